# Optimizing a Trainium2 kernel written in Bass

```python
import math
import jax, jax.numpy as jnp
from jax import lax
import numpy as np

D_MODEL = 1024
BATCH = 16
SEQ = 2048
DEPTH = 1
DEC_BATCH = 32
DEC_SEQ = 32
PAST_LEN = 1024

CHUNK = 64

FOX_HEADS = 8
HEAD_DIM = 64
FOX_WIDTH = FOX_HEADS * HEAD_DIM
Q_BLOCK = 128
ATTN_SCALE = HEAD_DIM ** -0.5

CONV_CH = D_MODEL // 2
CONV_WIDTH = 31

N_EXPERTS = 256
TOP_K = 8
N_GROUPS = 8
TOPK_GROUPS = 4
D_EXPERT = D_MODEL // 4
D_SHARED = D_EXPERT
ROUTED_SCALE = 2.5
EXPERT_BLOCK = 128

LN_EPS = 1e-5
DN_ALPHA = (2 * DEPTH) ** 0.25
DN_BETA = (8 * DEPTH) ** -0.25

OFF_Q = 0
OFF_K = FOX_WIDTH
OFF_V = 2 * FOX_WIDTH
OFF_F = 3 * FOX_WIDTH
OFF_GLU = OFF_F + FOX_HEADS
OFF_GA = OFF_GLU + 2 * CONV_CH
OFF_GB = OFF_GA + D_MODEL
N_IN = OFF_GB + D_MODEL

kernel_name = 'fox_conformer_moe_deepnorm_stream_step'


def _layer_norm(x, g, b):
    xf = x.astype(jnp.float32)
    mu = xf.mean(-1, keepdims=True)
    var = jnp.square(xf - mu).mean(-1, keepdims=True)
    return ((xf - mu) * lax.rsqrt(var + LN_EPS) * g.astype(jnp.float32) + b.astype(jnp.float32)).astype(x.dtype)


def _heads(t):
    return t.reshape(*t.shape[:-1], FOX_HEADS, HEAD_DIM)


def _mixer_inputs(x, w_in, b_in):
    z = x @ w_in + b_in
    q = _heads(z[..., OFF_Q:OFF_K])
    k = _heads(z[..., OFF_K:OFF_V])
    v = _heads(z[..., OFF_V:OFF_F])
    logf = jax.nn.log_sigmoid(z[..., OFF_F:OFF_GLU].astype(jnp.float32))
    glu = z[..., OFF_GLU:OFF_GA]
    u = glu[..., :CONV_CH] * jax.nn.sigmoid(glu[..., CONV_CH:])
    return q, k, v, logf, u, z[..., OFF_GA:OFF_GB], z[..., OFF_GB:]


def _fox_prompt(q, k, v, logf):
    b, s = q.shape[0], q.shape[1]
    c = jnp.cumsum(logf, axis=1).transpose(0, 2, 1)
    kpos = jnp.arange(s)

    def block(i):
        s0 = i * Q_BLOCK
        qb = lax.dynamic_slice_in_dim(q, s0, Q_BLOCK, axis=1)
        cq = lax.dynamic_slice_in_dim(c, s0, Q_BLOCK, axis=2)
        logits = jnp.einsum('bqhd,bkhd->bhqk', qb, k, preferred_element_type=jnp.float32) * ATTN_SCALE
        logits = logits + cq[..., :, None] - c[..., None, :]
        qpos = s0 + jnp.arange(Q_BLOCK)
        logits = jnp.where(kpos[None, :] <= qpos[:, None], logits, -jnp.inf)
        probs = jax.nn.softmax(logits, axis=-1)
        return jnp.einsum('bhqk,bkhd->bqhd', probs.astype(v.dtype), v)

    out = lax.map(block, jnp.arange(s // Q_BLOCK))
    return out.transpose(1, 0, 2, 3, 4).reshape(b, s, FOX_WIDTH)


def _fox_sample(q, k, v, logf, ck, cv, clogf):
    t, p = q.shape[1], ck.shape[1]
    k_all = jnp.concatenate([ck.astype(k.dtype), k], axis=1)
    v_all = jnp.concatenate([cv.astype(v.dtype), v], axis=1)
    c = jnp.cumsum(jnp.concatenate([clogf.astype(jnp.float32), logf], axis=1), axis=1).transpose(0, 2, 1)
    logits = jnp.einsum('bqhd,bkhd->bhqk', q, k_all, preferred_element_type=jnp.float32) * ATTN_SCALE
    logits = logits + c[..., p:, None] - c[..., None, :]
    mask = jnp.arange(p + t)[None, :] <= p + jnp.arange(t)[:, None]
    probs = jax.nn.softmax(jnp.where(mask, logits, -jnp.inf), axis=-1)
    out = jnp.einsum('bhqk,bkhd->bqhd', probs.astype(v.dtype), v_all)
    return out.reshape(q.shape[0], t, FOX_WIDTH)


def _conv_module(u_ext, conv_w, conv_b, cln_g, cln_b, w_b, b_b):
    h = lax.conv_general_dilated(u_ext, conv_w[:, None, :].astype(u_ext.dtype), (1,), 'VALID',
                                 dimension_numbers=('NWC', 'WIO', 'NWC'), feature_group_count=CONV_CH)
    h = jax.nn.silu(_layer_norm(h + conv_b, cln_g, cln_b))
    return h @ w_b + b_b


def _merge(x, attn, conv_out, g_a, g_b, w_a, w_out, ln_g, ln_b):
    m = jax.nn.sigmoid(g_a) * (attn @ w_a) + jax.nn.sigmoid(g_b) * conv_out
    return _layer_norm(DN_ALPHA * x + m @ w_out, ln_g, ln_b)


def _swiglu(x, wg, wu, wd):
    return (jax.nn.silu(x @ wg) * (x @ wu)) @ wd


def _moe_ffn(x, w_router, b_router, w_e_gate, w_e_up, w_e_down, w_s_gate, w_s_up, w_s_down):
    lead = x.shape[:-1]
    xt = x.reshape(-1, D_MODEL)
    t = xt.shape[0]
    scores = jax.nn.sigmoid(jnp.dot(xt, w_router, preferred_element_type=jnp.float32))
    sel = scores + b_router.astype(jnp.float32)
    gscore = lax.top_k(sel.reshape(t, N_GROUPS, N_EXPERTS // N_GROUPS), 2)[0].sum(-1)
    _, gidx = lax.top_k(gscore, TOPK_GROUPS)
    gmask = jax.nn.one_hot(gidx, N_GROUPS, dtype=jnp.float32).sum(1)
    sel = jnp.where(jnp.repeat(gmask, N_EXPERTS // N_GROUPS, axis=1) > 0, sel, -jnp.inf)
    _, eidx = lax.top_k(sel, TOP_K)
    gate = jnp.take_along_axis(scores, eidx, axis=1)
    gate = gate / gate.sum(-1, keepdims=True) * ROUTED_SCALE
    n_assign = t * TOP_K
    flat_e = eidx.reshape(n_assign)
    flat_tok = jnp.arange(n_assign, dtype=jnp.int32) // TOP_K
    flat_w = gate.reshape(n_assign)
    order = jnp.argsort(flat_e)
    se = flat_e[order]
    counts = jnp.bincount(flat_e, length=N_EXPERTS)
    starts = jnp.cumsum(counts) - counts
    pcounts = (counts + EXPERT_BLOCK - 1) // EXPERT_BLOCK * EXPERT_BLOCK
    pends = jnp.cumsum(pcounts)
    pstarts = pends - pcounts
    dest = pstarts[se] + jnp.arange(n_assign, dtype=jnp.int32) - starts[se]
    n_blocks = -(-n_assign // EXPERT_BLOCK) + N_EXPERTS
    n_rows = n_blocks * EXPERT_BLOCK
    buf_tok = jnp.full((n_rows,), t, jnp.int32).at[dest].set(flat_tok[order])
    buf_w = jnp.zeros((n_rows,), jnp.float32).at[dest].set(flat_w[order])
    blk_e = jnp.minimum(jnp.searchsorted(pends, jnp.arange(n_blocks, dtype=jnp.int32) * EXPERT_BLOCK,
                                         side='right'), N_EXPERTS - 1)
    xpad = jnp.concatenate([xt, jnp.zeros((1, D_MODEL), xt.dtype)], axis=0)

    def run_block(args):
        tok, w, e = args
        yb = _swiglu(xpad[tok], w_e_gate[e], w_e_up[e], w_e_down[e])
        return yb * w[:, None].astype(yb.dtype)

    yb = lax.map(run_block, (buf_tok.reshape(n_blocks, EXPERT_BLOCK),
                             buf_w.reshape(n_blocks, EXPERT_BLOCK), blk_e))
    routed = jnp.zeros((t + 1, D_MODEL), yb.dtype).at[buf_tok].add(yb.reshape(n_rows, D_MODEL))[:t]
    out = routed.astype(xt.dtype) + _swiglu(xt, w_s_gate, w_s_up, w_s_down)
    return out.reshape(*lead, D_MODEL)


def setup_inputs(seed: int = 0) -> dict:
    key = jax.random.key(seed)
    ks = jax.random.split(key, 32)
    nrm = lambda k, shape, scale: jax.random.normal(k, shape, jnp.float32) * scale
    forget_bias = jnp.asarray(np.linspace(1.0, 4.0, FOX_HEADS), jnp.float32)
    col_scale = np.ones((N_IN,), np.float32)
    col_scale[OFF_V:OFF_F] = DN_BETA
    b_off = np.zeros((N_IN,), np.float32)
    b_off[OFF_F:OFF_GLU] = np.linspace(1.0, 4.0, FOX_HEADS)
    gain = lambda k, n: 1.0 + nrm(k, (DEPTH, n), 0.02)
    return {
        'x_prompt': nrm(ks[0], (BATCH, SEQ, D_MODEL), 1.0),
        'x_sample': nrm(ks[1], (DEC_BATCH, DEC_SEQ, D_MODEL), 1.0),
        'cache_k': nrm(ks[2], (DEPTH, DEC_BATCH, PAST_LEN, FOX_HEADS, HEAD_DIM), 1.0),
        'cache_v': nrm(ks[3], (DEPTH, DEC_BATCH, PAST_LEN, FOX_HEADS, HEAD_DIM), DN_BETA),
        'cache_logf': jax.nn.log_sigmoid(nrm(ks[4], (DEPTH, DEC_BATCH, PAST_LEN, FOX_HEADS), 1.0) + forget_bias),
        'state_conv': nrm(ks[5], (DEPTH, DEC_BATCH, CONV_WIDTH - 1, CONV_CH), 0.5),
        'w_in': nrm(ks[6], (DEPTH, D_MODEL, N_IN), D_MODEL ** -0.5) * jnp.asarray(col_scale),
        'b_in': nrm(ks[7], (DEPTH, N_IN), 0.02) + jnp.asarray(b_off),
        'conv_w': nrm(ks[8], (DEPTH, CONV_WIDTH, CONV_CH), CONV_WIDTH ** -0.5),
        'conv_b': nrm(ks[9], (DEPTH, CONV_CH), 0.02),
        'conv_ln_g': gain(ks[10], CONV_CH),
        'conv_ln_b': nrm(ks[11], (DEPTH, CONV_CH), 0.02),
        'w_a': nrm(ks[12], (DEPTH, FOX_WIDTH, D_MODEL), FOX_WIDTH ** -0.5 * DN_BETA),
        'w_b': nrm(ks[13], (DEPTH, CONV_CH, D_MODEL), CONV_CH ** -0.5 * DN_BETA),
        'b_b': nrm(ks[14], (DEPTH, D_MODEL), 0.02),
        'w_out': nrm(ks[15], (DEPTH, D_MODEL, D_MODEL), D_MODEL ** -0.5 * DN_BETA),
        'ln1_g': gain(ks[16], D_MODEL),
        'ln1_b': nrm(ks[17], (DEPTH, D_MODEL), 0.02),
        'w_router': nrm(ks[18], (DEPTH, D_MODEL, N_EXPERTS), D_MODEL ** -0.5),
        'b_router': nrm(ks[19], (DEPTH, N_EXPERTS), 0.01),
        'w_e_gate': nrm(ks[20], (DEPTH, N_EXPERTS, D_MODEL, D_EXPERT), D_MODEL ** -0.5 * DN_BETA),
        'w_e_up': nrm(ks[21], (DEPTH, N_EXPERTS, D_MODEL, D_EXPERT), D_MODEL ** -0.5 * DN_BETA),
        'w_e_down': nrm(ks[22], (DEPTH, N_EXPERTS, D_EXPERT, D_MODEL), D_EXPERT ** -0.5 * DN_BETA),
        'w_s_gate': nrm(ks[23], (DEPTH, D_MODEL, D_SHARED), D_MODEL ** -0.5 * DN_BETA),
        'w_s_up': nrm(ks[24], (DEPTH, D_MODEL, D_SHARED), D_MODEL ** -0.5 * DN_BETA),
        'w_s_down': nrm(ks[25], (DEPTH, D_SHARED, D_MODEL), D_SHARED ** -0.5 * DN_BETA),
        'ln2_g': gain(ks[26], D_MODEL),
        'ln2_b': nrm(ks[27], (DEPTH, D_MODEL), 0.02),
    }


def reference(x_prompt, x_sample, cache_k, cache_v, cache_logf, state_conv,
              w_in, b_in, conv_w, conv_b, conv_ln_g, conv_ln_b, w_a, w_b, b_b, w_out, ln1_g, ln1_b,
              w_router, b_router, w_e_gate, w_e_up, w_e_down, w_s_gate, w_s_up, w_s_down, ln2_g, ln2_b):
    hp, hs = x_prompt, x_sample
    kp, vp, fp, cp, ks_, vs_, fs_, cs_ = [], [], [], [], [], [], [], []
    for l in range(DEPTH):
        ffn = (w_router[l], b_router[l], w_e_gate[l], w_e_up[l], w_e_down[l],
               w_s_gate[l], w_s_up[l], w_s_down[l])
        conv_p = (conv_w[l], conv_b[l], conv_ln_g[l], conv_ln_b[l], w_b[l], b_b[l])
        q, k, v, logf, u, g_a, g_b = _mixer_inputs(hp, w_in[l], b_in[l])
        attn = _fox_prompt(q, k, v, logf)
        u_ext = jnp.pad(u, ((0, 0), (CONV_WIDTH - 1, 0), (0, 0)))
        mid = _merge(hp, attn, _conv_module(u_ext, *conv_p), g_a, g_b, w_a[l], w_out[l], ln1_g[l], ln1_b[l])
        hp = _layer_norm(DN_ALPHA * mid + _moe_ffn(mid, *ffn), ln2_g[l], ln2_b[l])
        kp.append(k)
        vp.append(v)
        fp.append(logf)
        cp.append(u_ext[:, -(CONV_WIDTH - 1):])
        q, k, v, logf, u, g_a, g_b = _mixer_inputs(hs, w_in[l], b_in[l])
        attn = _fox_sample(q, k, v, logf, cache_k[l], cache_v[l], cache_logf[l])
        u_ext = jnp.concatenate([state_conv[l].astype(u.dtype), u], axis=1)
        mid = _merge(hs, attn, _conv_module(u_ext, *conv_p), g_a, g_b, w_a[l], w_out[l], ln1_g[l], ln1_b[l])
        hs = _layer_norm(DN_ALPHA * mid + _moe_ffn(mid, *ffn), ln2_g[l], ln2_b[l])
        ks_.append(k)
        vs_.append(v)
        fs_.append(logf)
        cs_.append(u_ext[:, -(CONV_WIDTH - 1):])
    new_k_prompt = jnp.stack(kp)
    new_v_prompt = jnp.stack(vp)
    new_logf_prompt = jnp.stack(fp)
    new_conv_prompt = jnp.stack(cp)
    new_k_sample = jnp.stack(ks_)
    new_v_sample = jnp.stack(vs_)
    new_logf_sample = jnp.stack(fs_)
    new_conv_sample = jnp.stack(cs_)
    return (hp, hs, new_k_prompt, new_v_prompt, new_logf_prompt, new_conv_prompt,
            new_k_sample, new_v_sample, new_logf_sample, new_conv_sample)
```

```python
from contextlib import ExitStack

import numpy as np
import ml_dtypes
import concourse.bass as bass
import concourse.mybir as mybir
from concourse.bass_utils import run_bass_kernel_spmd

F32 = mybir.dt.float32
BF16 = mybir.dt.bfloat16
I32 = mybir.dt.int32
U32 = mybir.dt.uint32
AF = mybir.ActivationFunctionType
ALU = mybir.AluOpType


class Tr:
    __slots__ = ("sem", "count", "name")

    def __init__(self, sem, name):
        self.sem = sem
        self.count = 0
        self.name = name


class Buf:
    __slots__ = ("t", "w", "r", "name")

    def __init__(self, t, name=""):
        self.t = t
        self.w = None
        self.r = {}
        self.name = name


class Eng:
    def __init__(self, name, tr):
        self.name = name
        self.tr = tr
        self.ops = []
        self.waited = {}
        self.dma_trs = []
        self.dma_i = 0


class Prog:
    def __init__(self, nc, n_dma=(28, 28, 6)):
        self.nc = nc
        self.stack = ExitStack()
        self.scopes = []
        mk = lambda n: Tr(self.stack.enter_context(nc.semaphore(n)), n)
        self.pe = Eng("pe", mk("s_pe"))
        self.act = Eng("act", mk("s_act"))
        self.dve = Eng("dve", mk("s_dve"))
        self.pool = Eng("pool", mk("s_pool"))
        self.sp = Eng("sp", mk("s_sp"))
        self.engs = [self.pe, self.act, self.dve, self.pool, self.sp]
        for e, n in zip((self.sp, self.pool, self.act), n_dma):
            e.dma_trs = [mk("d_%s%d" % (e.name, i)) for i in range(n)]
        self.banks = [Buf(self.stack.enter_context(nc.psum_tensor("psb%d" % i, [128, 512], F32)), "ps%d" % i)
                      for i in range(8)]
        self.bank_i = 0
        self.final = []
        self.n_ops = 0
        self.sched = True
        self.pending = []

    def sb(self, name, shape, dtype):
        st = self.scopes[-1] if self.scopes else self.stack
        self.n_sb = getattr(self, "n_sb", 0) + 1
        return Buf(st.enter_context(self.nc.sbuf_tensor("sb%d_%s" % (self.n_sb, name), list(shape), dtype)), name)

    def push_scope(self):
        self.scopes.append(ExitStack())

    def pop_scope(self):
        self.barrier()
        self.flush()
        self.scopes.pop().close()

    def ps(self, lo=0, hi=8):
        n = hi - lo
        b = self.banks[lo + (self.bank_i % n)]
        self.bank_i += 1
        return b

    def _deps(self, eng, reads, writes, extra=()):
        deps = {}

        def add(tok):
            if tok is None:
                return
            tr, v = tok
            if deps.get(tr, 0) < v:
                deps[tr] = v

        for b in reads:
            add(b.w)
        for b in writes:
            add(b.w)
            for tr, v in b.r.items():
                add((tr, v))
        for tok in extra:
            add(tok)
        waits = []
        for tr, v in deps.items():
            if eng is self.pe and tr is eng.tr:
                continue
            if eng.waited.get(tr, 0) >= v:
                continue
            eng.waited[tr] = v
            waits.append((tr, v))
        return waits

    @staticmethod
    def _mark(tok, reads, writes):
        tr, v = tok
        for b in reads:
            if b.r.get(tr, 0) < v:
                b.r[tr] = v
        for b in writes:
            b.w = tok
            b.r = {}

    def op(self, eng, fn, reads=(), writes=(), sig=True, c=0.3):
        if eng is self.pe and c == 0.3:
            c = 0.07
        if self.sched:
            self.pending.append(("op", eng, (fn, tuple(reads), tuple(writes), sig), tuple(reads), tuple(writes), c, 0))
            return None
        return self._op_now(eng, fn, reads, writes, sig)

    def dma(self, eng, out, in_, reads=(), writes=(), out_final=False, slow=False, fn=None, nbytes=None, **kw):
        if nbytes is None:
            try:
                ap = out if out is not None else None
                nbytes = 1
                for s_ in ap.shape:
                    nbytes *= int(s_)
                nbytes *= 2 if ap.dtype == BF16 else 4
            except Exception:
                nbytes = 262144
        if self.sched:
            self.pending.append(("dma", eng, (out, in_, tuple(reads), tuple(writes), out_final, slow, fn, kw), tuple(reads), tuple(writes),
                                 1.1 if fn is not None else 0.08, nbytes))
            return None
        return self._dma_now(eng, out, in_, reads, writes, out_final, slow, fn, **kw)

    def _op_now(self, eng, fn, reads=(), writes=(), sig=True):
        waits = self._deps(eng, reads, writes)
        tr = eng.tr
        if sig:
            tr.count += 1
            tok = (tr, tr.count)
        else:
            tok = (tr, tr.count + 1)

        def run(h, waits=waits, fn=fn, sig=sig, tr=tr):
            for tr2, v in waits:
                h.wait_ge(tr2.sem, v)
            ins = fn(h)
            if sig:
                ins.then_inc(tr.sem, 1)

        eng.ops.append(run)
        self.n_ops += 1
        self._mark(tok, reads, writes)
        return tok

    def _dma_now(self, eng, out, in_, reads=(), writes=(), out_final=False, slow=False, fn=None, **kw):
        tr = eng.dma_trs[eng.dma_i % len(eng.dma_trs)]
        eng.dma_i += 1
        extra = [(tr, tr.count)] if tr.count else []
        waits = self._deps(eng, reads, writes, extra)
        tr.count += 16
        tok = (tr, tr.count)
        if slow:
            kw["allow_slow_non_contiguous"] = True

        def run(h, waits=waits, tr=tr, out=out, in_=in_, kw=kw, fn=fn):
            for tr2, v in waits:
                h.wait_ge(tr2.sem, v)
            if fn is not None:
                ins = fn(h)
            else:
                ins = h.dma_start(out=out, in_=in_, **kw)
            ins.then_inc(tr.sem, 16)

        eng.ops.append(run)
        self.n_ops += 1
        self._mark(tok, reads, writes)
        if out_final:
            self.final.append(tok)
        return tok

    def drain(self):
        pend = self.pending
        self.pending = []
        n = len(pend)
        if n == 0:
            return
        import heapq
        lastw, readers = {}, {}
        deps = [None] * n
        succ = [[] for _ in range(n)]
        indeg = [0] * n
        for i, (_, eng, _, reads, writes, _, _) in enumerate(pend):
            d = set()
            for b in reads:
                w = lastw.get(id(b))
                if w is not None:
                    d.add(w)
            for b in writes:
                w = lastw.get(id(b))
                if w is not None:
                    d.add(w)
                for r_ in readers.get(id(b), ()):
                    d.add(r_)
            d.discard(i)
            deps[i] = d
            for j in d:
                succ[j].append(i)
            indeg[i] = len(d)
            for b in reads:
                readers.setdefault(id(b), []).append(i)
            for b in writes:
                lastw[id(b)] = i
                readers[id(b)] = []
        fin = [0.0] * n
        eng_free = {id(e): 0.0 for e in self.engs}
        ready = {id(e): [] for e in self.engs}
        for i in range(n):
            if indeg[i] == 0:
                heapq.heappush(ready[id(pend[i][1])], (0.0, i))
        dma_free = 0.0
        order = []
        LAT = 0.15
        WIN = 6000
        done = 0
        lo = 0
        scheduled = [False] * n
        while done < n:
            best = None
            for e in self.engs:
                h = ready[id(e)]
                if not h:
                    continue
                cand = None
                tmp_ = []
                k = 0
                while h and k < 8:
                    rt, i = heapq.heappop(h)
                    tmp_.append((rt, i))
                    k += 1
                    if i - lo > WIN:
                        continue
                    st_ = max(rt, eng_free[id(e)])
                    key = (st_, i)
                    if cand is None or key < cand[0]:
                        cand = (key, rt, i)
                for x in tmp_:
                    heapq.heappush(h, x)
                if cand is not None and (best is None or cand[0] < best[0][0]):
                    best = (cand, e)
            if best is None:
                cands = [(h[0][1], e) for e in self.engs for h in [ready[id(e)]] if h]
                i_min = None
                for e in self.engs:
                    for rt, i in ready[id(e)]:
                        if i_min is None or i < i_min[0]:
                            i_min = (i, rt, e)
                i, rt, e = i_min
                best = ((((max(rt, eng_free[id(e)])), i), rt, i), e)
            (key, rt, i), e = best
            h = ready[id(e)]
            h.remove((rt, i))
            heapq.heapify(h)
            kind, _, _, _, _, cost, nbytes = pend[i]
            st_ = key[0]
            if kind == "dma":
                eng_free[id(e)] = st_ + cost
                t0_ = max(st_ + cost, dma_free)
                dma_free = t0_ + nbytes / 250000.0
                fin[i] = dma_free + 1.8
            else:
                eng_free[id(e)] = st_ + cost
                fin[i] = st_ + cost
            scheduled[i] = True
            order.append(i)
            done += 1
            while lo < n and scheduled[lo]:
                lo += 1
            for j in succ[i]:
                indeg[j] -= 1
                if indeg[j] == 0:
                    rt_j = 0.0
                    for d_ in deps[j]:
                        l_ = 0.0 if pend[d_][1] is pend[j][1] and pend[j][1] is self.pe else LAT
                        if fin[d_] + l_ > rt_j:
                            rt_j = fin[d_] + l_
                    heapq.heappush(ready[id(pend[j][1])], (rt_j, j))
        self.sim_time = getattr(self, "sim_time", 0.0) + max(fin)
        for i in order:
            kind, eng, args, _, _, _, _ = pend[i]
            if kind == "op":
                fn, reads, writes, sig = args
                self._op_now(eng, fn, reads, writes, sig)
            else:
                out, in_, reads, writes, out_final, slow, fn, kw = args
                self._dma_now(eng, out, in_, reads, writes, out_final, slow, fn, **kw)

    def barrier(self):
        self.drain()
        toks = []
        for e in self.engs:
            if e.tr.count:
                toks.append((e.tr, e.tr.count))
            for tr in e.dma_trs:
                if tr.count:
                    toks.append((tr, tr.count))
        for e in self.engs:
            waits = []
            for tr, v in toks:
                if tr is e.tr:
                    continue
                if e.waited.get(tr, 0) >= v:
                    continue
                e.waited[tr] = v
                waits.append((tr, v))
            if waits:
                def run(h, waits=waits):
                    for tr2, v in waits:
                        h.wait_ge(tr2.sem, v)
                e.ops.append(run)

    def flush(self):
        nc = self.nc
        pend = {e.name: e.ops for e in self.engs}
        for e in self.engs:
            e.ops = []
        with nc.Block() as block:
            @block.tensor
            def _(h):
                for f in pend["pe"]:
                    f(h)

            @block.scalar
            def _(h):
                for f in pend["act"]:
                    f(h)

            @block.vector
            def _(h):
                for f in pend["dve"]:
                    f(h)

            @block.gpsimd
            def _(h):
                for f in pend["pool"]:
                    f(h)

            @block.sync
            def _(h):
                for f in pend["sp"]:
                    f(h)

    def finish(self):
        self.drain()
        waits = []
        for tr, v in self.final:
            if self.sp.waited.get(tr, 0) < v:
                self.sp.waited[tr] = v
                waits.append((tr, v))

        def run(h, waits=waits):
            for tr2, v in waits:
                h.wait_ge(tr2.sem, v)

        self.sp.ops.append(run)
        self.barrier()
        self.flush()
        while self.scopes:
            self.scopes.pop().close()
        self.stack.close()


D = 1024
NCORES = 8
SEQ = 2048
NSEQ_P = 2
NSUB_S = 4
TS = 32
PAST = 1024
H = 8
DH = 64
CC = 512
KW = 31
NE = 256
DE = 256
OFF_Q, OFF_K, OFF_V, OFF_F, OFF_GLU, OFF_GA, OFF_GB, N_IN = 0, 512, 1024, 1536, 1544, 2568, 3592, 4616
NA = OFF_GA
TP = NSEQ_P * SEQ
TT = TP + 128
DN_ALPHA = 2.0 ** 0.25
LN_EPS = 1e-5

PC_BQ, PC_BGLU, PC_BGA, PC_BGB, PC_BB, PC_CB, PC_CG, PC_CBE, PC_CW, PC_BF, PC_BK, PC_N = 0, 4, 12, 20, 28, 36, 40, 44, 48, 172, 173, 177


def _n(ap):
    n = 1
    for s_ in ap.shape[1:]:
        n *= int(s_)
    return n


def _mm(P, psb, out_ap, lhsT, rhs, start, stop, reads, sig=None):
    c = max(_n(rhs), 64) / 2400.0 * (4.0 if lhsT.dtype == F32 else 1.0) + 0.01
    P.op(P.pe, lambda e: e.matmul(out_ap, lhsT, rhs, start=start, stop=stop), reads=reads, writes=[psb],
         sig=stop if sig is None else sig, c=c)


def _act(P, out_ap, in_ap, func, reads, writes, bias=None, scale=1.0):
    c = _n(out_ap) / 1400.0 + 0.22
    if bias is None:
        P.op(P.act, lambda e: e.activation(out_ap, in_ap, func, scale=scale), reads=reads, writes=writes, c=c)
    else:
        P.op(P.act, lambda e: e.activation(out_ap, in_ap, func, bias=bias, scale=scale), reads=reads, writes=writes, c=c)


def _tt(P, eng, out_ap, a, b, op, reads, writes):
    c = _n(out_ap) / (960.0 if eng is P.dve else 600.0) + (0.12 if eng is P.dve else 0.3)
    P.op(eng, lambda e: e.tensor_tensor(out_ap, a, b, op), reads=reads, writes=writes, c=c)


def _stt(P, out_ap, in0, scalar, in1, op0, op1, reads, writes):
    P.op(P.dve, lambda e: e.scalar_tensor_tensor(out_ap, in0, scalar, in1, op0, op1), reads=reads, writes=writes, c=_n(out_ap) / 960.0 + 0.12)


def _ts(P, eng, out_ap, in0, s1, s2, op0, op1, reads, writes):
    c = _n(out_ap) / 960.0 + 0.12
    if s2 is None:
        P.op(eng, lambda e: e.tensor_scalar(out_ap, in0, s1, None, op0), reads=reads, writes=writes, c=c)
    else:
        P.op(eng, lambda e: e.tensor_scalar(out_ap, in0, s1, s2, op0, op1), reads=reads, writes=writes, c=c)


def _copy(P, eng, out_ap, in_ap, reads, writes):
    if eng is P.act:
        P.op(eng, lambda e: e.copy(out_ap, in_ap), reads=reads, writes=writes, c=_n(out_ap) / 1400.0 + 0.22)
    else:
        P.op(eng, lambda e: e.tensor_copy(out_ap, in_ap), reads=reads, writes=writes,
             c=_n(out_ap) / (960.0 if eng is P.dve else 280.0) + (0.12 if eng is P.dve else 0.3))


def load_w_cast(P, dst, src_ap, kchunks, ncols, col0=0):
    v = src_ap.rearrange("(kc p) n -> p kc n", p=128)
    c = 0
    while c < ncols:
        n = min(2048, ncols - c)
        P.dma(P.pool, dst.t[:, :, c:c + n], v[:, :, col0 + c:col0 + c + n], reads=[], writes=[dst])
        c += n


def pass_a(P, T, NB):
    nc = P.nc
    P.push_scope()
    cf = P.sb("cf", [128, 384], F32)
    cb = P.sb("cb", [128, 256], BF16)
    pcol = P.sb("pcol", [128, PC_N], F32)
    w_in = P.sb("w_in_a", [128, 8, NA], BF16)
    bkv = P.sb("bkv", [128, 1024], F32)
    bq8 = P.sb("bq8", [128, 4], F32)
    nbf = P.sb("nbf", [8, 1], F32)
    ones8 = P.sb("ones8", [8, 512], F32)
    onesb = P.sb("onesb", [128, 128], BF16)
    epsb = P.sb("epsb", [128, 1], F32)
    oneb = P.sb("oneb", [128, 1], F32)
    scr = Buf(None, "scratch")
    c3d = Buf(None, "c3d")
    P.dma(P.sp, cf.t[:, :], T["cf"], writes=[cf])
    P.dma(P.sp, cb.t[:, :], T["cb"][:, 0:256], writes=[cb])
    P.dma(P.sp, pcol.t[:, :], T["pcol"], writes=[pcol])
    P.dma(P.sp, bkv.t[:, :], T["b_in"][:, OFF_K:OFF_F].partition_broadcast(128), writes=[bkv])
    load_w_cast(P, w_in, T["w_in"], 8, NA)
    P.op(P.act, lambda e: e.mul(bq8.t[:, :], pcol.t[:, PC_BQ:PC_BQ + 4], 0.125), reads=[pcol], writes=[bq8])
    P.op(P.act, lambda e: e.mul(nbf.t[:, :], pcol.t[0:8, PC_BF:PC_BF + 1], -1.0), reads=[pcol], writes=[nbf])
    P.op(P.dve, lambda e: e.memset(ones8.t[:, :], 1.0), writes=[ones8])
    P.op(P.dve, lambda e: e.memset(onesb.t[:, :], 1.0), writes=[onesb])
    P.op(P.dve, lambda e: e.memset(epsb.t[:, :], LN_EPS), writes=[epsb])
    P.op(P.dve, lambda e: e.memset(oneb.t[:, :], 1.0), writes=[oneb])
    identf = cf.t[:, 0:128]
    lstrict = cf.t[:, 128:256]
    onesf = cf.t[:, 256:384]
    identb = cb.t[:, 0:128]
    tri = cb.t[:, 128:256]

    kT = P.sb("kT", [128, 4, SEQ], BF16)
    vsb = P.sb("vsb", [128, SEQ // 128, 512], BF16)
    negc = P.sb("negc", [128, SEQ // 128, 8], F32)
    xt = [P.sb("xt%d" % i, [128, 1024], F32) for i in range(2)]
    xT = P.sb("xT", [128, 8, NB], BF16)
    qz = P.sb("qz", [128, 8, NB], BF16)
    c3pad = P.sb("c3pad", [128, 8, NB], BF16)
    uT = P.sb("uT", [128, 4, 30 + NB], F32)
    sg = [P.sb("sg%d" % i, [128, NB], F32) for i in range(2)]
    kvf = [P.sb("kvf%d" % i, [128, 1024], F32) for i in range(2)]
    kbf = [P.sb("kbf%d" % i, [128, 512], BF16) for i in range(2)]
    lfneg = P.sb("lfneg", [8, NB], F32)
    cblk = P.sb("cblk", [8, NB], F32)
    carry = P.sb("carry", [8, 1], F32)
    c3p = P.sb("c3p", [8, 3, NB], BF16)
    ctmp = P.sb("ctmp", [8, NB], F32)
    et = ctmp
    lftok = P.sb("lftok", [128, NB // 128, 8], F32)
    pt = [P.sb("pt%d" % i, [128, NB], BF16) for i in range(3)]
    attnT = P.sb("attnT", [64, 8, NB], BF16)
    acc = [P.sb("cacc%d" % i, [128, NB], F32) for i in range(2)]
    rden = acc
    hc = P.sb("hc", [128, 4, NB], F32)
    hsq = P.sb("hsq", [128, NB], F32)
    mean = P.sb("mean", [128, NB], F32)
    rstd = P.sb("rstd", [128, NB], F32)
    hn = sg
    hT = P.sb("hT", [128, 4, NB], BF16)
    cvo = P.sb("cvo", [32, 512], F32)
    P.op(P.pool, lambda e: e.memset(qz.t[:, :, :], 0.0), writes=[qz])
    P.op(P.pool, lambda e: e.memset(c3pad.t[:, :, :], 0.0), writes=[c3pad])

    st = {"pt": 0, "x": 0, "kv": 0}

    def project_feature(col0, nfeat, nb, evac):
        psb = P.ps()
        for kc in range(8):
            _mm(P, psb, psb.t[0:nfeat, 0:nb], w_in.t[:, kc, col0:col0 + nfeat], xT.t[:, kc, 0:nb], kc == 0, kc == 7,
                [w_in, xT])
        evac(psb)

    def attention(h, qc0, nq, ktiles):
        ob = P.banks[4 + (h % 2)]
        db = P.banks[6 + (h % 2)]
        n = len(ktiles)
        for i, kt in enumerate(ktiles):
            nk, q0 = kt["nk"], kt["q0"]
            w = nq - q0
            sb_ = P.ps(0, 4)
            _mm(P, sb_, sb_.t[0:nk, 0:w], kt["kT"], qz.t[:, h, qc0 + q0:qc0 + nq], True, False, kt["reads"] + [qz])
            _mm(P, sb_, sb_.t[0:nk, 0:w], onesb.t[:, 0:nk], c3pad.t[:, h, qc0 + q0:qc0 + nq], False, True,
                [onesb, c3pad])
            p = pt[st["pt"] % 3]
            st["pt"] += 1
            _act(P, p.t[0:nk, 0:w], sb_.t[0:nk, 0:w], AF.Exp, kt["reads"] + [sb_], [p], bias=kt["bias"])
            if kt["tri"]:
                tw = min(nk, w)
                _tt(P, P.pool, p.t[0:nk, 0:tw], p.t[0:nk, 0:tw], tri[0:nk, 0:tw], ALU.mult, [p, cb], [p])
            _mm(P, ob, ob.t[0:64, q0:nq], kt["v"], p.t[0:nk, 0:w], i == 0, i == n - 1, kt["reads"] + [p])
            _mm(P, db, db.t[0:64, q0:nq], onesb.t[0:nk, 0:64], p.t[0:nk, 0:w], i == 0, i == n - 1, [onesb, p])
        rd = rden[h % 2]
        P.op(P.dve, lambda e: e.reciprocal(rd.t[0:64, 0:nq], db.t[0:64, 0:nq]), reads=[db], writes=[rd])
        _tt(P, P.dve, attnT.t[:, h, qc0:qc0 + nq], ob.t[0:64, 0:nq], rd.t[0:64, 0:nq], ALU.mult, [ob, rd], [attnT])


    def conv_ln(nb, uview, accview, c_list=range(4)):
        ps_s = P.ps()
        ps_q = P.ps()
        for c in range(4):
            for j in range(KW):
                a = acc[j % 2]
                col = pcol.t[:, PC_CW + c * KW + j:PC_CW + c * KW + j + 1]
                if j < 2:
                    _ts(P, P.dve, accview(a), uview(c, j), col, None, ALU.mult, None, [uT, pcol], [a])
                else:
                    _stt(P, accview(a), uview(c, j), col, accview(a), ALU.mult, ALU.add, [uT, pcol, a], [a])
            _stt(P, hc.t[:, c, 0:nb], acc[0].t[:, 0:nb], pcol.t[:, PC_CB + c:PC_CB + c + 1], acc[1].t[:, 0:nb],
                 ALU.add, ALU.add, [acc[0], acc[1], pcol], [hc])
            P.op(P.act, lambda e, c=c: e.activation(hsq.t[:, 0:nb], hc.t[:, c, 0:nb], AF.Square), reads=[hc], writes=[hsq])
            _mm(P, ps_s, ps_s.t[:, 0:nb], onesf, hc.t[:, c, 0:nb], c == 0, c == 3, [cf, hc], sig=True)
            _mm(P, ps_q, ps_q.t[:, 0:nb], onesf, hsq.t[:, 0:nb], c == 0, c == 3, [cf, hsq], sig=True)
        P.op(P.act, lambda e: e.mul(mean.t[:, 0:nb], ps_s.t[:, 0:nb], 1.0 / CC), reads=[ps_s], writes=[mean])
        m2 = hn[0]
        _tt(P, P.dve, m2.t[:, 0:nb], mean.t[:, 0:nb], mean.t[:, 0:nb], ALU.mult, [mean], [m2])
        _stt(P, rstd.t[:, 0:nb], ps_q.t[:, 0:nb], 1.0 / CC, m2.t[:, 0:nb], ALU.mult, ALU.subtract, [ps_q, m2], [rstd])
        _act(P, rstd.t[:, 0:nb], rstd.t[:, 0:nb], AF.Sqrt, [rstd], [rstd], bias=epsb.t[:, 0:1])
        P.op(P.dve, lambda e: e.reciprocal(rstd.t[:, 0:nb], rstd.t[:, 0:nb]), reads=[rstd], writes=[rstd])
        for c in range(4):
            x_ = hn[c % 2]
            _tt(P, P.dve, x_.t[:, 0:nb], hc.t[:, c, 0:nb], mean.t[:, 0:nb], ALU.subtract, [hc, mean], [x_])
            _tt(P, P.pool, x_.t[:, 0:nb], x_.t[:, 0:nb], rstd.t[:, 0:nb], ALU.mult, [x_, rstd], [x_])
            P.op(P.act, lambda e, c=c, x_=x_: e.activation(hT.t[:, c, 0:nb], x_.t[:, 0:nb], AF.Silu,
                                                       bias=pcol.t[:, PC_CBE + c:PC_CBE + c + 1],
                                                       scale=pcol.t[:, PC_CG + c:PC_CG + c + 1]),
                 reads=[x_, pcol], writes=[hT])

    def conv_state_out(uview30, dst):
        psb = P.ps()
        for c in range(4):
            P.op(P.pe, lambda e, c=c, psb=psb: e.transpose(psb.t[0:30, c * 128:(c + 1) * 128], uview30(c), identf),
                 reads=[uT, cf], writes=[psb], sig=(c == 3))
        _copy(P, P.act, cvo.t[0:30, :], psb.t[0:30, 0:512], [psb], [cvo])
        P.dma(P.sp, dst, cvo.t[0:30, :], reads=[cvo], out_final=True)

    def common_front(x_src, nb, subs):
        ntile = nb // 128
        for t in range(ntile):
            xb = xt[st["x"] % 2]
            st["x"] += 1
            P.dma(P.sp, xb.t[:, :], x_src[t * 128:(t + 1) * 128, :], writes=[xb])
            for g in range(2):
                psb = P.ps()
                for j in range(4):
                    kc = g * 4 + j
                    P.op(P.pe, lambda e, psb=psb, j=j, kc=kc, xb=xb: e.transpose(
                        psb.t[:, j * 128:(j + 1) * 128], xb.t[:, kc * 128:(kc + 1) * 128], identf),
                        reads=[xb, cf], writes=[psb], sig=(j == 3))
                eng = P.act if g == 0 else P.dve
                _copy(P, eng, xT.t[:, g * 4:(g + 1) * 4, t * 128:(t + 1) * 128],
                      psb.t[:, :].rearrange("p (a b) -> p a b", a=4), [psb], [xT])
        for c in range(4):
            def ev(psb, c=c):
                _act(P, qz.t[0:64, 2 * c, 0:nb], psb.t[0:64, 0:nb], AF.Identity, [psb, bq8], [qz],
                     bias=bq8.t[0:64, c:c + 1], scale=0.125)
                _act(P, qz.t[64:128, 2 * c + 1, 0:nb], psb.t[64:128, 0:nb], AF.Identity, [psb, bq8], [qz],
                     bias=bq8.t[64:128, c:c + 1], scale=0.125)
            project_feature(OFF_Q + c * 128, 128, nb, ev)
        def evf(psb):
            _act(P, et.t[:, 0:nb], psb.t[0:8, 0:nb], AF.Exp, [psb, nbf], [et], bias=nbf.t[:, 0:1], scale=-1.0)
            _act(P, lfneg.t[:, 0:nb], et.t[:, 0:nb], AF.Ln, [et], [lfneg], bias=oneb.t[0:8, 0:1])
        project_feature(OFF_F, 8, nb, evf)
        for s in subs:
            c0, n = s["c0"], s["n"]
            init = 0.0 if s["first"] else carry.t[:, 0:1]
            P.op(P.dve, lambda e, c0=c0, n=n, init=init: e.tensor_tensor_scan(
                cblk.t[:, c0:c0 + n], ones8.t[:, 0:n], lfneg.t[:, c0:c0 + n], init, ALU.mult, ALU.subtract),
                reads=[ones8, lfneg, carry], writes=[cblk])
        _copy(P, P.act, carry.t[:, 0:1], cblk.t[:, nb - 1:nb], [cblk], [carry])
        _copy(P, P.dve, c3p.t[:, 0, 0:nb], cblk.t[:, 0:nb], [cblk], [c3p])
        _tt(P, P.dve, ctmp.t[:, 0:nb], cblk.t[:, 0:nb], c3p.t[:, 0, 0:nb], ALU.subtract, [cblk, c3p], [ctmp])
        _copy(P, P.dve, c3p.t[:, 1, 0:nb], ctmp.t[:, 0:nb], [ctmp], [c3p])
        _tt(P, P.dve, c3p.t[:, 2, 0:nb], ctmp.t[:, 0:nb], c3p.t[:, 1, 0:nb], ALU.subtract, [ctmp, c3p], [c3p])
        P.dma(P.sp, T["c3_d"][:, :, 0:nb], c3p.t[:, :, 0:nb], reads=[c3p], writes=[c3d])
        P.dma(P.sp, c3pad.t[0:3, :, 0:nb], T["c3_d"][:, :, 0:nb].rearrange("h s n -> s h n"), reads=[c3d], writes=[c3pad])

    def glu(nb, uout, view=lambda a: a):
        for c in range(4):
            sgb = sg[c % 2]
            def evg(psb, sgb=sgb, c=c):
                _act(P, sgb.t[:, 0:nb], psb.t[:, 0:nb], AF.Sigmoid, [psb, pcol], [sgb],
                     bias=pcol.t[:, PC_BGLU + 4 + c:PC_BGLU + 5 + c])
            project_feature(OFF_GLU + CC + c * 128, 128, nb, evg)
            def eva(psb, sgb=sgb, c=c):
                _stt(P, uout(c), view(psb.t[:, 0:nb]), pcol.t[:, PC_BGLU + c:PC_BGLU + c + 1], view(sgb.t[:, 0:nb]),
                     ALU.add, ALU.mult, [psb, pcol, sgb], [uT])
            project_feature(OFF_GLU + c * 128, 128, nb, eva)

    def store_scratch(nb, tok0):
        P.dma(P.sp, T["attn_d"].rearrange("(h d) t -> d h t", d=64)[:, :, tok0:tok0 + nb], attnT.t[:, :, 0:nb],
              reads=[attnT], writes=[scr])
        P.dma(P.sp, T["h_d"].rearrange("(c p) t -> p c t", p=128)[:, :, tok0:tok0 + nb], hT.t[:, :, 0:nb],
              reads=[hT], writes=[scr])

    ntile = NB // 128
    for sq in range(NSEQ_P):
        for b in range(SEQ // NB):
            r0 = sq * SEQ + b * NB
            tile0 = b * ntile
            if b == 0:
                P.op(P.pool, lambda e: e.memset(uT.t[:, :, 0:30], 0.0), writes=[uT])
            else:
                _copy(P, P.pool, uT.t[:, :, 0:30], uT.t[:, :, NB:NB + 30], [uT], [uT])
            common_front(T["xp"][r0:r0 + NB, :], NB, [{"c0": 0, "n": NB, "first": b == 0}])
            for t in range(ntile):
                psb = P.ps()
                P.op(P.pe, lambda e, psb=psb, t=t: e.transpose(psb.t[:, 0:8], lfneg.t[:, t * 128:(t + 1) * 128], identf[0:8, 0:8]),
                     reads=[lfneg, cf], writes=[psb])
                P.op(P.pe, lambda e, psb=psb, t=t: e.transpose(psb.t[:, 8:16], cblk.t[:, t * 128:(t + 1) * 128], identf[0:8, 0:8]),
                     reads=[cblk, cf], writes=[psb])
                P.op(P.act, lambda e, psb=psb, t=t: e.mul(lftok.t[:, t, :], psb.t[:, 0:8], -1.0), reads=[psb], writes=[lftok])
                P.op(P.act, lambda e, psb=psb, j=tile0 + t: e.mul(negc.t[:, j, :], psb.t[:, 8:16], -1.0), reads=[psb], writes=[negc])
            P.dma(P.sp, T["lfp"][r0:r0 + NB, :].rearrange("(t p) h -> p t h", p=128), lftok.t[:, 0:ntile, :],
                  reads=[lftok], out_final=True)
            for t in range(ntile):
                j = tile0 + t
                kvb = kvf[st["kv"] % 2]
                kb = kbf[st["kv"] % 2]
                st["kv"] += 1
                for half in range(2):
                    psb = P.ps()
                    for kc in range(8):
                        _mm(P, psb, psb.t[:, 0:512], xT.t[:, kc, t * 128:(t + 1) * 128],
                            w_in.t[:, kc, OFF_K + half * 512:OFF_K + (half + 1) * 512], kc == 0, kc == 7, [xT, w_in])
                    _tt(P, P.dve, kvb.t[:, half * 512:(half + 1) * 512], psb.t[:, 0:512], bkv.t[:, half * 512:(half + 1) * 512],
                        ALU.add, [psb, bkv], [kvb])
                rr = r0 + t * 128
                P.dma(P.sp, T["kp"][rr:rr + 128, :], kvb.t[:, 0:512], reads=[kvb], out_final=True)
                P.dma(P.sp, T["vp"][rr:rr + 128, :], kvb.t[:, 512:1024], reads=[kvb], out_final=True)
                _copy(P, P.act, kb.t[:, :], kvb.t[:, 0:512], [kvb], [kb])
                _copy(P, P.pool, vsb.t[:, j, :], kvb.t[:, 512:1024], [kvb], [vsb])
                psb = P.ps()
                pbf = psb.t[:, :].bitcast(BF16)
                for c in range(4):
                    P.op(P.pe, lambda e, c=c, pbf=pbf, kb=kb: e.transpose(pbf[:, c * 128:(c + 1) * 128], kb.t[:, c * 128:(c + 1) * 128], identb),
                         reads=[kb, cb], writes=[psb], sig=(c == 3))
                _copy(P, P.dve, kT.t[:, :, j * 128:(j + 1) * 128], pbf[:, 0:512].rearrange("p (c n) -> p c n", c=4), [psb], [kT])
            glu(NB, lambda c: uT.t[:, c, 30:30 + NB])
            for h in range(8):
                kts = []
                for j in range(tile0 + ntile):
                    kts.append(dict(kT=kT.t[:, h // 2, j * 128:(j + 1) * 128], v=vsb.t[:, j, h * 64:(h + 1) * 64],
                                    bias=negc.t[:, j, h:h + 1], nk=128, q0=max(0, (j - tile0) * 128), tri=j >= tile0,
                                    reads=[kT, vsb, negc]))
                attention(h, 0, NB, kts)
            conv_ln(NB, lambda c, j: uT.t[:, c, j:j + NB], lambda a: a.t[:, 0:NB])
            store_scratch(NB, r0)
            if b == SEQ // NB - 1:
                conv_state_out(lambda c: uT.t[:, c, NB:NB + 30], T["cvp"][sq, :, :])
    uSv = uT.t[:, :, 0:NSUB_S * 62].rearrange("p c (s n) -> p c s n", s=NSUB_S)
    ckb = P.sb("ckb", [128, 8, 512], BF16)
    clfb = P.sb("clfb", [128, 8, 8], F32)
    sufs = P.sb("sufs", [128, 8, 8], F32)
    negcs = P.sb("negcs", [32, NSUB_S, 8], F32)
    kTn = P.sb("kTn", [128, 4, 128], BF16)
    vnew = P.sb("vnew", [32, NSUB_S, 512], BF16)
    kvs = kvf
    scv = kvf
    v4 = lambda a: a.rearrange("p (s n) -> p s n", s=NSUB_S)
    for s in range(NSUB_S):
        sc = scv[s % 2]
        P.dma(P.sp, sc.t[0:30, 0:512], T["sconv"][s, :, :], writes=[sc])
        psb = P.ps()
        for c in range(4):
            P.op(P.pe, lambda e, c=c, psb=psb, sc=sc: e.transpose(psb.t[:, c * 32:c * 32 + 30], sc.t[0:30, c * 128:(c + 1) * 128],
                                                               identf[0:30, 0:30]),
                 reads=[sc, cf], writes=[psb], sig=(c == 3))
        _copy(P, P.act, uSv[:, :, s, 0:30], psb.t[:, 0:128].rearrange("p (c n) -> p c n", c=4)[:, :, 0:30], [psb], [uT])
    common_front(T["xs"], 128, [{"c0": 32 * s, "n": 32, "first": True} for s in range(NSUB_S)])
    psb = P.ps()
    P.op(P.pe, lambda e, psb=psb: e.transpose(psb.t[:, 0:8], lfneg.t[:, 0:128], identf[0:8, 0:8]), reads=[lfneg, cf], writes=[psb])
    P.op(P.act, lambda e, psb=psb: e.mul(lftok.t[:, 0, :], psb.t[:, 0:8], -1.0), reads=[psb], writes=[lftok])
    P.dma(P.sp, T["lfs"], lftok.t[:, 0, :], reads=[lftok], out_final=True)
    for s in range(NSUB_S):
        psb = P.ps()
        P.op(P.pe, lambda e, psb=psb, s=s: e.transpose(psb.t[0:32, 0:8], cblk.t[:, 32 * s:32 * s + 32], identf[0:8, 0:8]),
             reads=[cblk, cf], writes=[psb])
        P.op(P.act, lambda e, psb=psb, s=s: e.mul(negcs.t[0:32, s, :], psb.t[0:32, 0:8], -1.0), reads=[psb], writes=[negcs])
        kvb = kvs[s % 2]
        for half in range(2):
            psb = P.ps()
            for kc in range(8):
                _mm(P, psb, psb.t[0:32, 0:512], xT.t[:, kc, 32 * s:32 * s + 32],
                    w_in.t[:, kc, OFF_K + half * 512:OFF_K + (half + 1) * 512], kc == 0, kc == 7, [xT, w_in])
            _tt(P, P.dve, kvb.t[0:32, half * 512:(half + 1) * 512], psb.t[0:32, 0:512], bkv.t[0:32, half * 512:(half + 1) * 512],
                ALU.add, [psb, bkv], [kvb])
        P.dma(P.sp, T["ks"][32 * s:32 * s + 32, :], kvb.t[0:32, 0:512], reads=[kvb], out_final=True)
        P.dma(P.sp, T["vs"][32 * s:32 * s + 32, :], kvb.t[0:32, 512:1024], reads=[kvb], out_final=True)
        _copy(P, P.act, vnew.t[0:32, s, :], kvb.t[0:32, 512:1024], [kvb], [vnew])
    for c in range(4):
        def evk(psb, c=c):
            _act(P, kTn.t[:, c, 0:128], psb.t[:, 0:128], AF.Identity, [psb, pcol], [kTn], bias=pcol.t[:, PC_BK + c:PC_BK + c + 1])
        project_feature(OFF_K + c * 128, 128, 128, evk)
    glu(128, lambda c: uSv[:, c, :, 30:62], v4)
    for s in range(NSUB_S):
        P.dma(P.pool, ckb.t[:, :, :], T["ck"][s].rearrange("(j p) n -> p j n", p=128), writes=[ckb])
        P.dma(P.pool, vsb.t[:, 0:8, :], T["cv"][s].rearrange("(j p) n -> p j n", p=128), writes=[vsb])
        P.dma(P.sp, clfb.t[:, :, :], T["clf"][s].rearrange("(j p) h -> p j h", p=128), writes=[clfb])
        for j in range(8):
            psb = P.ps()
            pbf = psb.t[:, :].bitcast(BF16)
            for c in range(4):
                P.op(P.pe, lambda e, c=c, j=j, pbf=pbf: e.transpose(pbf[:, c * 128:(c + 1) * 128], ckb.t[:, j, c * 128:(c + 1) * 128], identb),
                     reads=[ckb, cb], writes=[psb], sig=(c == 3))
            _copy(P, P.dve if j % 2 else P.act, kT.t[:, :, j * 128:(j + 1) * 128],
                  pbf[:, 0:512].rearrange("p (c n) -> p c n", c=4), [psb], [kT])
        psb = P.ps()
        for j in range(8):
            _mm(P, psb, psb.t[:, j * 8:(j + 1) * 8], lstrict, clfb.t[:, j, :], True, j == 7, [cf, clfb], sig=False)
            for j2 in range(j + 1, 8):
                _mm(P, psb, psb.t[:, j * 8:(j + 1) * 8], onesf, clfb.t[:, j2, :], False, j2 == 7, [cf, clfb], sig=False)
        P.op(P.pe, lambda e, psb=psb: e.transpose(psb.t[0:8, 64:72], clfb.t[0:8, 0, :], identf[0:8, 0:8]), reads=[clfb, cf], writes=[psb])
        _copy(P, P.dve, sufs.t[:, :, :], psb.t[:, 0:64].rearrange("p (j h) -> p j h", j=8), [psb], [sufs])
        for h in range(8):
            kts = []
            for j in range(8):
                kts.append(dict(kT=kT.t[:, h // 2, j * 128:(j + 1) * 128], v=vsb.t[:, j, h * 64:(h + 1) * 64],
                                bias=sufs.t[:, j, h:h + 1], nk=128, q0=0, tri=False, reads=[kT, vsb, sufs]))
            kts.append(dict(kT=kTn.t[:, h // 2, 32 * s:32 * s + 32], v=vnew.t[0:32, s, h * 64:(h + 1) * 64],
                            bias=negcs.t[0:32, s, h:h + 1], nk=32, q0=0, tri=True, reads=[kTn, vnew, negcs]))
            attention(h, 32 * s, 32, kts)
    conv_ln(128, lambda c, j: uSv[:, c, :, j:j + 32], lambda a: v4(a.t[:, 0:128]))
    store_scratch(128, TP)
    for s in range(NSUB_S):
        conv_state_out(lambda c, s=s: uSv[:, c, s, 32:62], T["cvs"][s, :, :])
    P.pop_scope()


CAP = 384
NSLOT = NE * CAP
NT = TT // 128


def layer_norm_tile(P, r, out_fn, tmp, eps_col):
    st6, mv, sc = tmp["st6"], tmp["mv"], tmp["sc"]
    for g in range(2):
        P.op(P.dve, lambda e, g=g: e.bn_stats(st6.t[:, g, :], r.t[:, g * 512:(g + 1) * 512]), reads=[r], writes=[st6])
    P.op(P.dve, lambda e: e.bn_aggr(mv.t[:, 0:2], st6.t[:, :, :].rearrange("p a b -> p (a b)")), reads=[st6], writes=[mv])
    _act(P, sc.t[:, 0:1], mv.t[:, 1:2], AF.Sqrt, [mv], [sc], bias=eps_col)
    P.op(P.dve, lambda e: e.reciprocal(sc.t[:, 0:1], sc.t[:, 0:1]), reads=[sc], writes=[sc])
    _ts(P, P.dve, sc.t[:, 1:2], mv.t[:, 0:1], -1.0, sc.t[:, 0:1], ALU.mult, ALU.mult, [mv, sc], [sc])
    out_fn(sc.t[:, 0:1], sc.t[:, 1:2])


def pass_b(P, T, G, NB):
    P.push_scope()
    cf = P.sb("cf", [128, 384], F32)
    cb = P.sb("cb", [128, 384], BF16)
    pcol = P.sb("pcol", [128, PC_N], F32)
    P.dma(P.sp, cf.t[:, :], T["cf"], writes=[cf])
    P.dma(P.sp, cb.t[:, :], T["cb"], writes=[cb])
    P.dma(P.sp, pcol.t[:, :], T["pcol"], writes=[pcol])
    identf = cf.t[:, 0:128]
    identb = cb.t[:, 0:128]
    ustrict = cb.t[:, 256:384]
    w_g = P.sb("w_g", [128, 8, 2048], BF16)
    w_a = P.sb("w_a", [128, 4, 1024], BF16)
    w_b = P.sb("w_b", [128, 4, 1024], BF16)
    w_o = P.sb("w_o", [128, 8, 1024], BF16)
    wr_hi = P.sb("wr_hi", [128, 8, 256], BF16)
    wr_lo = P.sb("wr_lo", [128, 8, 256], BF16)
    w_s = P.sb("w_s", [128, 8, 512], BF16)
    w_sd = P.sb("w_sd", [128, 2, 1024], BF16)
    lng = P.sb("lng", [128, 1024], F32)
    lnb = P.sb("lnb", [128, 1024], F32)
    brt = P.sb("brt", [128, 256], F32)
    cnt = P.sb("cnt", [128, 256], F32)
    onesb = P.sb("onesb", [128, 128], BF16)
    epsb = P.sb("epsb", [128, 1], F32)
    tokid = P.sb("tokid", [128, NT], I32)
    load_w_cast(P, w_g, T["w_in"], 8, 2048, OFF_GA)
    load_w_cast(P, w_a, T["w_a"], 4, 1024)
    load_w_cast(P, w_b, T["w_b"], 4, 1024)
    load_w_cast(P, w_o, T["w_out"], 8, 1024)
    load_w_cast(P, wr_hi, T["w_router"], 8, 256)
    load_w_cast(P, w_s, T["w_sg"], 8, 256)
    P.dma(P.pool, w_s.t[:, :, 256:512], T["w_su"].rearrange("(kc p) n -> p kc n", p=128), writes=[w_s])
    load_w_cast(P, w_sd, T["w_sd"], 2, 1024)
    P.dma(P.sp, lng.t[:, :], T["ln1_g"].partition_broadcast(128), writes=[lng])
    P.dma(P.sp, lnb.t[:, :], T["ln1_b"].partition_broadcast(128), writes=[lnb])
    P.dma(P.sp, brt.t[:, :], T["b_router"].partition_broadcast(128), writes=[brt])
    P.dma(P.sp, cnt.t[:, :], T["base1"], writes=[cnt])
    P.dma(P.sp, tokid.t[:, :], T["tokid"], writes=[tokid])
    P.op(P.dve, lambda e: e.memset(onesb.t[:, :], 1.0), writes=[onesb])
    P.op(P.dve, lambda e: e.memset(epsb.t[:, :], LN_EPS), writes=[epsb])
    wr32 = P.sb("wr32", [128, 8, 256], F32)
    P.dma(P.sp, wr32.t[:, :, :], T["w_router"].rearrange("(kc p) n -> p kc n", p=128), writes=[wr32])
    _tt(P, P.dve, wr_lo.t[:, :, :], wr32.t[:, :, :], wr_hi.t[:, :, :], ALU.subtract, [wr32, wr_hi], [wr_lo])

    nt = NB // 128
    at = P.sb("at", [128, 4, NB], BF16)
    ht = P.sb("ht", [128, 4, NB], BF16)
    xt = [P.sb("xt%d" % i, [128, 1024], F32) for i in range(nt)]
    xT = P.sb("xT", [128, 8, NB], BF16)
    sga = [P.sb("sga%d" % i, [128, NB], F32) for i in range(2)]
    sgb = [P.sb("sgb%d" % i, [128, NB], F32) for i in range(2)]
    t1 = [P.sb("t1%d" % i, [128, NB], F32) for i in range(2)]
    t2 = [P.sb("t2%d" % i, [128, NB], F32) for i in range(2)]
    mT = P.sb("mT", [128, 8, NB], BF16)
    rr = P.sb("rr", [128, 1024], F32)
    mid = [P.sb("mid%d" % i, [128, 1024], F32) for i in range(2)]
    mhi = [P.sb("mhi%d" % i, [128, 1024], BF16) for i in range(2)]
    mlo = P.sb("mlo", [128, 1024], BF16)
    mTh = P.sb("mTh", [128, 8, NB], BF16)
    mTl = P.sb("mTl", [128, 8, NB], BF16)
    tmp = {"st6": P.sb("st6", [128, 2, 6], F32), "mv": P.sb("mv", [128, 2], F32), "sc": P.sb("sc", [128, 2], F32)}
    rt = {n: P.sb("rt_" + n, [128, 256], F32) for n in ("scores", "sel", "selm", "emask", "gate", "sv", "junk")}
    emb = P.sb("emb", [128, 256], BF16)
    m8 = P.sb("m8", [128, 8, 8], F32)
    gs = P.sb("gs", [128, 8], F32)
    g8s = P.sb("g8s", [128, 8], F32)
    gmask = P.sb("gmask", [128, 8], F32)
    gneg = P.sb("gneg", [128, 8], F32)
    s8 = P.sb("s8", [128, 8], F32)
    den = P.sb("den", [128, 1], F32)
    gsh = [P.sb("gsh%d" % i, [128, NB], F32) for i in range(2)]
    hsT = P.sb("hsT", [128, 2, NB], BF16)
    pre = [P.sb("pre%d" % i, [128, 1024], F32) for i in range(2)]
    scr = Buf(None, "scr")
    slots_all, gates_all = G["slots"], G["gates"]
    st = {"m": 0}

    for b0 in range(0, TT, NB):
        nb = min(NB, TT - b0)
        ntile = nb // 128
        P.dma(P.sp, at.t[:, :, 0:nb], T["attn_d"].rearrange("(c p) t -> p c t", p=128)[:, :, b0:b0 + nb], writes=[at])
        P.dma(P.sp, ht.t[:, :, 0:nb], T["h_d"].rearrange("(c p) t -> p c t", p=128)[:, :, b0:b0 + nb], writes=[ht])
        for t in range(ntile):
            xb = xt[t]
            r0 = b0 + t * 128
            src_x = T["xp"][r0:r0 + 128, :] if r0 < TP else T["xs"]
            P.dma(P.sp, xb.t[:, :], src_x, writes=[xb])
            for g in range(2):
                psb = P.ps()
                for j in range(4):
                    kc = g * 4 + j
                    P.op(P.pe, lambda e, psb=psb, j=j, kc=kc, xb=xb: e.transpose(
                        psb.t[:, j * 128:(j + 1) * 128], xb.t[:, kc * 128:(kc + 1) * 128], identf),
                        reads=[xb, cf], writes=[psb], sig=(j == 3))
                _copy(P, P.act if g == 0 else P.dve, xT.t[:, g * 4:(g + 1) * 4, t * 128:(t + 1) * 128],
                      psb.t[:, :].rearrange("p (a b) -> p a b", a=4), [psb], [xT])
        for oc in range(8):
            i2 = oc % 2
            pa = P.ps()
            for c in range(4):
                _mm(P, pa, pa.t[:, 0:nb], w_a.t[:, c, oc * 128:(oc + 1) * 128], at.t[:, c, 0:nb], c == 0, c == 3, [w_a, at])
            pb = P.ps()
            for c in range(4):
                _mm(P, pb, pb.t[:, 0:nb], w_b.t[:, c, oc * 128:(oc + 1) * 128], ht.t[:, c, 0:nb], c == 0, c == 3, [w_b, ht])
            pga = P.ps()
            for kc in range(8):
                _mm(P, pga, pga.t[:, 0:nb], w_g.t[:, kc, oc * 128:(oc + 1) * 128], xT.t[:, kc, 0:nb], kc == 0, kc == 7, [w_g, xT])
            pgb = P.ps()
            for kc in range(8):
                _mm(P, pgb, pgb.t[:, 0:nb], w_g.t[:, kc, 1024 + oc * 128:1024 + (oc + 1) * 128], xT.t[:, kc, 0:nb], kc == 0, kc == 7, [w_g, xT])
            _act(P, sga[i2].t[:, 0:nb], pga.t[:, 0:nb], AF.Sigmoid, [pga, pcol], [sga[i2]], bias=pcol.t[:, PC_BGA + oc:PC_BGA + oc + 1])
            _act(P, sgb[i2].t[:, 0:nb], pgb.t[:, 0:nb], AF.Sigmoid, [pgb, pcol], [sgb[i2]], bias=pcol.t[:, PC_BGB + oc:PC_BGB + oc + 1])
            _tt(P, P.dve, t1[i2].t[:, 0:nb], pa.t[:, 0:nb], sga[i2].t[:, 0:nb], ALU.mult, [pa, sga[i2]], [t1[i2]])
            _stt(P, t2[i2].t[:, 0:nb], pb.t[:, 0:nb], pcol.t[:, PC_BB + oc:PC_BB + oc + 1], sgb[i2].t[:, 0:nb], ALU.add, ALU.mult,
                 [pb, pcol, sgb[i2]], [t2[i2]])
            _tt(P, P.pool, mT.t[:, oc, 0:nb], t1[i2].t[:, 0:nb], t2[i2].t[:, 0:nb], ALU.add, [t1[i2], t2[i2]], [mT])
        for t in range(ntile):
            tg = (b0 // 128) + t
            xb = xt[t]
            md = mid[st["m"] % 2]
            mh = mhi[st["m"] % 2]
            st["m"] += 1
            for half in range(2):
                psb = P.ps()
                for kc in range(8):
                    _mm(P, psb, psb.t[:, 0:512], mT.t[:, kc, t * 128:(t + 1) * 128], w_o.t[:, kc, half * 512:(half + 1) * 512],
                        kc == 0, kc == 7, [mT, w_o])
                _stt(P, rr.t[:, half * 512:(half + 1) * 512], xb.t[:, half * 512:(half + 1) * 512], DN_ALPHA, psb.t[:, 0:512],
                     ALU.mult, ALU.add, [xb, psb], [rr])
            def norm1(rstd, nmr, md=md):
                P.op(P.act, lambda e: e.activation(md.t[:, :], rr.t[:, :], AF.Identity, bias=nmr, scale=rstd), reads=[rr, tmp["sc"]], writes=[md])
            layer_norm_tile(P, rr, norm1, tmp, epsb.t[:, 0:1])
            _tt(P, P.dve, md.t[:, :], md.t[:, :], lng.t[:, :], ALU.mult, [md, lng], [md])
            _tt(P, P.pool, md.t[:, :], md.t[:, :], lnb.t[:, :], ALU.add, [md, lnb], [md])
            _copy(P, P.act, mh.t[:, :], md.t[:, :], [md], [mh])
            _tt(P, P.dve, mlo.t[:, :], md.t[:, :], mh.t[:, :], ALU.subtract, [md, mh], [mlo])
            if "mid_dbg" in T:
                P.dma(P.sp, T["mid_dbg"][tg * 128:(tg + 1) * 128, :], md.t[:, :], reads=[md], out_final=True)
            for srcb, dstb in ((mh, mTh), (mlo, mTl)):
                psb = P.ps()
                pbf = psb.t[:, :].bitcast(BF16)
                for kc in range(8):
                    P.op(P.pe, lambda e, kc=kc, pbf=pbf, srcb=srcb: e.transpose(pbf[:, kc * 128:(kc + 1) * 128], srcb.t[:, kc * 128:(kc + 1) * 128], identb),
                         reads=[srcb, cb], writes=[psb], sig=(kc == 7))
                _copy(P, P.act if srcb is mh else P.dve, dstb.t[:, :, t * 128:(t + 1) * 128], pbf.rearrange("p (c n) -> p c n", c=8), [psb], [dstb])
            psr = P.ps()
            k = 0
            for (a_, w_) in ((mTh, wr_hi), (mTh, wr_lo), (mTl, wr_hi)):
                for kc in range(8):
                    _mm(P, psr, psr.t[:, 0:256], a_.t[:, kc, t * 128:(t + 1) * 128], w_.t[:, kc, :], k == 0, k == 23, [a_, w_])
                    k += 1
            sc_, sel, selm, emask, gate, sv, junk = (rt[n] for n in ("scores", "sel", "selm", "emask", "gate", "sv", "junk"))
            _act(P, sc_.t[:, :], psr.t[:, 0:256], AF.Sigmoid, [psr], [sc_])
            _tt(P, P.dve, sel.t[:, :], sc_.t[:, :], brt.t[:, :], ALU.add, [sc_, brt], [sel])
            for g in range(8):
                P.op(P.dve, lambda e, g=g: e.max(m8.t[:, g, :], sel.t[:, g * 32:(g + 1) * 32]), reads=[sel], writes=[m8])
            _tt(P, P.dve, gs.t[:, :], m8.t[:, :, 0], m8.t[:, :, 1], ALU.add, [m8], [gs])
            P.op(P.dve, lambda e: e.max(g8s.t[:, :], gs.t[:, :]), reads=[gs], writes=[g8s])
            _ts(P, P.dve, gmask.t[:, :], gs.t[:, :], g8s.t[:, 3:4], None, ALU.is_ge, None, [gs, g8s], [gmask])
            _ts(P, P.dve, gneg.t[:, :], gmask.t[:, :], -1.0, 1e9, ALU.add, ALU.mult, [gmask], [gneg])
            for g in range(8):
                _ts(P, P.dve, selm.t[:, g * 32:(g + 1) * 32], sel.t[:, g * 32:(g + 1) * 32], gmask.t[:, g:g + 1], gneg.t[:, g:g + 1],
                    ALU.mult, ALU.add, [sel, gmask, gneg], [selm])
            P.op(P.dve, lambda e: e.max(m8.t[:, 0, :], selm.t[:, :]), reads=[selm], writes=[m8])
            _ts(P, P.dve, emask.t[:, :], selm.t[:, :], m8.t[:, 0, 7:8], None, ALU.is_ge, None, [selm, m8], [emask])
            _copy(P, P.pool, emb.t[:, :], emask.t[:, :], [emask], [emb])
            P.op(P.dve, lambda e: e.scalar_tensor_tensor(gate.t[:, :], sc_.t[:, :], 1.0, emask.t[:, :], ALU.mult, ALU.mult, accum_out=den.t[:, 0:1]),
                 reads=[sc_, emask], writes=[gate, den])
            P.op(P.dve, lambda e: e.reciprocal(den.t[:, 0:1], den.t[:, 0:1]), reads=[den], writes=[den])
            _ts(P, P.dve, gate.t[:, :], gate.t[:, :], den.t[:, 0:1], 2.5, ALU.mult, ALU.mult, [gate, den], [gate])
            pp = P.ps()
            _mm(P, pp, pp.t[:, 0:256], ustrict, emb.t[:, :], True, True, [cb, emb])
            pt_ = P.ps()
            _mm(P, pt_, pt_.t[:, 0:256], onesb.t[:, :], emb.t[:, :], True, True, [onesb, emb])
            _tt(P, P.dve, sv.t[:, :], pp.t[:, 0:256], cnt.t[:, :], ALU.add, [pp, cnt], [sv])
            _tt(P, P.pool, sv.t[:, :], sv.t[:, :], emask.t[:, :], ALU.mult, [sv, emask], [sv])
            _tt(P, P.dve, cnt.t[:, :], pt_.t[:, 0:256], cnt.t[:, :], ALU.add, [pt_, cnt], [cnt])
            P.op(P.dve, lambda e: e.max(s8.t[:, :], sv.t[:, :]), reads=[sv], writes=[s8])
            for k in range(8):
                P.op(P.dve, lambda e, k=k, tg=tg: e.scalar_tensor_tensor(junk.t[:, :], sv.t[:, :], s8.t[:, k:k + 1], gate.t[:, :], ALU.is_equal, ALU.mult,
                                                                     accum_out=gates_all.t[:, tg, k:k + 1]),
                     reads=[sv, s8, gate], writes=[junk, gates_all])
            _ts(P, P.dve, slots_all.t[:, tg, :], s8.t[:, :], -1.0, None, ALU.add, None, [s8], [slots_all])
            for k in range(8):
                P.dma(P.pool, None, None, reads=[slots_all, mh], writes=[scr],
                      fn=lambda h, k=k, tg=tg, mh=mh: h.indirect_dma_start(
                          out=T["xg_d"], out_offset=bass.IndirectOffsetOnAxis(ap=slots_all.t[:, tg, k:k + 1], axis=0),
                          in_=mh.t[:, :], in_offset=None))
        for j in range(2):
            pg = P.ps()
            for kc in range(8):
                _mm(P, pg, pg.t[:, 0:nb], w_s.t[:, kc, j * 128:(j + 1) * 128], mTh.t[:, kc, 0:nb], kc == 0, kc == 7, [w_s, mTh])
            pu = P.ps()
            for kc in range(8):
                _mm(P, pu, pu.t[:, 0:nb], w_s.t[:, kc, 256 + j * 128:256 + (j + 1) * 128], mTh.t[:, kc, 0:nb], kc == 0, kc == 7, [w_s, mTh])
            _act(P, gsh[j].t[:, 0:nb], pg.t[:, 0:nb], AF.Silu, [pg], [gsh[j]])
            _tt(P, P.dve, hsT.t[:, j, 0:nb], pu.t[:, 0:nb], gsh[j].t[:, 0:nb], ALU.mult, [pu, gsh[j]], [hsT])
        for t in range(ntile):
            tg = (b0 // 128) + t
            md = mid[(st["m"] - ntile + t) % 2]
            pr = pre[t % 2]
            for half in range(2):
                psb = P.ps()
                for j in range(2):
                    _mm(P, psb, psb.t[:, 0:512], hsT.t[:, j, t * 128:(t + 1) * 128], w_sd.t[:, j, half * 512:(half + 1) * 512], j == 0, j == 1,
                        [hsT, w_sd])
                _stt(P, pr.t[:, half * 512:(half + 1) * 512], md.t[:, half * 512:(half + 1) * 512], DN_ALPHA, psb.t[:, 0:512], ALU.mult, ALU.add,
                     [md, psb], [pr])
            P.dma(P.sp, T["pre_d"][tg * 128:(tg + 1) * 128, :], pr.t[:, :], reads=[pr], writes=[scr])
    P.pop_scope()


def pass_c(P, T, n_exp=NE):
    P.push_scope()
    NBLK = CAP // 128
    cb = P.sb("cb", [128, 128], BF16)
    P.dma(P.sp, cb.t[:, :], T["cb"][:, 0:128], writes=[cb])
    identb = cb.t[:, 0:128]
    NS = 2
    NW = 2
    sg_ = [P.sb("wsg%d" % i, [128, 8, DE], F32) for i in range(NS)]
    su_ = [P.sb("wsu%d" % i, [128, 8, DE], F32) for i in range(NS)]
    sd_ = [P.sb("wsd%d" % i, [128, 2, D], F32) for i in range(NS)]
    wg = [P.sb("wg%d" % i, [128, 8, DE], BF16) for i in range(NW)]
    wu = [P.sb("wu%d" % i, [128, 8, DE], BF16) for i in range(NW)]
    wd = [P.sb("wd%d" % i, [128, 2, D], BF16) for i in range(NW)]
    xg = [P.sb("xg%d" % i, [128, NBLK, D], BF16) for i in range(2)]
    xgT = [P.sb("xgT%d" % i, [128, 8, CAP], BF16) for i in range(2)]
    gsb = [P.sb("gsb%d" % i, [128, 2, CAP], F32) for i in range(2)]
    hTe = [P.sb("hTe%d" % i, [128, 2, CAP], BF16) for i in range(2)]
    yb = [P.sb("yb%d" % i, [128, D], BF16) for i in range(3)]
    scr = Buf(None, "scr_c")

    def loads(e):
        s = e % NS
        P.dma(P.sp, sg_[s].t[:, :, :], T["w_eg"][e].rearrange("(kc p) n -> p kc n", p=128), writes=[sg_[s]])
        P.dma(P.sp, su_[s].t[:, :, :], T["w_eu"][e].rearrange("(kc p) n -> p kc n", p=128), writes=[su_[s]])
        P.dma(P.sp, sd_[s].t[:, :, :], T["w_ed"][e].rearrange("(j p) n -> p j n", p=128), writes=[sd_[s]])
        P.dma(P.sp, xg[e % 2].t[:, :, :], T["xg_d"][e * CAP:(e + 1) * CAP, :].rearrange("(b p) n -> p b n", p=128), writes=[xg[e % 2]])

    def casts(e):
        s, i = e % NS, e % NW
        _copy(P, P.act, wg[i].t[:, :, :], sg_[s].t[:, :, :], [sg_[s]], [wg[i]])
        _copy(P, P.dve, wu[i].t[:, :, :], su_[s].t[:, :, :], [su_[s]], [wu[i]])
        _copy(P, P.pool, wd[i].t[:, :, :], sd_[s].t[:, :, :], [sd_[s]], [wd[i]])

    loads(0)
    loads(1)
    casts(0)
    yi = 0
    for e in range(n_exp):
        i, i2 = e % NW, e % 2
        for b in range(NBLK):
            psb = P.ps()
            pbf = psb.t[:, :].bitcast(BF16)
            for kc in range(8):
                P.op(P.pe, lambda ee, kc=kc, pbf=pbf, b=b, i2=i2: ee.transpose(pbf[:, kc * 128:(kc + 1) * 128], xg[i2].t[:, b, kc * 128:(kc + 1) * 128], identb),
                     reads=[xg[i2], cb], writes=[psb], sig=(kc == 7))
            _copy(P, P.act if b % 2 == 0 else P.dve, xgT[i2].t[:, :, b * 128:(b + 1) * 128], pbf.rearrange("p (c n) -> p c n", c=8), [psb], [xgT[i2]])
        if e + 1 < n_exp:
            casts(e + 1)
        for j in range(2):
            pg = P.ps()
            for kc in range(8):
                _mm(P, pg, pg.t[:, 0:CAP], wg[i].t[:, kc, j * 128:(j + 1) * 128], xgT[i2].t[:, kc, :], kc == 0, kc == 7, [wg[i], xgT[i2]])
            pu = P.ps()
            for kc in range(8):
                _mm(P, pu, pu.t[:, 0:CAP], wu[i].t[:, kc, j * 128:(j + 1) * 128], xgT[i2].t[:, kc, :], kc == 0, kc == 7, [wu[i], xgT[i2]])
            _act(P, gsb[i2].t[:, j, :], pg.t[:, 0:CAP], AF.Silu, [pg], [gsb[i2]])
            _tt(P, P.dve, hTe[i2].t[:, j, :], pu.t[:, 0:CAP], gsb[i2].t[:, j, :], ALU.mult, [pu, gsb[i2]], [hTe[i2]])
        if e + 2 < n_exp:
            loads(e + 2)
        for b in range(NBLK):
            y_ = yb[yi % 3]
            yi += 1
            for half in range(2):
                psb = P.ps()
                for j in range(2):
                    _mm(P, psb, psb.t[:, 0:512], hTe[i2].t[:, j, b * 128:(b + 1) * 128], wd[i].t[:, j, half * 512:(half + 1) * 512], j == 0, j == 1,
                        [hTe[i2], wd[i]])
                _copy(P, P.act if half == 0 else P.dve, y_.t[:, half * 512:(half + 1) * 512], psb.t[:, 0:512], [psb], [y_])
            r0 = e * CAP + b * 128
            P.dma(P.sp, T["ys_d"][r0:r0 + 128, :], y_.t[:, :], reads=[y_], writes=[scr])
    P.pop_scope()


def pass_d(P, T, G):
    P.push_scope()
    lng = P.sb("lng2", [128, D], F32)
    lnb = P.sb("lnb2", [128, D], F32)
    epsb = P.sb("epsb", [128, 1], F32)
    P.dma(P.sp, lng.t[:, :], T["ln2_g"].partition_broadcast(128), writes=[lng])
    P.dma(P.sp, lnb.t[:, :], T["ln2_b"].partition_broadcast(128), writes=[lnb])
    P.op(P.dve, lambda e: e.memset(epsb.t[:, :], LN_EPS), writes=[epsb])
    yk = [P.sb("yk%d" % i, [128, D], BF16) for i in range(16)]
    acc = [P.sb("acc%d" % i, [128, D], F32) for i in range(2)]
    yo = [P.sb("yo%d" % i, [128, D], F32) for i in range(2)]
    tmp = {"st6": P.sb("st6", [128, 2, 6], F32), "mv": P.sb("mv", [128, 2], F32), "sc": P.sb("sc", [128, 2], F32)}
    slots_all, gates_all = G["slots"], G["gates"]
    scr = Buf(None, "scr_d")
    for tg in range(NT):
        a = acc[tg % 2]
        o = yo[tg % 2]
        P.dma(P.sp, a.t[:, :], T["pre_d"][tg * 128:(tg + 1) * 128, :], reads=[scr], writes=[a])
        for k in range(8):
            y_ = yk[(tg * 8 + k) % 16]
            P.dma(P.pool, None, None, reads=[slots_all, scr], writes=[y_],
                  fn=lambda h, k=k, tg=tg, y_=y_: h.indirect_dma_start(
                      out=y_.t[:, :], out_offset=None, in_=T["ys_d"],
                      in_offset=bass.IndirectOffsetOnAxis(ap=slots_all.t[:, tg, k:k + 1], axis=0)))
        for k in range(8):
            y_ = yk[(tg * 8 + k) % 16]
            _stt(P, a.t[:, :], y_.t[:, :], gates_all.t[:, tg, k:k + 1], a.t[:, :], ALU.mult, ALU.add, [y_, gates_all, a], [a])
        def norm2(rstd, nmr, a=a, o=o):
            P.op(P.act, lambda e: e.activation(o.t[:, :], a.t[:, :], AF.Identity, bias=nmr, scale=rstd), reads=[a, tmp["sc"]], writes=[o])
        layer_norm_tile(P, a, norm2, tmp, epsb.t[:, 0:1])
        _tt(P, P.dve, o.t[:, :], o.t[:, :], lng.t[:, :], ALU.mult, [o, lng], [o])
        _tt(P, P.pool, o.t[:, :], o.t[:, :], lnb.t[:, :], ALU.add, [o, lnb], [o])
        dst = T["yp"][tg * 128:(tg + 1) * 128, :] if tg * 128 < TP else T["ys"]
        P.dma(P.sp, dst, o.t[:, :], reads=[o], out_final=True)
    P.pop_scope()


IN_SPECS_A = [
    ("xp", [TP, D], F32), ("xs", [128, D], F32), ("ck", [NSUB_S, PAST, 512], F32), ("cv", [NSUB_S, PAST, 512], F32),
    ("clf", [NSUB_S, PAST, 8], F32), ("sconv", [NSUB_S, 30, 512], F32), ("w_in", [D, N_IN], F32), ("b_in", [1, N_IN], F32),
    ("pcol", [128, PC_N], F32), ("cf", [128, 384], F32), ("cb", [128, 384], BF16),
]
IN_SPECS_B = [
    ("w_a", [512, D], F32), ("w_b", [512, D], F32), ("w_out", [D, D], F32), ("ln1_g", [1, D], F32), ("ln1_b", [1, D], F32),
    ("w_router", [D, NE], F32), ("b_router", [1, NE], F32), ("w_sg", [D, DE], F32), ("w_su", [D, DE], F32), ("w_sd", [DE, D], F32),
    ("base1", [128, NE], F32), ("tokid", [128, NT], I32),
]
IN_SPECS_C = [
    ("w_eg", [NE, D, DE], F32), ("w_eu", [NE, D, DE], F32), ("w_ed", [NE, DE, D], F32), ("ln2_g", [1, D], F32), ("ln2_b", [1, D], F32),
]
OUT_SPECS = [
    ("yp", [TP, D]), ("ys", [128, D]), ("kp", [TP, 512]), ("vp", [TP, 512]), ("lfp", [TP, 8]), ("cvp", [NSEQ_P, 30, 512]),
    ("ks", [128, 512]), ("vs", [128, 512]), ("lfs", [128, 8]), ("cvs", [NSUB_S, 30, 512]),
]


def build(stage="A", NB=512, NBB=256, debug=False):
    nc = bass.Bass("TRN2", target_bir_lowering=False)
    T = {}
    specs = list(IN_SPECS_A)
    if stage >= "B":
        specs += IN_SPECS_B
    if stage >= "C":
        specs += IN_SPECS_C
    for name, shape, dt in specs:
        T[name] = nc.dram_tensor(name, shape, dt, kind="ExternalInput").ap()
    for name, shape in OUT_SPECS:
        T[name] = nc.dram_tensor(name, shape, F32, kind="ExternalOutput").ap()
    dk = "ExternalOutput" if debug else "Internal"
    T["attn_d"] = nc.dram_tensor("attn_d", [512, TT], BF16, kind=dk).ap()
    T["h_d"] = nc.dram_tensor("h_d", [512, TT], BF16, kind=dk).ap()
    T["c3_d"] = nc.dram_tensor("c3_d", [8, 3, 512], BF16, kind="Internal").ap()
    P = Prog(nc)
    G = {}
    if stage >= "B":
        T["xg_d"] = nc.dram_tensor("xg_d", [NSLOT, D], BF16, kind="Internal").ap()
        T["pre_d"] = nc.dram_tensor("pre_d", [TT, D], F32, kind="Internal").ap()
        if debug:
            T["mid_dbg"] = nc.dram_tensor("mid_dbg", [TT, D], F32, kind="ExternalOutput").ap()
            T["slots_dbg"] = nc.dram_tensor("slots_dbg", [128, NT * 8], I32, kind="ExternalOutput").ap()
            T["gates_dbg"] = nc.dram_tensor("gates_dbg", [128, NT * 8], F32, kind="ExternalOutput").ap()
        G["slots"] = P.sb("slots_all", [128, NT, 8], I32)
        G["gates"] = P.sb("gates_all", [128, NT, 8], F32)
    pass_a(P, T, NB)
    if stage >= "B":
        pass_b(P, T, G, NBB)
        if stage >= "C":
            T["ys_d"] = nc.dram_tensor("ys_d", [NSLOT, D], BF16, kind="Internal").ap()
            pass_c(P, T)
            pass_d(P, T, G)
        if debug:
            P.dma(P.sp, T["slots_dbg"], G["slots"].t[:, :, :].rearrange("p a b -> p (a b)"), reads=[G["slots"]], out_final=True)
            P.dma(P.sp, T["gates_dbg"], G["gates"].t[:, :, :].rearrange("p a b -> p (a b)"), reads=[G["gates"]], out_final=True)
    P.finish()
    print('[build] ops', P.n_ops, 'sim_us %.1f' % getattr(P, 'sim_time', 0.0), flush=True)
    return nc, [s[0] for s in specs]


def host_consts(inp):
    b_in = np.asarray(inp["b_in"])[0]
    pcol = np.zeros((128, PC_N), np.float32)
    col = lambda v, n: np.ascontiguousarray(np.asarray(v).reshape(n, 128).T)
    pcol[:, PC_BQ:PC_BQ + 4] = col(b_in[OFF_Q:OFF_K], 4)
    pcol[:, PC_BGLU:PC_BGLU + 8] = col(b_in[OFF_GLU:OFF_GA], 8)
    pcol[:, PC_BGA:PC_BGA + 8] = col(b_in[OFF_GA:OFF_GB], 8)
    pcol[:, PC_BGB:PC_BGB + 8] = col(b_in[OFF_GB:N_IN], 8)
    pcol[:, PC_BB:PC_BB + 8] = col(inp["b_b"][0], 8)
    pcol[:, PC_CB:PC_CB + 4] = col(inp["conv_b"][0], 4)
    pcol[:, PC_CG:PC_CG + 4] = col(inp["conv_ln_g"][0], 4)
    pcol[:, PC_CBE:PC_CBE + 4] = col(inp["conv_ln_b"][0], 4)
    cw = np.asarray(inp["conv_w"])[0]
    pcol[:, PC_CW:PC_CW + 124] = cw.T.reshape(4, 128, KW).transpose(1, 0, 2).reshape(128, 4 * KW)
    pcol[0:8, PC_BF] = b_in[OFF_F:OFF_GLU]
    pcol[:, PC_BK:PC_BK + 4] = col(b_in[OFF_K:OFF_V], 4)
    cf = np.concatenate([np.eye(128, dtype=np.float32), np.tril(np.ones((128, 128), np.float32), -1),
                         np.ones((128, 128), np.float32)], axis=1)
    cb = np.concatenate([np.eye(128, dtype=np.float32), np.triu(np.ones((128, 128), np.float32)),
                         np.triu(np.ones((128, 128), np.float32), 1)], axis=1).astype(ml_dtypes.bfloat16)
    base1 = np.ascontiguousarray(np.broadcast_to((np.arange(NE, dtype=np.float32) * CAP + 1.0)[None, :], (128, NE)))
    tokid = np.ascontiguousarray((np.arange(NT, dtype=np.int32)[None, :] * 128 + np.arange(128, dtype=np.int32)[:, None]).astype(np.int32))
    return pcol, cf, cb, base1, tokid


def core_inputs(inp, c, consts):
    pcol, cf, cb, base1, tokid = consts
    m = {
        "xp": np.asarray(inp["x_prompt"])[2 * c:2 * c + 2].reshape(TP, D),
        "xs": np.asarray(inp["x_sample"])[4 * c:4 * c + 4].reshape(128, D),
        "ck": np.asarray(inp["cache_k"])[0, 4 * c:4 * c + 4].reshape(NSUB_S, PAST, 512),
        "cv": np.asarray(inp["cache_v"])[0, 4 * c:4 * c + 4].reshape(NSUB_S, PAST, 512),
        "clf": np.asarray(inp["cache_logf"])[0, 4 * c:4 * c + 4],
        "sconv": np.asarray(inp["state_conv"])[0, 4 * c:4 * c + 4],
        "w_in": np.asarray(inp["w_in"])[0], "b_in": np.asarray(inp["b_in"]),
        "pcol": pcol, "cf": cf, "cb": cb, "base1": base1, "tokid": tokid,
        "w_a": np.asarray(inp["w_a"])[0], "w_b": np.asarray(inp["w_b"])[0], "w_out": np.asarray(inp["w_out"])[0],
        "ln1_g": np.asarray(inp["ln1_g"]), "ln1_b": np.asarray(inp["ln1_b"]),
        "w_router": np.asarray(inp["w_router"])[0], "b_router": np.asarray(inp["b_router"]),
        "w_sg": np.asarray(inp["w_s_gate"])[0], "w_su": np.asarray(inp["w_s_up"])[0], "w_sd": np.asarray(inp["w_s_down"])[0],
        "w_eg": np.asarray(inp["w_e_gate"])[0], "w_eu": np.asarray(inp["w_e_up"])[0], "w_ed": np.asarray(inp["w_e_down"])[0],
        "ln2_g": np.asarray(inp["ln2_g"]), "ln2_b": np.asarray(inp["ln2_b"]),
    }
    return m


def assemble(results):
    cat = lambda k: np.concatenate([r[k] for r in results], axis=0)
    yp = cat("yp").reshape(16, SEQ, D)
    ys = cat("ys").reshape(32, TS, D)
    kp = cat("kp").reshape(1, 16, SEQ, H, DH)
    vp = cat("vp").reshape(1, 16, SEQ, H, DH)
    lfp = cat("lfp").reshape(1, 16, SEQ, H)
    cvp = cat("cvp").reshape(1, 16, 30, CC)
    ks = cat("ks").reshape(1, 32, TS, H, DH)
    vs = cat("vs").reshape(1, 32, TS, H, DH)
    lfs = cat("lfs").reshape(1, 32, TS, H)
    cvs = cat("cvs").reshape(1, 32, 30, CC)
    return (yp, ys, kp, vp, lfp, cvp, ks, vs, lfs, cvs)


def kernel(**inputs):
    nc, names = build("C")
    consts = host_consts(inputs)
    in_maps = []
    for c in range(NCORES):
        m = core_inputs(inputs, c, consts)
        in_maps.append({k: (m[k] if m[k].flags["C_CONTIGUOUS"] else np.ascontiguousarray(m[k])) for k in names})
    res = run_bass_kernel_spmd(nc, in_maps, core_ids=list(range(NCORES)))
    return assemble(res.results)
```

```python
from contextlib import ExitStack

import numpy as np
import ml_dtypes
import concourse.bass as bass
import concourse.mybir as mybir
from concourse.bass_utils import run_bass_kernel_spmd

F32 = mybir.dt.float32
BF16 = mybir.dt.bfloat16
I32 = mybir.dt.int32
U32 = mybir.dt.uint32
AF = mybir.ActivationFunctionType
ALU = mybir.AluOpType


class Tr:
    __slots__ = ("sem", "count", "name")

    def __init__(self, sem, name):
        self.sem = sem
        self.count = 0
        self.name = name


class Buf:
    __slots__ = ("t", "w", "r", "name")

    def __init__(self, t, name=""):
        self.t = t
        self.w = None
        self.r = {}
        self.name = name


class Eng:
    def __init__(self, name, tr):
        self.name = name
        self.tr = tr
        self.ops = []
        self.waited = {}
        self.dma_trs = []
        self.dma_i = 0
        self.deferred = []


class Prog:
    def __init__(self, nc, n_dma=(28, 28, 6)):
        self.nc = nc
        self.stack = ExitStack()
        self.scopes = []
        mk = lambda n: Tr(self.stack.enter_context(nc.semaphore(n)), n)
        self.pe = Eng("pe", mk("s_pe"))
        self.act = Eng("act", mk("s_act"))
        self.dve = Eng("dve", mk("s_dve"))
        self.pool = Eng("pool", mk("s_pool"))
        self.sp = Eng("sp", mk("s_sp"))
        self.engs = [self.pe, self.act, self.dve, self.pool, self.sp]
        for e, n in zip((self.sp, self.pool, self.act), n_dma):
            e.dma_trs = [mk("d_%s%d" % (e.name, i)) for i in range(n)]
        self.banks = [Buf(self.stack.enter_context(nc.psum_tensor("psb%d" % i, [128, 512], F32)), "ps%d" % i)
                      for i in range(8)]
        self.bank_i = 0
        self.final = []
        self.n_ops = 0
        self.sched = True
        self.pending = []

    def sb(self, name, shape, dtype):
        st = self.scopes[-1] if self.scopes else self.stack
        self.n_sb = getattr(self, "n_sb", 0) + 1
        return Buf(st.enter_context(self.nc.sbuf_tensor("sb%d_%s" % (self.n_sb, name), list(shape), dtype)), name)

    def push_scope(self):
        self.scopes.append(ExitStack())

    def pop_scope(self):
        self.barrier()
        self.flush()
        self.scopes.pop().close()

    def ps(self, lo=0, hi=8):
        n = hi - lo
        b = self.banks[lo + (self.bank_i % n)]
        self.bank_i += 1
        return b

    def _deps(self, eng, reads, writes, extra=()):
        deps = {}

        def add(tok):
            if tok is None:
                return
            tr, v = tok
            if deps.get(tr, 0) < v:
                deps[tr] = v

        for b in reads:
            add(b.w)
        for b in writes:
            add(b.w)
            for tr, v in b.r.items():
                add((tr, v))
        for tok in extra:
            add(tok)
        waits = []
        for tr, v in deps.items():
            if eng is self.pe and tr is eng.tr:
                continue
            if eng.waited.get(tr, 0) >= v:
                continue
            eng.waited[tr] = v
            waits.append((tr, v))
        return waits

    @staticmethod
    def _mark(tok, reads, writes):
        tr, v = tok
        for b in reads:
            if b.r.get(tr, 0) < v:
                b.r[tr] = v
        for b in writes:
            b.w = tok
            b.r = {}

    def op(self, eng, fn, reads=(), writes=(), sig=True, c=0.3):
        if eng is self.pe and c == 0.3:
            c = 0.07
        if self.sched:
            self.pending.append(("op", eng, (fn, tuple(reads), tuple(writes), sig), tuple(reads), tuple(writes), c, 0))
            return None
        return self._op_now(eng, fn, reads, writes, sig)

    def dma(self, eng, out, in_, reads=(), writes=(), out_final=False, slow=False, fn=None, nbytes=None, **kw):
        if nbytes is None:
            try:
                ap = out if out is not None else None
                nbytes = 1
                for s_ in ap.shape:
                    nbytes *= int(s_)
                nbytes *= 2 if ap.dtype == BF16 else 4
            except Exception:
                nbytes = 262144
        if self.sched:
            self.pending.append(("dma", eng, (out, in_, tuple(reads), tuple(writes), out_final, slow, fn, kw), tuple(reads), tuple(writes),
                                 1.1 if fn is not None else 0.08, nbytes))
            return None
        return self._dma_now(eng, out, in_, reads, writes, out_final, slow, fn, **kw)

    def _op_now(self, eng, fn, reads=(), writes=(), sig=True):
        sig = True
        waits = self._deps(eng, reads, writes)
        tr = eng.tr
        if sig:
            tr.count += 1
            tok = (tr, tr.count)
        else:
            tok = None

        def run(h, waits=waits, fn=fn, sig=sig, tr=tr):
            for tr2, v in waits:
                h.wait_ge(tr2.sem, v)
            ins = fn(h)
            if sig:
                ins.then_inc(tr.sem, 1)

        eng.ops.append(run)
        self.n_ops += 1
        if tok is None:
            eng.deferred.append((reads, writes))
        else:
            for r_, w_ in eng.deferred:
                self._mark(tok, r_, w_)
            eng.deferred = []
            self._mark(tok, reads, writes)
        return tok

    def _dma_now(self, eng, out, in_, reads=(), writes=(), out_final=False, slow=False, fn=None, **kw):
        tr = eng.dma_trs[eng.dma_i % len(eng.dma_trs)]
        eng.dma_i += 1
        extra = [(tr, tr.count)] if tr.count else []
        waits = self._deps(eng, reads, writes, extra)
        tr.count += 16
        tok = (tr, tr.count)
        if slow:
            kw["allow_slow_non_contiguous"] = True

        def run(h, waits=waits, tr=tr, out=out, in_=in_, kw=kw, fn=fn):
            for tr2, v in waits:
                h.wait_ge(tr2.sem, v)
            if fn is not None:
                ins = fn(h)
            else:
                ins = h.dma_start(out=out, in_=in_, **kw)
            ins.then_inc(tr.sem, 16)

        eng.ops.append(run)
        self.n_ops += 1
        self._mark(tok, reads, writes)
        if out_final:
            self.final.append(tok)
        return tok

    def drain(self):
        pend = self.pending
        self.pending = []
        n = len(pend)
        if n == 0:
            return
        import heapq
        lastw, readers = {}, {}
        close = list(range(n))
        nxt = {}
        for i in range(n - 1, -1, -1):
            kind, eng = pend[i][0], pend[i][1]
            pass
        deps = [None] * n
        succ = [[] for _ in range(n)]
        indeg = [0] * n
        for i, (_, eng, _, reads, writes, _, _) in enumerate(pend):
            d = set()
            for b in reads:
                w = lastw.get(id(b))
                if w is not None:
                    d.add(w)
            for b in writes:
                w = lastw.get(id(b))
                if w is not None:
                    d.add(w)
                for r_ in readers.get(id(b), ()):
                    d.add(r_)
            d.discard(i)
            if not (pend[i][0] == "op" and eng is self.pe):
                d = {close[w] for w in d}
            else:
                d = {(w if pend[w][1] is self.pe else close[w]) for w in d}
            d.discard(i)
            deps[i] = d
            for j in d:
                succ[j].append(i)
            indeg[i] = len(d)
            for b in reads:
                readers.setdefault(id(b), []).append(i)
            for b in writes:
                lastw[id(b)] = i
                readers[id(b)] = []
        fin = [0.0] * n
        eng_free = {id(e): 0.0 for e in self.engs}
        ready = {id(e): [] for e in self.engs}
        for i in range(n):
            if indeg[i] == 0:
                heapq.heappush(ready[id(pend[i][1])], (0.0, i))
        dma_free = 0.0
        order = []
        LAT = 0.15
        WIN = 6000
        done = 0
        lo = 0
        scheduled = [False] * n
        while done < n:
            best = None
            for e in self.engs:
                h = ready[id(e)]
                if not h:
                    continue
                cand = None
                tmp_ = []
                k = 0
                while h and k < 8:
                    rt, i = heapq.heappop(h)
                    tmp_.append((rt, i))
                    k += 1
                    if i - lo > WIN:
                        continue
                    st_ = max(rt, eng_free[id(e)])
                    key = (st_, i)
                    if cand is None or key < cand[0]:
                        cand = (key, rt, i)
                for x in tmp_:
                    heapq.heappush(h, x)
                if cand is not None and (best is None or cand[0] < best[0][0]):
                    best = (cand, e)
            if best is None:
                cands = [(h[0][1], e) for e in self.engs for h in [ready[id(e)]] if h]
                i_min = None
                for e in self.engs:
                    for rt, i in ready[id(e)]:
                        if i_min is None or i < i_min[0]:
                            i_min = (i, rt, e)
                i, rt, e = i_min
                best = ((((max(rt, eng_free[id(e)])), i), rt, i), e)
            (key, rt, i), e = best
            h = ready[id(e)]
            h.remove((rt, i))
            heapq.heapify(h)
            kind, _, _, _, _, cost, nbytes = pend[i]
            st_ = key[0]
            if kind == "dma":
                eng_free[id(e)] = st_ + cost
                t0_ = max(st_ + cost, dma_free)
                dma_free = t0_ + nbytes / 250000.0
                fin[i] = dma_free + 1.8
            else:
                eng_free[id(e)] = st_ + cost
                fin[i] = st_ + cost
            scheduled[i] = True
            order.append(i)
            done += 1
            while lo < n and scheduled[lo]:
                lo += 1
            for j in succ[i]:
                indeg[j] -= 1
                if indeg[j] == 0:
                    rt_j = 0.0
                    for d_ in deps[j]:
                        l_ = 0.0 if pend[d_][1] is pend[j][1] and pend[j][1] is self.pe else LAT
                        if fin[d_] + l_ > rt_j:
                            rt_j = fin[d_] + l_
                    heapq.heappush(ready[id(pend[j][1])], (rt_j, j))
        self.sim_time = getattr(self, "sim_time", 0.0) + max(fin)
        for i in order:
            kind, eng, args, _, _, _, _ = pend[i]
            if kind == "op":
                fn, reads, writes, sig = args
                self._op_now(eng, fn, reads, writes, sig)
            else:
                out, in_, reads, writes, out_final, slow, fn, kw = args
                self._dma_now(eng, out, in_, reads, writes, out_final, slow, fn, **kw)

    def barrier(self):
        self.drain()
        toks = []
        for e in self.engs:
            if e.tr.count:
                toks.append((e.tr, e.tr.count))
            for tr in e.dma_trs:
                if tr.count:
                    toks.append((tr, tr.count))
        for e in self.engs:
            waits = []
            for tr, v in toks:
                if tr is e.tr:
                    continue
                if e.waited.get(tr, 0) >= v:
                    continue
                e.waited[tr] = v
                waits.append((tr, v))
            if waits:
                def run(h, waits=waits):
                    for tr2, v in waits:
                        h.wait_ge(tr2.sem, v)
                e.ops.append(run)

    def flush(self):
        nc = self.nc
        pend = {e.name: e.ops for e in self.engs}
        for e in self.engs:
            e.ops = []
        with nc.Block() as block:
            @block.tensor
            def _(h):
                for f in pend["pe"]:
                    f(h)

            @block.scalar
            def _(h):
                for f in pend["act"]:
                    f(h)

            @block.vector
            def _(h):
                for f in pend["dve"]:
                    f(h)

            @block.gpsimd
            def _(h):
                for f in pend["pool"]:
                    f(h)

            @block.sync
            def _(h):
                for f in pend["sp"]:
                    f(h)

    def finish(self):
        self.drain()
        waits = []
        for tr, v in self.final:
            if self.sp.waited.get(tr, 0) < v:
                self.sp.waited[tr] = v
                waits.append((tr, v))

        def run(h, waits=waits):
            for tr2, v in waits:
                h.wait_ge(tr2.sem, v)

        self.sp.ops.append(run)
        self.barrier()
        self.flush()
        while self.scopes:
            self.scopes.pop().close()
        self.stack.close()


D = 1024
NCORES = 8
SEQ = 2048
NSEQ_P = 2
NSUB_S = 4
TS = 32
PAST = 1024
H = 8
DH = 64
CC = 512
KW = 31
NE = 256
DE = 256
OFF_Q, OFF_K, OFF_V, OFF_F, OFF_GLU, OFF_GA, OFF_GB, N_IN = 0, 512, 1024, 1536, 1544, 2568, 3592, 4616
NA = OFF_GA
TP = NSEQ_P * SEQ
TT = TP + 128
DN_ALPHA = 2.0 ** 0.25
LN_EPS = 1e-5

PC_BQ, PC_BGLU, PC_BGA, PC_BGB, PC_BB, PC_CB, PC_CG, PC_CBE, PC_CW, PC_BF, PC_BK, PC_N = 0, 4, 12, 20, 28, 36, 40, 44, 48, 172, 173, 177


def _n(ap):
    n = 1
    for s_ in ap.shape[1:]:
        n *= int(s_)
    return n


def _mm(P, psb, out_ap, lhsT, rhs, start, stop, reads, sig=None):
    c = max(_n(rhs), 64) / 2400.0 * (4.0 if lhsT.dtype == F32 else 1.0) + 0.01
    P.op(P.pe, lambda e: e.matmul(out_ap, lhsT, rhs, start=start, stop=stop), reads=reads, writes=[psb],
         sig=stop if sig is None else sig, c=c)


def _act(P, out_ap, in_ap, func, reads, writes, bias=None, scale=1.0):
    c = _n(out_ap) / 1400.0 + 0.22
    if bias is None:
        P.op(P.act, lambda e: e.activation(out_ap, in_ap, func, scale=scale), reads=reads, writes=writes, c=c)
    else:
        P.op(P.act, lambda e: e.activation(out_ap, in_ap, func, bias=bias, scale=scale), reads=reads, writes=writes, c=c)


def _tt(P, eng, out_ap, a, b, op, reads, writes):
    c = _n(out_ap) / (960.0 if eng is P.dve else 600.0) + (0.12 if eng is P.dve else 0.3)
    P.op(eng, lambda e: e.tensor_tensor(out_ap, a, b, op), reads=reads, writes=writes, c=c)


def _stt(P, out_ap, in0, scalar, in1, op0, op1, reads, writes):
    P.op(P.dve, lambda e: e.scalar_tensor_tensor(out_ap, in0, scalar, in1, op0, op1), reads=reads, writes=writes, c=_n(out_ap) / 960.0 + 0.12)


def _ts(P, eng, out_ap, in0, s1, s2, op0, op1, reads, writes):
    c = _n(out_ap) / 960.0 + 0.12
    if s2 is None:
        P.op(eng, lambda e: e.tensor_scalar(out_ap, in0, s1, None, op0), reads=reads, writes=writes, c=c)
    else:
        P.op(eng, lambda e: e.tensor_scalar(out_ap, in0, s1, s2, op0, op1), reads=reads, writes=writes, c=c)


def _copy(P, eng, out_ap, in_ap, reads, writes):
    if eng is P.act:
        P.op(eng, lambda e: e.copy(out_ap, in_ap), reads=reads, writes=writes, c=_n(out_ap) / 1400.0 + 0.22)
    else:
        P.op(eng, lambda e: e.tensor_copy(out_ap, in_ap), reads=reads, writes=writes,
             c=_n(out_ap) / (960.0 if eng is P.dve else 280.0) + (0.12 if eng is P.dve else 0.3))


def load_w_cast(P, dst, src_ap, kchunks, ncols, col0=0):
    v = src_ap.rearrange("(kc p) n -> p kc n", p=128)
    c = 0
    while c < ncols:
        n = min(2048, ncols - c)
        P.dma(P.pool, dst.t[:, :, c:c + n], v[:, :, col0 + c:col0 + c + n], reads=[], writes=[dst])
        c += n


def pass_a(P, T, NB):
    nc = P.nc
    P.push_scope()
    cf = P.sb("cf", [128, 384], F32)
    cb = P.sb("cb", [128, 256], BF16)
    pcol = P.sb("pcol", [128, PC_N], F32)
    w_in = P.sb("w_in_a", [128, 8, NA], BF16)
    bkv = P.sb("bkv", [128, 1024], F32)
    bq8 = P.sb("bq8", [128, 4], F32)
    nbf = P.sb("nbf", [8, 1], F32)
    ones8 = P.sb("ones8", [8, 512], F32)
    onesb = P.sb("onesb", [128, 128], BF16)
    epsb = P.sb("epsb", [128, 1], F32)
    oneb = P.sb("oneb", [128, 1], F32)
    scr = Buf(None, "scratch")
    c3d = Buf(None, "c3d")
    P.dma(P.sp, cf.t[:, :], T["cf"], writes=[cf])
    P.dma(P.sp, cb.t[:, :], T["cb"][:, 0:256], writes=[cb])
    P.dma(P.sp, pcol.t[:, :], T["pcol"], writes=[pcol])
    P.dma(P.sp, bkv.t[:, :], T["b_in"][:, OFF_K:OFF_F].partition_broadcast(128), writes=[bkv])
    load_w_cast(P, w_in, T["w_in"], 8, NA)
    P.op(P.act, lambda e: e.mul(bq8.t[:, :], pcol.t[:, PC_BQ:PC_BQ + 4], 0.125), reads=[pcol], writes=[bq8])
    P.op(P.act, lambda e: e.mul(nbf.t[:, :], pcol.t[0:8, PC_BF:PC_BF + 1], -1.0), reads=[pcol], writes=[nbf])
    P.op(P.dve, lambda e: e.memset(ones8.t[:, :], 1.0), writes=[ones8])
    P.op(P.dve, lambda e: e.memset(onesb.t[:, :], 1.0), writes=[onesb])
    P.op(P.dve, lambda e: e.memset(epsb.t[:, :], LN_EPS), writes=[epsb])
    P.op(P.dve, lambda e: e.memset(oneb.t[:, :], 1.0), writes=[oneb])
    identf = cf.t[:, 0:128]
    lstrict = cf.t[:, 128:256]
    onesf = cf.t[:, 256:384]
    identb = cb.t[:, 0:128]
    tri = cb.t[:, 128:256]

    kT = P.sb("kT", [128, 4, SEQ], BF16)
    vsb = P.sb("vsb", [128, SEQ // 128, 512], BF16)
    negc = P.sb("negc", [128, SEQ // 128, 8], F32)
    xt = [P.sb("xt%d" % i, [128, 1024], F32) for i in range(2)]
    xT = P.sb("xT", [128, 8, NB], BF16)
    qz = P.sb("qz", [128, 8, NB], BF16)
    c3pad = P.sb("c3pad", [128, 8, NB], BF16)
    uT = P.sb("uT", [128, 4, 30 + NB], F32)
    sg = [P.sb("sg%d" % i, [128, NB], F32) for i in range(2)]
    kvf = [P.sb("kvf%d" % i, [128, 1024], F32) for i in range(2)]
    kbf = [P.sb("kbf%d" % i, [128, 512], BF16) for i in range(2)]
    lfneg = P.sb("lfneg", [8, NB], F32)
    cblk = P.sb("cblk", [8, NB], F32)
    carry = P.sb("carry", [8, 1], F32)
    c3p = P.sb("c3p", [8, 3, NB], BF16)
    ctmp = P.sb("ctmp", [8, NB], F32)
    et = ctmp
    lftok = P.sb("lftok", [128, NB // 128, 8], F32)
    pt = [P.sb("pt%d" % i, [128, NB], BF16) for i in range(3)]
    attnT = P.sb("attnT", [64, 8, NB], BF16)
    acc = [P.sb("cacc%d" % i, [128, NB], F32) for i in range(2)]
    rden = acc
    hc = P.sb("hc", [128, 4, NB], F32)
    hsq = P.sb("hsq", [128, NB], F32)
    mean = P.sb("mean", [128, NB], F32)
    rstd = P.sb("rstd", [128, NB], F32)
    hn = sg
    hT = P.sb("hT", [128, 4, NB], BF16)
    ubf = P.sb("ubf", [128, 4, 30 + NB], BF16)
    diag = [P.sb("diag%d" % i, [128, 128], BF16) for i in range(6)]
    cvo = P.sb("cvo", [32, 512], F32)
    P.op(P.pool, lambda e: e.memset(qz.t[:, :, :], 0.0), writes=[qz])
    P.op(P.pool, lambda e: e.memset(c3pad.t[:, :, :], 0.0), writes=[c3pad])

    st = {"pt": 0, "x": 0, "kv": 0, "dg": 0}

    def project_feature(col0, nfeat, nb, evac):
        psb = P.ps()
        for kc in range(8):
            _mm(P, psb, psb.t[0:nfeat, 0:nb], w_in.t[:, kc, col0:col0 + nfeat], xT.t[:, kc, 0:nb], kc == 0, kc == 7,
                [w_in, xT])
        evac(psb)

    def attention(h, qc0, nq, ktiles):
        ob = P.banks[4 + (h % 2)]
        db = P.banks[6 + (h % 2)]
        n = len(ktiles)
        for i, kt in enumerate(ktiles):
            nk, q0 = kt["nk"], kt["q0"]
            w = nq - q0
            sb_ = P.ps(0, 4)
            _mm(P, sb_, sb_.t[0:nk, 0:w], kt["kT"], qz.t[:, h, qc0 + q0:qc0 + nq], True, False, kt["reads"] + [qz])
            _mm(P, sb_, sb_.t[0:nk, 0:w], onesb.t[:, 0:nk], c3pad.t[:, h, qc0 + q0:qc0 + nq], False, True,
                [onesb, c3pad])
            p = pt[st["pt"] % 3]
            st["pt"] += 1
            _act(P, p.t[0:nk, 0:w], sb_.t[0:nk, 0:w], AF.Exp, kt["reads"] + [sb_], [p], bias=kt["bias"])
            if kt["tri"]:
                tw = min(nk, w)
                _tt(P, P.pool, p.t[0:nk, 0:tw], p.t[0:nk, 0:tw], tri[0:nk, 0:tw], ALU.mult, [p, cb], [p])
            _mm(P, ob, ob.t[0:64, q0:nq], kt["v"], p.t[0:nk, 0:w], i == 0, i == n - 1, kt["reads"] + [p])
            _mm(P, db, db.t[0:64, q0:nq], onesb.t[0:nk, 0:64], p.t[0:nk, 0:w], i == 0, i == n - 1, [onesb, p])
        rd = rden[h % 2]
        _act(P, rd.t[0:64, 0:nq], db.t[0:64, 0:nq], AF.Ln, [db], [rd])
        _act(P, rd.t[0:64, 0:nq], rd.t[0:64, 0:nq], AF.Exp, [rd], [rd], scale=-1.0)
        _tt(P, P.dve, attnT.t[:, h, qc0:qc0 + nq], ob.t[0:64, 0:nq], rd.t[0:64, 0:nq], ALU.mult, [ob, rd], [attnT])


    def conv_ln(nb, uview, accview, c_list=range(4)):
        ps_s = P.ps()
        ps_q = P.ps()
        for c in range(4):
            _copy(P, P.pool, ubf.t[:, c, :], uT.t[:, c, :], [uT], [ubf])
        for c in range(4):
            psc = P.ps()
            for j in range(KW):
                dg = diag[st["dg"] % len(diag)]
                st["dg"] += 1
                col = pcol.t[:, PC_CW + c * KW + j:PC_CW + c * KW + j + 1]
                P.op(P.act, lambda e, dg=dg, col=col: e.activation(dg.t[:, :], identb, AF.Identity, scale=col), reads=[cb, pcol], writes=[dg], c=0.32)
                _mm(P, psc, accview(psc), dg.t[:, :], uview(c, j), j == 0, j == KW - 1, [dg, ubf], sig=True)
            _act(P, hc.t[:, c, 0:nb], psc.t[:, 0:nb], AF.Identity, [psc, pcol], [hc], bias=pcol.t[:, PC_CB + c:PC_CB + c + 1])
            P.op(P.act, lambda e, c=c: e.activation(hsq.t[:, 0:nb], hc.t[:, c, 0:nb], AF.Square), reads=[hc], writes=[hsq], c=nb / 1400.0 + 0.22)
            _mm(P, ps_s, ps_s.t[:, 0:nb], onesf, hc.t[:, c, 0:nb], c == 0, c == 3, [cf, hc], sig=True)
            _mm(P, ps_q, ps_q.t[:, 0:nb], onesf, hsq.t[:, 0:nb], c == 0, c == 3, [cf, hsq], sig=True)
        P.op(P.act, lambda e: e.mul(mean.t[:, 0:nb], ps_s.t[:, 0:nb], 1.0 / CC), reads=[ps_s], writes=[mean])
        m2 = hn[0]
        _tt(P, P.dve, m2.t[:, 0:nb], mean.t[:, 0:nb], mean.t[:, 0:nb], ALU.mult, [mean], [m2])
        _stt(P, rstd.t[:, 0:nb], ps_q.t[:, 0:nb], 1.0 / CC, m2.t[:, 0:nb], ALU.mult, ALU.subtract, [ps_q, m2], [rstd])
        _act(P, rstd.t[:, 0:nb], rstd.t[:, 0:nb], AF.Sqrt, [rstd], [rstd], bias=epsb.t[:, 0:1])
        P.op(P.dve, lambda e: e.reciprocal(rstd.t[:, 0:nb], rstd.t[:, 0:nb]), reads=[rstd], writes=[rstd])
        for c in range(4):
            x_ = hn[c % 2]
            _tt(P, P.dve, x_.t[:, 0:nb], hc.t[:, c, 0:nb], mean.t[:, 0:nb], ALU.subtract, [hc, mean], [x_])
            _tt(P, P.pool, x_.t[:, 0:nb], x_.t[:, 0:nb], rstd.t[:, 0:nb], ALU.mult, [x_, rstd], [x_])
            P.op(P.act, lambda e, c=c, x_=x_: e.activation(hT.t[:, c, 0:nb], x_.t[:, 0:nb], AF.Silu,
                                                       bias=pcol.t[:, PC_CBE + c:PC_CBE + c + 1],
                                                       scale=pcol.t[:, PC_CG + c:PC_CG + c + 1]),
                 reads=[x_, pcol], writes=[hT])

    def conv_state_out(uview30, dst):
        psb = P.ps()
        for c in range(4):
            P.op(P.pe, lambda e, c=c, psb=psb: e.transpose(psb.t[0:30, c * 128:(c + 1) * 128], uview30(c), identf),
                 reads=[uT, cf], writes=[psb], sig=(c == 3))
        _copy(P, P.act, cvo.t[0:30, :], psb.t[0:30, 0:512], [psb], [cvo])
        P.dma(P.sp, dst, cvo.t[0:30, :], reads=[cvo], out_final=True)

    def common_front(x_src, nb, subs):
        ntile = nb // 128
        for t in range(ntile):
            xb = xt[st["x"] % 2]
            st["x"] += 1
            P.dma(P.sp, xb.t[:, :], x_src[t * 128:(t + 1) * 128, :], writes=[xb])
            for g in range(2):
                psb = P.ps()
                for j in range(4):
                    kc = g * 4 + j
                    P.op(P.pe, lambda e, psb=psb, j=j, kc=kc, xb=xb: e.transpose(
                        psb.t[:, j * 128:(j + 1) * 128], xb.t[:, kc * 128:(kc + 1) * 128], identf),
                        reads=[xb, cf], writes=[psb], sig=(j == 3))
                eng = P.act if g == 0 else P.dve
                _copy(P, eng, xT.t[:, g * 4:(g + 1) * 4, t * 128:(t + 1) * 128],
                      psb.t[:, :].rearrange("p (a b) -> p a b", a=4), [psb], [xT])
        for c in range(4):
            def ev(psb, c=c):
                _act(P, qz.t[0:64, 2 * c, 0:nb], psb.t[0:64, 0:nb], AF.Identity, [psb, bq8], [qz],
                     bias=bq8.t[0:64, c:c + 1], scale=0.125)
                _act(P, qz.t[64:128, 2 * c + 1, 0:nb], psb.t[64:128, 0:nb], AF.Identity, [psb, bq8], [qz],
                     bias=bq8.t[64:128, c:c + 1], scale=0.125)
            project_feature(OFF_Q + c * 128, 128, nb, ev)
        def evf(psb):
            _act(P, et.t[:, 0:nb], psb.t[0:8, 0:nb], AF.Exp, [psb, nbf], [et], bias=nbf.t[:, 0:1], scale=-1.0)
            _act(P, lfneg.t[:, 0:nb], et.t[:, 0:nb], AF.Ln, [et], [lfneg], bias=oneb.t[0:8, 0:1])
        project_feature(OFF_F, 8, nb, evf)
        for s in subs:
            c0, n = s["c0"], s["n"]
            init = 0.0 if s["first"] else carry.t[:, 0:1]
            P.op(P.dve, lambda e, c0=c0, n=n, init=init: e.tensor_tensor_scan(
                cblk.t[:, c0:c0 + n], ones8.t[:, 0:n], lfneg.t[:, c0:c0 + n], init, ALU.mult, ALU.subtract),
                reads=[ones8, lfneg, carry], writes=[cblk])
        _copy(P, P.act, carry.t[:, 0:1], cblk.t[:, nb - 1:nb], [cblk], [carry])
        _copy(P, P.dve, c3p.t[:, 0, 0:nb], cblk.t[:, 0:nb], [cblk], [c3p])
        _tt(P, P.dve, ctmp.t[:, 0:nb], cblk.t[:, 0:nb], c3p.t[:, 0, 0:nb], ALU.subtract, [cblk, c3p], [ctmp])
        _copy(P, P.dve, c3p.t[:, 1, 0:nb], ctmp.t[:, 0:nb], [ctmp], [c3p])
        _tt(P, P.dve, c3p.t[:, 2, 0:nb], ctmp.t[:, 0:nb], c3p.t[:, 1, 0:nb], ALU.subtract, [ctmp, c3p], [c3p])
        P.dma(P.sp, T["c3_d"][:, :, 0:nb], c3p.t[:, :, 0:nb], reads=[c3p], writes=[c3d])
        P.dma(P.sp, c3pad.t[0:3, :, 0:nb], T["c3_d"][:, :, 0:nb].rearrange("h s n -> s h n"), reads=[c3d], writes=[c3pad])

    def glu(nb, uout, view=lambda a: a):
        for c in range(4):
            sgb = sg[c % 2]
            def evg(psb, sgb=sgb, c=c):
                _act(P, sgb.t[:, 0:nb], psb.t[:, 0:nb], AF.Sigmoid, [psb, pcol], [sgb],
                     bias=pcol.t[:, PC_BGLU + 4 + c:PC_BGLU + 5 + c])
            project_feature(OFF_GLU + CC + c * 128, 128, nb, evg)
            def eva(psb, sgb=sgb, c=c):
                _stt(P, uout(c), view(psb.t[:, 0:nb]), pcol.t[:, PC_BGLU + c:PC_BGLU + c + 1], view(sgb.t[:, 0:nb]),
                     ALU.add, ALU.mult, [psb, pcol, sgb], [uT])
            project_feature(OFF_GLU + c * 128, 128, nb, eva)

    def store_scratch(nb, tok0):
        P.dma(P.sp, T["attn_d"].rearrange("(h d) t -> d h t", d=64)[:, :, tok0:tok0 + nb], attnT.t[:, :, 0:nb],
              reads=[attnT], writes=[scr])
        P.dma(P.sp, T["h_d"].rearrange("(c p) t -> p c t", p=128)[:, :, tok0:tok0 + nb], hT.t[:, :, 0:nb],
              reads=[hT], writes=[scr])

    ntile = NB // 128
    for sq in range(NSEQ_P):
        for b in range(SEQ // NB):
            r0 = sq * SEQ + b * NB
            tile0 = b * ntile
            if b == 0:
                P.op(P.pool, lambda e: e.memset(uT.t[:, :, 0:30], 0.0), writes=[uT])
            else:
                _copy(P, P.pool, uT.t[:, :, 0:30], uT.t[:, :, NB:NB + 30], [uT], [uT])
            common_front(T["xp"][r0:r0 + NB, :], NB, [{"c0": 0, "n": NB, "first": b == 0}])
            for t in range(ntile):
                psb = P.ps()
                P.op(P.pe, lambda e, psb=psb, t=t: e.transpose(psb.t[:, 0:8], lfneg.t[:, t * 128:(t + 1) * 128], identf[0:8, 0:8]),
                     reads=[lfneg, cf], writes=[psb])
                P.op(P.pe, lambda e, psb=psb, t=t: e.transpose(psb.t[:, 8:16], cblk.t[:, t * 128:(t + 1) * 128], identf[0:8, 0:8]),
                     reads=[cblk, cf], writes=[psb])
                P.op(P.act, lambda e, psb=psb, t=t: e.mul(lftok.t[:, t, :], psb.t[:, 0:8], -1.0), reads=[psb], writes=[lftok])
                P.op(P.act, lambda e, psb=psb, j=tile0 + t: e.mul(negc.t[:, j, :], psb.t[:, 8:16], -1.0), reads=[psb], writes=[negc])
            P.dma(P.sp, T["lfp"][r0:r0 + NB, :].rearrange("(t p) h -> p t h", p=128), lftok.t[:, 0:ntile, :],
                  reads=[lftok], out_final=True)
            for t in range(ntile):
                j = tile0 + t
                kvb = kvf[st["kv"] % 2]
                kb = kbf[st["kv"] % 2]
                st["kv"] += 1
                for half in range(2):
                    psb = P.ps()
                    for kc in range(8):
                        _mm(P, psb, psb.t[:, 0:512], xT.t[:, kc, t * 128:(t + 1) * 128],
                            w_in.t[:, kc, OFF_K + half * 512:OFF_K + (half + 1) * 512], kc == 0, kc == 7, [xT, w_in])
                    _tt(P, P.dve, kvb.t[:, half * 512:(half + 1) * 512], psb.t[:, 0:512], bkv.t[:, half * 512:(half + 1) * 512],
                        ALU.add, [psb, bkv], [kvb])
                rr = r0 + t * 128
                P.dma(P.sp, T["kp"][rr:rr + 128, :], kvb.t[:, 0:512], reads=[kvb], out_final=True)
                P.dma(P.sp, T["vp"][rr:rr + 128, :], kvb.t[:, 512:1024], reads=[kvb], out_final=True)
                _copy(P, P.act, kb.t[:, :], kvb.t[:, 0:512], [kvb], [kb])
                _copy(P, P.pool, vsb.t[:, j, :], kvb.t[:, 512:1024], [kvb], [vsb])
                psb = P.ps()
                pbf = psb.t[:, :].bitcast(BF16)
                for c in range(4):
                    P.op(P.pe, lambda e, c=c, pbf=pbf, kb=kb: e.transpose(pbf[:, c * 128:(c + 1) * 128], kb.t[:, c * 128:(c + 1) * 128], identb),
                         reads=[kb, cb], writes=[psb], sig=(c == 3))
                _copy(P, P.dve, kT.t[:, :, j * 128:(j + 1) * 128], pbf[:, 0:512].rearrange("p (c n) -> p c n", c=4), [psb], [kT])
            glu(NB, lambda c: uT.t[:, c, 30:30 + NB])
            for h in range(8):
                kts = []
                for j in range(tile0 + ntile):
                    kts.append(dict(kT=kT.t[:, h // 2, j * 128:(j + 1) * 128], v=vsb.t[:, j, h * 64:(h + 1) * 64],
                                    bias=negc.t[:, j, h:h + 1], nk=128, q0=max(0, (j - tile0) * 128), tri=j >= tile0,
                                    reads=[kT, vsb, negc]))
                attention(h, 0, NB, kts)
            conv_ln(NB, lambda c, j: ubf.t[:, c, j:j + NB], lambda a: a.t[:, 0:NB])
            store_scratch(NB, r0)
            if b == SEQ // NB - 1:
                conv_state_out(lambda c: uT.t[:, c, NB:NB + 30], T["cvp"][sq, :, :])
    uSv = uT.t[:, :, 0:NSUB_S * 62].rearrange("p c (s n) -> p c s n", s=NSUB_S)
    ckb = P.sb("ckb", [128, 8, 512], BF16)
    clfb = P.sb("clfb", [128, 8, 8], F32)
    sufs = P.sb("sufs", [128, 8, 8], F32)
    negcs = P.sb("negcs", [32, NSUB_S, 8], F32)
    kTn = P.sb("kTn", [128, 4, 128], BF16)
    vnew = P.sb("vnew", [32, NSUB_S, 512], BF16)
    kvs = kvf
    scv = kvf
    v4 = lambda a: a.rearrange("p (s n) -> p s n", s=NSUB_S)
    for s in range(NSUB_S):
        sc = scv[s % 2]
        P.dma(P.sp, sc.t[0:30, 0:512], T["sconv"][s, :, :], writes=[sc])
        psb = P.ps()
        for c in range(4):
            P.op(P.pe, lambda e, c=c, psb=psb, sc=sc: e.transpose(psb.t[:, c * 32:c * 32 + 30], sc.t[0:30, c * 128:(c + 1) * 128],
                                                               identf[0:30, 0:30]),
                 reads=[sc, cf], writes=[psb], sig=(c == 3))
        _copy(P, P.act, uSv[:, :, s, 0:30], psb.t[:, 0:128].rearrange("p (c n) -> p c n", c=4)[:, :, 0:30], [psb], [uT])
    common_front(T["xs"], 128, [{"c0": 32 * s, "n": 32, "first": True} for s in range(NSUB_S)])
    psb = P.ps()
    P.op(P.pe, lambda e, psb=psb: e.transpose(psb.t[:, 0:8], lfneg.t[:, 0:128], identf[0:8, 0:8]), reads=[lfneg, cf], writes=[psb])
    P.op(P.act, lambda e, psb=psb: e.mul(lftok.t[:, 0, :], psb.t[:, 0:8], -1.0), reads=[psb], writes=[lftok])
    P.dma(P.sp, T["lfs"], lftok.t[:, 0, :], reads=[lftok], out_final=True)
    for s in range(NSUB_S):
        psb = P.ps()
        P.op(P.pe, lambda e, psb=psb, s=s: e.transpose(psb.t[0:32, 0:8], cblk.t[:, 32 * s:32 * s + 32], identf[0:8, 0:8]),
             reads=[cblk, cf], writes=[psb])
        P.op(P.act, lambda e, psb=psb, s=s: e.mul(negcs.t[0:32, s, :], psb.t[0:32, 0:8], -1.0), reads=[psb], writes=[negcs])
        kvb = kvs[s % 2]
        for half in range(2):
            psb = P.ps()
            for kc in range(8):
                _mm(P, psb, psb.t[0:32, 0:512], xT.t[:, kc, 32 * s:32 * s + 32],
                    w_in.t[:, kc, OFF_K + half * 512:OFF_K + (half + 1) * 512], kc == 0, kc == 7, [xT, w_in])
            _tt(P, P.dve, kvb.t[0:32, half * 512:(half + 1) * 512], psb.t[0:32, 0:512], bkv.t[0:32, half * 512:(half + 1) * 512],
                ALU.add, [psb, bkv], [kvb])
        P.dma(P.sp, T["ks"][32 * s:32 * s + 32, :], kvb.t[0:32, 0:512], reads=[kvb], out_final=True)
        P.dma(P.sp, T["vs"][32 * s:32 * s + 32, :], kvb.t[0:32, 512:1024], reads=[kvb], out_final=True)
        _copy(P, P.act, vnew.t[0:32, s, :], kvb.t[0:32, 512:1024], [kvb], [vnew])
    for c in range(4):
        def evk(psb, c=c):
            _act(P, kTn.t[:, c, 0:128], psb.t[:, 0:128], AF.Identity, [psb, pcol], [kTn], bias=pcol.t[:, PC_BK + c:PC_BK + c + 1])
        project_feature(OFF_K + c * 128, 128, 128, evk)
    glu(128, lambda c: uSv[:, c, :, 30:62], v4)
    for s in range(NSUB_S):
        P.dma(P.pool, ckb.t[:, :, :], T["ck"][s].rearrange("(j p) n -> p j n", p=128), writes=[ckb])
        P.dma(P.pool, vsb.t[:, 0:8, :], T["cv"][s].rearrange("(j p) n -> p j n", p=128), writes=[vsb])
        P.dma(P.sp, clfb.t[:, :, :], T["clf"][s].rearrange("(j p) h -> p j h", p=128), writes=[clfb])
        for j in range(8):
            psb = P.ps()
            pbf = psb.t[:, :].bitcast(BF16)
            for c in range(4):
                P.op(P.pe, lambda e, c=c, j=j, pbf=pbf: e.transpose(pbf[:, c * 128:(c + 1) * 128], ckb.t[:, j, c * 128:(c + 1) * 128], identb),
                     reads=[ckb, cb], writes=[psb], sig=(c == 3))
            _copy(P, P.dve if j % 2 else P.act, kT.t[:, :, j * 128:(j + 1) * 128],
                  pbf[:, 0:512].rearrange("p (c n) -> p c n", c=4), [psb], [kT])
        psb = P.ps()
        for j in range(8):
            _mm(P, psb, psb.t[:, j * 8:(j + 1) * 8], lstrict, clfb.t[:, j, :], True, j == 7, [cf, clfb], sig=False)
            for j2 in range(j + 1, 8):
                _mm(P, psb, psb.t[:, j * 8:(j + 1) * 8], onesf, clfb.t[:, j2, :], False, j2 == 7, [cf, clfb], sig=False)
        P.op(P.pe, lambda e, psb=psb: e.transpose(psb.t[0:8, 64:72], clfb.t[0:8, 0, :], identf[0:8, 0:8]), reads=[clfb, cf], writes=[psb])
        _copy(P, P.dve, sufs.t[:, :, :], psb.t[:, 0:64].rearrange("p (j h) -> p j h", j=8), [psb], [sufs])
        for h in range(8):
            kts = []
            for j in range(8):
                kts.append(dict(kT=kT.t[:, h // 2, j * 128:(j + 1) * 128], v=vsb.t[:, j, h * 64:(h + 1) * 64],
                                bias=sufs.t[:, j, h:h + 1], nk=128, q0=0, tri=False, reads=[kT, vsb, sufs]))
            kts.append(dict(kT=kTn.t[:, h // 2, 32 * s:32 * s + 32], v=vnew.t[0:32, s, h * 64:(h + 1) * 64],
                            bias=negcs.t[0:32, s, h:h + 1], nk=32, q0=0, tri=True, reads=[kTn, vnew, negcs]))
            attention(h, 32 * s, 32, kts)
    uSb = ubf.t[:, :, 0:NSUB_S * 62].rearrange("p c (s n) -> p c s n", s=NSUB_S)
    conv_ln(128, lambda c, j: uSb[:, c, :, j:j + 32], lambda a: v4(a.t[:, 0:128]))
    store_scratch(128, TP)
    for s in range(NSUB_S):
        conv_state_out(lambda c, s=s: uSv[:, c, s, 32:62], T["cvs"][s, :, :])
    P.pop_scope()


CAP = 384
NSLOT = NE * CAP
NT = TT // 128


def layer_norm_tile(P, r, out_fn, tmp, eps_col):
    st6, mv, sc = tmp["st6"], tmp["mv"], tmp["sc"]
    for g in range(2):
        P.op(P.dve, lambda e, g=g: e.bn_stats(st6.t[:, g, :], r.t[:, g * 512:(g + 1) * 512]), reads=[r], writes=[st6])
    P.op(P.dve, lambda e: e.bn_aggr(mv.t[:, 0:2], st6.t[:, :, :].rearrange("p a b -> p (a b)")), reads=[st6], writes=[mv])
    _act(P, sc.t[:, 0:1], mv.t[:, 1:2], AF.Sqrt, [mv], [sc], bias=eps_col)
    P.op(P.dve, lambda e: e.reciprocal(sc.t[:, 0:1], sc.t[:, 0:1]), reads=[sc], writes=[sc])
    _ts(P, P.dve, sc.t[:, 1:2], mv.t[:, 0:1], -1.0, sc.t[:, 0:1], ALU.mult, ALU.mult, [mv, sc], [sc])
    out_fn(sc.t[:, 0:1], sc.t[:, 1:2])


def pass_b(P, T, G, NB):
    P.push_scope()
    cf = P.sb("cf", [128, 384], F32)
    cb = P.sb("cb", [128, 384], BF16)
    pcol = P.sb("pcol", [128, PC_N], F32)
    P.dma(P.sp, cf.t[:, :], T["cf"], writes=[cf])
    P.dma(P.sp, cb.t[:, :], T["cb"], writes=[cb])
    P.dma(P.sp, pcol.t[:, :], T["pcol"], writes=[pcol])
    identf = cf.t[:, 0:128]
    identb = cb.t[:, 0:128]
    ustrict = cb.t[:, 256:384]
    w_g = P.sb("w_g", [128, 8, 2048], BF16)
    w_a = P.sb("w_a", [128, 4, 1024], BF16)
    w_b = P.sb("w_b", [128, 4, 1024], BF16)
    w_o = P.sb("w_o", [128, 8, 1024], BF16)
    wr_hi = P.sb("wr_hi", [128, 8, 256], BF16)
    wr_lo = P.sb("wr_lo", [128, 8, 256], BF16)
    w_s = P.sb("w_s", [128, 8, 512], BF16)
    w_sd = P.sb("w_sd", [128, 2, 1024], BF16)
    lng = P.sb("lng", [128, 1024], F32)
    lnb = P.sb("lnb", [128, 1024], F32)
    brt = P.sb("brt", [128, 256], F32)
    cnt = P.sb("cnt", [128, 256], F32)
    onesb = P.sb("onesb", [128, 128], BF16)
    epsb = P.sb("epsb", [128, 1], F32)
    tokid = P.sb("tokid", [128, NT], I32)
    load_w_cast(P, w_g, T["w_in"], 8, 2048, OFF_GA)
    load_w_cast(P, w_a, T["w_a"], 4, 1024)
    load_w_cast(P, w_b, T["w_b"], 4, 1024)
    load_w_cast(P, w_o, T["w_out"], 8, 1024)
    load_w_cast(P, wr_hi, T["w_router"], 8, 256)
    load_w_cast(P, w_s, T["w_sg"], 8, 256)
    P.dma(P.pool, w_s.t[:, :, 256:512], T["w_su"].rearrange("(kc p) n -> p kc n", p=128), writes=[w_s])
    load_w_cast(P, w_sd, T["w_sd"], 2, 1024)
    P.dma(P.sp, lng.t[:, :], T["ln1_g"].partition_broadcast(128), writes=[lng])
    P.dma(P.sp, lnb.t[:, :], T["ln1_b"].partition_broadcast(128), writes=[lnb])
    P.dma(P.sp, brt.t[:, :], T["b_router"].partition_broadcast(128), writes=[brt])
    P.dma(P.sp, cnt.t[:, :], T["base1"], writes=[cnt])
    P.dma(P.sp, tokid.t[:, :], T["tokid"], writes=[tokid])
    P.op(P.dve, lambda e: e.memset(onesb.t[:, :], 1.0), writes=[onesb])
    P.op(P.dve, lambda e: e.memset(epsb.t[:, :], LN_EPS), writes=[epsb])
    wr32 = P.sb("wr32", [128, 8, 256], F32)
    P.dma(P.sp, wr32.t[:, :, :], T["w_router"].rearrange("(kc p) n -> p kc n", p=128), writes=[wr32])
    _tt(P, P.dve, wr_lo.t[:, :, :], wr32.t[:, :, :], wr_hi.t[:, :, :], ALU.subtract, [wr32, wr_hi], [wr_lo])

    nt = NB // 128
    at = P.sb("at", [128, 4, NB], BF16)
    ht = P.sb("ht", [128, 4, NB], BF16)
    xt = [P.sb("xt%d" % i, [128, 1024], F32) for i in range(nt)]
    xT = P.sb("xT", [128, 8, NB], BF16)
    sga = [P.sb("sga%d" % i, [128, NB], F32) for i in range(2)]
    sgb = [P.sb("sgb%d" % i, [128, NB], F32) for i in range(2)]
    t1 = [P.sb("t1%d" % i, [128, NB], F32) for i in range(2)]
    t2 = [P.sb("t2%d" % i, [128, NB], F32) for i in range(2)]
    mT = P.sb("mT", [128, 8, NB], BF16)
    rr = P.sb("rr", [128, 1024], F32)
    mid = [P.sb("mid%d" % i, [128, 1024], F32) for i in range(2)]
    mhi = [P.sb("mhi%d" % i, [128, 1024], BF16) for i in range(2)]
    mlo = P.sb("mlo", [128, 1024], BF16)
    mTh = P.sb("mTh", [128, 8, NB], BF16)
    mTl = P.sb("mTl", [128, 8, NB], BF16)
    tmp = {"st6": P.sb("st6", [128, 2, 6], F32), "mv": P.sb("mv", [128, 2], F32), "sc": P.sb("sc", [128, 2], F32)}
    rt = {n: P.sb("rt_" + n, [128, 256], F32) for n in ("scores", "sel", "selm", "emask", "gate", "sv", "junk")}
    emb = P.sb("emb", [128, 256], BF16)
    m8 = P.sb("m8", [128, 8, 8], F32)
    gs = P.sb("gs", [128, 8], F32)
    g8s = P.sb("g8s", [128, 8], F32)
    gmask = P.sb("gmask", [128, 8], F32)
    gneg = P.sb("gneg", [128, 8], F32)
    s8 = P.sb("s8", [128, 8], F32)
    den = P.sb("den", [128, 1], F32)
    gsh = [P.sb("gsh%d" % i, [128, NB], F32) for i in range(2)]
    hsT = P.sb("hsT", [128, 2, NB], BF16)
    pre = [P.sb("pre%d" % i, [128, 1024], F32) for i in range(2)]
    scr = Buf(None, "scr")
    slots_all, gates_all = G["slots"], G["gates"]
    st = {"m": 0}

    for b0 in range(0, TT, NB):
        nb = min(NB, TT - b0)
        ntile = nb // 128
        P.dma(P.sp, at.t[:, :, 0:nb], T["attn_d"].rearrange("(c p) t -> p c t", p=128)[:, :, b0:b0 + nb], writes=[at])
        P.dma(P.sp, ht.t[:, :, 0:nb], T["h_d"].rearrange("(c p) t -> p c t", p=128)[:, :, b0:b0 + nb], writes=[ht])
        for t in range(ntile):
            xb = xt[t]
            r0 = b0 + t * 128
            src_x = T["xp"][r0:r0 + 128, :] if r0 < TP else T["xs"]
            P.dma(P.sp, xb.t[:, :], src_x, writes=[xb])
            for g in range(2):
                psb = P.ps()
                for j in range(4):
                    kc = g * 4 + j
                    P.op(P.pe, lambda e, psb=psb, j=j, kc=kc, xb=xb: e.transpose(
                        psb.t[:, j * 128:(j + 1) * 128], xb.t[:, kc * 128:(kc + 1) * 128], identf),
                        reads=[xb, cf], writes=[psb], sig=(j == 3))
                _copy(P, P.act if g == 0 else P.dve, xT.t[:, g * 4:(g + 1) * 4, t * 128:(t + 1) * 128],
                      psb.t[:, :].rearrange("p (a b) -> p a b", a=4), [psb], [xT])
        for oc in range(8):
            i2 = oc % 2
            pa = P.ps()
            for c in range(4):
                _mm(P, pa, pa.t[:, 0:nb], w_a.t[:, c, oc * 128:(oc + 1) * 128], at.t[:, c, 0:nb], c == 0, c == 3, [w_a, at])
            pb = P.ps()
            for c in range(4):
                _mm(P, pb, pb.t[:, 0:nb], w_b.t[:, c, oc * 128:(oc + 1) * 128], ht.t[:, c, 0:nb], c == 0, c == 3, [w_b, ht])
            pga = P.ps()
            for kc in range(8):
                _mm(P, pga, pga.t[:, 0:nb], w_g.t[:, kc, oc * 128:(oc + 1) * 128], xT.t[:, kc, 0:nb], kc == 0, kc == 7, [w_g, xT])
            pgb = P.ps()
            for kc in range(8):
                _mm(P, pgb, pgb.t[:, 0:nb], w_g.t[:, kc, 1024 + oc * 128:1024 + (oc + 1) * 128], xT.t[:, kc, 0:nb], kc == 0, kc == 7, [w_g, xT])
            _act(P, sga[i2].t[:, 0:nb], pga.t[:, 0:nb], AF.Sigmoid, [pga, pcol], [sga[i2]], bias=pcol.t[:, PC_BGA + oc:PC_BGA + oc + 1])
            _act(P, sgb[i2].t[:, 0:nb], pgb.t[:, 0:nb], AF.Sigmoid, [pgb, pcol], [sgb[i2]], bias=pcol.t[:, PC_BGB + oc:PC_BGB + oc + 1])
            _tt(P, P.dve, t1[i2].t[:, 0:nb], pa.t[:, 0:nb], sga[i2].t[:, 0:nb], ALU.mult, [pa, sga[i2]], [t1[i2]])
            _stt(P, t2[i2].t[:, 0:nb], pb.t[:, 0:nb], pcol.t[:, PC_BB + oc:PC_BB + oc + 1], sgb[i2].t[:, 0:nb], ALU.add, ALU.mult,
                 [pb, pcol, sgb[i2]], [t2[i2]])
            _tt(P, P.pool, mT.t[:, oc, 0:nb], t1[i2].t[:, 0:nb], t2[i2].t[:, 0:nb], ALU.add, [t1[i2], t2[i2]], [mT])
        for t in range(ntile):
            tg = (b0 // 128) + t
            xb = xt[t]
            md = mid[st["m"] % 2]
            mh = mhi[st["m"] % 2]
            st["m"] += 1
            for half in range(2):
                psb = P.ps()
                for kc in range(8):
                    _mm(P, psb, psb.t[:, 0:512], mT.t[:, kc, t * 128:(t + 1) * 128], w_o.t[:, kc, half * 512:(half + 1) * 512],
                        kc == 0, kc == 7, [mT, w_o])
                _stt(P, rr.t[:, half * 512:(half + 1) * 512], xb.t[:, half * 512:(half + 1) * 512], DN_ALPHA, psb.t[:, 0:512],
                     ALU.mult, ALU.add, [xb, psb], [rr])
            def norm1(rstd, nmr, md=md):
                P.op(P.act, lambda e: e.activation(md.t[:, :], rr.t[:, :], AF.Identity, bias=nmr, scale=rstd), reads=[rr, tmp["sc"]], writes=[md])
            layer_norm_tile(P, rr, norm1, tmp, epsb.t[:, 0:1])
            _tt(P, P.dve, md.t[:, :], md.t[:, :], lng.t[:, :], ALU.mult, [md, lng], [md])
            _tt(P, P.pool, md.t[:, :], md.t[:, :], lnb.t[:, :], ALU.add, [md, lnb], [md])
            _copy(P, P.act, mh.t[:, :], md.t[:, :], [md], [mh])
            _tt(P, P.dve, mlo.t[:, :], md.t[:, :], mh.t[:, :], ALU.subtract, [md, mh], [mlo])
            if "mid_dbg" in T:
                P.dma(P.sp, T["mid_dbg"][tg * 128:(tg + 1) * 128, :], md.t[:, :], reads=[md], out_final=True)
            for srcb, dstb in ((mh, mTh), (mlo, mTl)):
                psb = P.ps()
                pbf = psb.t[:, :].bitcast(BF16)
                for kc in range(8):
                    P.op(P.pe, lambda e, kc=kc, pbf=pbf, srcb=srcb: e.transpose(pbf[:, kc * 128:(kc + 1) * 128], srcb.t[:, kc * 128:(kc + 1) * 128], identb),
                         reads=[srcb, cb], writes=[psb], sig=(kc == 7))
                _copy(P, P.act if srcb is mh else P.dve, dstb.t[:, :, t * 128:(t + 1) * 128], pbf.rearrange("p (c n) -> p c n", c=8), [psb], [dstb])
            psr = P.ps()
            k = 0
            for (a_, w_) in ((mTh, wr_hi), (mTh, wr_lo), (mTl, wr_hi)):
                for kc in range(8):
                    _mm(P, psr, psr.t[:, 0:256], a_.t[:, kc, t * 128:(t + 1) * 128], w_.t[:, kc, :], k == 0, k == 23, [a_, w_])
                    k += 1
            sc_, sel, selm, emask, gate, sv, junk = (rt[n] for n in ("scores", "sel", "selm", "emask", "gate", "sv", "junk"))
            _act(P, sc_.t[:, :], psr.t[:, 0:256], AF.Sigmoid, [psr], [sc_])
            _tt(P, P.dve, sel.t[:, :], sc_.t[:, :], brt.t[:, :], ALU.add, [sc_, brt], [sel])
            for g in range(8):
                P.op(P.dve, lambda e, g=g: e.max(m8.t[:, g, :], sel.t[:, g * 32:(g + 1) * 32]), reads=[sel], writes=[m8])
            _tt(P, P.dve, gs.t[:, :], m8.t[:, :, 0], m8.t[:, :, 1], ALU.add, [m8], [gs])
            P.op(P.dve, lambda e: e.max(g8s.t[:, :], gs.t[:, :]), reads=[gs], writes=[g8s])
            _ts(P, P.dve, gmask.t[:, :], gs.t[:, :], g8s.t[:, 3:4], None, ALU.is_ge, None, [gs, g8s], [gmask])
            _ts(P, P.dve, gneg.t[:, :], gmask.t[:, :], -1.0, 1e9, ALU.add, ALU.mult, [gmask], [gneg])
            for g in range(8):
                _ts(P, P.dve, selm.t[:, g * 32:(g + 1) * 32], sel.t[:, g * 32:(g + 1) * 32], gmask.t[:, g:g + 1], gneg.t[:, g:g + 1],
                    ALU.mult, ALU.add, [sel, gmask, gneg], [selm])
            P.op(P.dve, lambda e: e.max(m8.t[:, 0, :], selm.t[:, :]), reads=[selm], writes=[m8])
            _ts(P, P.dve, emask.t[:, :], selm.t[:, :], m8.t[:, 0, 7:8], None, ALU.is_ge, None, [selm, m8], [emask])
            _copy(P, P.pool, emb.t[:, :], emask.t[:, :], [emask], [emb])
            P.op(P.dve, lambda e: e.scalar_tensor_tensor(gate.t[:, :], sc_.t[:, :], 1.0, emask.t[:, :], ALU.mult, ALU.mult, accum_out=den.t[:, 0:1]),
                 reads=[sc_, emask], writes=[gate, den])
            P.op(P.dve, lambda e: e.reciprocal(den.t[:, 0:1], den.t[:, 0:1]), reads=[den], writes=[den])
            _ts(P, P.dve, gate.t[:, :], gate.t[:, :], den.t[:, 0:1], 2.5, ALU.mult, ALU.mult, [gate, den], [gate])
            pp = P.ps()
            _mm(P, pp, pp.t[:, 0:256], ustrict, emb.t[:, :], True, True, [cb, emb])
            pt_ = P.ps()
            _mm(P, pt_, pt_.t[:, 0:256], onesb.t[:, :], emb.t[:, :], True, True, [onesb, emb])
            _tt(P, P.dve, sv.t[:, :], pp.t[:, 0:256], cnt.t[:, :], ALU.add, [pp, cnt], [sv])
            _tt(P, P.pool, sv.t[:, :], sv.t[:, :], emask.t[:, :], ALU.mult, [sv, emask], [sv])
            _tt(P, P.dve, cnt.t[:, :], pt_.t[:, 0:256], cnt.t[:, :], ALU.add, [pt_, cnt], [cnt])
            P.op(P.dve, lambda e: e.max(s8.t[:, :], sv.t[:, :]), reads=[sv], writes=[s8])
            for k in range(8):
                P.op(P.dve, lambda e, k=k, tg=tg: e.scalar_tensor_tensor(junk.t[:, :], sv.t[:, :], s8.t[:, k:k + 1], gate.t[:, :], ALU.is_equal, ALU.mult,
                                                                     accum_out=gates_all.t[:, tg, k:k + 1]),
                     reads=[sv, s8, gate], writes=[junk, gates_all])
            _ts(P, P.dve, slots_all.t[:, tg, :], s8.t[:, :], -1.0, None, ALU.add, None, [s8], [slots_all])
            for k in range(8):
                P.dma(P.pool, None, None, reads=[slots_all, mh], writes=[scr],
                      fn=lambda h, k=k, tg=tg, mh=mh: h.indirect_dma_start(
                          out=T["xg_d"], out_offset=bass.IndirectOffsetOnAxis(ap=slots_all.t[:, tg, k:k + 1], axis=0),
                          in_=mh.t[:, :], in_offset=None))
        for j in range(2):
            pg = P.ps()
            for kc in range(8):
                _mm(P, pg, pg.t[:, 0:nb], w_s.t[:, kc, j * 128:(j + 1) * 128], mTh.t[:, kc, 0:nb], kc == 0, kc == 7, [w_s, mTh])
            pu = P.ps()
            for kc in range(8):
                _mm(P, pu, pu.t[:, 0:nb], w_s.t[:, kc, 256 + j * 128:256 + (j + 1) * 128], mTh.t[:, kc, 0:nb], kc == 0, kc == 7, [w_s, mTh])
            _act(P, gsh[j].t[:, 0:nb], pg.t[:, 0:nb], AF.Silu, [pg], [gsh[j]])
            _tt(P, P.dve, hsT.t[:, j, 0:nb], pu.t[:, 0:nb], gsh[j].t[:, 0:nb], ALU.mult, [pu, gsh[j]], [hsT])
        for t in range(ntile):
            tg = (b0 // 128) + t
            md = mid[(st["m"] - ntile + t) % 2]
            pr = pre[t % 2]
            for half in range(2):
                psb = P.ps()
                for j in range(2):
                    _mm(P, psb, psb.t[:, 0:512], hsT.t[:, j, t * 128:(t + 1) * 128], w_sd.t[:, j, half * 512:(half + 1) * 512], j == 0, j == 1,
                        [hsT, w_sd])
                _stt(P, pr.t[:, half * 512:(half + 1) * 512], md.t[:, half * 512:(half + 1) * 512], DN_ALPHA, psb.t[:, 0:512], ALU.mult, ALU.add,
                     [md, psb], [pr])
            P.dma(P.sp, T["pre_d"][tg * 128:(tg + 1) * 128, :], pr.t[:, :], reads=[pr], writes=[scr])
    P.pop_scope()


def pass_c(P, T, n_exp=NE):
    P.push_scope()
    NBLK = CAP // 128
    cb = P.sb("cb", [128, 128], BF16)
    P.dma(P.sp, cb.t[:, :], T["cb"][:, 0:128], writes=[cb])
    identb = cb.t[:, 0:128]
    NS = 2
    NW = 2
    sg_ = [P.sb("wsg%d" % i, [128, 8, DE], F32) for i in range(NS)]
    su_ = [P.sb("wsu%d" % i, [128, 8, DE], F32) for i in range(NS)]
    sd_ = [P.sb("wsd%d" % i, [128, 2, D], F32) for i in range(NS)]
    wg = [P.sb("wg%d" % i, [128, 8, DE], BF16) for i in range(NW)]
    wu = [P.sb("wu%d" % i, [128, 8, DE], BF16) for i in range(NW)]
    wd = [P.sb("wd%d" % i, [128, 2, D], BF16) for i in range(NW)]
    xg = [P.sb("xg%d" % i, [128, NBLK, D], BF16) for i in range(2)]
    xgT = [P.sb("xgT%d" % i, [128, 8, CAP], BF16) for i in range(2)]
    gsb = [P.sb("gsb%d" % i, [128, 2, CAP], F32) for i in range(2)]
    hTe = [P.sb("hTe%d" % i, [128, 2, CAP], BF16) for i in range(2)]
    yb = [P.sb("yb%d" % i, [128, D], BF16) for i in range(3)]
    scr = Buf(None, "scr_c")

    def loads(e):
        s = e % NS
        P.dma(P.sp, sg_[s].t[:, :, :], T["w_eg"][e].rearrange("(kc p) n -> p kc n", p=128), writes=[sg_[s]])
        P.dma(P.sp, su_[s].t[:, :, :], T["w_eu"][e].rearrange("(kc p) n -> p kc n", p=128), writes=[su_[s]])
        P.dma(P.sp, sd_[s].t[:, :, :], T["w_ed"][e].rearrange("(j p) n -> p j n", p=128), writes=[sd_[s]])
        P.dma(P.sp, xg[e % 2].t[:, :, :], T["xg_d"][e * CAP:(e + 1) * CAP, :].rearrange("(b p) n -> p b n", p=128), writes=[xg[e % 2]])

    def casts(e):
        s, i = e % NS, e % NW
        _copy(P, P.act, wg[i].t[:, :, :], sg_[s].t[:, :, :], [sg_[s]], [wg[i]])
        _copy(P, P.dve, wu[i].t[:, :, :], su_[s].t[:, :, :], [su_[s]], [wu[i]])
        _copy(P, P.pool, wd[i].t[:, :, :], sd_[s].t[:, :, :], [sd_[s]], [wd[i]])

    loads(0)
    loads(1)
    casts(0)
    yi = 0
    for e in range(n_exp):
        i, i2 = e % NW, e % 2
        for b in range(NBLK):
            psb = P.ps()
            pbf = psb.t[:, :].bitcast(BF16)
            for kc in range(8):
                P.op(P.pe, lambda ee, kc=kc, pbf=pbf, b=b, i2=i2: ee.transpose(pbf[:, kc * 128:(kc + 1) * 128], xg[i2].t[:, b, kc * 128:(kc + 1) * 128], identb),
                     reads=[xg[i2], cb], writes=[psb], sig=(kc == 7))
            _copy(P, P.act if b % 2 == 0 else P.dve, xgT[i2].t[:, :, b * 128:(b + 1) * 128], pbf.rearrange("p (c n) -> p c n", c=8), [psb], [xgT[i2]])
        if e + 1 < n_exp:
            casts(e + 1)
        for j in range(2):
            pg = P.ps()
            for kc in range(8):
                _mm(P, pg, pg.t[:, 0:CAP], wg[i].t[:, kc, j * 128:(j + 1) * 128], xgT[i2].t[:, kc, :], kc == 0, kc == 7, [wg[i], xgT[i2]])
            pu = P.ps()
            for kc in range(8):
                _mm(P, pu, pu.t[:, 0:CAP], wu[i].t[:, kc, j * 128:(j + 1) * 128], xgT[i2].t[:, kc, :], kc == 0, kc == 7, [wu[i], xgT[i2]])
            _act(P, gsb[i2].t[:, j, :], pg.t[:, 0:CAP], AF.Silu, [pg], [gsb[i2]])
            _tt(P, P.dve, hTe[i2].t[:, j, :], pu.t[:, 0:CAP], gsb[i2].t[:, j, :], ALU.mult, [pu, gsb[i2]], [hTe[i2]])
        if e + 2 < n_exp:
            loads(e + 2)
        for b in range(NBLK):
            y_ = yb[yi % 3]
            yi += 1
            for half in range(2):
                psb = P.ps()
                for j in range(2):
                    _mm(P, psb, psb.t[:, 0:512], hTe[i2].t[:, j, b * 128:(b + 1) * 128], wd[i].t[:, j, half * 512:(half + 1) * 512], j == 0, j == 1,
                        [hTe[i2], wd[i]])
                _copy(P, P.act if half == 0 else P.dve, y_.t[:, half * 512:(half + 1) * 512], psb.t[:, 0:512], [psb], [y_])
            r0 = e * CAP + b * 128
            P.dma(P.sp, T["ys_d"][r0:r0 + 128, :], y_.t[:, :], reads=[y_], writes=[scr])
    P.pop_scope()


def pass_d(P, T, G):
    P.push_scope()
    lng = P.sb("lng2", [128, D], F32)
    lnb = P.sb("lnb2", [128, D], F32)
    epsb = P.sb("epsb", [128, 1], F32)
    P.dma(P.sp, lng.t[:, :], T["ln2_g"].partition_broadcast(128), writes=[lng])
    P.dma(P.sp, lnb.t[:, :], T["ln2_b"].partition_broadcast(128), writes=[lnb])
    P.op(P.dve, lambda e: e.memset(epsb.t[:, :], LN_EPS), writes=[epsb])
    yk = [P.sb("yk%d" % i, [128, D], BF16) for i in range(16)]
    acc = [P.sb("acc%d" % i, [128, D], F32) for i in range(2)]
    yo = [P.sb("yo%d" % i, [128, D], F32) for i in range(2)]
    tmp = {"st6": P.sb("st6", [128, 2, 6], F32), "mv": P.sb("mv", [128, 2], F32), "sc": P.sb("sc", [128, 2], F32)}
    slots_all, gates_all = G["slots"], G["gates"]
    scr = Buf(None, "scr_d")
    for tg in range(NT):
        a = acc[tg % 2]
        o = yo[tg % 2]
        P.dma(P.sp, a.t[:, :], T["pre_d"][tg * 128:(tg + 1) * 128, :], reads=[scr], writes=[a])
        for k in range(8):
            y_ = yk[(tg * 8 + k) % 16]
            P.dma(P.pool, None, None, reads=[slots_all, scr], writes=[y_],
                  fn=lambda h, k=k, tg=tg, y_=y_: h.indirect_dma_start(
                      out=y_.t[:, :], out_offset=None, in_=T["ys_d"],
                      in_offset=bass.IndirectOffsetOnAxis(ap=slots_all.t[:, tg, k:k + 1], axis=0)))
        for k in range(8):
            y_ = yk[(tg * 8 + k) % 16]
            _stt(P, a.t[:, :], y_.t[:, :], gates_all.t[:, tg, k:k + 1], a.t[:, :], ALU.mult, ALU.add, [y_, gates_all, a], [a])
        def norm2(rstd, nmr, a=a, o=o):
            P.op(P.act, lambda e: e.activation(o.t[:, :], a.t[:, :], AF.Identity, bias=nmr, scale=rstd), reads=[a, tmp["sc"]], writes=[o])
        layer_norm_tile(P, a, norm2, tmp, epsb.t[:, 0:1])
        _tt(P, P.dve, o.t[:, :], o.t[:, :], lng.t[:, :], ALU.mult, [o, lng], [o])
        _tt(P, P.pool, o.t[:, :], o.t[:, :], lnb.t[:, :], ALU.add, [o, lnb], [o])
        dst = T["yp"][tg * 128:(tg + 1) * 128, :] if tg * 128 < TP else T["ys"]
        P.dma(P.sp, dst, o.t[:, :], reads=[o], out_final=True)
    P.pop_scope()


IN_SPECS_A = [
    ("xp", [TP, D], F32), ("xs", [128, D], F32), ("ck", [NSUB_S, PAST, 512], F32), ("cv", [NSUB_S, PAST, 512], F32),
    ("clf", [NSUB_S, PAST, 8], F32), ("sconv", [NSUB_S, 30, 512], F32), ("w_in", [D, N_IN], F32), ("b_in", [1, N_IN], F32),
    ("pcol", [128, PC_N], F32), ("cf", [128, 384], F32), ("cb", [128, 384], BF16),
]
IN_SPECS_B = [
    ("w_a", [512, D], F32), ("w_b", [512, D], F32), ("w_out", [D, D], F32), ("ln1_g", [1, D], F32), ("ln1_b", [1, D], F32),
    ("w_router", [D, NE], F32), ("b_router", [1, NE], F32), ("w_sg", [D, DE], F32), ("w_su", [D, DE], F32), ("w_sd", [DE, D], F32),
    ("base1", [128, NE], F32), ("tokid", [128, NT], I32),
]
IN_SPECS_C = [
    ("w_eg", [NE, D, DE], F32), ("w_eu", [NE, D, DE], F32), ("w_ed", [NE, DE, D], F32), ("ln2_g", [1, D], F32), ("ln2_b", [1, D], F32),
]
OUT_SPECS = [
    ("yp", [TP, D]), ("ys", [128, D]), ("kp", [TP, 512]), ("vp", [TP, 512]), ("lfp", [TP, 8]), ("cvp", [NSEQ_P, 30, 512]),
    ("ks", [128, 512]), ("vs", [128, 512]), ("lfs", [128, 8]), ("cvs", [NSUB_S, 30, 512]),
]


def build(stage="A", NB=512, NBB=256, debug=False):
    nc = bass.Bass("TRN2", target_bir_lowering=False)
    T = {}
    specs = list(IN_SPECS_A)
    if stage >= "B":
        specs += IN_SPECS_B
    if stage >= "C":
        specs += IN_SPECS_C
    for name, shape, dt in specs:
        T[name] = nc.dram_tensor(name, shape, dt, kind="ExternalInput").ap()
    for name, shape in OUT_SPECS:
        T[name] = nc.dram_tensor(name, shape, F32, kind="ExternalOutput").ap()
    dk = "ExternalOutput" if debug else "Internal"
    T["attn_d"] = nc.dram_tensor("attn_d", [512, TT], BF16, kind=dk).ap()
    T["h_d"] = nc.dram_tensor("h_d", [512, TT], BF16, kind=dk).ap()
    T["c3_d"] = nc.dram_tensor("c3_d", [8, 3, 512], BF16, kind="Internal").ap()
    P = Prog(nc)
    G = {}
    if stage >= "B":
        T["xg_d"] = nc.dram_tensor("xg_d", [NSLOT, D], BF16, kind="Internal").ap()
        T["pre_d"] = nc.dram_tensor("pre_d", [TT, D], F32, kind="Internal").ap()
        if debug:
            T["mid_dbg"] = nc.dram_tensor("mid_dbg", [TT, D], F32, kind="ExternalOutput").ap()
            T["slots_dbg"] = nc.dram_tensor("slots_dbg", [128, NT * 8], I32, kind="ExternalOutput").ap()
            T["gates_dbg"] = nc.dram_tensor("gates_dbg", [128, NT * 8], F32, kind="ExternalOutput").ap()
        G["slots"] = P.sb("slots_all", [128, NT, 8], I32)
        G["gates"] = P.sb("gates_all", [128, NT, 8], F32)
    pass_a(P, T, NB)
    if stage >= "B":
        pass_b(P, T, G, NBB)
        if stage >= "C":
            T["ys_d"] = nc.dram_tensor("ys_d", [NSLOT, D], BF16, kind="Internal").ap()
            pass_c(P, T)
            pass_d(P, T, G)
        if debug:
            P.dma(P.sp, T["slots_dbg"], G["slots"].t[:, :, :].rearrange("p a b -> p (a b)"), reads=[G["slots"]], out_final=True)
            P.dma(P.sp, T["gates_dbg"], G["gates"].t[:, :, :].rearrange("p a b -> p (a b)"), reads=[G["gates"]], out_final=True)
    P.finish()
    print('[build] ops', P.n_ops, 'sim_us %.1f' % getattr(P, 'sim_time', 0.0), flush=True)
    return nc, [s[0] for s in specs]


def host_consts(inp):
    b_in = np.asarray(inp["b_in"])[0]
    pcol = np.zeros((128, PC_N), np.float32)
    col = lambda v, n: np.ascontiguousarray(np.asarray(v).reshape(n, 128).T)
    pcol[:, PC_BQ:PC_BQ + 4] = col(b_in[OFF_Q:OFF_K], 4)
    pcol[:, PC_BGLU:PC_BGLU + 8] = col(b_in[OFF_GLU:OFF_GA], 8)
    pcol[:, PC_BGA:PC_BGA + 8] = col(b_in[OFF_GA:OFF_GB], 8)
    pcol[:, PC_BGB:PC_BGB + 8] = col(b_in[OFF_GB:N_IN], 8)
    pcol[:, PC_BB:PC_BB + 8] = col(inp["b_b"][0], 8)
    pcol[:, PC_CB:PC_CB + 4] = col(inp["conv_b"][0], 4)
    pcol[:, PC_CG:PC_CG + 4] = col(inp["conv_ln_g"][0], 4)
    pcol[:, PC_CBE:PC_CBE + 4] = col(inp["conv_ln_b"][0], 4)
    cw = np.asarray(inp["conv_w"])[0]
    pcol[:, PC_CW:PC_CW + 124] = cw.T.reshape(4, 128, KW).transpose(1, 0, 2).reshape(128, 4 * KW)
    pcol[0:8, PC_BF] = b_in[OFF_F:OFF_GLU]
    pcol[:, PC_BK:PC_BK + 4] = col(b_in[OFF_K:OFF_V], 4)
    cf = np.concatenate([np.eye(128, dtype=np.float32), np.tril(np.ones((128, 128), np.float32), -1),
                         np.ones((128, 128), np.float32)], axis=1)
    cb = np.concatenate([np.eye(128, dtype=np.float32), np.triu(np.ones((128, 128), np.float32)),
                         np.triu(np.ones((128, 128), np.float32), 1)], axis=1).astype(ml_dtypes.bfloat16)
    base1 = np.ascontiguousarray(np.broadcast_to((np.arange(NE, dtype=np.float32) * CAP + 1.0)[None, :], (128, NE)))
    tokid = np.ascontiguousarray((np.arange(NT, dtype=np.int32)[None, :] * 128 + np.arange(128, dtype=np.int32)[:, None]).astype(np.int32))
    return pcol, cf, cb, base1, tokid


def core_inputs(inp, c, consts):
    pcol, cf, cb, base1, tokid = consts
    m = {
        "xp": np.asarray(inp["x_prompt"])[2 * c:2 * c + 2].reshape(TP, D),
        "xs": np.asarray(inp["x_sample"])[4 * c:4 * c + 4].reshape(128, D),
        "ck": np.asarray(inp["cache_k"])[0, 4 * c:4 * c + 4].reshape(NSUB_S, PAST, 512),
        "cv": np.asarray(inp["cache_v"])[0, 4 * c:4 * c + 4].reshape(NSUB_S, PAST, 512),
        "clf": np.asarray(inp["cache_logf"])[0, 4 * c:4 * c + 4],
        "sconv": np.asarray(inp["state_conv"])[0, 4 * c:4 * c + 4],
        "w_in": np.asarray(inp["w_in"])[0], "b_in": np.asarray(inp["b_in"]),
        "pcol": pcol, "cf": cf, "cb": cb, "base1": base1, "tokid": tokid,
        "w_a": np.asarray(inp["w_a"])[0], "w_b": np.asarray(inp["w_b"])[0], "w_out": np.asarray(inp["w_out"])[0],
        "ln1_g": np.asarray(inp["ln1_g"]), "ln1_b": np.asarray(inp["ln1_b"]),
        "w_router": np.asarray(inp["w_router"])[0], "b_router": np.asarray(inp["b_router"]),
        "w_sg": np.asarray(inp["w_s_gate"])[0], "w_su": np.asarray(inp["w_s_up"])[0], "w_sd": np.asarray(inp["w_s_down"])[0],
        "w_eg": np.asarray(inp["w_e_gate"])[0], "w_eu": np.asarray(inp["w_e_up"])[0], "w_ed": np.asarray(inp["w_e_down"])[0],
        "ln2_g": np.asarray(inp["ln2_g"]), "ln2_b": np.asarray(inp["ln2_b"]),
    }
    return m


def assemble(results):
    cat = lambda k: np.concatenate([r[k] for r in results], axis=0)
    yp = cat("yp").reshape(16, SEQ, D)
    ys = cat("ys").reshape(32, TS, D)
    kp = cat("kp").reshape(1, 16, SEQ, H, DH)
    vp = cat("vp").reshape(1, 16, SEQ, H, DH)
    lfp = cat("lfp").reshape(1, 16, SEQ, H)
    cvp = cat("cvp").reshape(1, 16, 30, CC)
    ks = cat("ks").reshape(1, 32, TS, H, DH)
    vs = cat("vs").reshape(1, 32, TS, H, DH)
    lfs = cat("lfs").reshape(1, 32, TS, H)
    cvs = cat("cvs").reshape(1, 32, 30, CC)
    return (yp, ys, kp, vp, lfp, cvp, ks, vs, lfs, cvs)


def kernel(**inputs):
    nc, names = build("C")
    consts = host_consts(inputs)
    in_maps = []
    for c in range(NCORES):
        m = core_inputs(inputs, c, consts)
        in_maps.append({k: (m[k] if m[k].flags["C_CONTIGUOUS"] else np.ascontiguousarray(m[k])) for k in names})
    res = run_bass_kernel_spmd(nc, in_maps, core_ids=list(range(NCORES)))
    return assemble(res.results)
```

```python
from contextlib import ExitStack

import numpy as np
import ml_dtypes
import concourse.bass as bass
import concourse.mybir as mybir
from concourse.bass_utils import run_bass_kernel_spmd

F32 = mybir.dt.float32
BF16 = mybir.dt.bfloat16
I32 = mybir.dt.int32
U32 = mybir.dt.uint32
AF = mybir.ActivationFunctionType
ALU = mybir.AluOpType


class Tr:
    __slots__ = ("sem", "count", "name")

    def __init__(self, sem, name):
        self.sem = sem
        self.count = 0
        self.name = name


class Buf:
    __slots__ = ("t", "w", "r", "name")

    def __init__(self, t, name=""):
        self.t = t
        self.w = None
        self.r = {}
        self.name = name


class Eng:
    def __init__(self, name, tr):
        self.name = name
        self.tr = tr
        self.ops = []
        self.waited = {}
        self.dma_trs = []
        self.dma_i = 0
        self.deferred = []


class Prog:
    def __init__(self, nc, n_dma=(28, 28, 6)):
        self.nc = nc
        self.stack = ExitStack()
        self.scopes = []
        mk = lambda n: Tr(self.stack.enter_context(nc.semaphore(n)), n)
        self.pe = Eng("pe", mk("s_pe"))
        self.act = Eng("act", mk("s_act"))
        self.dve = Eng("dve", mk("s_dve"))
        self.pool = Eng("pool", mk("s_pool"))
        self.sp = Eng("sp", mk("s_sp"))
        self.engs = [self.pe, self.act, self.dve, self.pool, self.sp]
        for e, n in zip((self.sp, self.pool, self.act), n_dma):
            e.dma_trs = [mk("d_%s%d" % (e.name, i)) for i in range(n)]
        self.banks = [Buf(self.stack.enter_context(nc.psum_tensor("psb%d" % i, [128, 512], F32)), "ps%d" % i)
                      for i in range(8)]
        self.bank_i = 0
        self.final = []
        self.n_ops = 0
        self.sched = True
        self.pending = []

    def sb(self, name, shape, dtype):
        st = self.scopes[-1] if self.scopes else self.stack
        self.n_sb = getattr(self, "n_sb", 0) + 1
        return Buf(st.enter_context(self.nc.sbuf_tensor("sb%d_%s" % (self.n_sb, name), list(shape), dtype)), name)

    def push_scope(self):
        self.scopes.append(ExitStack())

    def pop_scope(self):
        self.barrier()
        self.flush()
        self.scopes.pop().close()

    def ps(self, lo=0, hi=8):
        n = hi - lo
        b = self.banks[lo + (self.bank_i % n)]
        self.bank_i += 1
        return b

    def _deps(self, eng, reads, writes, extra=()):
        deps = {}

        def add(tok):
            if tok is None:
                return
            tr, v = tok
            if deps.get(tr, 0) < v:
                deps[tr] = v

        for b in reads:
            add(b.w)
        for b in writes:
            add(b.w)
            for tr, v in b.r.items():
                add((tr, v))
        for tok in extra:
            add(tok)
        waits = []
        for tr, v in deps.items():
            if eng is self.pe and tr is eng.tr:
                continue
            if eng.waited.get(tr, 0) >= v:
                continue
            eng.waited[tr] = v
            waits.append((tr, v))
        return waits

    @staticmethod
    def _mark(tok, reads, writes):
        tr, v = tok
        for b in reads:
            if b.r.get(tr, 0) < v:
                b.r[tr] = v
        for b in writes:
            b.w = tok
            b.r = {}

    def op(self, eng, fn, reads=(), writes=(), sig=True, c=0.3):
        if eng is self.pe and c == 0.3:
            c = 0.07
        if self.sched:
            self.pending.append(("op", eng, (fn, tuple(reads), tuple(writes), sig), tuple(reads), tuple(writes), c, 0))
            return None
        return self._op_now(eng, fn, reads, writes, sig)

    def dma(self, eng, out, in_, reads=(), writes=(), out_final=False, slow=False, fn=None, nbytes=None, **kw):
        if nbytes is None:
            try:
                ap = out if out is not None else None
                nbytes = 1
                for s_ in ap.shape:
                    nbytes *= int(s_)
                nbytes *= 2 if ap.dtype == BF16 else 4
            except Exception:
                nbytes = 262144
        if self.sched:
            self.pending.append(("dma", eng, (out, in_, tuple(reads), tuple(writes), out_final, slow, fn, kw), tuple(reads), tuple(writes),
                                 1.1 if fn is not None else 0.08, nbytes))
            return None
        return self._dma_now(eng, out, in_, reads, writes, out_final, slow, fn, **kw)

    def _op_now(self, eng, fn, reads=(), writes=(), sig=True):
        sig = True
        waits = self._deps(eng, reads, writes)
        tr = eng.tr
        if sig:
            tr.count += 1
            tok = (tr, tr.count)
        else:
            tok = None

        def run(h, waits=waits, fn=fn, sig=sig, tr=tr):
            for tr2, v in waits:
                h.wait_ge(tr2.sem, v)
            ins = fn(h)
            if sig:
                ins.then_inc(tr.sem, 1)

        eng.ops.append(run)
        self.n_ops += 1
        if tok is None:
            eng.deferred.append((reads, writes))
        else:
            for r_, w_ in eng.deferred:
                self._mark(tok, r_, w_)
            eng.deferred = []
            self._mark(tok, reads, writes)
        return tok

    def _dma_now(self, eng, out, in_, reads=(), writes=(), out_final=False, slow=False, fn=None, **kw):
        tr = eng.dma_trs[eng.dma_i % len(eng.dma_trs)]
        eng.dma_i += 1
        extra = [(tr, tr.count)] if tr.count else []
        waits = self._deps(eng, reads, writes, extra)
        tr.count += 16
        tok = (tr, tr.count)
        if slow:
            kw["allow_slow_non_contiguous"] = True

        def run(h, waits=waits, tr=tr, out=out, in_=in_, kw=kw, fn=fn):
            for tr2, v in waits:
                h.wait_ge(tr2.sem, v)
            if fn is not None:
                ins = fn(h)
            else:
                ins = h.dma_start(out=out, in_=in_, **kw)
            ins.then_inc(tr.sem, 16)

        eng.ops.append(run)
        self.n_ops += 1
        self._mark(tok, reads, writes)
        if out_final:
            self.final.append(tok)
        return tok

    def drain(self):
        pend = self.pending
        self.pending = []
        n = len(pend)
        if n == 0:
            return
        import heapq
        lastw, readers = {}, {}
        close = list(range(n))
        nxt = {}
        for i in range(n - 1, -1, -1):
            kind, eng = pend[i][0], pend[i][1]
            pass
        deps = [None] * n
        succ = [[] for _ in range(n)]
        indeg = [0] * n
        for i, (_, eng, _, reads, writes, _, _) in enumerate(pend):
            d = set()
            for b in reads:
                w = lastw.get(id(b))
                if w is not None:
                    d.add(w)
            for b in writes:
                w = lastw.get(id(b))
                if w is not None:
                    d.add(w)
                for r_ in readers.get(id(b), ()):
                    d.add(r_)
            d.discard(i)
            if not (pend[i][0] == "op" and eng is self.pe):
                d = {close[w] for w in d}
            else:
                d = {(w if pend[w][1] is self.pe else close[w]) for w in d}
            d.discard(i)
            deps[i] = d
            for j in d:
                succ[j].append(i)
            indeg[i] = len(d)
            for b in reads:
                readers.setdefault(id(b), []).append(i)
            for b in writes:
                lastw[id(b)] = i
                readers[id(b)] = []
        fin = [0.0] * n
        eng_free = {id(e): 0.0 for e in self.engs}
        ready = {id(e): [] for e in self.engs}
        for i in range(n):
            if indeg[i] == 0:
                heapq.heappush(ready[id(pend[i][1])], (0.0, i))
        dma_free = 0.0
        order = []
        LAT = 0.15
        WIN = 6000
        done = 0
        lo = 0
        scheduled = [False] * n
        while done < n:
            best = None
            for e in self.engs:
                h = ready[id(e)]
                if not h:
                    continue
                cand = None
                tmp_ = []
                k = 0
                while h and k < 8:
                    rt, i = heapq.heappop(h)
                    tmp_.append((rt, i))
                    k += 1
                    if i - lo > WIN:
                        continue
                    st_ = max(rt, eng_free[id(e)])
                    key = (st_, i)
                    if cand is None or key < cand[0]:
                        cand = (key, rt, i)
                for x in tmp_:
                    heapq.heappush(h, x)
                if cand is not None and (best is None or cand[0] < best[0][0]):
                    best = (cand, e)
            if best is None:
                cands = [(h[0][1], e) for e in self.engs for h in [ready[id(e)]] if h]
                i_min = None
                for e in self.engs:
                    for rt, i in ready[id(e)]:
                        if i_min is None or i < i_min[0]:
                            i_min = (i, rt, e)
                i, rt, e = i_min
                best = ((((max(rt, eng_free[id(e)])), i), rt, i), e)
            (key, rt, i), e = best
            h = ready[id(e)]
            h.remove((rt, i))
            heapq.heapify(h)
            kind, _, _, _, _, cost, nbytes = pend[i]
            st_ = key[0]
            if kind == "dma":
                eng_free[id(e)] = st_ + cost
                t0_ = max(st_ + cost, dma_free)
                dma_free = t0_ + nbytes / 400000.0
                fin[i] = dma_free + 1.8
            else:
                eng_free[id(e)] = st_ + cost
                fin[i] = st_ + cost
            scheduled[i] = True
            order.append(i)
            done += 1
            while lo < n and scheduled[lo]:
                lo += 1
            for j in succ[i]:
                indeg[j] -= 1
                if indeg[j] == 0:
                    rt_j = 0.0
                    for d_ in deps[j]:
                        l_ = 0.0 if pend[d_][1] is pend[j][1] and pend[j][1] is self.pe else LAT
                        if fin[d_] + l_ > rt_j:
                            rt_j = fin[d_] + l_
                    heapq.heappush(ready[id(pend[j][1])], (rt_j, j))
        self.sim_time = getattr(self, "sim_time", 0.0) + max(fin)
        for i in order:
            kind, eng, args, _, _, _, _ = pend[i]
            if kind == "op":
                fn, reads, writes, sig = args
                self._op_now(eng, fn, reads, writes, sig)
            else:
                out, in_, reads, writes, out_final, slow, fn, kw = args
                self._dma_now(eng, out, in_, reads, writes, out_final, slow, fn, **kw)

    def barrier(self):
        self.drain()
        toks = []
        for e in self.engs:
            if e.tr.count:
                toks.append((e.tr, e.tr.count))
            for tr in e.dma_trs:
                if tr.count:
                    toks.append((tr, tr.count))
        for e in self.engs:
            waits = []
            for tr, v in toks:
                if tr is e.tr:
                    continue
                if e.waited.get(tr, 0) >= v:
                    continue
                e.waited[tr] = v
                waits.append((tr, v))
            if waits:
                def run(h, waits=waits):
                    for tr2, v in waits:
                        h.wait_ge(tr2.sem, v)
                e.ops.append(run)

    def flush(self):
        nc = self.nc
        pend = {e.name: e.ops for e in self.engs}
        for e in self.engs:
            e.ops = []
        with nc.Block() as block:
            @block.tensor
            def _(h):
                for f in pend["pe"]:
                    f(h)

            @block.scalar
            def _(h):
                for f in pend["act"]:
                    f(h)

            @block.vector
            def _(h):
                for f in pend["dve"]:
                    f(h)

            @block.gpsimd
            def _(h):
                for f in pend["pool"]:
                    f(h)

            @block.sync
            def _(h):
                for f in pend["sp"]:
                    f(h)

    def finish(self):
        self.drain()
        waits = []
        for tr, v in self.final:
            if self.sp.waited.get(tr, 0) < v:
                self.sp.waited[tr] = v
                waits.append((tr, v))

        def run(h, waits=waits):
            for tr2, v in waits:
                h.wait_ge(tr2.sem, v)

        self.sp.ops.append(run)
        self.barrier()
        self.flush()
        while self.scopes:
            self.scopes.pop().close()
        self.stack.close()


D = 1024
NCORES = 8
SEQ = 2048
NSEQ_P = 2
NSUB_S = 4
TS = 32
PAST = 1024
H = 8
DH = 64
CC = 512
KW = 31
NE = 256
DE = 256
OFF_Q, OFF_K, OFF_V, OFF_F, OFF_GLU, OFF_GA, OFF_GB, N_IN = 0, 512, 1024, 1536, 1544, 2568, 3592, 4616
NA = OFF_GA
TP = NSEQ_P * SEQ
TT = TP + 128
DN_ALPHA = 2.0 ** 0.25
LN_EPS = 1e-5

PC_BQ, PC_BGLU, PC_BGA, PC_BGB, PC_BB, PC_CB, PC_CG, PC_CBE, PC_CW, PC_BF, PC_BK, PC_N = 0, 4, 12, 20, 28, 36, 40, 44, 48, 172, 173, 177


def _n(ap):
    n = 1
    for s_ in ap.shape[1:]:
        n *= int(s_)
    return n


def _mm(P, psb, out_ap, lhsT, rhs, start, stop, reads, sig=None):
    c = max(_n(rhs), 64) / 2400.0 * (4.0 if lhsT.dtype == F32 else 1.0) + 0.01
    P.op(P.pe, lambda e: e.matmul(out_ap, lhsT, rhs, start=start, stop=stop), reads=reads, writes=[psb],
         sig=stop if sig is None else sig, c=c)


def _act(P, out_ap, in_ap, func, reads, writes, bias=None, scale=1.0):
    c = _n(out_ap) / 1050.0 + 0.27
    if bias is None:
        P.op(P.act, lambda e: e.activation(out_ap, in_ap, func, scale=scale), reads=reads, writes=writes, c=c)
    else:
        P.op(P.act, lambda e: e.activation(out_ap, in_ap, func, bias=bias, scale=scale), reads=reads, writes=writes, c=c)


def _tt(P, eng, out_ap, a, b, op, reads, writes):
    c = _n(out_ap) / (950.0 if eng is P.dve else 440.0) + (0.17 if eng is P.dve else 0.15)
    P.op(eng, lambda e: e.tensor_tensor(out_ap, a, b, op), reads=reads, writes=writes, c=c)


def _stt(P, out_ap, in0, scalar, in1, op0, op1, reads, writes):
    P.op(P.dve, lambda e: e.scalar_tensor_tensor(out_ap, in0, scalar, in1, op0, op1), reads=reads, writes=writes, c=_n(out_ap) / 960.0 + 0.12)


def _ts(P, eng, out_ap, in0, s1, s2, op0, op1, reads, writes):
    c = _n(out_ap) / 960.0 + 0.12
    if s2 is None:
        P.op(eng, lambda e: e.tensor_scalar(out_ap, in0, s1, None, op0), reads=reads, writes=writes, c=c)
    else:
        P.op(eng, lambda e: e.tensor_scalar(out_ap, in0, s1, s2, op0, op1), reads=reads, writes=writes, c=c)


def _copy(P, eng, out_ap, in_ap, reads, writes):
    if eng is P.act:
        P.op(eng, lambda e: e.copy(out_ap, in_ap), reads=reads, writes=writes, c=_n(out_ap) / 1050.0 + 0.27)
    else:
        P.op(eng, lambda e: e.tensor_copy(out_ap, in_ap), reads=reads, writes=writes,
             c=_n(out_ap) / (960.0 if eng is P.dve else 280.0) + (0.12 if eng is P.dve else 0.3))


def load_w_cast(P, dst, src_ap, kchunks, ncols, col0=0):
    v = src_ap.rearrange("(kc p) n -> p kc n", p=128)
    c = 0
    while c < ncols:
        n = min(2048, ncols - c)
        P.dma(P.pool, dst.t[:, :, c:c + n], v[:, :, col0 + c:col0 + c + n], reads=[], writes=[dst])
        c += n


def pass_a(P, T, NB):
    nc = P.nc
    P.push_scope()
    cf = P.sb("cf", [128, 384], F32)
    cb = P.sb("cb", [128, 256], BF16)
    pcol = P.sb("pcol", [128, PC_N], F32)
    w_in = P.sb("w_in_a", [128, 8, NA], BF16)
    bkv = P.sb("bkv", [128, 1024], F32)
    bq8 = P.sb("bq8", [128, 4], F32)
    nbf = P.sb("nbf", [8, 1], F32)
    ones8 = P.sb("ones8", [8, 512], F32)
    onesb = P.sb("onesb", [128, 128], BF16)
    epsb = P.sb("epsb", [128, 1], F32)
    oneb = P.sb("oneb", [128, 1], F32)
    scr = Buf(None, "scratch")
    c3d = Buf(None, "c3d")
    P.dma(P.sp, cf.t[:, :], T["cf"], writes=[cf])
    P.dma(P.sp, cb.t[:, :], T["cb"][:, 0:256], writes=[cb])
    P.dma(P.sp, pcol.t[:, :], T["pcol"], writes=[pcol])
    P.dma(P.sp, bkv.t[:, :], T["b_in"][:, OFF_K:OFF_F].partition_broadcast(128), writes=[bkv])
    load_w_cast(P, w_in, T["w_in"], 8, NA)
    P.op(P.act, lambda e: e.mul(bq8.t[:, :], pcol.t[:, PC_BQ:PC_BQ + 4], 0.125), reads=[pcol], writes=[bq8])
    P.op(P.act, lambda e: e.mul(nbf.t[:, :], pcol.t[0:8, PC_BF:PC_BF + 1], -1.0), reads=[pcol], writes=[nbf])
    P.op(P.dve, lambda e: e.memset(ones8.t[:, :], 1.0), writes=[ones8])
    P.op(P.dve, lambda e: e.memset(onesb.t[:, :], 1.0), writes=[onesb])
    P.op(P.dve, lambda e: e.memset(epsb.t[:, :], LN_EPS), writes=[epsb])
    P.op(P.dve, lambda e: e.memset(oneb.t[:, :], 1.0), writes=[oneb])
    identf = cf.t[:, 0:128]
    lstrict = cf.t[:, 128:256]
    onesf = cf.t[:, 256:384]
    identb = cb.t[:, 0:128]
    tri = cb.t[:, 128:256]

    kT = P.sb("kT", [128, 4, SEQ], BF16)
    vsb = P.sb("vsb", [128, SEQ // 128, 512], BF16)
    negc = P.sb("negc", [128, SEQ // 128, 8], F32)
    xt = [P.sb("xt%d" % i, [128, 1024], F32) for i in range(2)]
    xT = P.sb("xT", [128, 8, NB], BF16)
    qz = P.sb("qz", [128, 8, NB], BF16)
    c3pad = P.sb("c3pad", [128, 8, NB], BF16)
    uT = P.sb("uT", [128, 4, 30 + NB], F32)
    sg = [P.sb("sg%d" % i, [128, NB], F32) for i in range(2)]
    kvf = [P.sb("kvf%d" % i, [128, 1024], F32) for i in range(2)]
    kbf = [P.sb("kbf%d" % i, [128, 512], BF16) for i in range(2)]
    lfneg = P.sb("lfneg", [8, NB], F32)
    cblk = P.sb("cblk", [8, NB], F32)
    carry = P.sb("carry", [8, 1], F32)
    c3p = P.sb("c3p", [8, 3, NB], BF16)
    ctmp = P.sb("ctmp", [8, NB], F32)
    et = ctmp
    lftok = P.sb("lftok", [128, NB // 128, 8], F32)
    pt = [P.sb("pt%d" % i, [128, NB], BF16) for i in range(3)]
    attnT = P.sb("attnT", [64, 8, NB], BF16)
    acc = [P.sb("cacc%d" % i, [128, NB], F32) for i in range(2)]
    rden = acc
    hc = P.sb("hc", [128, 4, NB], F32)
    hsq = P.sb("hsq", [128, NB], F32)
    mean = P.sb("mean", [128, NB], F32)
    rstd = P.sb("rstd", [128, NB], F32)
    hn = sg
    hT = P.sb("hT", [128, 4, NB], BF16)
    ubf = P.sb("ubf", [128, 4, 30 + NB], BF16)
    diag = [P.sb("diag%d" % i, [128, 128], BF16) for i in range(6)]
    cvo = P.sb("cvo", [32, 512], F32)
    P.op(P.pool, lambda e: e.memset(qz.t[:, :, :], 0.0), writes=[qz])
    P.op(P.pool, lambda e: e.memset(c3pad.t[:, :, :], 0.0), writes=[c3pad])

    st = {"pt": 0, "x": 0, "kv": 0, "dg": 0}

    def project_feature(col0, nfeat, nb, evac):
        psb = P.ps()
        for kc in range(8):
            _mm(P, psb, psb.t[0:nfeat, 0:nb], w_in.t[:, kc, col0:col0 + nfeat], xT.t[:, kc, 0:nb], kc == 0, kc == 7,
                [w_in, xT])
        evac(psb)

    def attention(h, qc0, nq, ktiles):
        ob = P.banks[4 + (h % 2)]
        db = P.banks[6 + (h % 2)]
        n = len(ktiles)
        for i, kt in enumerate(ktiles):
            nk, q0 = kt["nk"], kt["q0"]
            w = nq - q0
            sb_ = P.ps(0, 4)
            _mm(P, sb_, sb_.t[0:nk, 0:w], kt["kT"], qz.t[:, h, qc0 + q0:qc0 + nq], True, False, kt["reads"] + [qz])
            _mm(P, sb_, sb_.t[0:nk, 0:w], onesb.t[:, 0:nk], c3pad.t[:, h, qc0 + q0:qc0 + nq], False, True,
                [onesb, c3pad])
            p = pt[st["pt"] % 3]
            st["pt"] += 1
            _act(P, p.t[0:nk, 0:w], sb_.t[0:nk, 0:w], AF.Exp, kt["reads"] + [sb_], [p], bias=kt["bias"])
            if kt["tri"]:
                tw = min(nk, w)
                _tt(P, P.pool, p.t[0:nk, 0:tw], p.t[0:nk, 0:tw], tri[0:nk, 0:tw], ALU.mult, [p, cb], [p])
            _mm(P, ob, ob.t[0:64, q0:nq], kt["v"], p.t[0:nk, 0:w], i == 0, i == n - 1, kt["reads"] + [p])
            _mm(P, db, db.t[0:64, q0:nq], onesb.t[0:nk, 0:64], p.t[0:nk, 0:w], i == 0, i == n - 1, [onesb, p])
        rd = rden[h % 2]
        _act(P, rd.t[0:64, 0:nq], db.t[0:64, 0:nq], AF.Ln, [db], [rd])
        _act(P, rd.t[0:64, 0:nq], rd.t[0:64, 0:nq], AF.Exp, [rd], [rd], scale=-1.0)
        _tt(P, P.dve, attnT.t[:, h, qc0:qc0 + nq], ob.t[0:64, 0:nq], rd.t[0:64, 0:nq], ALU.mult, [ob, rd], [attnT])


    def conv_ln(nb, uview, accview, c_list=range(4)):
        ps_s = P.ps()
        ps_q = P.ps()
        for c in range(4):
            _copy(P, P.pool, ubf.t[:, c, :], uT.t[:, c, :], [uT], [ubf])
        for c in range(4):
            psc = P.ps()
            for j in range(KW):
                dg = diag[st["dg"] % len(diag)]
                st["dg"] += 1
                col = pcol.t[:, PC_CW + c * KW + j:PC_CW + c * KW + j + 1]
                P.op(P.act, lambda e, dg=dg, col=col: e.activation(dg.t[:, :], identb, AF.Identity, scale=col), reads=[cb, pcol], writes=[dg], c=0.32)
                _mm(P, psc, accview(psc), dg.t[:, :], uview(c, j), j == 0, j == KW - 1, [dg, ubf], sig=True)
            _act(P, hc.t[:, c, 0:nb], psc.t[:, 0:nb], AF.Identity, [psc, pcol], [hc], bias=pcol.t[:, PC_CB + c:PC_CB + c + 1])
            P.op(P.act, lambda e, c=c: e.activation(hsq.t[:, 0:nb], hc.t[:, c, 0:nb], AF.Square), reads=[hc], writes=[hsq], c=nb / 1400.0 + 0.22)
            _mm(P, ps_s, ps_s.t[:, 0:nb], onesf, hc.t[:, c, 0:nb], c == 0, c == 3, [cf, hc], sig=True)
            _mm(P, ps_q, ps_q.t[:, 0:nb], onesf, hsq.t[:, 0:nb], c == 0, c == 3, [cf, hsq], sig=True)
        P.op(P.act, lambda e: e.mul(mean.t[:, 0:nb], ps_s.t[:, 0:nb], 1.0 / CC), reads=[ps_s], writes=[mean])
        m2 = hn[0]
        _tt(P, P.dve, m2.t[:, 0:nb], mean.t[:, 0:nb], mean.t[:, 0:nb], ALU.mult, [mean], [m2])
        _stt(P, rstd.t[:, 0:nb], ps_q.t[:, 0:nb], 1.0 / CC, m2.t[:, 0:nb], ALU.mult, ALU.subtract, [ps_q, m2], [rstd])
        _act(P, rstd.t[:, 0:nb], rstd.t[:, 0:nb], AF.Sqrt, [rstd], [rstd], bias=epsb.t[:, 0:1])
        P.op(P.dve, lambda e: e.reciprocal(rstd.t[:, 0:nb], rstd.t[:, 0:nb]), reads=[rstd], writes=[rstd])
        for c in range(4):
            x_ = hn[c % 2]
            _tt(P, P.dve, x_.t[:, 0:nb], hc.t[:, c, 0:nb], mean.t[:, 0:nb], ALU.subtract, [hc, mean], [x_])
            _tt(P, P.pool, x_.t[:, 0:nb], x_.t[:, 0:nb], rstd.t[:, 0:nb], ALU.mult, [x_, rstd], [x_])
            P.op(P.act, lambda e, c=c, x_=x_: e.activation(hT.t[:, c, 0:nb], x_.t[:, 0:nb], AF.Silu,
                                                       bias=pcol.t[:, PC_CBE + c:PC_CBE + c + 1],
                                                       scale=pcol.t[:, PC_CG + c:PC_CG + c + 1]),
                 reads=[x_, pcol], writes=[hT])

    def conv_state_out(uview30, dst):
        psb = P.ps()
        for c in range(4):
            P.op(P.pe, lambda e, c=c, psb=psb: e.transpose(psb.t[0:30, c * 128:(c + 1) * 128], uview30(c), identf),
                 reads=[uT, cf], writes=[psb], sig=(c == 3))
        _copy(P, P.act, cvo.t[0:30, :], psb.t[0:30, 0:512], [psb], [cvo])
        P.dma(P.sp, dst, cvo.t[0:30, :], reads=[cvo], out_final=True)

    def common_front(x_src, nb, subs):
        ntile = nb // 128
        for t in range(ntile):
            xb = xt[st["x"] % 2]
            st["x"] += 1
            P.dma(P.sp, xb.t[:, :], x_src[t * 128:(t + 1) * 128, :], writes=[xb])
            for g in range(2):
                psb = P.ps()
                for j in range(4):
                    kc = g * 4 + j
                    P.op(P.pe, lambda e, psb=psb, j=j, kc=kc, xb=xb: e.transpose(
                        psb.t[:, j * 128:(j + 1) * 128], xb.t[:, kc * 128:(kc + 1) * 128], identf),
                        reads=[xb, cf], writes=[psb], sig=(j == 3))
                eng = P.act if g == 0 else P.dve
                _copy(P, eng, xT.t[:, g * 4:(g + 1) * 4, t * 128:(t + 1) * 128],
                      psb.t[:, :].rearrange("p (a b) -> p a b", a=4), [psb], [xT])
        for c in range(4):
            def ev(psb, c=c):
                _act(P, qz.t[0:64, 2 * c, 0:nb], psb.t[0:64, 0:nb], AF.Identity, [psb, bq8], [qz],
                     bias=bq8.t[0:64, c:c + 1], scale=0.125)
                _act(P, qz.t[64:128, 2 * c + 1, 0:nb], psb.t[64:128, 0:nb], AF.Identity, [psb, bq8], [qz],
                     bias=bq8.t[64:128, c:c + 1], scale=0.125)
            project_feature(OFF_Q + c * 128, 128, nb, ev)
        def evf(psb):
            _act(P, et.t[:, 0:nb], psb.t[0:8, 0:nb], AF.Exp, [psb, nbf], [et], bias=nbf.t[:, 0:1], scale=-1.0)
            _act(P, lfneg.t[:, 0:nb], et.t[:, 0:nb], AF.Ln, [et], [lfneg], bias=oneb.t[0:8, 0:1])
        project_feature(OFF_F, 8, nb, evf)
        for s in subs:
            c0, n = s["c0"], s["n"]
            init = 0.0 if s["first"] else carry.t[:, 0:1]
            P.op(P.dve, lambda e, c0=c0, n=n, init=init: e.tensor_tensor_scan(
                cblk.t[:, c0:c0 + n], ones8.t[:, 0:n], lfneg.t[:, c0:c0 + n], init, ALU.mult, ALU.subtract),
                reads=[ones8, lfneg, carry], writes=[cblk])
        _copy(P, P.act, carry.t[:, 0:1], cblk.t[:, nb - 1:nb], [cblk], [carry])
        _copy(P, P.dve, c3p.t[:, 0, 0:nb], cblk.t[:, 0:nb], [cblk], [c3p])
        _tt(P, P.dve, ctmp.t[:, 0:nb], cblk.t[:, 0:nb], c3p.t[:, 0, 0:nb], ALU.subtract, [cblk, c3p], [ctmp])
        _copy(P, P.dve, c3p.t[:, 1, 0:nb], ctmp.t[:, 0:nb], [ctmp], [c3p])
        _tt(P, P.dve, c3p.t[:, 2, 0:nb], ctmp.t[:, 0:nb], c3p.t[:, 1, 0:nb], ALU.subtract, [ctmp, c3p], [c3p])
        P.dma(P.sp, T["c3_d"][:, :, 0:nb], c3p.t[:, :, 0:nb], reads=[c3p], writes=[c3d])
        P.dma(P.sp, c3pad.t[0:3, :, 0:nb], T["c3_d"][:, :, 0:nb].rearrange("h s n -> s h n"), reads=[c3d], writes=[c3pad])

    def glu(nb, uout, view=lambda a: a):
        for c in range(4):
            sgb = sg[c % 2]
            def evg(psb, sgb=sgb, c=c):
                _act(P, sgb.t[:, 0:nb], psb.t[:, 0:nb], AF.Sigmoid, [psb, pcol], [sgb],
                     bias=pcol.t[:, PC_BGLU + 4 + c:PC_BGLU + 5 + c])
            project_feature(OFF_GLU + CC + c * 128, 128, nb, evg)
            def eva(psb, sgb=sgb, c=c):
                _stt(P, uout(c), view(psb.t[:, 0:nb]), pcol.t[:, PC_BGLU + c:PC_BGLU + c + 1], view(sgb.t[:, 0:nb]),
                     ALU.add, ALU.mult, [psb, pcol, sgb], [uT])
            project_feature(OFF_GLU + c * 128, 128, nb, eva)

    def store_scratch(nb, tok0):
        P.dma(P.sp, T["attn_d"].rearrange("(h d) t -> d h t", d=64)[:, :, tok0:tok0 + nb], attnT.t[:, :, 0:nb],
              reads=[attnT], writes=[scr])
        P.dma(P.sp, T["h_d"].rearrange("(c p) t -> p c t", p=128)[:, :, tok0:tok0 + nb], hT.t[:, :, 0:nb],
              reads=[hT], writes=[scr])

    ntile = NB // 128
    for sq in range(NSEQ_P):
        for b in range(SEQ // NB):
            r0 = sq * SEQ + b * NB
            tile0 = b * ntile
            if b == 0:
                P.op(P.pool, lambda e: e.memset(uT.t[:, :, 0:30], 0.0), writes=[uT])
            else:
                _copy(P, P.pool, uT.t[:, :, 0:30], uT.t[:, :, NB:NB + 30], [uT], [uT])
            common_front(T["xp"][r0:r0 + NB, :], NB, [{"c0": 0, "n": NB, "first": b == 0}])
            for t in range(ntile):
                psb = P.ps()
                P.op(P.pe, lambda e, psb=psb, t=t: e.transpose(psb.t[:, 0:8], lfneg.t[:, t * 128:(t + 1) * 128], identf[0:8, 0:8]),
                     reads=[lfneg, cf], writes=[psb])
                P.op(P.pe, lambda e, psb=psb, t=t: e.transpose(psb.t[:, 8:16], cblk.t[:, t * 128:(t + 1) * 128], identf[0:8, 0:8]),
                     reads=[cblk, cf], writes=[psb])
                P.op(P.act, lambda e, psb=psb, t=t: e.mul(lftok.t[:, t, :], psb.t[:, 0:8], -1.0), reads=[psb], writes=[lftok])
                P.op(P.act, lambda e, psb=psb, j=tile0 + t: e.mul(negc.t[:, j, :], psb.t[:, 8:16], -1.0), reads=[psb], writes=[negc])
            P.dma(P.sp, T["lfp"][r0:r0 + NB, :].rearrange("(t p) h -> p t h", p=128), lftok.t[:, 0:ntile, :],
                  reads=[lftok], out_final=True)
            for t in range(ntile):
                j = tile0 + t
                kvb = kvf[st["kv"] % 2]
                kb = kbf[st["kv"] % 2]
                st["kv"] += 1
                for half in range(2):
                    psb = P.ps()
                    for kc in range(8):
                        _mm(P, psb, psb.t[:, 0:512], xT.t[:, kc, t * 128:(t + 1) * 128],
                            w_in.t[:, kc, OFF_K + half * 512:OFF_K + (half + 1) * 512], kc == 0, kc == 7, [xT, w_in])
                    _tt(P, P.dve, kvb.t[:, half * 512:(half + 1) * 512], psb.t[:, 0:512], bkv.t[:, half * 512:(half + 1) * 512],
                        ALU.add, [psb, bkv], [kvb])
                rr = r0 + t * 128
                P.dma(P.sp, T["kp"][rr:rr + 128, :], kvb.t[:, 0:512], reads=[kvb], out_final=True)
                P.dma(P.sp, T["vp"][rr:rr + 128, :], kvb.t[:, 512:1024], reads=[kvb], out_final=True)
                _copy(P, P.act, kb.t[:, :], kvb.t[:, 0:512], [kvb], [kb])
                _copy(P, P.pool, vsb.t[:, j, :], kvb.t[:, 512:1024], [kvb], [vsb])
                psb = P.ps()
                pbf = psb.t[:, :].bitcast(BF16)
                for c in range(4):
                    P.op(P.pe, lambda e, c=c, pbf=pbf, kb=kb: e.transpose(pbf[:, c * 128:(c + 1) * 128], kb.t[:, c * 128:(c + 1) * 128], identb),
                         reads=[kb, cb], writes=[psb], sig=(c == 3))
                _copy(P, P.dve, kT.t[:, :, j * 128:(j + 1) * 128], pbf[:, 0:512].rearrange("p (c n) -> p c n", c=4), [psb], [kT])
            glu(NB, lambda c: uT.t[:, c, 30:30 + NB])
            for h in range(8):
                kts = []
                for j in range(tile0 + ntile):
                    kts.append(dict(kT=kT.t[:, h // 2, j * 128:(j + 1) * 128], v=vsb.t[:, j, h * 64:(h + 1) * 64],
                                    bias=negc.t[:, j, h:h + 1], nk=128, q0=max(0, (j - tile0) * 128), tri=j >= tile0,
                                    reads=[kT, vsb, negc]))
                attention(h, 0, NB, kts)
            conv_ln(NB, lambda c, j: ubf.t[:, c, j:j + NB], lambda a: a.t[:, 0:NB])
            store_scratch(NB, r0)
            if b == SEQ // NB - 1:
                conv_state_out(lambda c: uT.t[:, c, NB:NB + 30], T["cvp"][sq, :, :])
    uSv = uT.t[:, :, 0:NSUB_S * 62].rearrange("p c (s n) -> p c s n", s=NSUB_S)
    ckb = P.sb("ckb", [128, 8, 512], BF16)
    clfb = P.sb("clfb", [128, 8, 8], F32)
    sufs = P.sb("sufs", [128, 8, 8], F32)
    negcs = P.sb("negcs", [32, NSUB_S, 8], F32)
    kTn = P.sb("kTn", [128, 4, 128], BF16)
    vnew = P.sb("vnew", [32, NSUB_S, 512], BF16)
    kvs = kvf
    scv = kvf
    v4 = lambda a: a.rearrange("p (s n) -> p s n", s=NSUB_S)
    for s in range(NSUB_S):
        sc = scv[s % 2]
        P.dma(P.sp, sc.t[0:30, 0:512], T["sconv"][s, :, :], writes=[sc])
        psb = P.ps()
        for c in range(4):
            P.op(P.pe, lambda e, c=c, psb=psb, sc=sc: e.transpose(psb.t[:, c * 32:c * 32 + 30], sc.t[0:30, c * 128:(c + 1) * 128],
                                                               identf[0:30, 0:30]),
                 reads=[sc, cf], writes=[psb], sig=(c == 3))
        _copy(P, P.act, uSv[:, :, s, 0:30], psb.t[:, 0:128].rearrange("p (c n) -> p c n", c=4)[:, :, 0:30], [psb], [uT])
    common_front(T["xs"], 128, [{"c0": 32 * s, "n": 32, "first": True} for s in range(NSUB_S)])
    psb = P.ps()
    P.op(P.pe, lambda e, psb=psb: e.transpose(psb.t[:, 0:8], lfneg.t[:, 0:128], identf[0:8, 0:8]), reads=[lfneg, cf], writes=[psb])
    P.op(P.act, lambda e, psb=psb: e.mul(lftok.t[:, 0, :], psb.t[:, 0:8], -1.0), reads=[psb], writes=[lftok])
    P.dma(P.sp, T["lfs"], lftok.t[:, 0, :], reads=[lftok], out_final=True)
    for s in range(NSUB_S):
        psb = P.ps()
        P.op(P.pe, lambda e, psb=psb, s=s: e.transpose(psb.t[0:32, 0:8], cblk.t[:, 32 * s:32 * s + 32], identf[0:8, 0:8]),
             reads=[cblk, cf], writes=[psb])
        P.op(P.act, lambda e, psb=psb, s=s: e.mul(negcs.t[0:32, s, :], psb.t[0:32, 0:8], -1.0), reads=[psb], writes=[negcs])
        kvb = kvs[s % 2]
        for half in range(2):
            psb = P.ps()
            for kc in range(8):
                _mm(P, psb, psb.t[0:32, 0:512], xT.t[:, kc, 32 * s:32 * s + 32],
                    w_in.t[:, kc, OFF_K + half * 512:OFF_K + (half + 1) * 512], kc == 0, kc == 7, [xT, w_in])
            _tt(P, P.dve, kvb.t[0:32, half * 512:(half + 1) * 512], psb.t[0:32, 0:512], bkv.t[0:32, half * 512:(half + 1) * 512],
                ALU.add, [psb, bkv], [kvb])
        P.dma(P.sp, T["ks"][32 * s:32 * s + 32, :], kvb.t[0:32, 0:512], reads=[kvb], out_final=True)
        P.dma(P.sp, T["vs"][32 * s:32 * s + 32, :], kvb.t[0:32, 512:1024], reads=[kvb], out_final=True)
        _copy(P, P.act, vnew.t[0:32, s, :], kvb.t[0:32, 512:1024], [kvb], [vnew])
    for c in range(4):
        def evk(psb, c=c):
            _act(P, kTn.t[:, c, 0:128], psb.t[:, 0:128], AF.Identity, [psb, pcol], [kTn], bias=pcol.t[:, PC_BK + c:PC_BK + c + 1])
        project_feature(OFF_K + c * 128, 128, 128, evk)
    glu(128, lambda c: uSv[:, c, :, 30:62], v4)
    for s in range(NSUB_S):
        P.dma(P.pool, ckb.t[:, :, :], T["ck"][s].rearrange("(j p) n -> p j n", p=128), writes=[ckb])
        P.dma(P.pool, vsb.t[:, 0:8, :], T["cv"][s].rearrange("(j p) n -> p j n", p=128), writes=[vsb])
        P.dma(P.sp, clfb.t[:, :, :], T["clf"][s].rearrange("(j p) h -> p j h", p=128), writes=[clfb])
        for j in range(8):
            psb = P.ps()
            pbf = psb.t[:, :].bitcast(BF16)
            for c in range(4):
                P.op(P.pe, lambda e, c=c, j=j, pbf=pbf: e.transpose(pbf[:, c * 128:(c + 1) * 128], ckb.t[:, j, c * 128:(c + 1) * 128], identb),
                     reads=[ckb, cb], writes=[psb], sig=(c == 3))
            _copy(P, P.dve if j % 2 else P.act, kT.t[:, :, j * 128:(j + 1) * 128],
                  pbf[:, 0:512].rearrange("p (c n) -> p c n", c=4), [psb], [kT])
        psb = P.ps()
        for j in range(8):
            _mm(P, psb, psb.t[:, j * 8:(j + 1) * 8], lstrict, clfb.t[:, j, :], True, j == 7, [cf, clfb], sig=False)
            for j2 in range(j + 1, 8):
                _mm(P, psb, psb.t[:, j * 8:(j + 1) * 8], onesf, clfb.t[:, j2, :], False, j2 == 7, [cf, clfb], sig=False)
        P.op(P.pe, lambda e, psb=psb: e.transpose(psb.t[0:8, 64:72], clfb.t[0:8, 0, :], identf[0:8, 0:8]), reads=[clfb, cf], writes=[psb])
        _copy(P, P.dve, sufs.t[:, :, :], psb.t[:, 0:64].rearrange("p (j h) -> p j h", j=8), [psb], [sufs])
        for h in range(8):
            kts = []
            for j in range(8):
                kts.append(dict(kT=kT.t[:, h // 2, j * 128:(j + 1) * 128], v=vsb.t[:, j, h * 64:(h + 1) * 64],
                                bias=sufs.t[:, j, h:h + 1], nk=128, q0=0, tri=False, reads=[kT, vsb, sufs]))
            kts.append(dict(kT=kTn.t[:, h // 2, 32 * s:32 * s + 32], v=vnew.t[0:32, s, h * 64:(h + 1) * 64],
                            bias=negcs.t[0:32, s, h:h + 1], nk=32, q0=0, tri=True, reads=[kTn, vnew, negcs]))
            attention(h, 32 * s, 32, kts)
    uSb = ubf.t[:, :, 0:NSUB_S * 62].rearrange("p c (s n) -> p c s n", s=NSUB_S)
    conv_ln(128, lambda c, j: uSb[:, c, :, j:j + 32], lambda a: v4(a.t[:, 0:128]))
    store_scratch(128, TP)
    for s in range(NSUB_S):
        conv_state_out(lambda c, s=s: uSv[:, c, s, 32:62], T["cvs"][s, :, :])
    P.pop_scope()


CAP = 384
NSLOT = NE * CAP
NT = TT // 128


def layer_norm_tile(P, r, out_fn, tmp, eps_col):
    st6, mv, sc = tmp["st6"], tmp["mv"], tmp["sc"]
    for g in range(2):
        P.op(P.dve, lambda e, g=g: e.bn_stats(st6.t[:, g, :], r.t[:, g * 512:(g + 1) * 512]), reads=[r], writes=[st6])
    P.op(P.dve, lambda e: e.bn_aggr(mv.t[:, 0:2], st6.t[:, :, :].rearrange("p a b -> p (a b)")), reads=[st6], writes=[mv])
    _act(P, sc.t[:, 0:1], mv.t[:, 1:2], AF.Sqrt, [mv], [sc], bias=eps_col)
    P.op(P.dve, lambda e: e.reciprocal(sc.t[:, 0:1], sc.t[:, 0:1]), reads=[sc], writes=[sc])
    _ts(P, P.dve, sc.t[:, 1:2], mv.t[:, 0:1], -1.0, sc.t[:, 0:1], ALU.mult, ALU.mult, [mv, sc], [sc])
    out_fn(sc.t[:, 0:1], sc.t[:, 1:2])


def pass_b(P, T, G, NB):
    P.push_scope()
    cf = P.sb("cf", [128, 384], F32)
    cb = P.sb("cb", [128, 384], BF16)
    pcol = P.sb("pcol", [128, PC_N], F32)
    P.dma(P.sp, cf.t[:, :], T["cf"], writes=[cf])
    P.dma(P.sp, cb.t[:, :], T["cb"], writes=[cb])
    P.dma(P.sp, pcol.t[:, :], T["pcol"], writes=[pcol])
    identf = cf.t[:, 0:128]
    identb = cb.t[:, 0:128]
    ustrict = cb.t[:, 256:384]
    w_g = P.sb("w_g", [128, 8, 2048], BF16)
    w_a = P.sb("w_a", [128, 4, 1024], BF16)
    w_b = P.sb("w_b", [128, 4, 1024], BF16)
    w_o = P.sb("w_o", [128, 8, 1024], BF16)
    wr_hi = P.sb("wr_hi", [128, 8, 256], BF16)
    wr_lo = P.sb("wr_lo", [128, 8, 256], BF16)
    w_s = P.sb("w_s", [128, 8, 512], BF16)
    w_sd = P.sb("w_sd", [128, 2, 1024], BF16)
    lng = P.sb("lng", [128, 1024], F32)
    lnb = P.sb("lnb", [128, 1024], F32)
    brt = P.sb("brt", [128, 256], F32)
    cnt = P.sb("cnt", [128, 256], F32)
    onesb = P.sb("onesb", [128, 128], BF16)
    epsb = P.sb("epsb", [128, 1], F32)
    tokid = P.sb("tokid", [128, NT], I32)
    load_w_cast(P, w_g, T["w_in"], 8, 2048, OFF_GA)
    load_w_cast(P, w_a, T["w_a"], 4, 1024)
    load_w_cast(P, w_b, T["w_b"], 4, 1024)
    load_w_cast(P, w_o, T["w_out"], 8, 1024)
    load_w_cast(P, wr_hi, T["w_router"], 8, 256)
    load_w_cast(P, w_s, T["w_sg"], 8, 256)
    P.dma(P.pool, w_s.t[:, :, 256:512], T["w_su"].rearrange("(kc p) n -> p kc n", p=128), writes=[w_s])
    load_w_cast(P, w_sd, T["w_sd"], 2, 1024)
    P.dma(P.sp, lng.t[:, :], T["ln1_g"].partition_broadcast(128), writes=[lng])
    P.dma(P.sp, lnb.t[:, :], T["ln1_b"].partition_broadcast(128), writes=[lnb])
    P.dma(P.sp, brt.t[:, :], T["b_router"].partition_broadcast(128), writes=[brt])
    P.dma(P.sp, cnt.t[:, :], T["base1"], writes=[cnt])
    P.dma(P.sp, tokid.t[:, :], T["tokid"], writes=[tokid])
    P.op(P.dve, lambda e: e.memset(onesb.t[:, :], 1.0), writes=[onesb])
    P.op(P.dve, lambda e: e.memset(epsb.t[:, :], LN_EPS), writes=[epsb])
    wr32 = P.sb("wr32", [128, 8, 256], F32)
    P.dma(P.sp, wr32.t[:, :, :], T["w_router"].rearrange("(kc p) n -> p kc n", p=128), writes=[wr32])
    _tt(P, P.dve, wr_lo.t[:, :, :], wr32.t[:, :, :], wr_hi.t[:, :, :], ALU.subtract, [wr32, wr_hi], [wr_lo])

    nt = NB // 128
    at = P.sb("at", [128, 4, NB], BF16)
    ht = P.sb("ht", [128, 4, NB], BF16)
    xt = [P.sb("xt%d" % i, [128, 1024], F32) for i in range(nt)]
    xT = P.sb("xT", [128, 8, NB], BF16)
    sga = [P.sb("sga%d" % i, [128, NB], F32) for i in range(2)]
    sgb = [P.sb("sgb%d" % i, [128, NB], F32) for i in range(2)]
    t1 = [P.sb("t1%d" % i, [128, NB], F32) for i in range(2)]
    t2 = [P.sb("t2%d" % i, [128, NB], F32) for i in range(2)]
    mT = P.sb("mT", [128, 8, NB], BF16)
    rr2 = [P.sb("rr%d" % i, [128, 1024], F32) for i in range(2)]
    mid = [P.sb("mid%d" % i, [128, 1024], F32) for i in range(2)]
    mhi = [P.sb("mhi%d" % i, [128, 1024], BF16) for i in range(2)]
    mlo2 = [P.sb("mlo%d" % i, [128, 1024], BF16) for i in range(2)]
    mTh = P.sb("mTh", [128, 8, NB], BF16)
    mTl = P.sb("mTl", [128, 8, NB], BF16)
    tmp2 = [{"st6": P.sb("st6%d" % i, [128, 2, 6], F32), "mv": P.sb("mv%d" % i, [128, 2], F32), "sc": P.sb("sc%d" % i, [128, 2], F32)}
            for i in range(2)]
    rt2 = [{n: P.sb("rt%d_%s" % (i, n), [128, 256], F32) for n in ("scores", "sel", "selm", "emask", "gate", "sv", "junk")} for i in range(2)]
    emb2 = [P.sb("emb%d" % i, [128, 256], BF16) for i in range(2)]
    m82 = [P.sb("m8%d" % i, [128, 8, 8], F32) for i in range(2)]
    gs2 = [P.sb("gs%d" % i, [128, 8], F32) for i in range(2)]
    g8s2 = [P.sb("g8s%d" % i, [128, 8], F32) for i in range(2)]
    gmask2 = [P.sb("gmask%d" % i, [128, 8], F32) for i in range(2)]
    gneg2 = [P.sb("gneg%d" % i, [128, 8], F32) for i in range(2)]
    s82 = [P.sb("s8%d" % i, [128, 8], F32) for i in range(2)]
    den2 = [P.sb("den%d" % i, [128, 1], F32) for i in range(2)]
    gsh = [P.sb("gsh%d" % i, [128, NB], F32) for i in range(2)]
    hsT = P.sb("hsT", [128, 2, NB], BF16)
    pre = [P.sb("pre%d" % i, [128, 1024], F32) for i in range(2)]
    scr = Buf(None, "scr")
    slots_all, gates_all = G["slots"], G["gates"]
    st = {"m": 0}

    for b0 in range(0, TT, NB):
        nb = min(NB, TT - b0)
        ntile = nb // 128
        P.dma(P.sp, at.t[:, :, 0:nb], T["attn_d"].rearrange("(c p) t -> p c t", p=128)[:, :, b0:b0 + nb], writes=[at])
        P.dma(P.sp, ht.t[:, :, 0:nb], T["h_d"].rearrange("(c p) t -> p c t", p=128)[:, :, b0:b0 + nb], writes=[ht])
        for t in range(ntile):
            xb = xt[t]
            r0 = b0 + t * 128
            src_x = T["xp"][r0:r0 + 128, :] if r0 < TP else T["xs"]
            P.dma(P.sp, xb.t[:, :], src_x, writes=[xb])
            for g in range(2):
                psb = P.ps()
                for j in range(4):
                    kc = g * 4 + j
                    P.op(P.pe, lambda e, psb=psb, j=j, kc=kc, xb=xb: e.transpose(
                        psb.t[:, j * 128:(j + 1) * 128], xb.t[:, kc * 128:(kc + 1) * 128], identf),
                        reads=[xb, cf], writes=[psb], sig=(j == 3))
                _copy(P, P.act if g == 0 else P.dve, xT.t[:, g * 4:(g + 1) * 4, t * 128:(t + 1) * 128],
                      psb.t[:, :].rearrange("p (a b) -> p a b", a=4), [psb], [xT])
        for oc in range(8):
            i2 = oc % 2
            pa = P.ps()
            for c in range(4):
                _mm(P, pa, pa.t[:, 0:nb], w_a.t[:, c, oc * 128:(oc + 1) * 128], at.t[:, c, 0:nb], c == 0, c == 3, [w_a, at])
            pb = P.ps()
            for c in range(4):
                _mm(P, pb, pb.t[:, 0:nb], w_b.t[:, c, oc * 128:(oc + 1) * 128], ht.t[:, c, 0:nb], c == 0, c == 3, [w_b, ht])
            pga = P.ps()
            for kc in range(8):
                _mm(P, pga, pga.t[:, 0:nb], w_g.t[:, kc, oc * 128:(oc + 1) * 128], xT.t[:, kc, 0:nb], kc == 0, kc == 7, [w_g, xT])
            pgb = P.ps()
            for kc in range(8):
                _mm(P, pgb, pgb.t[:, 0:nb], w_g.t[:, kc, 1024 + oc * 128:1024 + (oc + 1) * 128], xT.t[:, kc, 0:nb], kc == 0, kc == 7, [w_g, xT])
            _act(P, sga[i2].t[:, 0:nb], pga.t[:, 0:nb], AF.Sigmoid, [pga, pcol], [sga[i2]], bias=pcol.t[:, PC_BGA + oc:PC_BGA + oc + 1])
            _act(P, sgb[i2].t[:, 0:nb], pgb.t[:, 0:nb], AF.Sigmoid, [pgb, pcol], [sgb[i2]], bias=pcol.t[:, PC_BGB + oc:PC_BGB + oc + 1])
            _tt(P, P.dve, t1[i2].t[:, 0:nb], pa.t[:, 0:nb], sga[i2].t[:, 0:nb], ALU.mult, [pa, sga[i2]], [t1[i2]])
            _stt(P, t2[i2].t[:, 0:nb], pb.t[:, 0:nb], pcol.t[:, PC_BB + oc:PC_BB + oc + 1], sgb[i2].t[:, 0:nb], ALU.add, ALU.mult,
                 [pb, pcol, sgb[i2]], [t2[i2]])
            _tt(P, P.pool, mT.t[:, oc, 0:nb], t1[i2].t[:, 0:nb], t2[i2].t[:, 0:nb], ALU.add, [t1[i2], t2[i2]], [mT])
        for t in range(ntile):
            tg = (b0 // 128) + t
            tp = tg % 2
            rr, mlo, tmp, rt, emb, m8, gs, g8s, gmask, gneg, s8, den = (rr2[tp], mlo2[tp], tmp2[tp], rt2[tp], emb2[tp], m82[tp], gs2[tp],
                                                                      g8s2[tp], gmask2[tp], gneg2[tp], s82[tp], den2[tp])
            xb = xt[t]
            md = mid[st["m"] % 2]
            mh = mhi[st["m"] % 2]
            st["m"] += 1
            for half in range(2):
                psb = P.ps()
                for kc in range(8):
                    _mm(P, psb, psb.t[:, 0:512], mT.t[:, kc, t * 128:(t + 1) * 128], w_o.t[:, kc, half * 512:(half + 1) * 512],
                        kc == 0, kc == 7, [mT, w_o])
                _stt(P, rr.t[:, half * 512:(half + 1) * 512], xb.t[:, half * 512:(half + 1) * 512], DN_ALPHA, psb.t[:, 0:512],
                     ALU.mult, ALU.add, [xb, psb], [rr])
            def norm1(rstd, nmr, md=md, rr=rr, tmp=tmp):
                P.op(P.act, lambda e, md=md, rr=rr, nmr=nmr, rstd=rstd: e.activation(md.t[:, :], rr.t[:, :], AF.Identity, bias=nmr, scale=rstd), reads=[rr, tmp["sc"]], writes=[md])
            layer_norm_tile(P, rr, norm1, tmp, epsb.t[:, 0:1])
            _tt(P, P.dve, md.t[:, :], md.t[:, :], lng.t[:, :], ALU.mult, [md, lng], [md])
            _tt(P, P.pool, md.t[:, :], md.t[:, :], lnb.t[:, :], ALU.add, [md, lnb], [md])
            _copy(P, P.act, mh.t[:, :], md.t[:, :], [md], [mh])
            _tt(P, P.dve, mlo.t[:, :], md.t[:, :], mh.t[:, :], ALU.subtract, [md, mh], [mlo])
            if "mid_dbg" in T:
                P.dma(P.sp, T["mid_dbg"][tg * 128:(tg + 1) * 128, :], md.t[:, :], reads=[md], out_final=True)
            for srcb, dstb in ((mh, mTh), (mlo, mTl)):
                psb = P.ps()
                pbf = psb.t[:, :].bitcast(BF16)
                for kc in range(8):
                    P.op(P.pe, lambda e, kc=kc, pbf=pbf, srcb=srcb: e.transpose(pbf[:, kc * 128:(kc + 1) * 128], srcb.t[:, kc * 128:(kc + 1) * 128], identb),
                         reads=[srcb, cb], writes=[psb], sig=(kc == 7))
                _copy(P, P.act if srcb is mh else P.dve, dstb.t[:, :, t * 128:(t + 1) * 128], pbf.rearrange("p (c n) -> p c n", c=8), [psb], [dstb])
            psr = P.ps()
            k = 0
            for (a_, w_) in ((mTh, wr_hi), (mTh, wr_lo), (mTl, wr_hi)):
                for kc in range(8):
                    _mm(P, psr, psr.t[:, 0:256], a_.t[:, kc, t * 128:(t + 1) * 128], w_.t[:, kc, :], k == 0, k == 23, [a_, w_])
                    k += 1
            sc_, sel, selm, emask, gate, sv, junk = (rt[n] for n in ("scores", "sel", "selm", "emask", "gate", "sv", "junk"))
            _act(P, sc_.t[:, :], psr.t[:, 0:256], AF.Sigmoid, [psr], [sc_])
            _tt(P, P.dve, sel.t[:, :], sc_.t[:, :], brt.t[:, :], ALU.add, [sc_, brt], [sel])
            for g in range(8):
                P.op(P.dve, lambda e, g=g, m8=m8, sel=sel: e.max(m8.t[:, g, :], sel.t[:, g * 32:(g + 1) * 32]), reads=[sel], writes=[m8])
            _tt(P, P.dve, gs.t[:, :], m8.t[:, :, 0], m8.t[:, :, 1], ALU.add, [m8], [gs])
            P.op(P.dve, lambda e, g8s=g8s, gs=gs: e.max(g8s.t[:, :], gs.t[:, :]), reads=[gs], writes=[g8s])
            _ts(P, P.dve, gmask.t[:, :], gs.t[:, :], g8s.t[:, 3:4], None, ALU.is_ge, None, [gs, g8s], [gmask])
            _ts(P, P.dve, gneg.t[:, :], gmask.t[:, :], -1.0, 1e9, ALU.add, ALU.mult, [gmask], [gneg])
            for g in range(8):
                _ts(P, P.dve, selm.t[:, g * 32:(g + 1) * 32], sel.t[:, g * 32:(g + 1) * 32], gmask.t[:, g:g + 1], gneg.t[:, g:g + 1],
                    ALU.mult, ALU.add, [sel, gmask, gneg], [selm])
            P.op(P.dve, lambda e, m8=m8, selm=selm: e.max(m8.t[:, 0, :], selm.t[:, :]), reads=[selm], writes=[m8])
            _ts(P, P.dve, emask.t[:, :], selm.t[:, :], m8.t[:, 0, 7:8], None, ALU.is_ge, None, [selm, m8], [emask])
            _copy(P, P.pool, emb.t[:, :], emask.t[:, :], [emask], [emb])
            P.op(P.dve, lambda e, gate=gate, sc_=sc_, emask=emask, den=den: e.scalar_tensor_tensor(gate.t[:, :], sc_.t[:, :], 1.0, emask.t[:, :], ALU.mult, ALU.mult, accum_out=den.t[:, 0:1]),
                 reads=[sc_, emask], writes=[gate, den])
            P.op(P.dve, lambda e, den=den: e.reciprocal(den.t[:, 0:1], den.t[:, 0:1]), reads=[den], writes=[den])
            _ts(P, P.dve, gate.t[:, :], gate.t[:, :], den.t[:, 0:1], 2.5, ALU.mult, ALU.mult, [gate, den], [gate])
            pp = P.ps()
            _mm(P, pp, pp.t[:, 0:256], ustrict, emb.t[:, :], True, True, [cb, emb])
            pt_ = P.ps()
            _mm(P, pt_, pt_.t[:, 0:256], onesb.t[:, :], emb.t[:, :], True, True, [onesb, emb])
            _tt(P, P.dve, sv.t[:, :], pp.t[:, 0:256], cnt.t[:, :], ALU.add, [pp, cnt], [sv])
            _tt(P, P.pool, sv.t[:, :], sv.t[:, :], emask.t[:, :], ALU.mult, [sv, emask], [sv])
            _tt(P, P.dve, cnt.t[:, :], pt_.t[:, 0:256], cnt.t[:, :], ALU.add, [pt_, cnt], [cnt])
            P.op(P.dve, lambda e, s8=s8, sv=sv: e.max(s8.t[:, :], sv.t[:, :]), reads=[sv], writes=[s8])
            for k in range(8):
                P.op(P.dve, lambda e, k=k, tg=tg, junk=junk, sv=sv, s8=s8, gate=gate: e.scalar_tensor_tensor(junk.t[:, :], sv.t[:, :], s8.t[:, k:k + 1], gate.t[:, :], ALU.is_equal, ALU.mult,
                                                                     accum_out=gates_all.t[:, tg, k:k + 1]),
                     reads=[sv, s8, gate], writes=[junk, gates_all])
            _ts(P, P.dve, slots_all.t[:, tg, :], s8.t[:, :], -1.0, None, ALU.add, None, [s8], [slots_all])
            for k in range(8):
                P.dma(P.pool, None, None, reads=[slots_all, mh], writes=[scr],
                      fn=lambda h, k=k, tg=tg, mh=mh: h.indirect_dma_start(
                          out=T["xg_d"], out_offset=bass.IndirectOffsetOnAxis(ap=slots_all.t[:, tg, k:k + 1], axis=0),
                          in_=mh.t[:, :], in_offset=None))
        for j in range(2):
            pg = P.ps()
            for kc in range(8):
                _mm(P, pg, pg.t[:, 0:nb], w_s.t[:, kc, j * 128:(j + 1) * 128], mTh.t[:, kc, 0:nb], kc == 0, kc == 7, [w_s, mTh])
            pu = P.ps()
            for kc in range(8):
                _mm(P, pu, pu.t[:, 0:nb], w_s.t[:, kc, 256 + j * 128:256 + (j + 1) * 128], mTh.t[:, kc, 0:nb], kc == 0, kc == 7, [w_s, mTh])
            _act(P, gsh[j].t[:, 0:nb], pg.t[:, 0:nb], AF.Silu, [pg], [gsh[j]])
            _tt(P, P.dve, hsT.t[:, j, 0:nb], pu.t[:, 0:nb], gsh[j].t[:, 0:nb], ALU.mult, [pu, gsh[j]], [hsT])
        for t in range(ntile):
            tg = (b0 // 128) + t
            md = mid[(st["m"] - ntile + t) % 2]
            pr = pre[t % 2]
            for half in range(2):
                psb = P.ps()
                for j in range(2):
                    _mm(P, psb, psb.t[:, 0:512], hsT.t[:, j, t * 128:(t + 1) * 128], w_sd.t[:, j, half * 512:(half + 1) * 512], j == 0, j == 1,
                        [hsT, w_sd])
                _stt(P, pr.t[:, half * 512:(half + 1) * 512], md.t[:, half * 512:(half + 1) * 512], DN_ALPHA, psb.t[:, 0:512], ALU.mult, ALU.add,
                     [md, psb], [pr])
            P.dma(P.sp, T["pre_d"][tg * 128:(tg + 1) * 128, :], pr.t[:, :], reads=[pr], writes=[scr])
    P.pop_scope()


def pass_c(P, T, n_exp=NE):
    P.push_scope()
    NBLK = CAP // 128
    cb = P.sb("cb", [128, 128], BF16)
    P.dma(P.sp, cb.t[:, :], T["cb"][:, 0:128], writes=[cb])
    identb = cb.t[:, 0:128]
    NS = 4
    NW = 2
    sg_ = [P.sb("wsg%d" % i, [128, 8, DE], F32) for i in range(NS)]
    su_ = [P.sb("wsu%d" % i, [128, 8, DE], F32) for i in range(NS)]
    sd_ = [P.sb("wsd%d" % i, [128, 2, D], F32) for i in range(NS)]
    wg = [P.sb("wg%d" % i, [128, 8, DE], BF16) for i in range(NW)]
    wu = [P.sb("wu%d" % i, [128, 8, DE], BF16) for i in range(NW)]
    wd = [P.sb("wd%d" % i, [128, 2, D], BF16) for i in range(NW)]
    xg = [P.sb("xg%d" % i, [128, NBLK, D], BF16) for i in range(NS)]
    xgT = [P.sb("xgT%d" % i, [128, 8, CAP], BF16) for i in range(2)]
    gsb = [P.sb("gsb%d" % i, [128, 2, CAP], F32) for i in range(2)]
    hTe = [P.sb("hTe%d" % i, [128, 2, CAP], BF16) for i in range(2)]
    yb = [P.sb("yb%d" % i, [128, D], BF16) for i in range(3)]
    scr = Buf(None, "scr_c")

    def loads(e):
        s = e % NS
        P.dma(P.sp, sg_[s].t[:, :, :], T["w_eg"][e].rearrange("(p kc) n -> p kc n", kc=8), writes=[sg_[s]])
        P.dma(P.sp, su_[s].t[:, :, :], T["w_eu"][e].rearrange("(p kc) n -> p kc n", kc=8), writes=[su_[s]])
        P.dma(P.sp, sd_[s].t[:, :, :], T["w_ed"][e].rearrange("(p j) n -> p j n", j=2), writes=[sd_[s]])
        P.dma(P.sp, xg[e % NS].t[:, :, :], T["xg_d"][e * CAP:(e + 1) * CAP, :].rearrange("(b p) n -> p b n", p=128), writes=[xg[e % NS]])

    def casts(e):
        s, i = e % NS, e % NW
        _copy(P, P.act, wg[i].t[:, :, :], sg_[s].t[:, :, :], [sg_[s]], [wg[i]])
        _copy(P, P.dve, wu[i].t[:, :, :], su_[s].t[:, :, :], [su_[s]], [wu[i]])
        _copy(P, P.pool, wd[i].t[:, :, :], sd_[s].t[:, :, :], [sd_[s]], [wd[i]])

    for e0 in range(NS):
        loads(e0)
    casts(0)
    yi = 0
    for e in range(n_exp):
        i, i2 = e % NW, e % 2
        for b in range(NBLK):
            psb = P.ps()
            pbf = psb.t[:, :].bitcast(BF16)
            for kc in range(8):
                P.op(P.pe, lambda ee, kc=kc, pbf=pbf, b=b, e=e: ee.transpose(pbf[:, kc * 128:(kc + 1) * 128], xg[e % NS].t[:, b, :].rearrange("s (p k) -> s k p", k=8)[:, kc, :], identb),
                     reads=[xg[e % NS], cb], writes=[psb], sig=(kc == 7))
            _copy(P, P.act if b % 2 == 0 else P.dve, xgT[i2].t[:, :, b * 128:(b + 1) * 128], pbf.rearrange("p (c n) -> p c n", c=8), [psb], [xgT[i2]])
        if e + 1 < n_exp:
            casts(e + 1)
        for j in range(2):
            pg = P.ps()
            for kc in range(8):
                _mm(P, pg, pg.t[:, 0:CAP], wg[i].t[:, kc, :].rearrange("p (m j) -> p j m", j=2)[:, j, :], xgT[i2].t[:, kc, :], kc == 0, kc == 7, [wg[i], xgT[i2]])
            pu = P.ps()
            for kc in range(8):
                _mm(P, pu, pu.t[:, 0:CAP], wu[i].t[:, kc, :].rearrange("p (m j) -> p j m", j=2)[:, j, :], xgT[i2].t[:, kc, :], kc == 0, kc == 7, [wu[i], xgT[i2]])
            _act(P, gsb[i2].t[:, j, :], pg.t[:, 0:CAP], AF.Silu, [pg], [gsb[i2]])
            _tt(P, P.dve, hTe[i2].t[:, j, :], pu.t[:, 0:CAP], gsb[i2].t[:, j, :], ALU.mult, [pu, gsb[i2]], [hTe[i2]])
        if e + NS < n_exp:
            loads(e + NS)
        for b in range(NBLK):
            y_ = yb[yi % 3]
            yi += 1
            for half in range(2):
                psb = P.ps()
                for j in range(2):
                    _mm(P, psb, psb.t[:, 0:512], hTe[i2].t[:, j, b * 128:(b + 1) * 128], wd[i].t[:, j, half * 512:(half + 1) * 512], j == 0, j == 1,
                        [hTe[i2], wd[i]])
                _copy(P, P.act if half == 0 else P.dve, y_.t[:, half * 512:(half + 1) * 512], psb.t[:, 0:512], [psb], [y_])
            r0 = e * CAP + b * 128
            P.dma(P.sp, T["ys_d"][r0:r0 + 128, :], y_.t[:, :], reads=[y_], writes=[scr])
    P.pop_scope()


def pass_d(P, T, G):
    P.push_scope()
    lng = P.sb("lng2", [128, D], F32)
    lnb = P.sb("lnb2", [128, D], F32)
    epsb = P.sb("epsb", [128, 1], F32)
    P.dma(P.sp, lng.t[:, :], T["ln2_g"].partition_broadcast(128), writes=[lng])
    P.dma(P.sp, lnb.t[:, :], T["ln2_b"].partition_broadcast(128), writes=[lnb])
    P.op(P.dve, lambda e: e.memset(epsb.t[:, :], LN_EPS), writes=[epsb])
    yk = [P.sb("yk%d" % i, [128, D], BF16) for i in range(16)]
    acc = [P.sb("acc%d" % i, [128, D], F32) for i in range(2)]
    yo = [P.sb("yo%d" % i, [128, D], F32) for i in range(2)]
    tmp = {"st6": P.sb("st6", [128, 2, 6], F32), "mv": P.sb("mv", [128, 2], F32), "sc": P.sb("sc", [128, 2], F32)}
    slots_all, gates_all = G["slots"], G["gates"]
    scr = Buf(None, "scr_d")
    for tg in range(NT):
        a = acc[tg % 2]
        o = yo[tg % 2]
        P.dma(P.sp, a.t[:, :], T["pre_d"][tg * 128:(tg + 1) * 128, :], reads=[scr], writes=[a])
        for k in range(8):
            y_ = yk[(tg * 8 + k) % 16]
            P.dma(P.pool, None, None, reads=[slots_all, scr], writes=[y_],
                  fn=lambda h, k=k, tg=tg, y_=y_: h.indirect_dma_start(
                      out=y_.t[:, :], out_offset=None, in_=T["ys_d"],
                      in_offset=bass.IndirectOffsetOnAxis(ap=slots_all.t[:, tg, k:k + 1], axis=0)))
        for k in range(8):
            y_ = yk[(tg * 8 + k) % 16]
            _stt(P, a.t[:, :], y_.t[:, :], gates_all.t[:, tg, k:k + 1], a.t[:, :], ALU.mult, ALU.add, [y_, gates_all, a], [a])
        def norm2(rstd, nmr, a=a, o=o):
            P.op(P.act, lambda e: e.activation(o.t[:, :], a.t[:, :], AF.Identity, bias=nmr, scale=rstd), reads=[a, tmp["sc"]], writes=[o])
        layer_norm_tile(P, a, norm2, tmp, epsb.t[:, 0:1])
        _tt(P, P.dve, o.t[:, :], o.t[:, :], lng.t[:, :], ALU.mult, [o, lng], [o])
        _tt(P, P.pool, o.t[:, :], o.t[:, :], lnb.t[:, :], ALU.add, [o, lnb], [o])
        dst = T["yp"][tg * 128:(tg + 1) * 128, :] if tg * 128 < TP else T["ys"]
        P.dma(P.sp, dst, o.t[:, :], reads=[o], out_final=True)
    P.pop_scope()


IN_SPECS_A = [
    ("xp", [TP, D], F32), ("xs", [128, D], F32), ("ck", [NSUB_S, PAST, 512], F32), ("cv", [NSUB_S, PAST, 512], F32),
    ("clf", [NSUB_S, PAST, 8], F32), ("sconv", [NSUB_S, 30, 512], F32), ("w_in", [D, N_IN], F32), ("b_in", [1, N_IN], F32),
    ("pcol", [128, PC_N], F32), ("cf", [128, 384], F32), ("cb", [128, 384], BF16),
]
IN_SPECS_B = [
    ("w_a", [512, D], F32), ("w_b", [512, D], F32), ("w_out", [D, D], F32), ("ln1_g", [1, D], F32), ("ln1_b", [1, D], F32),
    ("w_router", [D, NE], F32), ("b_router", [1, NE], F32), ("w_sg", [D, DE], F32), ("w_su", [D, DE], F32), ("w_sd", [DE, D], F32),
    ("base1", [128, NE], F32), ("tokid", [128, NT], I32),
]
IN_SPECS_C = [
    ("w_eg", [NE, D, DE], F32), ("w_eu", [NE, D, DE], F32), ("w_ed", [NE, DE, D], F32), ("ln2_g", [1, D], F32), ("ln2_b", [1, D], F32),
]
OUT_SPECS = [
    ("yp", [TP, D]), ("ys", [128, D]), ("kp", [TP, 512]), ("vp", [TP, 512]), ("lfp", [TP, 8]), ("cvp", [NSEQ_P, 30, 512]),
    ("ks", [128, 512]), ("vs", [128, 512]), ("lfs", [128, 8]), ("cvs", [NSUB_S, 30, 512]),
]


def build(stage="A", NB=512, NBB=256, debug=False):
    nc = bass.Bass("TRN2", target_bir_lowering=False)
    T = {}
    specs = list(IN_SPECS_A)
    if stage >= "B":
        specs += IN_SPECS_B
    if stage >= "C":
        specs += IN_SPECS_C
    for name, shape, dt in specs:
        T[name] = nc.dram_tensor(name, shape, dt, kind="ExternalInput").ap()
    for name, shape in OUT_SPECS:
        T[name] = nc.dram_tensor(name, shape, F32, kind="ExternalOutput").ap()
    dk = "ExternalOutput" if debug else "Internal"
    T["attn_d"] = nc.dram_tensor("attn_d", [512, TT], BF16, kind=dk).ap()
    T["h_d"] = nc.dram_tensor("h_d", [512, TT], BF16, kind=dk).ap()
    T["c3_d"] = nc.dram_tensor("c3_d", [8, 3, 512], BF16, kind="Internal").ap()
    P = Prog(nc)
    G = {}
    if stage >= "B":
        T["xg_d"] = nc.dram_tensor("xg_d", [NSLOT, D], BF16, kind="Internal").ap()
        T["pre_d"] = nc.dram_tensor("pre_d", [TT, D], F32, kind="Internal").ap()
        if debug:
            T["mid_dbg"] = nc.dram_tensor("mid_dbg", [TT, D], F32, kind="ExternalOutput").ap()
            T["slots_dbg"] = nc.dram_tensor("slots_dbg", [128, NT * 8], I32, kind="ExternalOutput").ap()
            T["gates_dbg"] = nc.dram_tensor("gates_dbg", [128, NT * 8], F32, kind="ExternalOutput").ap()
        G["slots"] = P.sb("slots_all", [128, NT, 8], I32)
        G["gates"] = P.sb("gates_all", [128, NT, 8], F32)
    pass_a(P, T, NB)
    if stage >= "B":
        pass_b(P, T, G, NBB)
        if stage >= "C":
            T["ys_d"] = nc.dram_tensor("ys_d", [NSLOT, D], BF16, kind="Internal").ap()
            pass_c(P, T)
            pass_d(P, T, G)
        if debug:
            P.dma(P.sp, T["slots_dbg"], G["slots"].t[:, :, :].rearrange("p a b -> p (a b)"), reads=[G["slots"]], out_final=True)
            P.dma(P.sp, T["gates_dbg"], G["gates"].t[:, :, :].rearrange("p a b -> p (a b)"), reads=[G["gates"]], out_final=True)
    P.finish()
    print('[build] ops', P.n_ops, 'sim_us %.1f' % getattr(P, 'sim_time', 0.0), flush=True)
    return nc, [s[0] for s in specs]


def host_consts(inp):
    b_in = np.asarray(inp["b_in"])[0]
    pcol = np.zeros((128, PC_N), np.float32)
    col = lambda v, n: np.ascontiguousarray(np.asarray(v).reshape(n, 128).T)
    pcol[:, PC_BQ:PC_BQ + 4] = col(b_in[OFF_Q:OFF_K], 4)
    pcol[:, PC_BGLU:PC_BGLU + 8] = col(b_in[OFF_GLU:OFF_GA], 8)
    pcol[:, PC_BGA:PC_BGA + 8] = col(b_in[OFF_GA:OFF_GB], 8)
    pcol[:, PC_BGB:PC_BGB + 8] = col(b_in[OFF_GB:N_IN], 8)
    pcol[:, PC_BB:PC_BB + 8] = col(inp["b_b"][0], 8)
    pcol[:, PC_CB:PC_CB + 4] = col(inp["conv_b"][0], 4)
    pcol[:, PC_CG:PC_CG + 4] = col(inp["conv_ln_g"][0], 4)
    pcol[:, PC_CBE:PC_CBE + 4] = col(inp["conv_ln_b"][0], 4)
    cw = np.asarray(inp["conv_w"])[0]
    pcol[:, PC_CW:PC_CW + 124] = cw.T.reshape(4, 128, KW).transpose(1, 0, 2).reshape(128, 4 * KW)
    pcol[0:8, PC_BF] = b_in[OFF_F:OFF_GLU]
    pcol[:, PC_BK:PC_BK + 4] = col(b_in[OFF_K:OFF_V], 4)
    cf = np.concatenate([np.eye(128, dtype=np.float32), np.tril(np.ones((128, 128), np.float32), -1),
                         np.ones((128, 128), np.float32)], axis=1)
    cb = np.concatenate([np.eye(128, dtype=np.float32), np.triu(np.ones((128, 128), np.float32)),
                         np.triu(np.ones((128, 128), np.float32), 1)], axis=1).astype(ml_dtypes.bfloat16)
    base1 = np.ascontiguousarray(np.broadcast_to((np.arange(NE, dtype=np.float32) * CAP + 1.0)[None, :], (128, NE)))
    tokid = np.ascontiguousarray((np.arange(NT, dtype=np.int32)[None, :] * 128 + np.arange(128, dtype=np.int32)[:, None]).astype(np.int32))
    return pcol, cf, cb, base1, tokid


def core_inputs(inp, c, consts):
    pcol, cf, cb, base1, tokid = consts
    m = {
        "xp": np.asarray(inp["x_prompt"])[2 * c:2 * c + 2].reshape(TP, D),
        "xs": np.asarray(inp["x_sample"])[4 * c:4 * c + 4].reshape(128, D),
        "ck": np.asarray(inp["cache_k"])[0, 4 * c:4 * c + 4].reshape(NSUB_S, PAST, 512),
        "cv": np.asarray(inp["cache_v"])[0, 4 * c:4 * c + 4].reshape(NSUB_S, PAST, 512),
        "clf": np.asarray(inp["cache_logf"])[0, 4 * c:4 * c + 4],
        "sconv": np.asarray(inp["state_conv"])[0, 4 * c:4 * c + 4],
        "w_in": np.asarray(inp["w_in"])[0], "b_in": np.asarray(inp["b_in"]),
        "pcol": pcol, "cf": cf, "cb": cb, "base1": base1, "tokid": tokid,
        "w_a": np.asarray(inp["w_a"])[0], "w_b": np.asarray(inp["w_b"])[0], "w_out": np.asarray(inp["w_out"])[0],
        "ln1_g": np.asarray(inp["ln1_g"]), "ln1_b": np.asarray(inp["ln1_b"]),
        "w_router": np.asarray(inp["w_router"])[0], "b_router": np.asarray(inp["b_router"]),
        "w_sg": np.asarray(inp["w_s_gate"])[0], "w_su": np.asarray(inp["w_s_up"])[0], "w_sd": np.asarray(inp["w_s_down"])[0],
        "w_eg": np.asarray(inp["w_e_gate"])[0], "w_eu": np.asarray(inp["w_e_up"])[0], "w_ed": np.asarray(inp["w_e_down"])[0],
        "ln2_g": np.asarray(inp["ln2_g"]), "ln2_b": np.asarray(inp["ln2_b"]),
    }
    return m


def assemble(results):
    cat = lambda k: np.concatenate([r[k] for r in results], axis=0)
    yp = cat("yp").reshape(16, SEQ, D)
    ys = cat("ys").reshape(32, TS, D)
    kp = cat("kp").reshape(1, 16, SEQ, H, DH)
    vp = cat("vp").reshape(1, 16, SEQ, H, DH)
    lfp = cat("lfp").reshape(1, 16, SEQ, H)
    cvp = cat("cvp").reshape(1, 16, 30, CC)
    ks = cat("ks").reshape(1, 32, TS, H, DH)
    vs = cat("vs").reshape(1, 32, TS, H, DH)
    lfs = cat("lfs").reshape(1, 32, TS, H)
    cvs = cat("cvs").reshape(1, 32, 30, CC)
    return (yp, ys, kp, vp, lfp, cvp, ks, vs, lfs, cvs)


def kernel(**inputs):
    nc, names = build("C")
    consts = host_consts(inputs)
    in_maps = []
    for c in range(NCORES):
        m = core_inputs(inputs, c, consts)
        in_maps.append({k: (m[k] if m[k].flags["C_CONTIGUOUS"] else np.ascontiguousarray(m[k])) for k in names})
    res = run_bass_kernel_spmd(nc, in_maps, core_ids=list(range(NCORES)))
    return assemble(res.results)
```

```python
from contextlib import ExitStack

import numpy as np
import ml_dtypes
import concourse.bass as bass
import concourse.mybir as mybir
from concourse.bass_utils import run_bass_kernel_spmd

F32 = mybir.dt.float32
BF16 = mybir.dt.bfloat16
I32 = mybir.dt.int32
U32 = mybir.dt.uint32
AF = mybir.ActivationFunctionType
ALU = mybir.AluOpType


class Tr:
    __slots__ = ("sem", "count", "name")

    def __init__(self, sem, name):
        self.sem = sem
        self.count = 0
        self.name = name


class Buf:
    __slots__ = ("t", "w", "r", "name")

    def __init__(self, t, name=""):
        self.t = t
        self.w = None
        self.r = {}
        self.name = name


class Eng:
    def __init__(self, name, tr):
        self.name = name
        self.tr = tr
        self.ops = []
        self.waited = {}
        self.dma_trs = []
        self.dma_i = 0
        self.deferred = []


class Prog:
    def __init__(self, nc, n_dma=(28, 28, 6)):
        self.nc = nc
        self.stack = ExitStack()
        self.scopes = []
        mk = lambda n: Tr(self.stack.enter_context(nc.semaphore(n)), n)
        self.pe = Eng("pe", mk("s_pe"))
        self.act = Eng("act", mk("s_act"))
        self.dve = Eng("dve", mk("s_dve"))
        self.pool = Eng("pool", mk("s_pool"))
        self.sp = Eng("sp", mk("s_sp"))
        self.engs = [self.pe, self.act, self.dve, self.pool, self.sp]
        for e, n in zip((self.sp, self.pool, self.act), n_dma):
            e.dma_trs = [mk("d_%s%d" % (e.name, i)) for i in range(n)]
        self.banks = [Buf(self.stack.enter_context(nc.psum_tensor("psb%d" % i, [128, 512], F32)), "ps%d" % i)
                      for i in range(8)]
        self.bank_i = 0
        self.final = []
        self.n_ops = 0
        self.sched = True
        self.pending = []

    def sb(self, name, shape, dtype):
        st = self.scopes[-1] if self.scopes else self.stack
        self.n_sb = getattr(self, "n_sb", 0) + 1
        return Buf(st.enter_context(self.nc.sbuf_tensor("sb%d_%s" % (self.n_sb, name), list(shape), dtype)), name)

    def push_scope(self):
        self.scopes.append(ExitStack())

    def pop_scope(self):
        self.barrier()
        self.flush()
        self.scopes.pop().close()

    def ps(self, lo=0, hi=8):
        n = hi - lo
        b = self.banks[lo + (self.bank_i % n)]
        self.bank_i += 1
        return b

    def _deps(self, eng, reads, writes, extra=()):
        deps = {}

        def add(tok):
            if tok is None:
                return
            tr, v = tok
            if deps.get(tr, 0) < v:
                deps[tr] = v

        for b in reads:
            add(b.w)
        for b in writes:
            add(b.w)
            for tr, v in b.r.items():
                add((tr, v))
        for tok in extra:
            add(tok)
        waits = []
        for tr, v in deps.items():
            if eng is self.pe and tr is eng.tr:
                continue
            if eng.waited.get(tr, 0) >= v:
                continue
            eng.waited[tr] = v
            waits.append((tr, v))
        return waits

    @staticmethod
    def _mark(tok, reads, writes):
        tr, v = tok
        for b in reads:
            if b.r.get(tr, 0) < v:
                b.r[tr] = v
        for b in writes:
            b.w = tok
            b.r = {}

    def op(self, eng, fn, reads=(), writes=(), sig=True, c=0.3):
        if eng is self.pe and c == 0.3:
            c = 0.07
        if self.sched:
            self.pending.append(("op", eng, (fn, tuple(reads), tuple(writes), sig), tuple(reads), tuple(writes), c, 0))
            return None
        return self._op_now(eng, fn, reads, writes, sig)

    def dma(self, eng, out, in_, reads=(), writes=(), out_final=False, slow=False, fn=None, nbytes=None, **kw):
        if nbytes is None:
            try:
                ap = out if out is not None else None
                nbytes = 1
                for s_ in ap.shape:
                    nbytes *= int(s_)
                nbytes *= 2 if ap.dtype == BF16 else 4
            except Exception:
                nbytes = 262144
        if self.sched:
            self.pending.append(("dma", eng, (out, in_, tuple(reads), tuple(writes), out_final, slow, fn, kw), tuple(reads), tuple(writes),
                                 1.1 if fn is not None else 0.08, nbytes))
            return None
        return self._dma_now(eng, out, in_, reads, writes, out_final, slow, fn, **kw)

    def _op_now(self, eng, fn, reads=(), writes=(), sig=True):
        sig = True
        waits = self._deps(eng, reads, writes)
        tr = eng.tr
        if sig:
            tr.count += 1
            tok = (tr, tr.count)
        else:
            tok = None

        def run(h, waits=waits, fn=fn, sig=sig, tr=tr):
            for tr2, v in waits:
                h.wait_ge(tr2.sem, v)
            ins = fn(h)
            if sig:
                ins.then_inc(tr.sem, 1)

        eng.ops.append(run)
        self.n_ops += 1
        if tok is None:
            eng.deferred.append((reads, writes))
        else:
            for r_, w_ in eng.deferred:
                self._mark(tok, r_, w_)
            eng.deferred = []
            self._mark(tok, reads, writes)
        return tok

    def _dma_now(self, eng, out, in_, reads=(), writes=(), out_final=False, slow=False, fn=None, **kw):
        tr = eng.dma_trs[eng.dma_i % len(eng.dma_trs)]
        eng.dma_i += 1
        extra = [(tr, tr.count)] if tr.count else []
        waits = self._deps(eng, reads, writes, extra)
        tr.count += 16
        tok = (tr, tr.count)
        if slow:
            kw["allow_slow_non_contiguous"] = True

        def run(h, waits=waits, tr=tr, out=out, in_=in_, kw=kw, fn=fn):
            for tr2, v in waits:
                h.wait_ge(tr2.sem, v)
            if fn is not None:
                ins = fn(h)
            else:
                ins = h.dma_start(out=out, in_=in_, **kw)
            ins.then_inc(tr.sem, 16)

        eng.ops.append(run)
        self.n_ops += 1
        self._mark(tok, reads, writes)
        if out_final:
            self.final.append(tok)
        return tok

    def drain(self):
        pend = self.pending
        self.pending = []
        n = len(pend)
        if n == 0:
            return
        import heapq
        lastw, readers = {}, {}
        close = list(range(n))
        nxt = {}
        for i in range(n - 1, -1, -1):
            kind, eng = pend[i][0], pend[i][1]
            pass
        deps = [None] * n
        succ = [[] for _ in range(n)]
        indeg = [0] * n
        for i, (_, eng, _, reads, writes, _, _) in enumerate(pend):
            d = set()
            for b in reads:
                w = lastw.get(id(b))
                if w is not None:
                    d.add(w)
            for b in writes:
                w = lastw.get(id(b))
                if w is not None:
                    d.add(w)
                for r_ in readers.get(id(b), ()):
                    d.add(r_)
            d.discard(i)
            if not (pend[i][0] == "op" and eng is self.pe):
                d = {close[w] for w in d}
            else:
                d = {(w if pend[w][1] is self.pe else close[w]) for w in d}
            d.discard(i)
            deps[i] = d
            for j in d:
                succ[j].append(i)
            indeg[i] = len(d)
            for b in reads:
                readers.setdefault(id(b), []).append(i)
            for b in writes:
                lastw[id(b)] = i
                readers[id(b)] = []
        fin = [0.0] * n
        eng_free = {id(e): 0.0 for e in self.engs}
        ready = {id(e): [] for e in self.engs}
        for i in range(n):
            if indeg[i] == 0:
                heapq.heappush(ready[id(pend[i][1])], (0.0, i))
        dma_free = 0.0
        order = []
        LAT = 0.15
        WIN = 6000
        done = 0
        lo = 0
        scheduled = [False] * n
        while done < n:
            best = None
            for e in self.engs:
                h = ready[id(e)]
                if not h:
                    continue
                cand = None
                tmp_ = []
                k = 0
                while h and k < 8:
                    rt, i = heapq.heappop(h)
                    tmp_.append((rt, i))
                    k += 1
                    if i - lo > WIN:
                        continue
                    st_ = max(rt, eng_free[id(e)])
                    key = (st_, i)
                    if cand is None or key < cand[0]:
                        cand = (key, rt, i)
                for x in tmp_:
                    heapq.heappush(h, x)
                if cand is not None and (best is None or cand[0] < best[0][0]):
                    best = (cand, e)
            if best is None:
                cands = [(h[0][1], e) for e in self.engs for h in [ready[id(e)]] if h]
                i_min = None
                for e in self.engs:
                    for rt, i in ready[id(e)]:
                        if i_min is None or i < i_min[0]:
                            i_min = (i, rt, e)
                i, rt, e = i_min
                best = ((((max(rt, eng_free[id(e)])), i), rt, i), e)
            (key, rt, i), e = best
            h = ready[id(e)]
            h.remove((rt, i))
            heapq.heapify(h)
            kind, _, _, _, _, cost, nbytes = pend[i]
            st_ = key[0]
            if kind == "dma":
                eng_free[id(e)] = st_ + cost
                t0_ = max(st_ + cost, dma_free)
                dma_free = t0_ + nbytes / 400000.0
                fin[i] = dma_free + 1.8
            else:
                eng_free[id(e)] = st_ + cost
                fin[i] = st_ + cost
            scheduled[i] = True
            order.append(i)
            done += 1
            while lo < n and scheduled[lo]:
                lo += 1
            for j in succ[i]:
                indeg[j] -= 1
                if indeg[j] == 0:
                    rt_j = 0.0
                    for d_ in deps[j]:
                        l_ = 0.0 if pend[d_][1] is pend[j][1] and pend[j][1] is self.pe else LAT
                        if fin[d_] + l_ > rt_j:
                            rt_j = fin[d_] + l_
                    heapq.heappush(ready[id(pend[j][1])], (rt_j, j))
        self.sim_time = getattr(self, "sim_time", 0.0) + max(fin)
        for i in order:
            kind, eng, args, _, _, _, _ = pend[i]
            if kind == "op":
                fn, reads, writes, sig = args
                self._op_now(eng, fn, reads, writes, sig)
            else:
                out, in_, reads, writes, out_final, slow, fn, kw = args
                self._dma_now(eng, out, in_, reads, writes, out_final, slow, fn, **kw)

    def barrier(self):
        self.drain()
        toks = []
        for e in self.engs:
            if e.tr.count:
                toks.append((e.tr, e.tr.count))
            for tr in e.dma_trs:
                if tr.count:
                    toks.append((tr, tr.count))
        for e in self.engs:
            waits = []
            for tr, v in toks:
                if tr is e.tr:
                    continue
                if e.waited.get(tr, 0) >= v:
                    continue
                e.waited[tr] = v
                waits.append((tr, v))
            if waits:
                def run(h, waits=waits):
                    for tr2, v in waits:
                        h.wait_ge(tr2.sem, v)
                e.ops.append(run)

    def flush(self):
        nc = self.nc
        pend = {e.name: e.ops for e in self.engs}
        for e in self.engs:
            e.ops = []
        with nc.Block() as block:
            @block.tensor
            def _(h):
                for f in pend["pe"]:
                    f(h)

            @block.scalar
            def _(h):
                for f in pend["act"]:
                    f(h)

            @block.vector
            def _(h):
                for f in pend["dve"]:
                    f(h)

            @block.gpsimd
            def _(h):
                for f in pend["pool"]:
                    f(h)

            @block.sync
            def _(h):
                for f in pend["sp"]:
                    f(h)

    def finish(self):
        self.drain()
        waits = []
        for tr, v in self.final:
            if self.sp.waited.get(tr, 0) < v:
                self.sp.waited[tr] = v
                waits.append((tr, v))

        def run(h, waits=waits):
            for tr2, v in waits:
                h.wait_ge(tr2.sem, v)

        self.sp.ops.append(run)
        self.barrier()
        self.flush()
        while self.scopes:
            self.scopes.pop().close()
        self.stack.close()


D = 1024
NCORES = 8
SEQ = 2048
NSEQ_P = 2
NSUB_S = 4
TS = 32
PAST = 1024
H = 8
DH = 64
CC = 512
KW = 31
NE = 256
DE = 256
OFF_Q, OFF_K, OFF_V, OFF_F, OFF_GLU, OFF_GA, OFF_GB, N_IN = 0, 512, 1024, 1536, 1544, 2568, 3592, 4616
NA = OFF_GA
TP = NSEQ_P * SEQ
TT = TP + 128
DN_ALPHA = 2.0 ** 0.25
LN_EPS = 1e-5

PC_BQ, PC_BGLU, PC_BGA, PC_BGB, PC_BB, PC_CB, PC_CG, PC_CBE, PC_CW, PC_BF, PC_BK, PC_N = 0, 4, 12, 20, 28, 36, 40, 44, 48, 172, 173, 177


def _n(ap):
    n = 1
    for s_ in ap.shape[1:]:
        n *= int(s_)
    return n


def _mm(P, psb, out_ap, lhsT, rhs, start, stop, reads, sig=None):
    c = max(_n(rhs), 64) / 2400.0 * (4.0 if lhsT.dtype == F32 else 1.0) + 0.01
    P.op(P.pe, lambda e: e.matmul(out_ap, lhsT, rhs, start=start, stop=stop), reads=reads, writes=[psb],
         sig=stop if sig is None else sig, c=c)


def _act(P, out_ap, in_ap, func, reads, writes, bias=None, scale=1.0):
    c = _n(out_ap) / 1050.0 + 0.27
    if bias is None:
        P.op(P.act, lambda e: e.activation(out_ap, in_ap, func, scale=scale), reads=reads, writes=writes, c=c)
    else:
        P.op(P.act, lambda e: e.activation(out_ap, in_ap, func, bias=bias, scale=scale), reads=reads, writes=writes, c=c)


def _tt(P, eng, out_ap, a, b, op, reads, writes):
    c = _n(out_ap) / (950.0 if eng is P.dve else 440.0) + (0.17 if eng is P.dve else 0.15)
    P.op(eng, lambda e: e.tensor_tensor(out_ap, a, b, op), reads=reads, writes=writes, c=c)


def _stt(P, out_ap, in0, scalar, in1, op0, op1, reads, writes):
    P.op(P.dve, lambda e: e.scalar_tensor_tensor(out_ap, in0, scalar, in1, op0, op1), reads=reads, writes=writes, c=_n(out_ap) / 960.0 + 0.12)


def _ts(P, eng, out_ap, in0, s1, s2, op0, op1, reads, writes):
    c = _n(out_ap) / 960.0 + 0.12
    if s2 is None:
        P.op(eng, lambda e: e.tensor_scalar(out_ap, in0, s1, None, op0), reads=reads, writes=writes, c=c)
    else:
        P.op(eng, lambda e: e.tensor_scalar(out_ap, in0, s1, s2, op0, op1), reads=reads, writes=writes, c=c)


def _copy(P, eng, out_ap, in_ap, reads, writes):
    if eng is P.act:
        P.op(eng, lambda e: e.copy(out_ap, in_ap), reads=reads, writes=writes, c=_n(out_ap) / 1050.0 + 0.27)
    else:
        P.op(eng, lambda e: e.tensor_copy(out_ap, in_ap), reads=reads, writes=writes,
             c=_n(out_ap) / (960.0 if eng is P.dve else 280.0) + (0.12 if eng is P.dve else 0.3))


def load_w_cast(P, dst, src_ap, kchunks, ncols, col0=0):
    v = src_ap.rearrange("(kc p) n -> p kc n", p=128)
    c = 0
    while c < ncols:
        n = min(2048, ncols - c)
        P.dma(P.pool, dst.t[:, :, c:c + n], v[:, :, col0 + c:col0 + c + n], reads=[], writes=[dst])
        c += n


def pass_a(P, T, NB):
    nc = P.nc
    P.push_scope()
    cf = P.sb("cf", [128, 384], F32)
    cb = P.sb("cb", [128, 256], BF16)
    pcol = P.sb("pcol", [128, PC_N], F32)
    w_in = P.sb("w_in_a", [128, 8, NA], BF16)
    bkv = P.sb("bkv", [128, 1024], F32)
    bq8 = P.sb("bq8", [128, 4], F32)
    nbf = P.sb("nbf", [8, 1], F32)
    ones8 = P.sb("ones8", [8, 512], F32)
    onesb = P.sb("onesb", [128, 128], BF16)
    epsb = P.sb("epsb", [128, 1], F32)
    oneb = P.sb("oneb", [128, 1], F32)
    scr = Buf(None, "scratch")
    c3d = Buf(None, "c3d")
    P.dma(P.sp, cf.t[:, :], T["cf"], writes=[cf])
    P.dma(P.sp, cb.t[:, :], T["cb"][:, 0:256], writes=[cb])
    P.dma(P.sp, pcol.t[:, :], T["pcol"], writes=[pcol])
    P.dma(P.sp, bkv.t[:, :], T["b_in"][:, OFF_K:OFF_F].partition_broadcast(128), writes=[bkv])
    load_w_cast(P, w_in, T["w_in"], 8, NA)
    P.op(P.act, lambda e: e.mul(bq8.t[:, :], pcol.t[:, PC_BQ:PC_BQ + 4], 0.125), reads=[pcol], writes=[bq8])
    P.op(P.act, lambda e: e.mul(nbf.t[:, :], pcol.t[0:8, PC_BF:PC_BF + 1], -1.0), reads=[pcol], writes=[nbf])
    P.op(P.dve, lambda e: e.memset(ones8.t[:, :], 1.0), writes=[ones8])
    P.op(P.dve, lambda e: e.memset(onesb.t[:, :], 1.0), writes=[onesb])
    P.op(P.dve, lambda e: e.memset(epsb.t[:, :], LN_EPS), writes=[epsb])
    P.op(P.dve, lambda e: e.memset(oneb.t[:, :], 1.0), writes=[oneb])
    identf = cf.t[:, 0:128]
    lstrict = cf.t[:, 128:256]
    onesf = cf.t[:, 256:384]
    identb = cb.t[:, 0:128]
    tri = cb.t[:, 128:256]

    kT = P.sb("kT", [128, 4, SEQ], BF16)
    vsb = P.sb("vsb", [128, SEQ // 128, 512], BF16)
    negc = P.sb("negc", [128, SEQ // 128, 8], F32)
    xt = [P.sb("xt%d" % i, [128, 1024], F32) for i in range(2)]
    xT = P.sb("xT", [128, 8, NB], BF16)
    qz = P.sb("qz", [128, 8, NB], BF16)
    c3pad = P.sb("c3pad", [128, 8, NB], BF16)
    uT = P.sb("uT", [128, 4, 30 + NB], F32)
    sg = [P.sb("sg%d" % i, [128, NB], F32) for i in range(2)]
    kvf = [P.sb("kvf%d" % i, [128, 1024], F32) for i in range(2)]
    kbf = [P.sb("kbf%d" % i, [128, 512], BF16) for i in range(2)]
    lfneg = P.sb("lfneg", [8, NB], F32)
    cblk = P.sb("cblk", [8, NB], F32)
    carry = P.sb("carry", [8, 1], F32)
    c3p = P.sb("c3p", [8, 3, NB], BF16)
    ctmp = P.sb("ctmp", [8, NB], F32)
    et = ctmp
    lftok = P.sb("lftok", [128, NB // 128, 8], F32)
    pt = [P.sb("pt%d" % i, [128, NB], BF16) for i in range(3)]
    attnT = P.sb("attnT", [64, 8, NB], BF16)
    acc = [P.sb("cacc%d" % i, [128, NB], F32) for i in range(2)]
    rden = acc
    hc = P.sb("hc", [128, 4, NB], F32)
    hsq = P.sb("hsq", [128, NB], F32)
    mean = P.sb("mean", [128, NB], F32)
    rstd = P.sb("rstd", [128, NB], F32)
    hn = sg
    hT = P.sb("hT", [128, 4, NB], BF16)
    ubf = P.sb("ubf", [128, 4, 30 + NB], BF16)
    diag = [P.sb("diag%d" % i, [128, 128], BF16) for i in range(6)]
    cvo = P.sb("cvo", [32, 512], F32)
    P.op(P.pool, lambda e: e.memset(qz.t[:, :, :], 0.0), writes=[qz])
    P.op(P.pool, lambda e: e.memset(c3pad.t[:, :, :], 0.0), writes=[c3pad])

    st = {"pt": 0, "x": 0, "kv": 0, "dg": 0}

    def project_feature(col0, nfeat, nb, evac):
        psb = P.ps()
        for kc in range(8):
            _mm(P, psb, psb.t[0:nfeat, 0:nb], w_in.t[:, kc, col0:col0 + nfeat], xT.t[:, kc, 0:nb], kc == 0, kc == 7,
                [w_in, xT])
        evac(psb)

    def attention(h, qc0, nq, ktiles):
        ob = P.banks[4 + (h % 2)]
        db = P.banks[6 + (h % 2)]
        n = len(ktiles)
        for i, kt in enumerate(ktiles):
            nk, q0 = kt["nk"], kt["q0"]
            w = nq - q0
            sb_ = P.ps(0, 4)
            _mm(P, sb_, sb_.t[0:nk, 0:w], kt["kT"], qz.t[:, h, qc0 + q0:qc0 + nq], True, False, kt["reads"] + [qz])
            _mm(P, sb_, sb_.t[0:nk, 0:w], onesb.t[:, 0:nk], c3pad.t[:, h, qc0 + q0:qc0 + nq], False, True,
                [onesb, c3pad])
            p = pt[st["pt"] % 3]
            st["pt"] += 1
            _act(P, p.t[0:nk, 0:w], sb_.t[0:nk, 0:w], AF.Exp, kt["reads"] + [sb_], [p], bias=kt["bias"])
            if kt["tri"]:
                tw = min(nk, w)
                _tt(P, P.pool, p.t[0:nk, 0:tw], p.t[0:nk, 0:tw], tri[0:nk, 0:tw], ALU.mult, [p, cb], [p])
            _mm(P, ob, ob.t[0:64, q0:nq], kt["v"], p.t[0:nk, 0:w], i == 0, i == n - 1, kt["reads"] + [p])
            _mm(P, db, db.t[0:64, q0:nq], onesb.t[0:nk, 0:64], p.t[0:nk, 0:w], i == 0, i == n - 1, [onesb, p])
        rd = rden[h % 2]
        _act(P, rd.t[0:64, 0:nq], db.t[0:64, 0:nq], AF.Ln, [db], [rd])
        _act(P, rd.t[0:64, 0:nq], rd.t[0:64, 0:nq], AF.Exp, [rd], [rd], scale=-1.0)
        _tt(P, P.dve, attnT.t[:, h, qc0:qc0 + nq], ob.t[0:64, 0:nq], rd.t[0:64, 0:nq], ALU.mult, [ob, rd], [attnT])


    def conv_ln(nb, uview, accview, c_list=range(4)):
        ps_s = P.ps()
        ps_q = P.ps()
        for c in range(4):
            _copy(P, P.pool, ubf.t[:, c, :], uT.t[:, c, :], [uT], [ubf])
        for c in range(4):
            psc = P.ps()
            for j in range(KW):
                dg = diag[st["dg"] % len(diag)]
                st["dg"] += 1
                col = pcol.t[:, PC_CW + c * KW + j:PC_CW + c * KW + j + 1]
                P.op(P.act, lambda e, dg=dg, col=col: e.activation(dg.t[:, :], identb, AF.Identity, scale=col), reads=[cb, pcol], writes=[dg], c=0.32)
                _mm(P, psc, accview(psc), dg.t[:, :], uview(c, j), j == 0, j == KW - 1, [dg, ubf], sig=True)
            _act(P, hc.t[:, c, 0:nb], psc.t[:, 0:nb], AF.Identity, [psc, pcol], [hc], bias=pcol.t[:, PC_CB + c:PC_CB + c + 1])
            P.op(P.act, lambda e, c=c: e.activation(hsq.t[:, 0:nb], hc.t[:, c, 0:nb], AF.Square), reads=[hc], writes=[hsq], c=nb / 1400.0 + 0.22)
            _mm(P, ps_s, ps_s.t[:, 0:nb], onesf, hc.t[:, c, 0:nb], c == 0, c == 3, [cf, hc], sig=True)
            _mm(P, ps_q, ps_q.t[:, 0:nb], onesf, hsq.t[:, 0:nb], c == 0, c == 3, [cf, hsq], sig=True)
        P.op(P.act, lambda e: e.mul(mean.t[:, 0:nb], ps_s.t[:, 0:nb], 1.0 / CC), reads=[ps_s], writes=[mean])
        m2 = hn[0]
        _tt(P, P.dve, m2.t[:, 0:nb], mean.t[:, 0:nb], mean.t[:, 0:nb], ALU.mult, [mean], [m2])
        _stt(P, rstd.t[:, 0:nb], ps_q.t[:, 0:nb], 1.0 / CC, m2.t[:, 0:nb], ALU.mult, ALU.subtract, [ps_q, m2], [rstd])
        _act(P, rstd.t[:, 0:nb], rstd.t[:, 0:nb], AF.Sqrt, [rstd], [rstd], bias=epsb.t[:, 0:1])
        P.op(P.dve, lambda e: e.reciprocal(rstd.t[:, 0:nb], rstd.t[:, 0:nb]), reads=[rstd], writes=[rstd])
        for c in range(4):
            x_ = hn[c % 2]
            _tt(P, P.dve, x_.t[:, 0:nb], hc.t[:, c, 0:nb], mean.t[:, 0:nb], ALU.subtract, [hc, mean], [x_])
            _tt(P, P.pool, x_.t[:, 0:nb], x_.t[:, 0:nb], rstd.t[:, 0:nb], ALU.mult, [x_, rstd], [x_])
            P.op(P.act, lambda e, c=c, x_=x_: e.activation(hT.t[:, c, 0:nb], x_.t[:, 0:nb], AF.Silu,
                                                       bias=pcol.t[:, PC_CBE + c:PC_CBE + c + 1],
                                                       scale=pcol.t[:, PC_CG + c:PC_CG + c + 1]),
                 reads=[x_, pcol], writes=[hT])

    def conv_state_out(uview30, dst):
        psb = P.ps()
        for c in range(4):
            P.op(P.pe, lambda e, c=c, psb=psb: e.transpose(psb.t[0:30, c * 128:(c + 1) * 128], uview30(c), identf),
                 reads=[uT, cf], writes=[psb], sig=(c == 3))
        _copy(P, P.act, cvo.t[0:30, :], psb.t[0:30, 0:512], [psb], [cvo])
        P.dma(P.sp, dst, cvo.t[0:30, :], reads=[cvo], out_final=True)

    def common_front(x_src, nb, subs):
        ntile = nb // 128
        for t in range(ntile):
            xb = xt[st["x"] % 2]
            st["x"] += 1
            P.dma(P.sp, xb.t[:, :], x_src[t * 128:(t + 1) * 128, :], writes=[xb])
            for g in range(2):
                psb = P.ps()
                for j in range(4):
                    kc = g * 4 + j
                    P.op(P.pe, lambda e, psb=psb, j=j, kc=kc, xb=xb: e.transpose(
                        psb.t[:, j * 128:(j + 1) * 128], xb.t[:, kc * 128:(kc + 1) * 128], identf),
                        reads=[xb, cf], writes=[psb], sig=(j == 3))
                eng = P.act if g == 0 else P.dve
                _copy(P, eng, xT.t[:, g * 4:(g + 1) * 4, t * 128:(t + 1) * 128],
                      psb.t[:, :].rearrange("p (a b) -> p a b", a=4), [psb], [xT])
        for c in range(4):
            def ev(psb, c=c):
                _act(P, qz.t[0:64, 2 * c, 0:nb], psb.t[0:64, 0:nb], AF.Identity, [psb, bq8], [qz],
                     bias=bq8.t[0:64, c:c + 1], scale=0.125)
                _act(P, qz.t[64:128, 2 * c + 1, 0:nb], psb.t[64:128, 0:nb], AF.Identity, [psb, bq8], [qz],
                     bias=bq8.t[64:128, c:c + 1], scale=0.125)
            project_feature(OFF_Q + c * 128, 128, nb, ev)
        def evf(psb):
            _act(P, et.t[:, 0:nb], psb.t[0:8, 0:nb], AF.Exp, [psb, nbf], [et], bias=nbf.t[:, 0:1], scale=-1.0)
            _act(P, lfneg.t[:, 0:nb], et.t[:, 0:nb], AF.Ln, [et], [lfneg], bias=oneb.t[0:8, 0:1])
        project_feature(OFF_F, 8, nb, evf)
        for s in subs:
            c0, n = s["c0"], s["n"]
            init = 0.0 if s["first"] else carry.t[:, 0:1]
            P.op(P.dve, lambda e, c0=c0, n=n, init=init: e.tensor_tensor_scan(
                cblk.t[:, c0:c0 + n], ones8.t[:, 0:n], lfneg.t[:, c0:c0 + n], init, ALU.mult, ALU.subtract),
                reads=[ones8, lfneg, carry], writes=[cblk])
        _copy(P, P.act, carry.t[:, 0:1], cblk.t[:, nb - 1:nb], [cblk], [carry])
        _copy(P, P.dve, c3p.t[:, 0, 0:nb], cblk.t[:, 0:nb], [cblk], [c3p])
        _tt(P, P.dve, ctmp.t[:, 0:nb], cblk.t[:, 0:nb], c3p.t[:, 0, 0:nb], ALU.subtract, [cblk, c3p], [ctmp])
        _copy(P, P.dve, c3p.t[:, 1, 0:nb], ctmp.t[:, 0:nb], [ctmp], [c3p])
        _tt(P, P.dve, c3p.t[:, 2, 0:nb], ctmp.t[:, 0:nb], c3p.t[:, 1, 0:nb], ALU.subtract, [ctmp, c3p], [c3p])
        P.dma(P.sp, T["c3_d"][:, :, 0:nb], c3p.t[:, :, 0:nb], reads=[c3p], writes=[c3d])
        P.dma(P.sp, c3pad.t[0:3, :, 0:nb], T["c3_d"][:, :, 0:nb].rearrange("h s n -> s h n"), reads=[c3d], writes=[c3pad])

    def glu(nb, uout, view=lambda a: a):
        for c in range(4):
            sgb = sg[c % 2]
            def evg(psb, sgb=sgb, c=c):
                _act(P, sgb.t[:, 0:nb], psb.t[:, 0:nb], AF.Sigmoid, [psb, pcol], [sgb],
                     bias=pcol.t[:, PC_BGLU + 4 + c:PC_BGLU + 5 + c])
            project_feature(OFF_GLU + CC + c * 128, 128, nb, evg)
            def eva(psb, sgb=sgb, c=c):
                _stt(P, uout(c), view(psb.t[:, 0:nb]), pcol.t[:, PC_BGLU + c:PC_BGLU + c + 1], view(sgb.t[:, 0:nb]),
                     ALU.add, ALU.mult, [psb, pcol, sgb], [uT])
            project_feature(OFF_GLU + c * 128, 128, nb, eva)

    def store_scratch(nb, tok0):
        P.dma(P.sp, T["attn_d"].rearrange("(h d) t -> d h t", d=64)[:, :, tok0:tok0 + nb], attnT.t[:, :, 0:nb],
              reads=[attnT], writes=[scr])
        P.dma(P.sp, T["h_d"].rearrange("(c p) t -> p c t", p=128)[:, :, tok0:tok0 + nb], hT.t[:, :, 0:nb],
              reads=[hT], writes=[scr])

    ntile = NB // 128
    for sq in range(NSEQ_P):
        for b in range(SEQ // NB):
            r0 = sq * SEQ + b * NB
            tile0 = b * ntile
            if b == 0:
                P.op(P.pool, lambda e: e.memset(uT.t[:, :, 0:30], 0.0), writes=[uT])
            else:
                _copy(P, P.pool, uT.t[:, :, 0:30], uT.t[:, :, NB:NB + 30], [uT], [uT])
            common_front(T["xp"][r0:r0 + NB, :], NB, [{"c0": 0, "n": NB, "first": b == 0}])
            for t in range(ntile):
                psb = P.ps()
                P.op(P.pe, lambda e, psb=psb, t=t: e.transpose(psb.t[:, 0:8], lfneg.t[:, t * 128:(t + 1) * 128], identf[0:8, 0:8]),
                     reads=[lfneg, cf], writes=[psb])
                P.op(P.pe, lambda e, psb=psb, t=t: e.transpose(psb.t[:, 8:16], cblk.t[:, t * 128:(t + 1) * 128], identf[0:8, 0:8]),
                     reads=[cblk, cf], writes=[psb])
                P.op(P.act, lambda e, psb=psb, t=t: e.mul(lftok.t[:, t, :], psb.t[:, 0:8], -1.0), reads=[psb], writes=[lftok])
                P.op(P.act, lambda e, psb=psb, j=tile0 + t: e.mul(negc.t[:, j, :], psb.t[:, 8:16], -1.0), reads=[psb], writes=[negc])
            P.dma(P.sp, T["lfp"][r0:r0 + NB, :].rearrange("(t p) h -> p t h", p=128), lftok.t[:, 0:ntile, :],
                  reads=[lftok], out_final=True)
            for t in range(ntile):
                j = tile0 + t
                kvb = kvf[st["kv"] % 2]
                kb = kbf[st["kv"] % 2]
                st["kv"] += 1
                for half in range(2):
                    psb = P.ps()
                    for kc in range(8):
                        _mm(P, psb, psb.t[:, 0:512], xT.t[:, kc, t * 128:(t + 1) * 128],
                            w_in.t[:, kc, OFF_K + half * 512:OFF_K + (half + 1) * 512], kc == 0, kc == 7, [xT, w_in])
                    _tt(P, P.dve, kvb.t[:, half * 512:(half + 1) * 512], psb.t[:, 0:512], bkv.t[:, half * 512:(half + 1) * 512],
                        ALU.add, [psb, bkv], [kvb])
                rr = r0 + t * 128
                P.dma(P.sp, T["kp"][rr:rr + 128, :], kvb.t[:, 0:512], reads=[kvb], out_final=True)
                P.dma(P.sp, T["vp"][rr:rr + 128, :], kvb.t[:, 512:1024], reads=[kvb], out_final=True)
                _copy(P, P.act, kb.t[:, :], kvb.t[:, 0:512], [kvb], [kb])
                _copy(P, P.pool, vsb.t[:, j, :], kvb.t[:, 512:1024], [kvb], [vsb])
                psb = P.ps()
                pbf = psb.t[:, :].bitcast(BF16)
                for c in range(4):
                    P.op(P.pe, lambda e, c=c, pbf=pbf, kb=kb: e.transpose(pbf[:, c * 128:(c + 1) * 128], kb.t[:, c * 128:(c + 1) * 128], identb),
                         reads=[kb, cb], writes=[psb], sig=(c == 3))
                _copy(P, P.dve, kT.t[:, :, j * 128:(j + 1) * 128], pbf[:, 0:512].rearrange("p (c n) -> p c n", c=4), [psb], [kT])
            glu(NB, lambda c: uT.t[:, c, 30:30 + NB])
            for h in range(8):
                kts = []
                for j in range(tile0 + ntile):
                    kts.append(dict(kT=kT.t[:, h // 2, j * 128:(j + 1) * 128], v=vsb.t[:, j, h * 64:(h + 1) * 64],
                                    bias=negc.t[:, j, h:h + 1], nk=128, q0=max(0, (j - tile0) * 128), tri=j >= tile0,
                                    reads=[kT, vsb, negc]))
                attention(h, 0, NB, kts)
            conv_ln(NB, lambda c, j: ubf.t[:, c, j:j + NB], lambda a: a.t[:, 0:NB])
            store_scratch(NB, r0)
            if b == SEQ // NB - 1:
                conv_state_out(lambda c: uT.t[:, c, NB:NB + 30], T["cvp"][sq, :, :])
    uSv = uT.t[:, :, 0:NSUB_S * 62].rearrange("p c (s n) -> p c s n", s=NSUB_S)
    ckb = P.sb("ckb", [128, 8, 512], BF16)
    clfb = P.sb("clfb", [128, 8, 8], F32)
    sufs = P.sb("sufs", [128, 8, 8], F32)
    negcs = P.sb("negcs", [32, NSUB_S, 8], F32)
    kTn = P.sb("kTn", [128, 4, 128], BF16)
    vnew = P.sb("vnew", [32, NSUB_S, 512], BF16)
    kvs = kvf
    scv = kvf
    v4 = lambda a: a.rearrange("p (s n) -> p s n", s=NSUB_S)
    for s in range(NSUB_S):
        sc = scv[s % 2]
        P.dma(P.sp, sc.t[0:30, 0:512], T["sconv"][s, :, :], writes=[sc])
        psb = P.ps()
        for c in range(4):
            P.op(P.pe, lambda e, c=c, psb=psb, sc=sc: e.transpose(psb.t[:, c * 32:c * 32 + 30], sc.t[0:30, c * 128:(c + 1) * 128],
                                                               identf[0:30, 0:30]),
                 reads=[sc, cf], writes=[psb], sig=(c == 3))
        _copy(P, P.act, uSv[:, :, s, 0:30], psb.t[:, 0:128].rearrange("p (c n) -> p c n", c=4)[:, :, 0:30], [psb], [uT])
    common_front(T["xs"], 128, [{"c0": 32 * s, "n": 32, "first": True} for s in range(NSUB_S)])
    psb = P.ps()
    P.op(P.pe, lambda e, psb=psb: e.transpose(psb.t[:, 0:8], lfneg.t[:, 0:128], identf[0:8, 0:8]), reads=[lfneg, cf], writes=[psb])
    P.op(P.act, lambda e, psb=psb: e.mul(lftok.t[:, 0, :], psb.t[:, 0:8], -1.0), reads=[psb], writes=[lftok])
    P.dma(P.sp, T["lfs"], lftok.t[:, 0, :], reads=[lftok], out_final=True)
    for s in range(NSUB_S):
        psb = P.ps()
        P.op(P.pe, lambda e, psb=psb, s=s: e.transpose(psb.t[0:32, 0:8], cblk.t[:, 32 * s:32 * s + 32], identf[0:8, 0:8]),
             reads=[cblk, cf], writes=[psb])
        P.op(P.act, lambda e, psb=psb, s=s: e.mul(negcs.t[0:32, s, :], psb.t[0:32, 0:8], -1.0), reads=[psb], writes=[negcs])
        kvb = kvs[s % 2]
        for half in range(2):
            psb = P.ps()
            for kc in range(8):
                _mm(P, psb, psb.t[0:32, 0:512], xT.t[:, kc, 32 * s:32 * s + 32],
                    w_in.t[:, kc, OFF_K + half * 512:OFF_K + (half + 1) * 512], kc == 0, kc == 7, [xT, w_in])
            _tt(P, P.dve, kvb.t[0:32, half * 512:(half + 1) * 512], psb.t[0:32, 0:512], bkv.t[0:32, half * 512:(half + 1) * 512],
                ALU.add, [psb, bkv], [kvb])
        P.dma(P.sp, T["ks"][32 * s:32 * s + 32, :], kvb.t[0:32, 0:512], reads=[kvb], out_final=True)
        P.dma(P.sp, T["vs"][32 * s:32 * s + 32, :], kvb.t[0:32, 512:1024], reads=[kvb], out_final=True)
        _copy(P, P.act, vnew.t[0:32, s, :], kvb.t[0:32, 512:1024], [kvb], [vnew])
    for c in range(4):
        def evk(psb, c=c):
            _act(P, kTn.t[:, c, 0:128], psb.t[:, 0:128], AF.Identity, [psb, pcol], [kTn], bias=pcol.t[:, PC_BK + c:PC_BK + c + 1])
        project_feature(OFF_K + c * 128, 128, 128, evk)
    glu(128, lambda c: uSv[:, c, :, 30:62], v4)
    for s in range(NSUB_S):
        P.dma(P.pool, ckb.t[:, :, :], T["ck"][s].rearrange("(j p) n -> p j n", p=128), writes=[ckb])
        P.dma(P.pool, vsb.t[:, 0:8, :], T["cv"][s].rearrange("(j p) n -> p j n", p=128), writes=[vsb])
        P.dma(P.sp, clfb.t[:, :, :], T["clf"][s].rearrange("(j p) h -> p j h", p=128), writes=[clfb])
        for j in range(8):
            psb = P.ps()
            pbf = psb.t[:, :].bitcast(BF16)
            for c in range(4):
                P.op(P.pe, lambda e, c=c, j=j, pbf=pbf: e.transpose(pbf[:, c * 128:(c + 1) * 128], ckb.t[:, j, c * 128:(c + 1) * 128], identb),
                     reads=[ckb, cb], writes=[psb], sig=(c == 3))
            _copy(P, P.dve if j % 2 else P.act, kT.t[:, :, j * 128:(j + 1) * 128],
                  pbf[:, 0:512].rearrange("p (c n) -> p c n", c=4), [psb], [kT])
        psb = P.ps()
        for j in range(8):
            _mm(P, psb, psb.t[:, j * 8:(j + 1) * 8], lstrict, clfb.t[:, j, :], True, j == 7, [cf, clfb], sig=False)
            for j2 in range(j + 1, 8):
                _mm(P, psb, psb.t[:, j * 8:(j + 1) * 8], onesf, clfb.t[:, j2, :], False, j2 == 7, [cf, clfb], sig=False)
        P.op(P.pe, lambda e, psb=psb: e.transpose(psb.t[0:8, 64:72], clfb.t[0:8, 0, :], identf[0:8, 0:8]), reads=[clfb, cf], writes=[psb])
        _copy(P, P.dve, sufs.t[:, :, :], psb.t[:, 0:64].rearrange("p (j h) -> p j h", j=8), [psb], [sufs])
        for h in range(8):
            kts = []
            for j in range(8):
                kts.append(dict(kT=kT.t[:, h // 2, j * 128:(j + 1) * 128], v=vsb.t[:, j, h * 64:(h + 1) * 64],
                                bias=sufs.t[:, j, h:h + 1], nk=128, q0=0, tri=False, reads=[kT, vsb, sufs]))
            kts.append(dict(kT=kTn.t[:, h // 2, 32 * s:32 * s + 32], v=vnew.t[0:32, s, h * 64:(h + 1) * 64],
                            bias=negcs.t[0:32, s, h:h + 1], nk=32, q0=0, tri=True, reads=[kTn, vnew, negcs]))
            attention(h, 32 * s, 32, kts)
    uSb = ubf.t[:, :, 0:NSUB_S * 62].rearrange("p c (s n) -> p c s n", s=NSUB_S)
    conv_ln(128, lambda c, j: uSb[:, c, :, j:j + 32], lambda a: v4(a.t[:, 0:128]))
    store_scratch(128, TP)
    for s in range(NSUB_S):
        conv_state_out(lambda c, s=s: uSv[:, c, s, 32:62], T["cvs"][s, :, :])
    P.pop_scope()


CAP = 384
NSLOT = NE * CAP
NT = TT // 128


def layer_norm_tile(P, r, out_fn, tmp, eps_col):
    st6, mv, sc = tmp["st6"], tmp["mv"], tmp["sc"]
    for g in range(2):
        P.op(P.dve, lambda e, g=g: e.bn_stats(st6.t[:, g, :], r.t[:, g * 512:(g + 1) * 512]), reads=[r], writes=[st6])
    P.op(P.dve, lambda e: e.bn_aggr(mv.t[:, 0:2], st6.t[:, :, :].rearrange("p a b -> p (a b)")), reads=[st6], writes=[mv])
    _act(P, sc.t[:, 0:1], mv.t[:, 1:2], AF.Sqrt, [mv], [sc], bias=eps_col)
    P.op(P.dve, lambda e: e.reciprocal(sc.t[:, 0:1], sc.t[:, 0:1]), reads=[sc], writes=[sc])
    _ts(P, P.dve, sc.t[:, 1:2], mv.t[:, 0:1], -1.0, sc.t[:, 0:1], ALU.mult, ALU.mult, [mv, sc], [sc])
    out_fn(sc.t[:, 0:1], sc.t[:, 1:2])


def pass_b(P, T, G, NB):
    P.push_scope()
    cf = P.sb("cf", [128, 384], F32)
    cb = P.sb("cb", [128, 384], BF16)
    pcol = P.sb("pcol", [128, PC_N], F32)
    P.dma(P.sp, cf.t[:, :], T["cf"], writes=[cf])
    P.dma(P.sp, cb.t[:, :], T["cb"], writes=[cb])
    P.dma(P.sp, pcol.t[:, :], T["pcol"], writes=[pcol])
    identf = cf.t[:, 0:128]
    identb = cb.t[:, 0:128]
    ustrict = cb.t[:, 256:384]
    w_g = P.sb("w_g", [128, 8, 2048], BF16)
    w_a = P.sb("w_a", [128, 4, 1024], BF16)
    w_b = P.sb("w_b", [128, 4, 1024], BF16)
    w_o = P.sb("w_o", [128, 8, 1024], BF16)
    wr_hi = P.sb("wr_hi", [128, 8, 256], BF16)
    wr_lo = P.sb("wr_lo", [128, 8, 256], BF16)
    w_s = P.sb("w_s", [128, 8, 512], BF16)
    w_sd = P.sb("w_sd", [128, 2, 1024], BF16)
    lng = P.sb("lng", [128, 1024], F32)
    lnb = P.sb("lnb", [128, 1024], F32)
    brt = P.sb("brt", [128, 256], F32)
    cnt = P.sb("cnt", [128, 256], F32)
    onesb = P.sb("onesb", [128, 128], BF16)
    epsb = P.sb("epsb", [128, 1], F32)
    tokid = P.sb("tokid", [128, NT], I32)
    load_w_cast(P, w_g, T["w_in"], 8, 2048, OFF_GA)
    load_w_cast(P, w_a, T["w_a"], 4, 1024)
    load_w_cast(P, w_b, T["w_b"], 4, 1024)
    load_w_cast(P, w_o, T["w_out"], 8, 1024)
    load_w_cast(P, wr_hi, T["w_router"], 8, 256)
    load_w_cast(P, w_s, T["w_sg"], 8, 256)
    P.dma(P.pool, w_s.t[:, :, 256:512], T["w_su"].rearrange("(kc p) n -> p kc n", p=128), writes=[w_s])
    load_w_cast(P, w_sd, T["w_sd"], 2, 1024)
    P.dma(P.sp, lng.t[:, :], T["ln1_g"].partition_broadcast(128), writes=[lng])
    P.dma(P.sp, lnb.t[:, :], T["ln1_b"].partition_broadcast(128), writes=[lnb])
    P.dma(P.sp, brt.t[:, :], T["b_router"].partition_broadcast(128), writes=[brt])
    P.dma(P.sp, cnt.t[:, :], T["base1"], writes=[cnt])
    P.dma(P.sp, tokid.t[:, :], T["tokid"], writes=[tokid])
    P.op(P.dve, lambda e: e.memset(onesb.t[:, :], 1.0), writes=[onesb])
    P.op(P.dve, lambda e: e.memset(epsb.t[:, :], LN_EPS), writes=[epsb])
    wr32 = P.sb("wr32", [128, 8, 256], F32)
    P.dma(P.sp, wr32.t[:, :, :], T["w_router"].rearrange("(kc p) n -> p kc n", p=128), writes=[wr32])
    _tt(P, P.dve, wr_lo.t[:, :, :], wr32.t[:, :, :], wr_hi.t[:, :, :], ALU.subtract, [wr32, wr_hi], [wr_lo])

    nt = NB // 128
    at = P.sb("at", [128, 4, NB], BF16)
    ht = P.sb("ht", [128, 4, NB], BF16)
    xt = [P.sb("xt%d" % i, [128, 1024], F32) for i in range(nt)]
    xT = P.sb("xT", [128, 8, NB], BF16)
    sga = [P.sb("sga%d" % i, [128, NB], F32) for i in range(2)]
    sgb = [P.sb("sgb%d" % i, [128, NB], F32) for i in range(2)]
    t1 = [P.sb("t1%d" % i, [128, NB], F32) for i in range(2)]
    t2 = [P.sb("t2%d" % i, [128, NB], F32) for i in range(2)]
    mT = P.sb("mT", [128, 8, NB], BF16)
    rr2 = [P.sb("rr%d" % i, [128, 1024], F32) for i in range(2)]
    mid = [P.sb("mid%d" % i, [128, 1024], F32) for i in range(2)]
    mhi = [P.sb("mhi%d" % i, [128, 1024], BF16) for i in range(2)]
    mlo2 = [P.sb("mlo%d" % i, [128, 1024], BF16) for i in range(2)]
    mTh = P.sb("mTh", [128, 8, NB], BF16)
    mTl = P.sb("mTl", [128, 8, NB], BF16)
    tmp2 = [{"st6": P.sb("st6%d" % i, [128, 2, 6], F32), "mv": P.sb("mv%d" % i, [128, 2], F32), "sc": P.sb("sc%d" % i, [128, 2], F32)}
            for i in range(2)]
    rt2 = [{n: P.sb("rt%d_%s" % (i, n), [128, 256], F32) for n in ("scores", "sel", "selm", "emask", "gate", "sv", "junk")} for i in range(2)]
    emb2 = [P.sb("emb%d" % i, [128, 256], BF16) for i in range(2)]
    m82 = [P.sb("m8%d" % i, [128, 8, 8], F32) for i in range(2)]
    gs2 = [P.sb("gs%d" % i, [128, 8], F32) for i in range(2)]
    g8s2 = [P.sb("g8s%d" % i, [128, 8], F32) for i in range(2)]
    gmask2 = [P.sb("gmask%d" % i, [128, 8], F32) for i in range(2)]
    gneg2 = [P.sb("gneg%d" % i, [128, 8], F32) for i in range(2)]
    s82 = [P.sb("s8%d" % i, [128, 8], F32) for i in range(2)]
    den2 = [P.sb("den%d" % i, [128, 1], F32) for i in range(2)]
    gsh = [P.sb("gsh%d" % i, [128, NB], F32) for i in range(2)]
    hsT = P.sb("hsT", [128, 2, NB], BF16)
    pre = [P.sb("pre%d" % i, [128, 1024], F32) for i in range(2)]
    scr = Buf(None, "scr")
    slots_all, gates_all = G["slots"], G["gates"]
    st = {"m": 0}

    for b0 in range(0, TT, NB):
        nb = min(NB, TT - b0)
        ntile = nb // 128
        P.dma(P.sp, at.t[:, :, 0:nb], T["attn_d"].rearrange("(c p) t -> p c t", p=128)[:, :, b0:b0 + nb], writes=[at])
        P.dma(P.sp, ht.t[:, :, 0:nb], T["h_d"].rearrange("(c p) t -> p c t", p=128)[:, :, b0:b0 + nb], writes=[ht])
        for t in range(ntile):
            xb = xt[t]
            r0 = b0 + t * 128
            src_x = T["xp"][r0:r0 + 128, :] if r0 < TP else T["xs"]
            P.dma(P.sp, xb.t[:, :], src_x, writes=[xb])
            for g in range(2):
                psb = P.ps()
                for j in range(4):
                    kc = g * 4 + j
                    P.op(P.pe, lambda e, psb=psb, j=j, kc=kc, xb=xb: e.transpose(
                        psb.t[:, j * 128:(j + 1) * 128], xb.t[:, kc * 128:(kc + 1) * 128], identf),
                        reads=[xb, cf], writes=[psb], sig=(j == 3))
                _copy(P, P.act if g == 0 else P.dve, xT.t[:, g * 4:(g + 1) * 4, t * 128:(t + 1) * 128],
                      psb.t[:, :].rearrange("p (a b) -> p a b", a=4), [psb], [xT])
        for oc in range(8):
            i2 = oc % 2
            pa = P.ps()
            for c in range(4):
                _mm(P, pa, pa.t[:, 0:nb], w_a.t[:, c, oc * 128:(oc + 1) * 128], at.t[:, c, 0:nb], c == 0, c == 3, [w_a, at])
            pb = P.ps()
            for c in range(4):
                _mm(P, pb, pb.t[:, 0:nb], w_b.t[:, c, oc * 128:(oc + 1) * 128], ht.t[:, c, 0:nb], c == 0, c == 3, [w_b, ht])
            pga = P.ps()
            for kc in range(8):
                _mm(P, pga, pga.t[:, 0:nb], w_g.t[:, kc, oc * 128:(oc + 1) * 128], xT.t[:, kc, 0:nb], kc == 0, kc == 7, [w_g, xT])
            pgb = P.ps()
            for kc in range(8):
                _mm(P, pgb, pgb.t[:, 0:nb], w_g.t[:, kc, 1024 + oc * 128:1024 + (oc + 1) * 128], xT.t[:, kc, 0:nb], kc == 0, kc == 7, [w_g, xT])
            _act(P, sga[i2].t[:, 0:nb], pga.t[:, 0:nb], AF.Sigmoid, [pga, pcol], [sga[i2]], bias=pcol.t[:, PC_BGA + oc:PC_BGA + oc + 1])
            _act(P, sgb[i2].t[:, 0:nb], pgb.t[:, 0:nb], AF.Sigmoid, [pgb, pcol], [sgb[i2]], bias=pcol.t[:, PC_BGB + oc:PC_BGB + oc + 1])
            _tt(P, P.dve, t1[i2].t[:, 0:nb], pa.t[:, 0:nb], sga[i2].t[:, 0:nb], ALU.mult, [pa, sga[i2]], [t1[i2]])
            _stt(P, t2[i2].t[:, 0:nb], pb.t[:, 0:nb], pcol.t[:, PC_BB + oc:PC_BB + oc + 1], sgb[i2].t[:, 0:nb], ALU.add, ALU.mult,
                 [pb, pcol, sgb[i2]], [t2[i2]])
            _tt(P, P.pool, mT.t[:, oc, 0:nb], t1[i2].t[:, 0:nb], t2[i2].t[:, 0:nb], ALU.add, [t1[i2], t2[i2]], [mT])
        for t in range(ntile):
            tg = (b0 // 128) + t
            tp = tg % 2
            rr, mlo, tmp, rt, emb, m8, gs, g8s, gmask, gneg, s8, den = (rr2[tp], mlo2[tp], tmp2[tp], rt2[tp], emb2[tp], m82[tp], gs2[tp],
                                                                      g8s2[tp], gmask2[tp], gneg2[tp], s82[tp], den2[tp])
            xb = xt[t]
            md = mid[st["m"] % 2]
            mh = mhi[st["m"] % 2]
            st["m"] += 1
            for half in range(2):
                psb = P.ps()
                for kc in range(8):
                    _mm(P, psb, psb.t[:, 0:512], mT.t[:, kc, t * 128:(t + 1) * 128], w_o.t[:, kc, half * 512:(half + 1) * 512],
                        kc == 0, kc == 7, [mT, w_o])
                _stt(P, rr.t[:, half * 512:(half + 1) * 512], xb.t[:, half * 512:(half + 1) * 512], DN_ALPHA, psb.t[:, 0:512],
                     ALU.mult, ALU.add, [xb, psb], [rr])
            def norm1(rstd, nmr, md=md, rr=rr, tmp=tmp):
                P.op(P.act, lambda e, md=md, rr=rr, nmr=nmr, rstd=rstd: e.activation(md.t[:, :], rr.t[:, :], AF.Identity, bias=nmr, scale=rstd), reads=[rr, tmp["sc"]], writes=[md])
            layer_norm_tile(P, rr, norm1, tmp, epsb.t[:, 0:1])
            _tt(P, P.dve, md.t[:, :], md.t[:, :], lng.t[:, :], ALU.mult, [md, lng], [md])
            _tt(P, P.pool, md.t[:, :], md.t[:, :], lnb.t[:, :], ALU.add, [md, lnb], [md])
            _copy(P, P.act, mh.t[:, :], md.t[:, :], [md], [mh])
            _tt(P, P.dve, mlo.t[:, :], md.t[:, :], mh.t[:, :], ALU.subtract, [md, mh], [mlo])
            if "mid_dbg" in T:
                P.dma(P.sp, T["mid_dbg"][tg * 128:(tg + 1) * 128, :], md.t[:, :], reads=[md], out_final=True)
            for srcb, dstb in ((mh, mTh), (mlo, mTl)):
                psb = P.ps()
                pbf = psb.t[:, :].bitcast(BF16)
                for kc in range(8):
                    P.op(P.pe, lambda e, kc=kc, pbf=pbf, srcb=srcb: e.transpose(pbf[:, kc * 128:(kc + 1) * 128], srcb.t[:, kc * 128:(kc + 1) * 128], identb),
                         reads=[srcb, cb], writes=[psb], sig=(kc == 7))
                _copy(P, P.act if srcb is mh else P.dve, dstb.t[:, :, t * 128:(t + 1) * 128], pbf.rearrange("p (c n) -> p c n", c=8), [psb], [dstb])
            psr = P.ps()
            k = 0
            for (a_, w_) in ((mTh, wr_hi), (mTh, wr_lo), (mTl, wr_hi)):
                for kc in range(8):
                    _mm(P, psr, psr.t[:, 0:256], a_.t[:, kc, t * 128:(t + 1) * 128], w_.t[:, kc, :], k == 0, k == 23, [a_, w_])
                    k += 1
            sc_, sel, selm, emask, gate, sv, junk = (rt[n] for n in ("scores", "sel", "selm", "emask", "gate", "sv", "junk"))
            _act(P, sc_.t[:, :], psr.t[:, 0:256], AF.Sigmoid, [psr], [sc_])
            _tt(P, P.dve, sel.t[:, :], sc_.t[:, :], brt.t[:, :], ALU.add, [sc_, brt], [sel])
            for g in range(8):
                P.op(P.dve, lambda e, g=g, m8=m8, sel=sel: e.max(m8.t[:, g, :], sel.t[:, g * 32:(g + 1) * 32]), reads=[sel], writes=[m8])
            _tt(P, P.dve, gs.t[:, :], m8.t[:, :, 0], m8.t[:, :, 1], ALU.add, [m8], [gs])
            P.op(P.dve, lambda e, g8s=g8s, gs=gs: e.max(g8s.t[:, :], gs.t[:, :]), reads=[gs], writes=[g8s])
            _ts(P, P.dve, gmask.t[:, :], gs.t[:, :], g8s.t[:, 3:4], None, ALU.is_ge, None, [gs, g8s], [gmask])
            _ts(P, P.dve, gneg.t[:, :], gmask.t[:, :], -1.0, 1e9, ALU.add, ALU.mult, [gmask], [gneg])
            for g in range(8):
                _ts(P, P.dve, selm.t[:, g * 32:(g + 1) * 32], sel.t[:, g * 32:(g + 1) * 32], gmask.t[:, g:g + 1], gneg.t[:, g:g + 1],
                    ALU.mult, ALU.add, [sel, gmask, gneg], [selm])
            P.op(P.dve, lambda e, m8=m8, selm=selm: e.max(m8.t[:, 0, :], selm.t[:, :]), reads=[selm], writes=[m8])
            _ts(P, P.dve, emask.t[:, :], selm.t[:, :], m8.t[:, 0, 7:8], None, ALU.is_ge, None, [selm, m8], [emask])
            _copy(P, P.pool, emb.t[:, :], emask.t[:, :], [emask], [emb])
            P.op(P.dve, lambda e, gate=gate, sc_=sc_, emask=emask, den=den: e.scalar_tensor_tensor(gate.t[:, :], sc_.t[:, :], 1.0, emask.t[:, :], ALU.mult, ALU.mult, accum_out=den.t[:, 0:1]),
                 reads=[sc_, emask], writes=[gate, den])
            P.op(P.dve, lambda e, den=den: e.reciprocal(den.t[:, 0:1], den.t[:, 0:1]), reads=[den], writes=[den])
            _ts(P, P.dve, gate.t[:, :], gate.t[:, :], den.t[:, 0:1], 2.5, ALU.mult, ALU.mult, [gate, den], [gate])
            pp = P.ps()
            _mm(P, pp, pp.t[:, 0:256], ustrict, emb.t[:, :], True, True, [cb, emb])
            pt_ = P.ps()
            _mm(P, pt_, pt_.t[:, 0:256], onesb.t[:, :], emb.t[:, :], True, True, [onesb, emb])
            _tt(P, P.dve, sv.t[:, :], pp.t[:, 0:256], cnt.t[:, :], ALU.add, [pp, cnt], [sv])
            _tt(P, P.pool, sv.t[:, :], sv.t[:, :], emask.t[:, :], ALU.mult, [sv, emask], [sv])
            _tt(P, P.dve, cnt.t[:, :], pt_.t[:, 0:256], cnt.t[:, :], ALU.add, [pt_, cnt], [cnt])
            P.op(P.dve, lambda e, s8=s8, sv=sv: e.max(s8.t[:, :], sv.t[:, :]), reads=[sv], writes=[s8])
            for k in range(8):
                P.op(P.dve, lambda e, k=k, tg=tg, junk=junk, sv=sv, s8=s8, gate=gate: e.scalar_tensor_tensor(junk.t[:, :], sv.t[:, :], s8.t[:, k:k + 1], gate.t[:, :], ALU.is_equal, ALU.mult,
                                                                     accum_out=gates_all.t[:, tg, k:k + 1]),
                     reads=[sv, s8, gate], writes=[junk, gates_all])
            _ts(P, P.dve, slots_all.t[:, tg, :], s8.t[:, :], -1.0, None, ALU.add, None, [s8], [slots_all])
            for k in range(8):
                P.dma(P.pool, None, None, reads=[slots_all, mh], writes=[scr],
                      fn=lambda h, k=k, tg=tg, mh=mh: h.indirect_dma_start(
                          out=T["xg_d"], out_offset=bass.IndirectOffsetOnAxis(ap=slots_all.t[:, tg, k:k + 1], axis=0),
                          in_=mh.t[:, :], in_offset=None))
        for j in range(2):
            pg = P.ps()
            for kc in range(8):
                _mm(P, pg, pg.t[:, 0:nb], w_s.t[:, kc, j * 128:(j + 1) * 128], mTh.t[:, kc, 0:nb], kc == 0, kc == 7, [w_s, mTh])
            pu = P.ps()
            for kc in range(8):
                _mm(P, pu, pu.t[:, 0:nb], w_s.t[:, kc, 256 + j * 128:256 + (j + 1) * 128], mTh.t[:, kc, 0:nb], kc == 0, kc == 7, [w_s, mTh])
            _act(P, gsh[j].t[:, 0:nb], pg.t[:, 0:nb], AF.Silu, [pg], [gsh[j]])
            _tt(P, P.dve, hsT.t[:, j, 0:nb], pu.t[:, 0:nb], gsh[j].t[:, 0:nb], ALU.mult, [pu, gsh[j]], [hsT])
        for t in range(ntile):
            tg = (b0 // 128) + t
            md = mid[(st["m"] - ntile + t) % 2]
            pr = pre[t % 2]
            for half in range(2):
                psb = P.ps()
                for j in range(2):
                    _mm(P, psb, psb.t[:, 0:512], hsT.t[:, j, t * 128:(t + 1) * 128], w_sd.t[:, j, half * 512:(half + 1) * 512], j == 0, j == 1,
                        [hsT, w_sd])
                _stt(P, pr.t[:, half * 512:(half + 1) * 512], md.t[:, half * 512:(half + 1) * 512], DN_ALPHA, psb.t[:, 0:512], ALU.mult, ALU.add,
                     [md, psb], [pr])
            P.dma(P.sp, T["pre_d"][tg * 128:(tg + 1) * 128, :], pr.t[:, :], reads=[pr], writes=[scr])
    P.pop_scope()


def pass_c(P, T, n_exp=NE):
    P.push_scope()
    NBLK = CAP // 128
    cb = P.sb("cb", [128, 128], BF16)
    P.dma(P.sp, cb.t[:, :], T["cb"][:, 0:128], writes=[cb])
    identb = cb.t[:, 0:128]
    NS = 4
    NW = 2
    sg_ = [P.sb("wsg%d" % i, [128, 8, DE], F32) for i in range(NS)]
    su_ = [P.sb("wsu%d" % i, [128, 8, DE], F32) for i in range(NS)]
    sd_ = [P.sb("wsd%d" % i, [128, 2, D], F32) for i in range(NS)]
    wg = [P.sb("wg%d" % i, [128, 8, DE], BF16) for i in range(NW)]
    wu = [P.sb("wu%d" % i, [128, 8, DE], BF16) for i in range(NW)]
    wd = [P.sb("wd%d" % i, [128, 2, D], BF16) for i in range(NW)]
    xg = [P.sb("xg%d" % i, [128, NBLK, D], BF16) for i in range(NS)]
    xgT = [P.sb("xgT%d" % i, [128, 8, CAP], BF16) for i in range(2)]
    gsb = [P.sb("gsb%d" % i, [128, 2, CAP], F32) for i in range(2)]
    hTe = [P.sb("hTe%d" % i, [128, 2, CAP], BF16) for i in range(2)]
    yb = [P.sb("yb%d" % i, [128, D], BF16) for i in range(3)]
    scr = Buf(None, "scr_c")

    def loads(e):
        s = e % NS
        P.dma(P.sp, sg_[s].t[:, :, :], T["w_eg"][e].rearrange("(p kc) n -> p kc n", kc=8), writes=[sg_[s]])
        P.dma(P.sp, su_[s].t[:, :, :], T["w_eu"][e].rearrange("(p kc) n -> p kc n", kc=8), writes=[su_[s]])
        P.dma(P.sp, sd_[s].t[:, :, :], T["w_ed"][e].rearrange("(p j) n -> p j n", j=2), writes=[sd_[s]])
        P.dma(P.sp, xg[e % NS].t[:, :, :], T["xg_d"][e * CAP:(e + 1) * CAP, :].rearrange("(b p) n -> p b n", p=128), writes=[xg[e % NS]])

    def casts(e):
        s, i = e % NS, e % NW
        _copy(P, P.act, wg[i].t[:, :, :], sg_[s].t[:, :, :], [sg_[s]], [wg[i]])
        _copy(P, P.dve, wu[i].t[:, :, :], su_[s].t[:, :, :], [su_[s]], [wu[i]])
        _copy(P, P.pool, wd[i].t[:, :, :], sd_[s].t[:, :, :], [sd_[s]], [wd[i]])

    for e0 in range(NS):
        loads(e0)
    casts(0)
    yi = 0
    for e in range(n_exp):
        i, i2 = e % NW, e % 2
        for b in range(NBLK):
            psb = P.ps()
            pbf = psb.t[:, :].bitcast(BF16)
            for kc in range(8):
                P.op(P.pe, lambda ee, kc=kc, pbf=pbf, b=b, e=e: ee.transpose(pbf[:, kc * 128:(kc + 1) * 128], xg[e % NS].t[:, b, :].rearrange("s (p k) -> s k p", k=8)[:, kc, :], identb),
                     reads=[xg[e % NS], cb], writes=[psb], sig=(kc == 7))
            _copy(P, P.act if b % 2 == 0 else P.dve, xgT[i2].t[:, :, b * 128:(b + 1) * 128], pbf.rearrange("p (c n) -> p c n", c=8), [psb], [xgT[i2]])
        if e + 1 < n_exp:
            casts(e + 1)
        for j in range(2):
            pg = P.ps()
            for kc in range(8):
                _mm(P, pg, pg.t[:, 0:CAP], wg[i].t[:, kc, :].rearrange("p (m j) -> p j m", j=2)[:, j, :], xgT[i2].t[:, kc, :], kc == 0, kc == 7, [wg[i], xgT[i2]])
            pu = P.ps()
            for kc in range(8):
                _mm(P, pu, pu.t[:, 0:CAP], wu[i].t[:, kc, :].rearrange("p (m j) -> p j m", j=2)[:, j, :], xgT[i2].t[:, kc, :], kc == 0, kc == 7, [wu[i], xgT[i2]])
            _act(P, gsb[i2].t[:, j, :], pg.t[:, 0:CAP], AF.Silu, [pg], [gsb[i2]])
            _tt(P, P.dve, hTe[i2].t[:, j, :], pu.t[:, 0:CAP], gsb[i2].t[:, j, :], ALU.mult, [pu, gsb[i2]], [hTe[i2]])
        if e + NS < n_exp:
            loads(e + NS)
        for b in range(NBLK):
            y_ = yb[yi % 3]
            yi += 1
            for half in range(2):
                psb = P.ps()
                for j in range(2):
                    _mm(P, psb, psb.t[:, 0:512], hTe[i2].t[:, j, b * 128:(b + 1) * 128], wd[i].t[:, j, half * 512:(half + 1) * 512], j == 0, j == 1,
                        [hTe[i2], wd[i]])
                _copy(P, P.act if half == 0 else P.dve, y_.t[:, half * 512:(half + 1) * 512], psb.t[:, 0:512], [psb], [y_])
            r0 = e * CAP + b * 128
            P.dma(P.sp, T["ys_d"][r0:r0 + 128, :], y_.t[:, :], reads=[y_], writes=[scr])
    P.pop_scope()


def pass_d(P, T, G):
    P.push_scope()
    lng = P.sb("lng2", [128, D], F32)
    lnb = P.sb("lnb2", [128, D], F32)
    epsb = P.sb("epsb", [128, 1], F32)
    cb = P.sb("cbd", [128, 128], BF16)
    P.dma(P.sp, cb.t[:, :], T["cb"][:, 0:128], writes=[cb])
    P.dma(P.sp, lng.t[:, :], T["ln2_g"].partition_broadcast(128), writes=[lng])
    P.dma(P.sp, lnb.t[:, :], T["ln2_b"].partition_broadcast(128), writes=[lnb])
    P.op(P.dve, lambda e: e.memset(epsb.t[:, :], LN_EPS), writes=[epsb])
    identb = cb.t[:, 0:128]
    yk = [P.sb("yk%d" % i, [128, D], BF16) for i in range(16)]
    dgs = [P.sb("dgd%d" % i, [128, 128], BF16) for i in range(16)]
    acc = [P.sb("acc%d" % i, [128, D], F32) for i in range(3)]
    yo = [P.sb("yo%d" % i, [128, D], F32) for i in range(2)]
    tmp2 = [{"st6": P.sb("st6d%d" % i, [128, 2, 6], F32), "mv": P.sb("mvd%d" % i, [128, 2], F32), "sc": P.sb("scd%d" % i, [128, 2], F32)}
            for i in range(2)]
    slots_all, gates_all = G["slots"], G["gates"]
    scr = Buf(None, "scr_d")
    for tg in range(NT):
        a = acc[tg % 3]
        o = yo[tg % 2]
        tmp = tmp2[tg % 2]
        P.dma(P.sp, a.t[:, :], T["pre_d"][tg * 128:(tg + 1) * 128, :], reads=[scr], writes=[a])
        ys_ = []
        for k in range(8):
            y_ = yk[(tg * 8 + k) % 16]
            ys_.append(y_)
            P.dma(P.pool, None, None, reads=[slots_all, scr], writes=[y_],
                  fn=lambda h, k=k, tg=tg, y_=y_: h.indirect_dma_start(
                      out=y_.t[:, :], out_offset=None, in_=T["ys_d"],
                      in_offset=bass.IndirectOffsetOnAxis(ap=slots_all.t[:, tg, k:k + 1], axis=0)))
        dg_ = []
        for k in range(8):
            dg = dgs[(tg * 8 + k) % 16]
            dg_.append(dg)
            P.op(P.act, lambda e, dg=dg, k=k, tg=tg: e.activation(dg.t[:, :], identb, AF.Identity, scale=gates_all.t[:, tg, k:k + 1]),
                 reads=[cb, gates_all], writes=[dg], c=0.4)
        for half in range(2):
            psb = P.ps()
            for k in range(8):
                _mm(P, psb, psb.t[:, 0:512], dg_[k].t[:, :], ys_[k].t[:, half * 512:(half + 1) * 512], k == 0, k == 7, [dg_[k], ys_[k]])
            _tt(P, P.dve, a.t[:, half * 512:(half + 1) * 512], psb.t[:, 0:512], a.t[:, half * 512:(half + 1) * 512], ALU.add, [psb, a], [a])
        def norm2(rstd, nmr, a=a, o=o, tmp=tmp):
            P.op(P.act, lambda e, a=a, o=o, nmr=nmr, rstd=rstd: e.activation(o.t[:, :], a.t[:, :], AF.Identity, bias=nmr, scale=rstd),
                 reads=[a, tmp["sc"]], writes=[o], c=1.3)
        layer_norm_tile(P, a, norm2, tmp, epsb.t[:, 0:1])
        _tt(P, P.dve, o.t[:, :], o.t[:, :], lng.t[:, :], ALU.mult, [o, lng], [o])
        _tt(P, P.dve, o.t[:, :], o.t[:, :], lnb.t[:, :], ALU.add, [o, lnb], [o])
        dst = T["yp"][tg * 128:(tg + 1) * 128, :] if tg * 128 < TP else T["ys"]
        P.dma(P.sp, dst, o.t[:, :], reads=[o], out_final=True)
    P.pop_scope()


IN_SPECS_A = [
    ("xp", [TP, D], F32), ("xs", [128, D], F32), ("ck", [NSUB_S, PAST, 512], F32), ("cv", [NSUB_S, PAST, 512], F32),
    ("clf", [NSUB_S, PAST, 8], F32), ("sconv", [NSUB_S, 30, 512], F32), ("w_in", [D, N_IN], F32), ("b_in", [1, N_IN], F32),
    ("pcol", [128, PC_N], F32), ("cf", [128, 384], F32), ("cb", [128, 384], BF16),
]
IN_SPECS_B = [
    ("w_a", [512, D], F32), ("w_b", [512, D], F32), ("w_out", [D, D], F32), ("ln1_g", [1, D], F32), ("ln1_b", [1, D], F32),
    ("w_router", [D, NE], F32), ("b_router", [1, NE], F32), ("w_sg", [D, DE], F32), ("w_su", [D, DE], F32), ("w_sd", [DE, D], F32),
    ("base1", [128, NE], F32), ("tokid", [128, NT], I32),
]
IN_SPECS_C = [
    ("w_eg", [NE, D, DE], F32), ("w_eu", [NE, D, DE], F32), ("w_ed", [NE, DE, D], F32), ("ln2_g", [1, D], F32), ("ln2_b", [1, D], F32),
]
OUT_SPECS = [
    ("yp", [TP, D]), ("ys", [128, D]), ("kp", [TP, 512]), ("vp", [TP, 512]), ("lfp", [TP, 8]), ("cvp", [NSEQ_P, 30, 512]),
    ("ks", [128, 512]), ("vs", [128, 512]), ("lfs", [128, 8]), ("cvs", [NSUB_S, 30, 512]),
]


def build(stage="A", NB=512, NBB=256, debug=False):
    nc = bass.Bass("TRN2", target_bir_lowering=False)
    T = {}
    specs = list(IN_SPECS_A)
    if stage >= "B":
        specs += IN_SPECS_B
    if stage >= "C":
        specs += IN_SPECS_C
    for name, shape, dt in specs:
        T[name] = nc.dram_tensor(name, shape, dt, kind="ExternalInput").ap()
    for name, shape in OUT_SPECS:
        T[name] = nc.dram_tensor(name, shape, F32, kind="ExternalOutput").ap()
    dk = "ExternalOutput" if debug else "Internal"
    T["attn_d"] = nc.dram_tensor("attn_d", [512, TT], BF16, kind=dk).ap()
    T["h_d"] = nc.dram_tensor("h_d", [512, TT], BF16, kind=dk).ap()
    T["c3_d"] = nc.dram_tensor("c3_d", [8, 3, 512], BF16, kind="Internal").ap()
    P = Prog(nc)
    G = {}
    if stage >= "B":
        T["xg_d"] = nc.dram_tensor("xg_d", [NSLOT, D], BF16, kind="Internal").ap()
        T["pre_d"] = nc.dram_tensor("pre_d", [TT, D], F32, kind="Internal").ap()
        if debug:
            T["mid_dbg"] = nc.dram_tensor("mid_dbg", [TT, D], F32, kind="ExternalOutput").ap()
            T["slots_dbg"] = nc.dram_tensor("slots_dbg", [128, NT * 8], I32, kind="ExternalOutput").ap()
            T["gates_dbg"] = nc.dram_tensor("gates_dbg", [128, NT * 8], F32, kind="ExternalOutput").ap()
        G["slots"] = P.sb("slots_all", [128, NT, 8], I32)
        G["gates"] = P.sb("gates_all", [128, NT, 8], F32)
    pass_a(P, T, NB)
    if stage >= "B":
        pass_b(P, T, G, NBB)
        if stage >= "C":
            T["ys_d"] = nc.dram_tensor("ys_d", [NSLOT, D], BF16, kind="Internal").ap()
            pass_c(P, T)
            pass_d(P, T, G)
        if debug:
            P.dma(P.sp, T["slots_dbg"], G["slots"].t[:, :, :].rearrange("p a b -> p (a b)"), reads=[G["slots"]], out_final=True)
            P.dma(P.sp, T["gates_dbg"], G["gates"].t[:, :, :].rearrange("p a b -> p (a b)"), reads=[G["gates"]], out_final=True)
    P.finish()
    print('[build] ops', P.n_ops, 'sim_us %.1f' % getattr(P, 'sim_time', 0.0), flush=True)
    return nc, [s[0] for s in specs]


def host_consts(inp):
    b_in = np.asarray(inp["b_in"])[0]
    pcol = np.zeros((128, PC_N), np.float32)
    col = lambda v, n: np.ascontiguousarray(np.asarray(v).reshape(n, 128).T)
    pcol[:, PC_BQ:PC_BQ + 4] = col(b_in[OFF_Q:OFF_K], 4)
    pcol[:, PC_BGLU:PC_BGLU + 8] = col(b_in[OFF_GLU:OFF_GA], 8)
    pcol[:, PC_BGA:PC_BGA + 8] = col(b_in[OFF_GA:OFF_GB], 8)
    pcol[:, PC_BGB:PC_BGB + 8] = col(b_in[OFF_GB:N_IN], 8)
    pcol[:, PC_BB:PC_BB + 8] = col(inp["b_b"][0], 8)
    pcol[:, PC_CB:PC_CB + 4] = col(inp["conv_b"][0], 4)
    pcol[:, PC_CG:PC_CG + 4] = col(inp["conv_ln_g"][0], 4)
    pcol[:, PC_CBE:PC_CBE + 4] = col(inp["conv_ln_b"][0], 4)
    cw = np.asarray(inp["conv_w"])[0]
    pcol[:, PC_CW:PC_CW + 124] = cw.T.reshape(4, 128, KW).transpose(1, 0, 2).reshape(128, 4 * KW)
    pcol[0:8, PC_BF] = b_in[OFF_F:OFF_GLU]
    pcol[:, PC_BK:PC_BK + 4] = col(b_in[OFF_K:OFF_V], 4)
    cf = np.concatenate([np.eye(128, dtype=np.float32), np.tril(np.ones((128, 128), np.float32), -1),
                         np.ones((128, 128), np.float32)], axis=1)
    cb = np.concatenate([np.eye(128, dtype=np.float32), np.triu(np.ones((128, 128), np.float32)),
                         np.triu(np.ones((128, 128), np.float32), 1)], axis=1).astype(ml_dtypes.bfloat16)
    base1 = np.ascontiguousarray(np.broadcast_to((np.arange(NE, dtype=np.float32) * CAP + 1.0)[None, :], (128, NE)))
    tokid = np.ascontiguousarray((np.arange(NT, dtype=np.int32)[None, :] * 128 + np.arange(128, dtype=np.int32)[:, None]).astype(np.int32))
    return pcol, cf, cb, base1, tokid


def core_inputs(inp, c, consts):
    pcol, cf, cb, base1, tokid = consts
    m = {
        "xp": np.asarray(inp["x_prompt"])[2 * c:2 * c + 2].reshape(TP, D),
        "xs": np.asarray(inp["x_sample"])[4 * c:4 * c + 4].reshape(128, D),
        "ck": np.asarray(inp["cache_k"])[0, 4 * c:4 * c + 4].reshape(NSUB_S, PAST, 512),
        "cv": np.asarray(inp["cache_v"])[0, 4 * c:4 * c + 4].reshape(NSUB_S, PAST, 512),
        "clf": np.asarray(inp["cache_logf"])[0, 4 * c:4 * c + 4],
        "sconv": np.asarray(inp["state_conv"])[0, 4 * c:4 * c + 4],
        "w_in": np.asarray(inp["w_in"])[0], "b_in": np.asarray(inp["b_in"]),
        "pcol": pcol, "cf": cf, "cb": cb, "base1": base1, "tokid": tokid,
        "w_a": np.asarray(inp["w_a"])[0], "w_b": np.asarray(inp["w_b"])[0], "w_out": np.asarray(inp["w_out"])[0],
        "ln1_g": np.asarray(inp["ln1_g"]), "ln1_b": np.asarray(inp["ln1_b"]),
        "w_router": np.asarray(inp["w_router"])[0], "b_router": np.asarray(inp["b_router"]),
        "w_sg": np.asarray(inp["w_s_gate"])[0], "w_su": np.asarray(inp["w_s_up"])[0], "w_sd": np.asarray(inp["w_s_down"])[0],
        "w_eg": np.asarray(inp["w_e_gate"])[0], "w_eu": np.asarray(inp["w_e_up"])[0], "w_ed": np.asarray(inp["w_e_down"])[0],
        "ln2_g": np.asarray(inp["ln2_g"]), "ln2_b": np.asarray(inp["ln2_b"]),
    }
    return m


def assemble(results):
    cat = lambda k: np.concatenate([r[k] for r in results], axis=0)
    yp = cat("yp").reshape(16, SEQ, D)
    ys = cat("ys").reshape(32, TS, D)
    kp = cat("kp").reshape(1, 16, SEQ, H, DH)
    vp = cat("vp").reshape(1, 16, SEQ, H, DH)
    lfp = cat("lfp").reshape(1, 16, SEQ, H)
    cvp = cat("cvp").reshape(1, 16, 30, CC)
    ks = cat("ks").reshape(1, 32, TS, H, DH)
    vs = cat("vs").reshape(1, 32, TS, H, DH)
    lfs = cat("lfs").reshape(1, 32, TS, H)
    cvs = cat("cvs").reshape(1, 32, 30, CC)
    return (yp, ys, kp, vp, lfp, cvp, ks, vs, lfs, cvs)


def kernel(**inputs):
    nc, names = build("C")
    consts = host_consts(inputs)
    in_maps = []
    for c in range(NCORES):
        m = core_inputs(inputs, c, consts)
        in_maps.append({k: (m[k] if m[k].flags["C_CONTIGUOUS"] else np.ascontiguousarray(m[k])) for k in names})
    res = run_bass_kernel_spmd(nc, in_maps, core_ids=list(range(NCORES)))
    return assemble(res.results)
```

```python
from contextlib import ExitStack

import numpy as np
import ml_dtypes
import concourse.bass as bass
import concourse.mybir as mybir
from concourse.bass_utils import run_bass_kernel_spmd

F32 = mybir.dt.float32
BF16 = mybir.dt.bfloat16
I32 = mybir.dt.int32
U32 = mybir.dt.uint32
AF = mybir.ActivationFunctionType
ALU = mybir.AluOpType


class Tr:
    __slots__ = ("sem", "count", "name")

    def __init__(self, sem, name):
        self.sem = sem
        self.count = 0
        self.name = name


class Buf:
    __slots__ = ("t", "w", "r", "name")

    def __init__(self, t, name=""):
        self.t = t
        self.w = None
        self.r = {}
        self.name = name


class Eng:
    def __init__(self, name, tr):
        self.name = name
        self.tr = tr
        self.ops = []
        self.waited = {}
        self.dma_trs = []
        self.dma_i = 0
        self.deferred = []


class Prog:
    def __init__(self, nc, n_dma=(28, 28, 6)):
        self.nc = nc
        self.stack = ExitStack()
        self.scopes = []
        mk = lambda n: Tr(self.stack.enter_context(nc.semaphore(n)), n)
        self.pe = Eng("pe", mk("s_pe"))
        self.act = Eng("act", mk("s_act"))
        self.dve = Eng("dve", mk("s_dve"))
        self.pool = Eng("pool", mk("s_pool"))
        self.sp = Eng("sp", mk("s_sp"))
        self.engs = [self.pe, self.act, self.dve, self.pool, self.sp]
        for e, n in zip((self.sp, self.pool, self.act), n_dma):
            e.dma_trs = [mk("d_%s%d" % (e.name, i)) for i in range(n)]
        self.banks = [Buf(self.stack.enter_context(nc.psum_tensor("psb%d" % i, [128, 512], F32)), "ps%d" % i)
                      for i in range(8)]
        self.bank_i = 0
        self.final = []
        self.n_ops = 0
        self.sched = True
        self.pending = []

    def sb(self, name, shape, dtype):
        st = self.scopes[-1] if self.scopes else self.stack
        self.n_sb = getattr(self, "n_sb", 0) + 1
        return Buf(st.enter_context(self.nc.sbuf_tensor("sb%d_%s" % (self.n_sb, name), list(shape), dtype)), name)

    def push_scope(self):
        self.scopes.append(ExitStack())

    def pop_scope(self):
        self.barrier()
        self.flush()
        self.scopes.pop().close()

    def ps(self, lo=0, hi=8):
        n = hi - lo
        b = self.banks[lo + (self.bank_i % n)]
        self.bank_i += 1
        return b

    def _deps(self, eng, reads, writes, extra=()):
        deps = {}

        def add(tok):
            if tok is None:
                return
            tr, v = tok
            if deps.get(tr, 0) < v:
                deps[tr] = v

        for b in reads:
            add(b.w)
        for b in writes:
            add(b.w)
            for tr, v in b.r.items():
                add((tr, v))
        for tok in extra:
            add(tok)
        waits = []
        for tr, v in deps.items():
            if eng is self.pe and tr is eng.tr:
                continue
            if eng.waited.get(tr, 0) >= v:
                continue
            eng.waited[tr] = v
            waits.append((tr, v))
        return waits

    @staticmethod
    def _mark(tok, reads, writes):
        tr, v = tok
        for b in reads:
            if b.r.get(tr, 0) < v:
                b.r[tr] = v
        for b in writes:
            b.w = tok
            b.r = {}

    def op(self, eng, fn, reads=(), writes=(), sig=True, c=0.3):
        if eng is self.pe and c == 0.3:
            c = 0.07
        if self.sched:
            self.pending.append(("op", eng, (fn, tuple(reads), tuple(writes), sig), tuple(reads), tuple(writes), c, 0))
            return None
        return self._op_now(eng, fn, reads, writes, sig)

    def dma(self, eng, out, in_, reads=(), writes=(), out_final=False, slow=False, fn=None, nbytes=None, **kw):
        if nbytes is None:
            try:
                ap = out if out is not None else None
                nbytes = 1
                for s_ in ap.shape:
                    nbytes *= int(s_)
                nbytes *= 2 if ap.dtype == BF16 else 4
            except Exception:
                nbytes = 262144
        if self.sched:
            self.pending.append(("dma", eng, (out, in_, tuple(reads), tuple(writes), out_final, slow, fn, kw), tuple(reads), tuple(writes),
                                 1.1 if fn is not None else 0.08, nbytes))
            return None
        return self._dma_now(eng, out, in_, reads, writes, out_final, slow, fn, **kw)

    def _op_now(self, eng, fn, reads=(), writes=(), sig=True):
        sig = True
        waits = self._deps(eng, reads, writes)
        tr = eng.tr
        if sig:
            tr.count += 1
            tok = (tr, tr.count)
        else:
            tok = None

        def run(h, waits=waits, fn=fn, sig=sig, tr=tr):
            for tr2, v in waits:
                h.wait_ge(tr2.sem, v)
            ins = fn(h)
            if sig:
                ins.then_inc(tr.sem, 1)

        eng.ops.append(run)
        self.n_ops += 1
        if tok is None:
            eng.deferred.append((reads, writes))
        else:
            for r_, w_ in eng.deferred:
                self._mark(tok, r_, w_)
            eng.deferred = []
            self._mark(tok, reads, writes)
        return tok

    def _dma_now(self, eng, out, in_, reads=(), writes=(), out_final=False, slow=False, fn=None, **kw):
        tr = eng.dma_trs[eng.dma_i % len(eng.dma_trs)]
        eng.dma_i += 1
        extra = [(tr, tr.count)] if tr.count else []
        waits = self._deps(eng, reads, writes, extra)
        tr.count += 16
        tok = (tr, tr.count)
        if slow:
            kw["allow_slow_non_contiguous"] = True

        def run(h, waits=waits, tr=tr, out=out, in_=in_, kw=kw, fn=fn):
            for tr2, v in waits:
                h.wait_ge(tr2.sem, v)
            if fn is not None:
                ins = fn(h)
            else:
                ins = h.dma_start(out=out, in_=in_, **kw)
            ins.then_inc(tr.sem, 16)

        eng.ops.append(run)
        self.n_ops += 1
        self._mark(tok, reads, writes)
        if out_final:
            self.final.append(tok)
        return tok

    def drain(self):
        pend = self.pending
        self.pending = []
        n = len(pend)
        if n == 0:
            return
        import heapq
        lastw, readers = {}, {}
        close = list(range(n))
        nxt = {}
        for i in range(n - 1, -1, -1):
            kind, eng = pend[i][0], pend[i][1]
            pass
        deps = [None] * n
        succ = [[] for _ in range(n)]
        indeg = [0] * n
        for i, (_, eng, _, reads, writes, _, _) in enumerate(pend):
            d = set()
            for b in reads:
                w = lastw.get(id(b))
                if w is not None:
                    d.add(w)
            for b in writes:
                w = lastw.get(id(b))
                if w is not None:
                    d.add(w)
                for r_ in readers.get(id(b), ()):
                    d.add(r_)
            d.discard(i)
            if not (pend[i][0] == "op" and eng is self.pe):
                d = {close[w] for w in d}
            else:
                d = {(w if pend[w][1] is self.pe else close[w]) for w in d}
            d.discard(i)
            deps[i] = d
            for j in d:
                succ[j].append(i)
            indeg[i] = len(d)
            for b in reads:
                readers.setdefault(id(b), []).append(i)
            for b in writes:
                lastw[id(b)] = i
                readers[id(b)] = []
        fin = [0.0] * n
        eng_free = {id(e): 0.0 for e in self.engs}
        ready = {id(e): [] for e in self.engs}
        for i in range(n):
            if indeg[i] == 0:
                heapq.heappush(ready[id(pend[i][1])], (0.0, i))
        dma_free = 0.0
        order = []
        LAT = 0.15
        WIN = 6000
        done = 0
        lo = 0
        scheduled = [False] * n
        while done < n:
            best = None
            for e in self.engs:
                h = ready[id(e)]
                if not h:
                    continue
                cand = None
                tmp_ = []
                k = 0
                while h and k < 8:
                    rt, i = heapq.heappop(h)
                    tmp_.append((rt, i))
                    k += 1
                    if i - lo > WIN:
                        continue
                    st_ = max(rt, eng_free[id(e)])
                    key = (st_, i)
                    if cand is None or key < cand[0]:
                        cand = (key, rt, i)
                for x in tmp_:
                    heapq.heappush(h, x)
                if cand is not None and (best is None or cand[0] < best[0][0]):
                    best = (cand, e)
            if best is None:
                cands = [(h[0][1], e) for e in self.engs for h in [ready[id(e)]] if h]
                i_min = None
                for e in self.engs:
                    for rt, i in ready[id(e)]:
                        if i_min is None or i < i_min[0]:
                            i_min = (i, rt, e)
                i, rt, e = i_min
                best = ((((max(rt, eng_free[id(e)])), i), rt, i), e)
            (key, rt, i), e = best
            h = ready[id(e)]
            h.remove((rt, i))
            heapq.heapify(h)
            kind, _, _, _, _, cost, nbytes = pend[i]
            st_ = key[0]
            if kind == "dma":
                eng_free[id(e)] = st_ + cost
                t0_ = max(st_ + cost, dma_free)
                dma_free = t0_ + nbytes / 400000.0
                fin[i] = dma_free + 1.8
            else:
                eng_free[id(e)] = st_ + cost
                fin[i] = st_ + cost
            scheduled[i] = True
            order.append(i)
            done += 1
            while lo < n and scheduled[lo]:
                lo += 1
            for j in succ[i]:
                indeg[j] -= 1
                if indeg[j] == 0:
                    rt_j = 0.0
                    for d_ in deps[j]:
                        l_ = 0.0 if pend[d_][1] is pend[j][1] and pend[j][1] is self.pe else LAT
                        if fin[d_] + l_ > rt_j:
                            rt_j = fin[d_] + l_
                    heapq.heappush(ready[id(pend[j][1])], (rt_j, j))
        self.sim_time = getattr(self, "sim_time", 0.0) + max(fin)
        for i in order:
            kind, eng, args, _, _, _, _ = pend[i]
            if kind == "op":
                fn, reads, writes, sig = args
                self._op_now(eng, fn, reads, writes, sig)
            else:
                out, in_, reads, writes, out_final, slow, fn, kw = args
                self._dma_now(eng, out, in_, reads, writes, out_final, slow, fn, **kw)

    def barrier(self):
        self.drain()
        toks = []
        for e in self.engs:
            if e.tr.count:
                toks.append((e.tr, e.tr.count))
            for tr in e.dma_trs:
                if tr.count:
                    toks.append((tr, tr.count))
        for e in self.engs:
            waits = []
            for tr, v in toks:
                if tr is e.tr:
                    continue
                if e.waited.get(tr, 0) >= v:
                    continue
                e.waited[tr] = v
                waits.append((tr, v))
            if waits:
                def run(h, waits=waits):
                    for tr2, v in waits:
                        h.wait_ge(tr2.sem, v)
                e.ops.append(run)

    def flush(self):
        nc = self.nc
        pend = {e.name: e.ops for e in self.engs}
        for e in self.engs:
            e.ops = []
        with nc.Block() as block:
            @block.tensor
            def _(h):
                for f in pend["pe"]:
                    f(h)

            @block.scalar
            def _(h):
                for f in pend["act"]:
                    f(h)

            @block.vector
            def _(h):
                for f in pend["dve"]:
                    f(h)

            @block.gpsimd
            def _(h):
                for f in pend["pool"]:
                    f(h)

            @block.sync
            def _(h):
                for f in pend["sp"]:
                    f(h)

    def finish(self):
        self.drain()
        waits = []
        for tr, v in self.final:
            if self.sp.waited.get(tr, 0) < v:
                self.sp.waited[tr] = v
                waits.append((tr, v))

        def run(h, waits=waits):
            for tr2, v in waits:
                h.wait_ge(tr2.sem, v)

        self.sp.ops.append(run)
        self.barrier()
        self.flush()
        while self.scopes:
            self.scopes.pop().close()
        self.stack.close()


D = 1024
NCORES = 8
SEQ = 2048
NSEQ_P = 2
NSUB_S = 4
TS = 32
PAST = 1024
H = 8
DH = 64
CC = 512
KW = 31
NE = 256
DE = 256
OFF_Q, OFF_K, OFF_V, OFF_F, OFF_GLU, OFF_GA, OFF_GB, N_IN = 0, 512, 1024, 1536, 1544, 2568, 3592, 4616
NA = OFF_GA
TP = NSEQ_P * SEQ
TT = TP + 128
DN_ALPHA = 2.0 ** 0.25
LN_EPS = 1e-5

PC_BQ, PC_BGLU, PC_BGA, PC_BGB, PC_BB, PC_CB, PC_CG, PC_CBE, PC_CW, PC_BF, PC_BK, PC_N = 0, 4, 12, 20, 28, 36, 40, 44, 48, 172, 173, 177


def _n(ap):
    n = 1
    for s_ in ap.shape[1:]:
        n *= int(s_)
    return n


def _mm(P, psb, out_ap, lhsT, rhs, start, stop, reads, sig=None):
    c = max(_n(rhs), 64) / 2400.0 * (4.0 if lhsT.dtype == F32 else 1.0) + 0.01
    P.op(P.pe, lambda e: e.matmul(out_ap, lhsT, rhs, start=start, stop=stop), reads=reads, writes=[psb],
         sig=stop if sig is None else sig, c=c)


def _act(P, out_ap, in_ap, func, reads, writes, bias=None, scale=1.0):
    c = _n(out_ap) / 1050.0 + 0.27
    if bias is None:
        P.op(P.act, lambda e: e.activation(out_ap, in_ap, func, scale=scale), reads=reads, writes=writes, c=c)
    else:
        P.op(P.act, lambda e: e.activation(out_ap, in_ap, func, bias=bias, scale=scale), reads=reads, writes=writes, c=c)


def _tt(P, eng, out_ap, a, b, op, reads, writes):
    c = _n(out_ap) / (950.0 if eng is P.dve else 440.0) + (0.17 if eng is P.dve else 0.15)
    P.op(eng, lambda e: e.tensor_tensor(out_ap, a, b, op), reads=reads, writes=writes, c=c)


def _stt(P, out_ap, in0, scalar, in1, op0, op1, reads, writes):
    P.op(P.dve, lambda e: e.scalar_tensor_tensor(out_ap, in0, scalar, in1, op0, op1), reads=reads, writes=writes, c=_n(out_ap) / 960.0 + 0.12)


def _ts(P, eng, out_ap, in0, s1, s2, op0, op1, reads, writes):
    c = _n(out_ap) / 960.0 + 0.12
    if s2 is None:
        P.op(eng, lambda e: e.tensor_scalar(out_ap, in0, s1, None, op0), reads=reads, writes=writes, c=c)
    else:
        P.op(eng, lambda e: e.tensor_scalar(out_ap, in0, s1, s2, op0, op1), reads=reads, writes=writes, c=c)


def _copy(P, eng, out_ap, in_ap, reads, writes):
    if eng is P.act:
        P.op(eng, lambda e: e.copy(out_ap, in_ap), reads=reads, writes=writes, c=_n(out_ap) / 1050.0 + 0.27)
    else:
        P.op(eng, lambda e: e.tensor_copy(out_ap, in_ap), reads=reads, writes=writes,
             c=_n(out_ap) / (960.0 if eng is P.dve else 280.0) + (0.12 if eng is P.dve else 0.3))


def load_w_cast(P, dst, src_ap, kchunks, ncols, col0=0):
    v = src_ap.rearrange("(kc p) n -> p kc n", p=128)
    c = 0
    while c < ncols:
        n = min(2048, ncols - c)
        P.dma(P.pool, dst.t[:, :, c:c + n], v[:, :, col0 + c:col0 + c + n], reads=[], writes=[dst])
        c += n


def pass_a(P, T, NB):
    nc = P.nc
    P.push_scope()
    cf = P.sb("cf", [128, 384], F32)
    cb = P.sb("cb", [128, 256], BF16)
    pcol = P.sb("pcol", [128, PC_N], F32)
    w_in = P.sb("w_in_a", [128, 8, NA], BF16)
    bkv = P.sb("bkv", [128, 1024], F32)
    bq8 = P.sb("bq8", [128, 4], F32)
    nbf = P.sb("nbf", [8, 1], F32)
    ones8 = P.sb("ones8", [8, 512], F32)
    onesb = P.sb("onesb", [128, 128], BF16)
    epsb = P.sb("epsb", [128, 1], F32)
    oneb = P.sb("oneb", [128, 1], F32)
    scr = Buf(None, "scratch")
    c3d = Buf(None, "c3d")
    P.dma(P.sp, cf.t[:, :], T["cf"], writes=[cf])
    P.dma(P.sp, cb.t[:, :], T["cb"][:, 0:256], writes=[cb])
    P.dma(P.sp, pcol.t[:, :], T["pcol"], writes=[pcol])
    P.dma(P.sp, bkv.t[:, :], T["b_in"][:, OFF_K:OFF_F].partition_broadcast(128), writes=[bkv])
    load_w_cast(P, w_in, T["w_in"], 8, NA)
    P.op(P.act, lambda e: e.mul(bq8.t[:, :], pcol.t[:, PC_BQ:PC_BQ + 4], 0.125), reads=[pcol], writes=[bq8])
    P.op(P.act, lambda e: e.mul(nbf.t[:, :], pcol.t[0:8, PC_BF:PC_BF + 1], -1.0), reads=[pcol], writes=[nbf])
    P.op(P.dve, lambda e: e.memset(ones8.t[:, :], 1.0), writes=[ones8])
    P.op(P.dve, lambda e: e.memset(onesb.t[:, :], 1.0), writes=[onesb])
    P.op(P.dve, lambda e: e.memset(epsb.t[:, :], LN_EPS), writes=[epsb])
    P.op(P.dve, lambda e: e.memset(oneb.t[:, :], 1.0), writes=[oneb])
    identf = cf.t[:, 0:128]
    lstrict = cf.t[:, 128:256]
    onesf = cf.t[:, 256:384]
    identb = cb.t[:, 0:128]
    tri = cb.t[:, 128:256]

    kT = P.sb("kT", [128, 4, SEQ], BF16)
    vsb = P.sb("vsb", [128, SEQ // 128, 512], BF16)
    negc = P.sb("negc", [128, SEQ // 128, 8], F32)
    xt = [P.sb("xt%d" % i, [128, 1024], F32) for i in range(2)]
    xT = P.sb("xT", [128, 8, NB], BF16)
    qz = P.sb("qz", [128, 8, NB], BF16)
    c3pad = P.sb("c3pad", [128, 8, NB], BF16)
    uT = P.sb("uT", [128, 4, 30 + NB], F32)
    sg = [P.sb("sg%d" % i, [128, NB], F32) for i in range(2)]
    kvf = [P.sb("kvf%d" % i, [128, 1024], F32) for i in range(2)]
    kbf = [P.sb("kbf%d" % i, [128, 512], BF16) for i in range(2)]
    lfneg = P.sb("lfneg", [8, NB], F32)
    cblk = P.sb("cblk", [8, NB], F32)
    carry = P.sb("carry", [8, 1], F32)
    c3p = P.sb("c3p", [8, 3, NB], BF16)
    ctmp = P.sb("ctmp", [8, NB], F32)
    et = ctmp
    lftok = P.sb("lftok", [128, NB // 128, 8], F32)
    pt = [P.sb("pt%d" % i, [128, NB], BF16) for i in range(3)]
    attnT = P.sb("attnT", [64, 8, NB], BF16)
    acc = [P.sb("cacc%d" % i, [128, NB], F32) for i in range(2)]
    rden = acc
    hc = P.sb("hc", [128, 4, NB], F32)
    hsq = P.sb("hsq", [128, NB], F32)
    mean = P.sb("mean", [128, NB], F32)
    rstd = P.sb("rstd", [128, NB], F32)
    hn = sg
    hT = P.sb("hT", [128, 4, NB], BF16)
    ubf = P.sb("ubf", [128, 4, 30 + NB], BF16)
    diag = [P.sb("diag%d" % i, [128, 128], BF16) for i in range(6)]
    cvo = P.sb("cvo", [32, 512], F32)
    P.op(P.pool, lambda e: e.memset(qz.t[:, :, :], 0.0), writes=[qz])
    P.op(P.pool, lambda e: e.memset(c3pad.t[:, :, :], 0.0), writes=[c3pad])

    st = {"pt": 0, "x": 0, "kv": 0, "dg": 0}

    def project_feature(col0, nfeat, nb, evac):
        psb = P.ps()
        for kc in range(8):
            _mm(P, psb, psb.t[0:nfeat, 0:nb], w_in.t[:, kc, col0:col0 + nfeat], xT.t[:, kc, 0:nb], kc == 0, kc == 7,
                [w_in, xT])
        evac(psb)

    def attention(h, qc0, nq, ktiles):
        ob = P.banks[4 + (h % 2)]
        db = P.banks[6 + (h % 2)]
        n = len(ktiles)
        for i, kt in enumerate(ktiles):
            nk, q0 = kt["nk"], kt["q0"]
            w = nq - q0
            sb_ = P.ps(0, 4)
            _mm(P, sb_, sb_.t[0:nk, 0:w], kt["kT"], qz.t[:, h, qc0 + q0:qc0 + nq], True, False, kt["reads"] + [qz])
            _mm(P, sb_, sb_.t[0:nk, 0:w], onesb.t[:, 0:nk], c3pad.t[:, h, qc0 + q0:qc0 + nq], False, True,
                [onesb, c3pad])
            p = pt[st["pt"] % 3]
            st["pt"] += 1
            _act(P, p.t[0:nk, 0:w], sb_.t[0:nk, 0:w], AF.Exp, kt["reads"] + [sb_], [p], bias=kt["bias"])
            if kt["tri"]:
                tw = min(nk, w)
                _tt(P, P.pool, p.t[0:nk, 0:tw], p.t[0:nk, 0:tw], tri[0:nk, 0:tw], ALU.mult, [p, cb], [p])
            _mm(P, ob, ob.t[0:64, q0:nq], kt["v"], p.t[0:nk, 0:w], i == 0, i == n - 1, kt["reads"] + [p])
            _mm(P, db, db.t[0:64, q0:nq], onesb.t[0:nk, 0:64], p.t[0:nk, 0:w], i == 0, i == n - 1, [onesb, p])
        rd = rden[h % 2]
        _act(P, rd.t[0:64, 0:nq], db.t[0:64, 0:nq], AF.Ln, [db], [rd])
        _act(P, rd.t[0:64, 0:nq], rd.t[0:64, 0:nq], AF.Exp, [rd], [rd], scale=-1.0)
        _tt(P, P.dve, attnT.t[:, h, qc0:qc0 + nq], ob.t[0:64, 0:nq], rd.t[0:64, 0:nq], ALU.mult, [ob, rd], [attnT])


    def conv_ln(nb, uview, accview, c_list=range(4)):
        ps_s = P.ps()
        ps_q = P.ps()
        for c in range(4):
            _copy(P, P.pool, ubf.t[:, c, :], uT.t[:, c, :], [uT], [ubf])
        for c in range(4):
            psc = P.ps()
            for j in range(KW):
                dg = diag[st["dg"] % len(diag)]
                st["dg"] += 1
                col = pcol.t[:, PC_CW + c * KW + j:PC_CW + c * KW + j + 1]
                P.op(P.act, lambda e, dg=dg, col=col: e.activation(dg.t[:, :], identb, AF.Identity, scale=col), reads=[cb, pcol], writes=[dg], c=0.32)
                _mm(P, psc, accview(psc), dg.t[:, :], uview(c, j), j == 0, j == KW - 1, [dg, ubf], sig=True)
            _act(P, hc.t[:, c, 0:nb], psc.t[:, 0:nb], AF.Identity, [psc, pcol], [hc], bias=pcol.t[:, PC_CB + c:PC_CB + c + 1])
            P.op(P.act, lambda e, c=c: e.activation(hsq.t[:, 0:nb], hc.t[:, c, 0:nb], AF.Square), reads=[hc], writes=[hsq], c=nb / 1400.0 + 0.22)
            _mm(P, ps_s, ps_s.t[:, 0:nb], onesf, hc.t[:, c, 0:nb], c == 0, c == 3, [cf, hc], sig=True)
            _mm(P, ps_q, ps_q.t[:, 0:nb], onesf, hsq.t[:, 0:nb], c == 0, c == 3, [cf, hsq], sig=True)
        P.op(P.act, lambda e: e.mul(mean.t[:, 0:nb], ps_s.t[:, 0:nb], 1.0 / CC), reads=[ps_s], writes=[mean])
        m2 = hn[0]
        _tt(P, P.dve, m2.t[:, 0:nb], mean.t[:, 0:nb], mean.t[:, 0:nb], ALU.mult, [mean], [m2])
        _stt(P, rstd.t[:, 0:nb], ps_q.t[:, 0:nb], 1.0 / CC, m2.t[:, 0:nb], ALU.mult, ALU.subtract, [ps_q, m2], [rstd])
        _act(P, rstd.t[:, 0:nb], rstd.t[:, 0:nb], AF.Sqrt, [rstd], [rstd], bias=epsb.t[:, 0:1])
        P.op(P.dve, lambda e: e.reciprocal(rstd.t[:, 0:nb], rstd.t[:, 0:nb]), reads=[rstd], writes=[rstd])
        for c in range(4):
            x_ = hn[c % 2]
            _tt(P, P.dve, x_.t[:, 0:nb], hc.t[:, c, 0:nb], mean.t[:, 0:nb], ALU.subtract, [hc, mean], [x_])
            _tt(P, P.pool, x_.t[:, 0:nb], x_.t[:, 0:nb], rstd.t[:, 0:nb], ALU.mult, [x_, rstd], [x_])
            P.op(P.act, lambda e, c=c, x_=x_: e.activation(hT.t[:, c, 0:nb], x_.t[:, 0:nb], AF.Silu,
                                                       bias=pcol.t[:, PC_CBE + c:PC_CBE + c + 1],
                                                       scale=pcol.t[:, PC_CG + c:PC_CG + c + 1]),
                 reads=[x_, pcol], writes=[hT])

    def conv_state_out(uview30, dst):
        psb = P.ps()
        for c in range(4):
            P.op(P.pe, lambda e, c=c, psb=psb: e.transpose(psb.t[0:30, c * 128:(c + 1) * 128], uview30(c), identf),
                 reads=[uT, cf], writes=[psb], sig=(c == 3))
        _copy(P, P.act, cvo.t[0:30, :], psb.t[0:30, 0:512], [psb], [cvo])
        P.dma(P.sp, dst, cvo.t[0:30, :], reads=[cvo], out_final=True)

    def common_front(x_src, nb, subs):
        ntile = nb // 128
        for t in range(ntile):
            xb = xt[st["x"] % 2]
            st["x"] += 1
            P.dma(P.sp, xb.t[:, :], x_src[t * 128:(t + 1) * 128, :], writes=[xb])
            for g in range(2):
                psb = P.ps()
                for j in range(4):
                    kc = g * 4 + j
                    P.op(P.pe, lambda e, psb=psb, j=j, kc=kc, xb=xb: e.transpose(
                        psb.t[:, j * 128:(j + 1) * 128], xb.t[:, kc * 128:(kc + 1) * 128], identf),
                        reads=[xb, cf], writes=[psb], sig=(j == 3))
                eng = P.act if g == 0 else P.dve
                _copy(P, eng, xT.t[:, g * 4:(g + 1) * 4, t * 128:(t + 1) * 128],
                      psb.t[:, :].rearrange("p (a b) -> p a b", a=4), [psb], [xT])
        for c in range(4):
            def ev(psb, c=c):
                _act(P, qz.t[0:64, 2 * c, 0:nb], psb.t[0:64, 0:nb], AF.Identity, [psb, bq8], [qz],
                     bias=bq8.t[0:64, c:c + 1], scale=0.125)
                _act(P, qz.t[64:128, 2 * c + 1, 0:nb], psb.t[64:128, 0:nb], AF.Identity, [psb, bq8], [qz],
                     bias=bq8.t[64:128, c:c + 1], scale=0.125)
            project_feature(OFF_Q + c * 128, 128, nb, ev)
        def evf(psb):
            _act(P, et.t[:, 0:nb], psb.t[0:8, 0:nb], AF.Exp, [psb, nbf], [et], bias=nbf.t[:, 0:1], scale=-1.0)
            _act(P, lfneg.t[:, 0:nb], et.t[:, 0:nb], AF.Ln, [et], [lfneg], bias=oneb.t[0:8, 0:1])
        project_feature(OFF_F, 8, nb, evf)
        for s in subs:
            c0, n = s["c0"], s["n"]
            init = 0.0 if s["first"] else carry.t[:, 0:1]
            P.op(P.dve, lambda e, c0=c0, n=n, init=init: e.tensor_tensor_scan(
                cblk.t[:, c0:c0 + n], ones8.t[:, 0:n], lfneg.t[:, c0:c0 + n], init, ALU.mult, ALU.subtract),
                reads=[ones8, lfneg, carry], writes=[cblk])
        _copy(P, P.act, carry.t[:, 0:1], cblk.t[:, nb - 1:nb], [cblk], [carry])
        _copy(P, P.dve, c3p.t[:, 0, 0:nb], cblk.t[:, 0:nb], [cblk], [c3p])
        _tt(P, P.dve, ctmp.t[:, 0:nb], cblk.t[:, 0:nb], c3p.t[:, 0, 0:nb], ALU.subtract, [cblk, c3p], [ctmp])
        _copy(P, P.dve, c3p.t[:, 1, 0:nb], ctmp.t[:, 0:nb], [ctmp], [c3p])
        _tt(P, P.dve, c3p.t[:, 2, 0:nb], ctmp.t[:, 0:nb], c3p.t[:, 1, 0:nb], ALU.subtract, [ctmp, c3p], [c3p])
        P.dma(P.sp, T["c3_d"][:, :, 0:nb], c3p.t[:, :, 0:nb], reads=[c3p], writes=[c3d])
        P.dma(P.sp, c3pad.t[0:3, :, 0:nb], T["c3_d"][:, :, 0:nb].rearrange("h s n -> s h n"), reads=[c3d], writes=[c3pad])

    def glu(nb, uout, view=lambda a: a):
        for c in range(4):
            sgb = sg[c % 2]
            def evg(psb, sgb=sgb, c=c):
                _act(P, sgb.t[:, 0:nb], psb.t[:, 0:nb], AF.Sigmoid, [psb, pcol], [sgb],
                     bias=pcol.t[:, PC_BGLU + 4 + c:PC_BGLU + 5 + c])
            project_feature(OFF_GLU + CC + c * 128, 128, nb, evg)
            def eva(psb, sgb=sgb, c=c):
                _stt(P, uout(c), view(psb.t[:, 0:nb]), pcol.t[:, PC_BGLU + c:PC_BGLU + c + 1], view(sgb.t[:, 0:nb]),
                     ALU.add, ALU.mult, [psb, pcol, sgb], [uT])
            project_feature(OFF_GLU + c * 128, 128, nb, eva)

    def store_scratch(nb, tok0):
        P.dma(P.sp, T["attn_d"].rearrange("(h d) t -> d h t", d=64)[:, :, tok0:tok0 + nb], attnT.t[:, :, 0:nb],
              reads=[attnT], writes=[scr])
        P.dma(P.sp, T["h_d"].rearrange("(c p) t -> p c t", p=128)[:, :, tok0:tok0 + nb], hT.t[:, :, 0:nb],
              reads=[hT], writes=[scr])

    ntile = NB // 128
    for sq in range(NSEQ_P):
        for b in range(SEQ // NB):
            r0 = sq * SEQ + b * NB
            tile0 = b * ntile
            if b == 0:
                P.op(P.pool, lambda e: e.memset(uT.t[:, :, 0:30], 0.0), writes=[uT])
            else:
                _copy(P, P.pool, uT.t[:, :, 0:30], uT.t[:, :, NB:NB + 30], [uT], [uT])
            common_front(T["xp"][r0:r0 + NB, :], NB, [{"c0": 0, "n": NB, "first": b == 0}])
            for t in range(ntile):
                psb = P.ps()
                P.op(P.pe, lambda e, psb=psb, t=t: e.transpose(psb.t[:, 0:8], lfneg.t[:, t * 128:(t + 1) * 128], identf[0:8, 0:8]),
                     reads=[lfneg, cf], writes=[psb])
                P.op(P.pe, lambda e, psb=psb, t=t: e.transpose(psb.t[:, 8:16], cblk.t[:, t * 128:(t + 1) * 128], identf[0:8, 0:8]),
                     reads=[cblk, cf], writes=[psb])
                P.op(P.act, lambda e, psb=psb, t=t: e.mul(lftok.t[:, t, :], psb.t[:, 0:8], -1.0), reads=[psb], writes=[lftok])
                P.op(P.act, lambda e, psb=psb, j=tile0 + t: e.mul(negc.t[:, j, :], psb.t[:, 8:16], -1.0), reads=[psb], writes=[negc])
            P.dma(P.sp, T["lfp"][r0:r0 + NB, :].rearrange("(t p) h -> p t h", p=128), lftok.t[:, 0:ntile, :],
                  reads=[lftok], out_final=True)
            for t in range(ntile):
                j = tile0 + t
                kvb = kvf[st["kv"] % 2]
                kb = kbf[st["kv"] % 2]
                st["kv"] += 1
                for half in range(2):
                    psb = P.ps()
                    for kc in range(8):
                        _mm(P, psb, psb.t[:, 0:512], xT.t[:, kc, t * 128:(t + 1) * 128],
                            w_in.t[:, kc, OFF_K + half * 512:OFF_K + (half + 1) * 512], kc == 0, kc == 7, [xT, w_in])
                    _tt(P, P.dve, kvb.t[:, half * 512:(half + 1) * 512], psb.t[:, 0:512], bkv.t[:, half * 512:(half + 1) * 512],
                        ALU.add, [psb, bkv], [kvb])
                rr = r0 + t * 128
                P.dma(P.sp, T["kp"][rr:rr + 128, :], kvb.t[:, 0:512], reads=[kvb], out_final=True)
                P.dma(P.sp, T["vp"][rr:rr + 128, :], kvb.t[:, 512:1024], reads=[kvb], out_final=True)
                _copy(P, P.act, kb.t[:, :], kvb.t[:, 0:512], [kvb], [kb])
                _copy(P, P.pool, vsb.t[:, j, :], kvb.t[:, 512:1024], [kvb], [vsb])
                psb = P.ps()
                pbf = psb.t[:, :].bitcast(BF16)
                for c in range(4):
                    P.op(P.pe, lambda e, c=c, pbf=pbf, kb=kb: e.transpose(pbf[:, c * 128:(c + 1) * 128], kb.t[:, c * 128:(c + 1) * 128], identb),
                         reads=[kb, cb], writes=[psb], sig=(c == 3))
                _copy(P, P.dve, kT.t[:, :, j * 128:(j + 1) * 128], pbf[:, 0:512].rearrange("p (c n) -> p c n", c=4), [psb], [kT])
            glu(NB, lambda c: uT.t[:, c, 30:30 + NB])
            for h in range(8):
                kts = []
                for j in range(tile0 + ntile):
                    kts.append(dict(kT=kT.t[:, h // 2, j * 128:(j + 1) * 128], v=vsb.t[:, j, h * 64:(h + 1) * 64],
                                    bias=negc.t[:, j, h:h + 1], nk=128, q0=max(0, (j - tile0) * 128), tri=j >= tile0,
                                    reads=[kT, vsb, negc]))
                attention(h, 0, NB, kts)
            conv_ln(NB, lambda c, j: ubf.t[:, c, j:j + NB], lambda a: a.t[:, 0:NB])
            store_scratch(NB, r0)
            if b == SEQ // NB - 1:
                conv_state_out(lambda c: uT.t[:, c, NB:NB + 30], T["cvp"][sq, :, :])
    uSv = uT.t[:, :, 0:NSUB_S * 62].rearrange("p c (s n) -> p c s n", s=NSUB_S)
    ckb = P.sb("ckb", [128, 8, 512], BF16)
    clfb = P.sb("clfb", [128, 8, 8], F32)
    sufs = P.sb("sufs", [128, 8, 8], F32)
    negcs = P.sb("negcs", [32, NSUB_S, 8], F32)
    kTn = P.sb("kTn", [128, 4, 128], BF16)
    vnew = P.sb("vnew", [32, NSUB_S, 512], BF16)
    kvs = kvf
    scv = kvf
    v4 = lambda a: a.rearrange("p (s n) -> p s n", s=NSUB_S)
    for s in range(NSUB_S):
        sc = scv[s % 2]
        P.dma(P.sp, sc.t[0:30, 0:512], T["sconv"][s, :, :], writes=[sc])
        psb = P.ps()
        for c in range(4):
            P.op(P.pe, lambda e, c=c, psb=psb, sc=sc: e.transpose(psb.t[:, c * 32:c * 32 + 30], sc.t[0:30, c * 128:(c + 1) * 128],
                                                               identf[0:30, 0:30]),
                 reads=[sc, cf], writes=[psb], sig=(c == 3))
        _copy(P, P.act, uSv[:, :, s, 0:30], psb.t[:, 0:128].rearrange("p (c n) -> p c n", c=4)[:, :, 0:30], [psb], [uT])
    common_front(T["xs"], 128, [{"c0": 32 * s, "n": 32, "first": True} for s in range(NSUB_S)])
    psb = P.ps()
    P.op(P.pe, lambda e, psb=psb: e.transpose(psb.t[:, 0:8], lfneg.t[:, 0:128], identf[0:8, 0:8]), reads=[lfneg, cf], writes=[psb])
    P.op(P.act, lambda e, psb=psb: e.mul(lftok.t[:, 0, :], psb.t[:, 0:8], -1.0), reads=[psb], writes=[lftok])
    P.dma(P.sp, T["lfs"], lftok.t[:, 0, :], reads=[lftok], out_final=True)
    for s in range(NSUB_S):
        psb = P.ps()
        P.op(P.pe, lambda e, psb=psb, s=s: e.transpose(psb.t[0:32, 0:8], cblk.t[:, 32 * s:32 * s + 32], identf[0:8, 0:8]),
             reads=[cblk, cf], writes=[psb])
        P.op(P.act, lambda e, psb=psb, s=s: e.mul(negcs.t[0:32, s, :], psb.t[0:32, 0:8], -1.0), reads=[psb], writes=[negcs])
        kvb = kvs[s % 2]
        for half in range(2):
            psb = P.ps()
            for kc in range(8):
                _mm(P, psb, psb.t[0:32, 0:512], xT.t[:, kc, 32 * s:32 * s + 32],
                    w_in.t[:, kc, OFF_K + half * 512:OFF_K + (half + 1) * 512], kc == 0, kc == 7, [xT, w_in])
            _tt(P, P.dve, kvb.t[0:32, half * 512:(half + 1) * 512], psb.t[0:32, 0:512], bkv.t[0:32, half * 512:(half + 1) * 512],
                ALU.add, [psb, bkv], [kvb])
        P.dma(P.sp, T["ks"][32 * s:32 * s + 32, :], kvb.t[0:32, 0:512], reads=[kvb], out_final=True)
        P.dma(P.sp, T["vs"][32 * s:32 * s + 32, :], kvb.t[0:32, 512:1024], reads=[kvb], out_final=True)
        _copy(P, P.act, vnew.t[0:32, s, :], kvb.t[0:32, 512:1024], [kvb], [vnew])
    for c in range(4):
        def evk(psb, c=c):
            _act(P, kTn.t[:, c, 0:128], psb.t[:, 0:128], AF.Identity, [psb, pcol], [kTn], bias=pcol.t[:, PC_BK + c:PC_BK + c + 1])
        project_feature(OFF_K + c * 128, 128, 128, evk)
    glu(128, lambda c: uSv[:, c, :, 30:62], v4)
    for s in range(NSUB_S):
        P.dma(P.pool, ckb.t[:, :, :], T["ck"][s].rearrange("(j p) n -> p j n", p=128), writes=[ckb])
        P.dma(P.pool, vsb.t[:, 0:8, :], T["cv"][s].rearrange("(j p) n -> p j n", p=128), writes=[vsb])
        P.dma(P.sp, clfb.t[:, :, :], T["clf"][s].rearrange("(j p) h -> p j h", p=128), writes=[clfb])
        for j in range(8):
            psb = P.ps()
            pbf = psb.t[:, :].bitcast(BF16)
            for c in range(4):
                P.op(P.pe, lambda e, c=c, j=j, pbf=pbf: e.transpose(pbf[:, c * 128:(c + 1) * 128], ckb.t[:, j, c * 128:(c + 1) * 128], identb),
                     reads=[ckb, cb], writes=[psb], sig=(c == 3))
            _copy(P, P.dve if j % 2 else P.act, kT.t[:, :, j * 128:(j + 1) * 128],
                  pbf[:, 0:512].rearrange("p (c n) -> p c n", c=4), [psb], [kT])
        psb = P.ps()
        for j in range(8):
            _mm(P, psb, psb.t[:, j * 8:(j + 1) * 8], lstrict, clfb.t[:, j, :], True, j == 7, [cf, clfb], sig=False)
            for j2 in range(j + 1, 8):
                _mm(P, psb, psb.t[:, j * 8:(j + 1) * 8], onesf, clfb.t[:, j2, :], False, j2 == 7, [cf, clfb], sig=False)
        P.op(P.pe, lambda e, psb=psb: e.transpose(psb.t[0:8, 64:72], clfb.t[0:8, 0, :], identf[0:8, 0:8]), reads=[clfb, cf], writes=[psb])
        _copy(P, P.dve, sufs.t[:, :, :], psb.t[:, 0:64].rearrange("p (j h) -> p j h", j=8), [psb], [sufs])
        for h in range(8):
            kts = []
            for j in range(8):
                kts.append(dict(kT=kT.t[:, h // 2, j * 128:(j + 1) * 128], v=vsb.t[:, j, h * 64:(h + 1) * 64],
                                bias=sufs.t[:, j, h:h + 1], nk=128, q0=0, tri=False, reads=[kT, vsb, sufs]))
            kts.append(dict(kT=kTn.t[:, h // 2, 32 * s:32 * s + 32], v=vnew.t[0:32, s, h * 64:(h + 1) * 64],
                            bias=negcs.t[0:32, s, h:h + 1], nk=32, q0=0, tri=True, reads=[kTn, vnew, negcs]))
            attention(h, 32 * s, 32, kts)
    uSb = ubf.t[:, :, 0:NSUB_S * 62].rearrange("p c (s n) -> p c s n", s=NSUB_S)
    conv_ln(128, lambda c, j: uSb[:, c, :, j:j + 32], lambda a: v4(a.t[:, 0:128]))
    store_scratch(128, TP)
    for s in range(NSUB_S):
        conv_state_out(lambda c, s=s: uSv[:, c, s, 32:62], T["cvs"][s, :, :])
    P.pop_scope()


CAP = 384
NSLOT = NE * CAP
NT = TT // 128


def layer_norm_tile(P, r, out_fn, tmp, eps_col):
    st6, mv, sc = tmp["st6"], tmp["mv"], tmp["sc"]
    for g in range(2):
        P.op(P.dve, lambda e, g=g: e.bn_stats(st6.t[:, g, :], r.t[:, g * 512:(g + 1) * 512]), reads=[r], writes=[st6])
    P.op(P.dve, lambda e: e.bn_aggr(mv.t[:, 0:2], st6.t[:, :, :].rearrange("p a b -> p (a b)")), reads=[st6], writes=[mv])
    _ts(P, P.dve, sc.t[:, 0:1], mv.t[:, 1:2], LN_EPS, None, ALU.add, None, [mv], [sc])
    P.op(P.pool, lambda e: e.tensor_tensor(sc.t[:, 0:1], sc.t[:, 0:1], eps_col, ALU.pow), reads=[sc], writes=[sc], c=0.25)
    _ts(P, P.dve, sc.t[:, 1:2], mv.t[:, 0:1], -1.0, sc.t[:, 0:1], ALU.mult, ALU.mult, [mv, sc], [sc])
    out_fn(sc.t[:, 0:1], sc.t[:, 1:2])


def pass_b(P, T, G, NB):
    P.push_scope()
    cf = P.sb("cf", [128, 384], F32)
    cb = P.sb("cb", [128, 384], BF16)
    pcol = P.sb("pcol", [128, PC_N], F32)
    P.dma(P.sp, cf.t[:, :], T["cf"], writes=[cf])
    P.dma(P.sp, cb.t[:, :], T["cb"], writes=[cb])
    P.dma(P.sp, pcol.t[:, :], T["pcol"], writes=[pcol])
    identf = cf.t[:, 0:128]
    identb = cb.t[:, 0:128]
    ustrict = cb.t[:, 256:384]
    w_g = P.sb("w_g", [128, 8, 2048], BF16)
    w_a = P.sb("w_a", [128, 4, 1024], BF16)
    w_b = P.sb("w_b", [128, 4, 1024], BF16)
    w_o = P.sb("w_o", [128, 8, 1024], BF16)
    wr_hi = P.sb("wr_hi", [128, 8, 256], BF16)
    wr_lo = P.sb("wr_lo", [128, 8, 256], BF16)
    w_s = P.sb("w_s", [128, 8, 512], BF16)
    w_sd = P.sb("w_sd", [128, 2, 1024], BF16)
    lng = P.sb("lng", [128, 1024], F32)
    lnb = P.sb("lnb", [128, 1024], F32)
    brt = P.sb("brt", [128, 256], F32)
    cnt = P.sb("cnt", [128, 256], F32)
    onesb = P.sb("onesb", [128, 128], BF16)
    epsb = P.sb("epsb", [128, 1], F32)
    tokid = P.sb("tokid", [128, NT], I32)
    load_w_cast(P, w_g, T["w_in"], 8, 2048, OFF_GA)
    load_w_cast(P, w_a, T["w_a"], 4, 1024)
    load_w_cast(P, w_b, T["w_b"], 4, 1024)
    load_w_cast(P, w_o, T["w_out"], 8, 1024)
    load_w_cast(P, wr_hi, T["w_router"], 8, 256)
    load_w_cast(P, w_s, T["w_sg"], 8, 256)
    P.dma(P.pool, w_s.t[:, :, 256:512], T["w_su"].rearrange("(kc p) n -> p kc n", p=128), writes=[w_s])
    load_w_cast(P, w_sd, T["w_sd"], 2, 1024)
    P.dma(P.sp, lng.t[:, :], T["ln1_g"].partition_broadcast(128), writes=[lng])
    P.dma(P.sp, lnb.t[:, :], T["ln1_b"].partition_broadcast(128), writes=[lnb])
    P.dma(P.sp, brt.t[:, :], T["b_router"].partition_broadcast(128), writes=[brt])
    P.dma(P.sp, cnt.t[:, :], T["base1"], writes=[cnt])
    P.dma(P.sp, tokid.t[:, :], T["tokid"], writes=[tokid])
    P.op(P.dve, lambda e: e.memset(onesb.t[:, :], 1.0), writes=[onesb])
    P.op(P.dve, lambda e: e.memset(epsb.t[:, :], -0.5), writes=[epsb])
    wr32 = P.sb("wr32", [128, 8, 256], F32)
    P.dma(P.sp, wr32.t[:, :, :], T["w_router"].rearrange("(kc p) n -> p kc n", p=128), writes=[wr32])
    _tt(P, P.dve, wr_lo.t[:, :, :], wr32.t[:, :, :], wr_hi.t[:, :, :], ALU.subtract, [wr32, wr_hi], [wr_lo])

    nt = NB // 128
    at = P.sb("at", [128, 4, NB], BF16)
    ht = P.sb("ht", [128, 4, NB], BF16)
    xt = [P.sb("xt%d" % i, [128, 1024], F32) for i in range(nt)]
    xT = P.sb("xT", [128, 8, NB], BF16)
    sga = [P.sb("sga%d" % i, [128, NB], F32) for i in range(2)]
    sgb = [P.sb("sgb%d" % i, [128, NB], F32) for i in range(2)]
    t1 = [P.sb("t1%d" % i, [128, NB], F32) for i in range(2)]
    t2 = [P.sb("t2%d" % i, [128, NB], F32) for i in range(2)]
    mT = P.sb("mT", [128, 8, NB], BF16)
    rr2 = [P.sb("rr%d" % i, [128, 1024], F32) for i in range(2)]
    mid = [P.sb("mid%d" % i, [128, 1024], F32) for i in range(2)]
    mhi = [P.sb("mhi%d" % i, [128, 1024], BF16) for i in range(2)]
    mlo2 = [P.sb("mlo%d" % i, [128, 1024], BF16) for i in range(2)]
    mTh = P.sb("mTh", [128, 8, NB], BF16)
    mTl = P.sb("mTl", [128, 8, NB], BF16)
    tmp2 = [{"st6": P.sb("st6%d" % i, [128, 2, 6], F32), "mv": P.sb("mv%d" % i, [128, 2], F32), "sc": P.sb("sc%d" % i, [128, 2], F32)}
            for i in range(2)]
    rt2 = [{n: P.sb("rt%d_%s" % (i, n), [128, 256], F32) for n in ("scores", "sel", "selm", "emask", "gate", "sv", "junk")} for i in range(2)]
    emb2 = [P.sb("emb%d" % i, [128, 256], BF16) for i in range(2)]
    m82 = [P.sb("m8%d" % i, [128, 8, 8], F32) for i in range(2)]
    gs2 = [P.sb("gs%d" % i, [128, 8], F32) for i in range(2)]
    g8s2 = [P.sb("g8s%d" % i, [128, 8], F32) for i in range(2)]
    gmask2 = [P.sb("gmask%d" % i, [128, 8], F32) for i in range(2)]
    gneg2 = [P.sb("gneg%d" % i, [128, 8], F32) for i in range(2)]
    s82 = [P.sb("s8%d" % i, [128, 8], F32) for i in range(2)]
    den2 = [P.sb("den%d" % i, [128, 1], F32) for i in range(2)]
    gsh = [P.sb("gsh%d" % i, [128, NB], F32) for i in range(2)]
    hsT = P.sb("hsT", [128, 2, NB], BF16)
    pre = [P.sb("pre%d" % i, [128, 1024], F32) for i in range(2)]
    scr = Buf(None, "scr")
    slots_all, gates_all = G["slots"], G["gates"]
    st = {"m": 0}

    for b0 in range(0, TT, NB):
        nb = min(NB, TT - b0)
        ntile = nb // 128
        P.dma(P.sp, at.t[:, :, 0:nb], T["attn_d"].rearrange("(c p) t -> p c t", p=128)[:, :, b0:b0 + nb], writes=[at])
        P.dma(P.sp, ht.t[:, :, 0:nb], T["h_d"].rearrange("(c p) t -> p c t", p=128)[:, :, b0:b0 + nb], writes=[ht])
        for t in range(ntile):
            xb = xt[t]
            r0 = b0 + t * 128
            src_x = T["xp"][r0:r0 + 128, :] if r0 < TP else T["xs"]
            P.dma(P.sp, xb.t[:, :], src_x, writes=[xb])
            for g in range(2):
                psb = P.ps()
                for j in range(4):
                    kc = g * 4 + j
                    P.op(P.pe, lambda e, psb=psb, j=j, kc=kc, xb=xb: e.transpose(
                        psb.t[:, j * 128:(j + 1) * 128], xb.t[:, kc * 128:(kc + 1) * 128], identf),
                        reads=[xb, cf], writes=[psb], sig=(j == 3))
                _copy(P, P.act if g == 0 else P.dve, xT.t[:, g * 4:(g + 1) * 4, t * 128:(t + 1) * 128],
                      psb.t[:, :].rearrange("p (a b) -> p a b", a=4), [psb], [xT])
        for oc in range(8):
            i2 = oc % 2
            pa = P.ps()
            for c in range(4):
                _mm(P, pa, pa.t[:, 0:nb], w_a.t[:, c, oc * 128:(oc + 1) * 128], at.t[:, c, 0:nb], c == 0, c == 3, [w_a, at])
            pb = P.ps()
            for c in range(4):
                _mm(P, pb, pb.t[:, 0:nb], w_b.t[:, c, oc * 128:(oc + 1) * 128], ht.t[:, c, 0:nb], c == 0, c == 3, [w_b, ht])
            pga = P.ps()
            for kc in range(8):
                _mm(P, pga, pga.t[:, 0:nb], w_g.t[:, kc, oc * 128:(oc + 1) * 128], xT.t[:, kc, 0:nb], kc == 0, kc == 7, [w_g, xT])
            pgb = P.ps()
            for kc in range(8):
                _mm(P, pgb, pgb.t[:, 0:nb], w_g.t[:, kc, 1024 + oc * 128:1024 + (oc + 1) * 128], xT.t[:, kc, 0:nb], kc == 0, kc == 7, [w_g, xT])
            _act(P, sga[i2].t[:, 0:nb], pga.t[:, 0:nb], AF.Sigmoid, [pga, pcol], [sga[i2]], bias=pcol.t[:, PC_BGA + oc:PC_BGA + oc + 1])
            _act(P, sgb[i2].t[:, 0:nb], pgb.t[:, 0:nb], AF.Sigmoid, [pgb, pcol], [sgb[i2]], bias=pcol.t[:, PC_BGB + oc:PC_BGB + oc + 1])
            _tt(P, P.dve, t1[i2].t[:, 0:nb], pa.t[:, 0:nb], sga[i2].t[:, 0:nb], ALU.mult, [pa, sga[i2]], [t1[i2]])
            _stt(P, t2[i2].t[:, 0:nb], pb.t[:, 0:nb], pcol.t[:, PC_BB + oc:PC_BB + oc + 1], sgb[i2].t[:, 0:nb], ALU.add, ALU.mult,
                 [pb, pcol, sgb[i2]], [t2[i2]])
            _tt(P, P.pool, mT.t[:, oc, 0:nb], t1[i2].t[:, 0:nb], t2[i2].t[:, 0:nb], ALU.add, [t1[i2], t2[i2]], [mT])
        for t in range(ntile):
            tg = (b0 // 128) + t
            tp = tg % 2
            rr, mlo, tmp, rt, emb, m8, gs, g8s, gmask, gneg, s8, den = (rr2[tp], mlo2[tp], tmp2[tp], rt2[tp], emb2[tp], m82[tp], gs2[tp],
                                                                      g8s2[tp], gmask2[tp], gneg2[tp], s82[tp], den2[tp])
            xb = xt[t]
            md = mid[st["m"] % 2]
            mh = mhi[st["m"] % 2]
            st["m"] += 1
            for half in range(2):
                psb = P.ps()
                for kc in range(8):
                    _mm(P, psb, psb.t[:, 0:512], mT.t[:, kc, t * 128:(t + 1) * 128], w_o.t[:, kc, half * 512:(half + 1) * 512],
                        kc == 0, kc == 7, [mT, w_o])
                _stt(P, rr.t[:, half * 512:(half + 1) * 512], xb.t[:, half * 512:(half + 1) * 512], DN_ALPHA, psb.t[:, 0:512],
                     ALU.mult, ALU.add, [xb, psb], [rr])
            def norm1(rstd, nmr, md=md, rr=rr, tmp=tmp):
                P.op(P.act, lambda e, md=md, rr=rr, nmr=nmr, rstd=rstd: e.activation(md.t[:, :], rr.t[:, :], AF.Identity, bias=nmr, scale=rstd), reads=[rr, tmp["sc"]], writes=[md])
            layer_norm_tile(P, rr, norm1, tmp, epsb.t[:, 0:1])
            _tt(P, P.dve, md.t[:, :], md.t[:, :], lng.t[:, :], ALU.mult, [md, lng], [md])
            _tt(P, P.dve, md.t[:, :], md.t[:, :], lnb.t[:, :], ALU.add, [md, lnb], [md])
            _copy(P, P.act, mh.t[:, :], md.t[:, :], [md], [mh])
            _tt(P, P.dve, mlo.t[:, :], md.t[:, :], mh.t[:, :], ALU.subtract, [md, mh], [mlo])
            if "mid_dbg" in T:
                P.dma(P.sp, T["mid_dbg"][tg * 128:(tg + 1) * 128, :], md.t[:, :], reads=[md], out_final=True)
            for srcb, dstb in ((mh, mTh), (mlo, mTl)):
                psb = P.ps()
                pbf = psb.t[:, :].bitcast(BF16)
                for kc in range(8):
                    P.op(P.pe, lambda e, kc=kc, pbf=pbf, srcb=srcb: e.transpose(pbf[:, kc * 128:(kc + 1) * 128], srcb.t[:, kc * 128:(kc + 1) * 128], identb),
                         reads=[srcb, cb], writes=[psb], sig=(kc == 7))
                _copy(P, P.act if srcb is mh else P.dve, dstb.t[:, :, t * 128:(t + 1) * 128], pbf.rearrange("p (c n) -> p c n", c=8), [psb], [dstb])
            psr = P.ps()
            k = 0
            for (a_, w_) in ((mTh, wr_hi), (mTh, wr_lo), (mTl, wr_hi)):
                for kc in range(8):
                    _mm(P, psr, psr.t[:, 0:256], a_.t[:, kc, t * 128:(t + 1) * 128], w_.t[:, kc, :], k == 0, k == 23, [a_, w_])
                    k += 1
            sc_, sel, selm, emask, gate, sv, junk = (rt[n] for n in ("scores", "sel", "selm", "emask", "gate", "sv", "junk"))
            _act(P, sc_.t[:, :], psr.t[:, 0:256], AF.Sigmoid, [psr], [sc_])
            _tt(P, P.dve, sel.t[:, :], sc_.t[:, :], brt.t[:, :], ALU.add, [sc_, brt], [sel])
            for g in range(8):
                P.op(P.dve, lambda e, g=g, m8=m8, sel=sel: e.max(m8.t[:, g, :], sel.t[:, g * 32:(g + 1) * 32]), reads=[sel], writes=[m8])
            _tt(P, P.dve, gs.t[:, :], m8.t[:, :, 0], m8.t[:, :, 1], ALU.add, [m8], [gs])
            P.op(P.dve, lambda e, g8s=g8s, gs=gs: e.max(g8s.t[:, :], gs.t[:, :]), reads=[gs], writes=[g8s])
            _ts(P, P.dve, gmask.t[:, :], gs.t[:, :], g8s.t[:, 3:4], None, ALU.is_ge, None, [gs, g8s], [gmask])
            _ts(P, P.dve, gneg.t[:, :], gmask.t[:, :], -1.0, 1e9, ALU.add, ALU.mult, [gmask], [gneg])
            for g in range(8):
                _ts(P, P.dve, selm.t[:, g * 32:(g + 1) * 32], sel.t[:, g * 32:(g + 1) * 32], gmask.t[:, g:g + 1], gneg.t[:, g:g + 1],
                    ALU.mult, ALU.add, [sel, gmask, gneg], [selm])
            P.op(P.dve, lambda e, m8=m8, selm=selm: e.max(m8.t[:, 0, :], selm.t[:, :]), reads=[selm], writes=[m8])
            _ts(P, P.dve, emask.t[:, :], selm.t[:, :], m8.t[:, 0, 7:8], None, ALU.is_ge, None, [selm, m8], [emask])
            _copy(P, P.pool, emb.t[:, :], emask.t[:, :], [emask], [emb])
            P.op(P.dve, lambda e, gate=gate, sc_=sc_, emask=emask, den=den: e.scalar_tensor_tensor(gate.t[:, :], sc_.t[:, :], 1.0, emask.t[:, :], ALU.mult, ALU.mult, accum_out=den.t[:, 0:1]),
                 reads=[sc_, emask], writes=[gate, den])
            P.op(P.dve, lambda e, den=den: e.reciprocal(den.t[:, 0:1], den.t[:, 0:1]), reads=[den], writes=[den])
            _ts(P, P.dve, gate.t[:, :], gate.t[:, :], den.t[:, 0:1], 2.5, ALU.mult, ALU.mult, [gate, den], [gate])
            pp = P.ps()
            _mm(P, pp, pp.t[:, 0:256], ustrict, emb.t[:, :], True, True, [cb, emb])
            pt_ = P.ps()
            _mm(P, pt_, pt_.t[:, 0:256], onesb.t[:, :], emb.t[:, :], True, True, [onesb, emb])
            _tt(P, P.dve, sv.t[:, :], pp.t[:, 0:256], cnt.t[:, :], ALU.add, [pp, cnt], [sv])
            _tt(P, P.pool, sv.t[:, :], sv.t[:, :], emask.t[:, :], ALU.mult, [sv, emask], [sv])
            _tt(P, P.dve, cnt.t[:, :], pt_.t[:, 0:256], cnt.t[:, :], ALU.add, [pt_, cnt], [cnt])
            P.op(P.dve, lambda e, s8=s8, sv=sv: e.max(s8.t[:, :], sv.t[:, :]), reads=[sv], writes=[s8])
            for k in range(8):
                P.op(P.dve, lambda e, k=k, tg=tg, junk=junk, sv=sv, s8=s8, gate=gate: e.scalar_tensor_tensor(junk.t[:, :], sv.t[:, :], s8.t[:, k:k + 1], gate.t[:, :], ALU.is_equal, ALU.mult,
                                                                     accum_out=gates_all.t[:, tg, k:k + 1]),
                     reads=[sv, s8, gate], writes=[junk, gates_all])
            _ts(P, P.dve, slots_all.t[:, tg, :], s8.t[:, :], -1.0, None, ALU.add, None, [s8], [slots_all])
            for k in range(8):
                P.dma(P.pool, None, None, reads=[slots_all, mh], writes=[scr],
                      fn=lambda h, k=k, tg=tg, mh=mh: h.indirect_dma_start(
                          out=T["xg_d"], out_offset=bass.IndirectOffsetOnAxis(ap=slots_all.t[:, tg, k:k + 1], axis=0),
                          in_=mh.t[:, :], in_offset=None))
        for j in range(2):
            pg = P.ps()
            for kc in range(8):
                _mm(P, pg, pg.t[:, 0:nb], w_s.t[:, kc, j * 128:(j + 1) * 128], mTh.t[:, kc, 0:nb], kc == 0, kc == 7, [w_s, mTh])
            pu = P.ps()
            for kc in range(8):
                _mm(P, pu, pu.t[:, 0:nb], w_s.t[:, kc, 256 + j * 128:256 + (j + 1) * 128], mTh.t[:, kc, 0:nb], kc == 0, kc == 7, [w_s, mTh])
            _act(P, gsh[j].t[:, 0:nb], pg.t[:, 0:nb], AF.Silu, [pg], [gsh[j]])
            _tt(P, P.dve, hsT.t[:, j, 0:nb], pu.t[:, 0:nb], gsh[j].t[:, 0:nb], ALU.mult, [pu, gsh[j]], [hsT])
        for t in range(ntile):
            tg = (b0 // 128) + t
            md = mid[(st["m"] - ntile + t) % 2]
            pr = pre[t % 2]
            for half in range(2):
                psb = P.ps()
                for j in range(2):
                    _mm(P, psb, psb.t[:, 0:512], hsT.t[:, j, t * 128:(t + 1) * 128], w_sd.t[:, j, half * 512:(half + 1) * 512], j == 0, j == 1,
                        [hsT, w_sd])
                _stt(P, pr.t[:, half * 512:(half + 1) * 512], md.t[:, half * 512:(half + 1) * 512], DN_ALPHA, psb.t[:, 0:512], ALU.mult, ALU.add,
                     [md, psb], [pr])
            P.dma(P.sp, T["pre_d"][tg * 128:(tg + 1) * 128, :], pr.t[:, :], reads=[pr], writes=[scr])
    P.pop_scope()


def pass_c(P, T, n_exp=NE):
    P.push_scope()
    NBLK = CAP // 128
    cb = P.sb("cb", [128, 128], BF16)
    P.dma(P.sp, cb.t[:, :], T["cb"][:, 0:128], writes=[cb])
    identb = cb.t[:, 0:128]
    NS = 4
    NW = 2
    sg_ = [P.sb("wsg%d" % i, [128, 8, DE], F32) for i in range(NS)]
    su_ = [P.sb("wsu%d" % i, [128, 8, DE], F32) for i in range(NS)]
    sd_ = [P.sb("wsd%d" % i, [128, 2, D], F32) for i in range(NS)]
    wg = [P.sb("wg%d" % i, [128, 8, DE], BF16) for i in range(NW)]
    wu = [P.sb("wu%d" % i, [128, 8, DE], BF16) for i in range(NW)]
    wd = [P.sb("wd%d" % i, [128, 2, D], BF16) for i in range(NW)]
    xg = [P.sb("xg%d" % i, [128, NBLK, D], BF16) for i in range(NS)]
    xgT = [P.sb("xgT%d" % i, [128, 8, CAP], BF16) for i in range(2)]
    gsb = [P.sb("gsb%d" % i, [128, 2, CAP], F32) for i in range(2)]
    hTe = [P.sb("hTe%d" % i, [128, 2, CAP], BF16) for i in range(2)]
    yb = [P.sb("yb%d" % i, [128, D], BF16) for i in range(3)]
    scr = Buf(None, "scr_c")

    def loads(e):
        s = e % NS
        P.dma(P.sp, sg_[s].t[:, :, :], T["w_eg"][e].rearrange("(p kc) n -> p kc n", kc=8), writes=[sg_[s]])
        P.dma(P.sp, su_[s].t[:, :, :], T["w_eu"][e].rearrange("(p kc) n -> p kc n", kc=8), writes=[su_[s]])
        P.dma(P.sp, sd_[s].t[:, :, :], T["w_ed"][e].rearrange("(p j) n -> p j n", j=2), writes=[sd_[s]])
        P.dma(P.sp, xg[e % NS].t[:, :, :], T["xg_d"][e * CAP:(e + 1) * CAP, :].rearrange("(b p) n -> p b n", p=128), writes=[xg[e % NS]])

    def casts(e):
        s, i = e % NS, e % NW
        _copy(P, P.act, wg[i].t[:, :, :], sg_[s].t[:, :, :], [sg_[s]], [wg[i]])
        _copy(P, P.dve, wu[i].t[:, :, :], su_[s].t[:, :, :], [su_[s]], [wu[i]])
        _copy(P, P.pool, wd[i].t[:, :, :], sd_[s].t[:, :, :], [sd_[s]], [wd[i]])

    for e0 in range(NS):
        loads(e0)
    casts(0)
    yi = 0
    for e in range(n_exp):
        i, i2 = e % NW, e % 2
        for b in range(NBLK):
            psb = P.ps()
            pbf = psb.t[:, :].bitcast(BF16)
            for kc in range(8):
                P.op(P.pe, lambda ee, kc=kc, pbf=pbf, b=b, e=e: ee.transpose(pbf[:, kc * 128:(kc + 1) * 128], xg[e % NS].t[:, b, :].rearrange("s (p k) -> s k p", k=8)[:, kc, :], identb),
                     reads=[xg[e % NS], cb], writes=[psb], sig=(kc == 7))
            _copy(P, P.act if b % 2 == 0 else P.dve, xgT[i2].t[:, :, b * 128:(b + 1) * 128], pbf.rearrange("p (c n) -> p c n", c=8), [psb], [xgT[i2]])
        if e + 1 < n_exp:
            casts(e + 1)
        for j in range(2):
            pg = P.ps()
            for kc in range(8):
                _mm(P, pg, pg.t[:, 0:CAP], wg[i].t[:, kc, :].rearrange("p (m j) -> p j m", j=2)[:, j, :], xgT[i2].t[:, kc, :], kc == 0, kc == 7, [wg[i], xgT[i2]])
            pu = P.ps()
            for kc in range(8):
                _mm(P, pu, pu.t[:, 0:CAP], wu[i].t[:, kc, :].rearrange("p (m j) -> p j m", j=2)[:, j, :], xgT[i2].t[:, kc, :], kc == 0, kc == 7, [wu[i], xgT[i2]])
            _act(P, gsb[i2].t[:, j, :], pg.t[:, 0:CAP], AF.Silu, [pg], [gsb[i2]])
            _tt(P, P.dve, hTe[i2].t[:, j, :], pu.t[:, 0:CAP], gsb[i2].t[:, j, :], ALU.mult, [pu, gsb[i2]], [hTe[i2]])
        if e + NS < n_exp:
            loads(e + NS)
        for b in range(NBLK):
            y_ = yb[yi % 3]
            yi += 1
            for half in range(2):
                psb = P.ps()
                for j in range(2):
                    _mm(P, psb, psb.t[:, 0:512], hTe[i2].t[:, j, b * 128:(b + 1) * 128], wd[i].t[:, j, half * 512:(half + 1) * 512], j == 0, j == 1,
                        [hTe[i2], wd[i]])
                _copy(P, P.act if half == 0 else P.dve, y_.t[:, half * 512:(half + 1) * 512], psb.t[:, 0:512], [psb], [y_])
            r0 = e * CAP + b * 128
            P.dma(P.sp, T["ys_d"][r0:r0 + 128, :], y_.t[:, :], reads=[y_], writes=[scr])
    P.pop_scope()


def pass_d(P, T, G):
    P.push_scope()
    lng = P.sb("lng2", [128, D], F32)
    lnb = P.sb("lnb2", [128, D], F32)
    epsb = P.sb("epsb", [128, 1], F32)
    cb = P.sb("cbd", [128, 128], BF16)
    P.dma(P.sp, cb.t[:, :], T["cb"][:, 0:128], writes=[cb])
    P.dma(P.sp, lng.t[:, :], T["ln2_g"].partition_broadcast(128), writes=[lng])
    P.dma(P.sp, lnb.t[:, :], T["ln2_b"].partition_broadcast(128), writes=[lnb])
    P.op(P.dve, lambda e: e.memset(epsb.t[:, :], -0.5), writes=[epsb])
    identb = cb.t[:, 0:128]
    yk = [P.sb("yk%d" % i, [128, D], BF16) for i in range(16)]
    dgs = [P.sb("dgd%d" % i, [128, 128], BF16) for i in range(16)]
    acc = [P.sb("acc%d" % i, [128, D], F32) for i in range(3)]
    yo = [P.sb("yo%d" % i, [128, D], F32) for i in range(2)]
    tmp2 = [{"st6": P.sb("st6d%d" % i, [128, 2, 6], F32), "mv": P.sb("mvd%d" % i, [128, 2], F32), "sc": P.sb("scd%d" % i, [128, 2], F32)}
            for i in range(2)]
    slots_all, gates_all = G["slots"], G["gates"]
    scr = Buf(None, "scr_d")
    for tg in range(NT):
        a = acc[tg % 3]
        o = yo[tg % 2]
        tmp = tmp2[tg % 2]
        P.dma(P.sp, a.t[:, :], T["pre_d"][tg * 128:(tg + 1) * 128, :], reads=[scr], writes=[a])
        ys_ = []
        for k in range(8):
            y_ = yk[(tg * 8 + k) % 16]
            ys_.append(y_)
            P.dma(P.pool, None, None, reads=[slots_all, scr], writes=[y_],
                  fn=lambda h, k=k, tg=tg, y_=y_: h.indirect_dma_start(
                      out=y_.t[:, :], out_offset=None, in_=T["ys_d"],
                      in_offset=bass.IndirectOffsetOnAxis(ap=slots_all.t[:, tg, k:k + 1], axis=0)))
        dg_ = []
        for k in range(8):
            dg = dgs[(tg * 8 + k) % 16]
            dg_.append(dg)
            P.op(P.act, lambda e, dg=dg, k=k, tg=tg: e.activation(dg.t[:, :], identb, AF.Identity, scale=gates_all.t[:, tg, k:k + 1]),
                 reads=[cb, gates_all], writes=[dg], c=0.4)
        for half in range(2):
            psb = P.ps()
            for k in range(8):
                _mm(P, psb, psb.t[:, 0:512], dg_[k].t[:, :], ys_[k].t[:, half * 512:(half + 1) * 512], k == 0, k == 7, [dg_[k], ys_[k]])
            _tt(P, P.dve, a.t[:, half * 512:(half + 1) * 512], psb.t[:, 0:512], a.t[:, half * 512:(half + 1) * 512], ALU.add, [psb, a], [a])
        def norm2(rstd, nmr, a=a, o=o, tmp=tmp):
            P.op(P.act, lambda e, a=a, o=o, nmr=nmr, rstd=rstd: e.activation(o.t[:, :], a.t[:, :], AF.Identity, bias=nmr, scale=rstd),
                 reads=[a, tmp["sc"]], writes=[o], c=1.3)
        layer_norm_tile(P, a, norm2, tmp, epsb.t[:, 0:1])
        _tt(P, P.dve, o.t[:, :], o.t[:, :], lng.t[:, :], ALU.mult, [o, lng], [o])
        _tt(P, P.dve, o.t[:, :], o.t[:, :], lnb.t[:, :], ALU.add, [o, lnb], [o])
        dst = T["yp"][tg * 128:(tg + 1) * 128, :] if tg * 128 < TP else T["ys"]
        P.dma(P.sp, dst, o.t[:, :], reads=[o], out_final=True)
    P.pop_scope()


IN_SPECS_A = [
    ("xp", [TP, D], F32), ("xs", [128, D], F32), ("ck", [NSUB_S, PAST, 512], F32), ("cv", [NSUB_S, PAST, 512], F32),
    ("clf", [NSUB_S, PAST, 8], F32), ("sconv", [NSUB_S, 30, 512], F32), ("w_in", [D, N_IN], F32), ("b_in", [1, N_IN], F32),
    ("pcol", [128, PC_N], F32), ("cf", [128, 384], F32), ("cb", [128, 384], BF16),
]
IN_SPECS_B = [
    ("w_a", [512, D], F32), ("w_b", [512, D], F32), ("w_out", [D, D], F32), ("ln1_g", [1, D], F32), ("ln1_b", [1, D], F32),
    ("w_router", [D, NE], F32), ("b_router", [1, NE], F32), ("w_sg", [D, DE], F32), ("w_su", [D, DE], F32), ("w_sd", [DE, D], F32),
    ("base1", [128, NE], F32), ("tokid", [128, NT], I32),
]
IN_SPECS_C = [
    ("w_eg", [NE, D, DE], F32), ("w_eu", [NE, D, DE], F32), ("w_ed", [NE, DE, D], F32), ("ln2_g", [1, D], F32), ("ln2_b", [1, D], F32),
]
OUT_SPECS = [
    ("yp", [TP, D]), ("ys", [128, D]), ("kp", [TP, 512]), ("vp", [TP, 512]), ("lfp", [TP, 8]), ("cvp", [NSEQ_P, 30, 512]),
    ("ks", [128, 512]), ("vs", [128, 512]), ("lfs", [128, 8]), ("cvs", [NSUB_S, 30, 512]),
]


def build(stage="A", NB=512, NBB=256, debug=False):
    nc = bass.Bass("TRN2", target_bir_lowering=False)
    T = {}
    specs = list(IN_SPECS_A)
    if stage >= "B":
        specs += IN_SPECS_B
    if stage >= "C":
        specs += IN_SPECS_C
    for name, shape, dt in specs:
        T[name] = nc.dram_tensor(name, shape, dt, kind="ExternalInput").ap()
    for name, shape in OUT_SPECS:
        T[name] = nc.dram_tensor(name, shape, F32, kind="ExternalOutput").ap()
    dk = "ExternalOutput" if debug else "Internal"
    T["attn_d"] = nc.dram_tensor("attn_d", [512, TT], BF16, kind=dk).ap()
    T["h_d"] = nc.dram_tensor("h_d", [512, TT], BF16, kind=dk).ap()
    T["c3_d"] = nc.dram_tensor("c3_d", [8, 3, 512], BF16, kind="Internal").ap()
    P = Prog(nc)
    G = {}
    if stage >= "B":
        T["xg_d"] = nc.dram_tensor("xg_d", [NSLOT, D], BF16, kind="Internal").ap()
        T["pre_d"] = nc.dram_tensor("pre_d", [TT, D], F32, kind="Internal").ap()
        if debug:
            T["mid_dbg"] = nc.dram_tensor("mid_dbg", [TT, D], F32, kind="ExternalOutput").ap()
            T["slots_dbg"] = nc.dram_tensor("slots_dbg", [128, NT * 8], I32, kind="ExternalOutput").ap()
            T["gates_dbg"] = nc.dram_tensor("gates_dbg", [128, NT * 8], F32, kind="ExternalOutput").ap()
        G["slots"] = P.sb("slots_all", [128, NT, 8], I32)
        G["gates"] = P.sb("gates_all", [128, NT, 8], F32)
    pass_a(P, T, NB)
    if stage >= "B":
        pass_b(P, T, G, NBB)
        if stage >= "C":
            T["ys_d"] = nc.dram_tensor("ys_d", [NSLOT, D], BF16, kind="Internal").ap()
            pass_c(P, T)
            pass_d(P, T, G)
        if debug:
            P.dma(P.sp, T["slots_dbg"], G["slots"].t[:, :, :].rearrange("p a b -> p (a b)"), reads=[G["slots"]], out_final=True)
            P.dma(P.sp, T["gates_dbg"], G["gates"].t[:, :, :].rearrange("p a b -> p (a b)"), reads=[G["gates"]], out_final=True)
    P.finish()
    print('[build] ops', P.n_ops, 'sim_us %.1f' % getattr(P, 'sim_time', 0.0), flush=True)
    return nc, [s[0] for s in specs]


def host_consts(inp):
    b_in = np.asarray(inp["b_in"])[0]
    pcol = np.zeros((128, PC_N), np.float32)
    col = lambda v, n: np.ascontiguousarray(np.asarray(v).reshape(n, 128).T)
    pcol[:, PC_BQ:PC_BQ + 4] = col(b_in[OFF_Q:OFF_K], 4)
    pcol[:, PC_BGLU:PC_BGLU + 8] = col(b_in[OFF_GLU:OFF_GA], 8)
    pcol[:, PC_BGA:PC_BGA + 8] = col(b_in[OFF_GA:OFF_GB], 8)
    pcol[:, PC_BGB:PC_BGB + 8] = col(b_in[OFF_GB:N_IN], 8)
    pcol[:, PC_BB:PC_BB + 8] = col(inp["b_b"][0], 8)
    pcol[:, PC_CB:PC_CB + 4] = col(inp["conv_b"][0], 4)
    pcol[:, PC_CG:PC_CG + 4] = col(inp["conv_ln_g"][0], 4)
    pcol[:, PC_CBE:PC_CBE + 4] = col(inp["conv_ln_b"][0], 4)
    cw = np.asarray(inp["conv_w"])[0]
    pcol[:, PC_CW:PC_CW + 124] = cw.T.reshape(4, 128, KW).transpose(1, 0, 2).reshape(128, 4 * KW)
    pcol[0:8, PC_BF] = b_in[OFF_F:OFF_GLU]
    pcol[:, PC_BK:PC_BK + 4] = col(b_in[OFF_K:OFF_V], 4)
    cf = np.concatenate([np.eye(128, dtype=np.float32), np.tril(np.ones((128, 128), np.float32), -1),
                         np.ones((128, 128), np.float32)], axis=1)
    cb = np.concatenate([np.eye(128, dtype=np.float32), np.triu(np.ones((128, 128), np.float32)),
                         np.triu(np.ones((128, 128), np.float32), 1)], axis=1).astype(ml_dtypes.bfloat16)
    base1 = np.ascontiguousarray(np.broadcast_to((np.arange(NE, dtype=np.float32) * CAP + 1.0)[None, :], (128, NE)))
    tokid = np.ascontiguousarray((np.arange(NT, dtype=np.int32)[None, :] * 128 + np.arange(128, dtype=np.int32)[:, None]).astype(np.int32))
    return pcol, cf, cb, base1, tokid


def core_inputs(inp, c, consts):
    pcol, cf, cb, base1, tokid = consts
    m = {
        "xp": np.asarray(inp["x_prompt"])[2 * c:2 * c + 2].reshape(TP, D),
        "xs": np.asarray(inp["x_sample"])[4 * c:4 * c + 4].reshape(128, D),
        "ck": np.asarray(inp["cache_k"])[0, 4 * c:4 * c + 4].reshape(NSUB_S, PAST, 512),
        "cv": np.asarray(inp["cache_v"])[0, 4 * c:4 * c + 4].reshape(NSUB_S, PAST, 512),
        "clf": np.asarray(inp["cache_logf"])[0, 4 * c:4 * c + 4],
        "sconv": np.asarray(inp["state_conv"])[0, 4 * c:4 * c + 4],
        "w_in": np.asarray(inp["w_in"])[0], "b_in": np.asarray(inp["b_in"]),
        "pcol": pcol, "cf": cf, "cb": cb, "base1": base1, "tokid": tokid,
        "w_a": np.asarray(inp["w_a"])[0], "w_b": np.asarray(inp["w_b"])[0], "w_out": np.asarray(inp["w_out"])[0],
        "ln1_g": np.asarray(inp["ln1_g"]), "ln1_b": np.asarray(inp["ln1_b"]),
        "w_router": np.asarray(inp["w_router"])[0], "b_router": np.asarray(inp["b_router"]),
        "w_sg": np.asarray(inp["w_s_gate"])[0], "w_su": np.asarray(inp["w_s_up"])[0], "w_sd": np.asarray(inp["w_s_down"])[0],
        "w_eg": np.asarray(inp["w_e_gate"])[0], "w_eu": np.asarray(inp["w_e_up"])[0], "w_ed": np.asarray(inp["w_e_down"])[0],
        "ln2_g": np.asarray(inp["ln2_g"]), "ln2_b": np.asarray(inp["ln2_b"]),
    }
    return m


def assemble(results):
    cat = lambda k: np.concatenate([r[k] for r in results], axis=0)
    yp = cat("yp").reshape(16, SEQ, D)
    ys = cat("ys").reshape(32, TS, D)
    kp = cat("kp").reshape(1, 16, SEQ, H, DH)
    vp = cat("vp").reshape(1, 16, SEQ, H, DH)
    lfp = cat("lfp").reshape(1, 16, SEQ, H)
    cvp = cat("cvp").reshape(1, 16, 30, CC)
    ks = cat("ks").reshape(1, 32, TS, H, DH)
    vs = cat("vs").reshape(1, 32, TS, H, DH)
    lfs = cat("lfs").reshape(1, 32, TS, H)
    cvs = cat("cvs").reshape(1, 32, 30, CC)
    return (yp, ys, kp, vp, lfp, cvp, ks, vs, lfs, cvs)


def kernel(**inputs):
    nc, names = build("C")
    consts = host_consts(inputs)
    in_maps = []
    for c in range(NCORES):
        m = core_inputs(inputs, c, consts)
        in_maps.append({k: (m[k] if m[k].flags["C_CONTIGUOUS"] else np.ascontiguousarray(m[k])) for k in names})
    res = run_bass_kernel_spmd(nc, in_maps, core_ids=list(range(NCORES)))
    return assemble(res.results)
```
